# Optimizing a Trainium2 kernel written in Bass

```python
import math
import jax, jax.numpy as jnp
from jax import lax
import numpy as np

D_MODEL = 2048
BATCH = 4
SEQ = 4096
DEPTH = 2

D_FF = 5632
SGU_WIDTH = D_MODEL // 2
SGU_GROUPS = 8
SGU_GROUP_CH = SGU_WIDTH // SGU_GROUPS
SGU_CHUNK = 128
ATT_HEADS = 8
ATT_HEAD_DIM = 128
ATT_WIDTH = ATT_HEADS * ATT_HEAD_DIM
IDX_HEADS = 16
IDX_DIM = 64
TOPK_MAX = 256
Q_BLOCK = 128
NUM_BUCKETS = 32
MAX_DISTANCE = 128
NORM_EPS = 1e-6
LN_EPS = 1e-5

IN_SIZES = (SGU_WIDTH, SGU_WIDTH, ATT_WIDTH, ATT_WIDTH, ATT_WIDTH,
            IDX_HEADS * IDX_DIM, IDX_DIM, IDX_HEADS)
IN_COLS = sum(IN_SIZES)
IN_SPLITS = tuple(int(s) for s in np.cumsum(IN_SIZES)[:-1])

kernel_name = "hybrid_gated_sgu_dsa_macaron"


def rmsnorm(x, g):
    xf = x.astype(jnp.float32)
    y = xf * lax.rsqrt(jnp.mean(xf * xf, axis=-1, keepdims=True) + NORM_EPS)
    return (y * g.astype(jnp.float32)).astype(x.dtype)


def layernorm(x, g, b):
    xf = x.astype(jnp.float32)
    mu = jnp.mean(xf, axis=-1, keepdims=True)
    var = jnp.mean(jnp.square(xf - mu), axis=-1, keepdims=True)
    y = (xf - mu) * lax.rsqrt(var + LN_EPS)
    return (y * g.astype(jnp.float32) + b.astype(jnp.float32)).astype(x.dtype)


def swiglu(h, w_in, w_out):
    a, b = jnp.split(h @ w_in, 2, axis=-1)
    return (jax.nn.silu(a) * b) @ w_out


def t5_causal_bucket(n):
    max_exact = NUM_BUCKETS // 2
    nf = jnp.maximum(n, 1).astype(jnp.float32)
    large = max_exact + (jnp.log(nf / max_exact) / math.log(MAX_DISTANCE / max_exact)
                         * (NUM_BUCKETS - max_exact)).astype(jnp.int32)
    large = jnp.minimum(large, NUM_BUCKETS - 1)
    return jnp.where(n < max_exact, n, large)


def spatial_gating(z_u, z_v, ln_g, ln_b, w_s, b_s):
    B, S, _ = z_v.shape
    n_chunks = S // SGU_CHUNK
    v = layernorm(z_v, ln_g, ln_b).reshape(B, n_chunks, SGU_CHUNK, SGU_GROUPS, SGU_GROUP_CH)
    causal = jnp.tril(jnp.ones((SGU_CHUNK, SGU_CHUNK), dtype=bool))
    ws = jnp.where(causal[None], w_s, jnp.zeros_like(w_s))
    mixed = jnp.einsum('gts,bnsgc->bntgc', ws, v) + b_s.T[None, None, :, :, None]
    return z_u * mixed.reshape(B, S, SGU_WIDTH)


def dsa_attention(q, k, v, q_idx, k_idx, w_idx, rel_bias):
    B, S = q.shape[0], q.shape[1]
    top_k = min(TOPK_MAX, S // 4)
    n_blocks = S // Q_BLOCK
    key_pos = jnp.arange(S, dtype=jnp.int32)
    idx_scale = IDX_DIM ** -0.5
    head_w_scale = IDX_HEADS ** -0.5
    att_scale = ATT_HEAD_DIM ** -0.5

    def to_blocks(a):
        return jnp.moveaxis(a.reshape(B, n_blocks, Q_BLOCK, *a.shape[2:]), 1, 0)

    gather = jax.vmap(lambda table, ids: table[ids])

    def block(args):
        qb, qib, wb, start = args
        q_pos = start + jnp.arange(Q_BLOCK, dtype=jnp.int32)
        causal = key_pos[None, :] <= q_pos[:, None]
        dots = jnp.einsum('bthd,bsd->bths', qib, k_idx).astype(jnp.float32) * idx_scale
        score = jnp.einsum('bths,bth->bts', jax.nn.relu(dots), wb.astype(jnp.float32) * head_w_scale)
        score = jnp.where(causal[None], score, -jnp.inf)
        _, sel = lax.top_k(score, top_k)
        k_sel = gather(k, sel)
        v_sel = gather(v, sel)
        dist = q_pos[None, :, None] - sel
        valid = dist >= 0
        bias = rel_bias[t5_causal_bucket(jnp.maximum(dist, 0))]
        logits = jnp.einsum('bthd,btkhd->bthk', qb, k_sel).astype(jnp.float32) * att_scale
        logits = logits + jnp.moveaxis(bias.astype(jnp.float32), -1, 2)
        logits = jnp.where(valid[:, :, None, :], logits, -jnp.inf)
        p = jax.nn.softmax(logits, axis=-1).astype(v.dtype)
        return jnp.einsum('bthk,btkhd->bthd', p, v_sel)

    starts = jnp.arange(n_blocks, dtype=jnp.int32) * Q_BLOCK
    out = lax.map(block, (to_blocks(q), to_blocks(q_idx), to_blocks(w_idx), starts))
    return jnp.moveaxis(out, 0, 1).reshape(B, S, ATT_WIDTH)


def setup_inputs(seed: int = 0) -> dict:
    key = jax.random.key(seed)
    ks = jax.random.split(key, 24)
    f32 = jnp.float32

    def w(k, shape, fan_in):
        return jax.random.normal(k, shape, f32) * fan_in ** -0.5

    def gain(k, shape):
        return 1.0 + 0.02 * jax.random.normal(k, shape, f32)

    L = DEPTH
    return {
        "x": jax.random.normal(ks[0], (BATCH, SEQ, D_MODEL), f32),
        "ffn1_norm_pre": gain(ks[1], (L, D_MODEL)),
        "ffn1_norm_post": gain(ks[2], (L, D_MODEL)),
        "ffn1_w_in": w(ks[3], (L, D_MODEL, 2 * D_FF), D_MODEL),
        "ffn1_w_out": w(ks[4], (L, D_FF, D_MODEL), D_FF),
        "mix_norm_pre": gain(ks[5], (L, D_MODEL)),
        "mix_norm_post": gain(ks[6], (L, D_MODEL)),
        "w_in": w(ks[7], (L, D_MODEL, IN_COLS), D_MODEL),
        "sgu_ln_g": gain(ks[8], (L, SGU_WIDTH)),
        "sgu_ln_b": 0.02 * jax.random.normal(ks[9], (L, SGU_WIDTH), f32),
        "sgu_w_s": w(ks[10], (L, SGU_GROUPS, SGU_CHUNK, SGU_CHUNK), SGU_CHUNK),
        "sgu_b": gain(ks[11], (L, SGU_GROUPS, SGU_CHUNK)),
        "rel_bias": 0.5 * jax.random.normal(ks[12], (NUM_BUCKETS, ATT_HEADS), f32),
        "w_branch_a": w(ks[13], (L, SGU_WIDTH, D_MODEL), SGU_WIDTH),
        "w_branch_b": w(ks[14], (L, ATT_WIDTH, D_MODEL), ATT_WIDTH),
        "w_gate": w(ks[15], (L, D_MODEL, 2 * D_MODEL), D_MODEL),
        "w_out": w(ks[16], (L, D_MODEL, D_MODEL), D_MODEL),
        "ffn2_norm_pre": gain(ks[17], (L, D_MODEL)),
        "ffn2_norm_post": gain(ks[18], (L, D_MODEL)),
        "ffn2_w_in": w(ks[19], (L, D_MODEL, 2 * D_FF), D_MODEL),
        "ffn2_w_out": w(ks[20], (L, D_FF, D_MODEL), D_FF),
    }


def reference(x, ffn1_norm_pre, ffn1_norm_post, ffn1_w_in, ffn1_w_out,
              mix_norm_pre, mix_norm_post, w_in, sgu_ln_g, sgu_ln_b, sgu_w_s, sgu_b,
              rel_bias, w_branch_a, w_branch_b, w_gate, w_out,
              ffn2_norm_pre, ffn2_norm_post, ffn2_w_in, ffn2_w_out):
    B, S, _ = x.shape
    for l in range(DEPTH):
        f = swiglu(rmsnorm(x, ffn1_norm_pre[l]), ffn1_w_in[l], ffn1_w_out[l])
        x = x + 0.5 * rmsnorm(f, ffn1_norm_post[l])

        h = rmsnorm(x, mix_norm_pre[l])
        z_u, z_v, q, k, v, q_idx, k_idx, w_idx = jnp.split(h @ w_in[l], IN_SPLITS, axis=-1)

        y_a = spatial_gating(jax.nn.gelu(z_u), jax.nn.gelu(z_v),
                             sgu_ln_g[l], sgu_ln_b[l], sgu_w_s[l], sgu_b[l])

        y_b = dsa_attention(q.reshape(B, S, ATT_HEADS, ATT_HEAD_DIM),
                            k.reshape(B, S, ATT_HEADS, ATT_HEAD_DIM),
                            v.reshape(B, S, ATT_HEADS, ATT_HEAD_DIM),
                            q_idx.reshape(B, S, IDX_HEADS, IDX_DIM),
                            k_idx, w_idx, rel_bias)

        g_a, g_b = jnp.split(jax.nn.sigmoid(h @ w_gate[l]), 2, axis=-1)
        merged = g_a * (y_a @ w_branch_a[l]) + g_b * (y_b @ w_branch_b[l])
        x = x + rmsnorm(merged @ w_out[l], mix_norm_post[l])

        f = swiglu(rmsnorm(x, ffn2_norm_pre[l]), ffn2_w_in[l], ffn2_w_out[l])
        x = x + 0.5 * rmsnorm(f, ffn2_norm_post[l])
    return x
```

```python
import numpy as np
from concourse.bass_utils import run_bass_kernel_spmd
from contextlib import ExitStack
import numpy as np
import concourse.bass as bass
import concourse.mybir as mybir

F32 = mybir.dt.float32
BF16 = mybir.dt.bfloat16
AF = mybir.ActivationFunctionType
ALU = mybir.AluOpType

ENGS = ("pe", "act", "dve", "pool", "sp")


class Buf:
    __slots__ = ("name", "lastw", "readers")

    def __init__(self, name):
        self.name = name
        self.lastw = None
        self.readers = {}


class Lane:
    def __init__(self, prog, name):
        self.sem = prog.nc.alloc_semaphore(name=name)
        self.count = 0
        self.last = None


class Op:
    __slots__ = ("eng", "fn", "waits", "flag", "val", "lane", "laneval", "idx")

    def __init__(self, eng, fn):
        self.eng = eng
        self.fn = fn
        self.waits = []
        self.flag = False
        self.val = None
        self.lane = None
        self.laneval = None


class Prog:
    def __init__(self, nc):
        self.nc = nc
        self.q = {e: [] for e in ENGS}
        self.sem = {e: nc.alloc_semaphore(name="sem_" + e) for e in ENGS}
        self.es = ExitStack()
        self.lanes = {}
        self.n_sb = 0

    def sbuf(self, name, shape, dtype):
        return self.es.enter_context(self.nc.sbuf_tensor(name, list(shape), dtype))

    def psum(self, name, shape, dtype=F32):
        return self.es.enter_context(self.nc.psum_tensor(name, list(shape), dtype))

    def lane(self, name):
        if name not in self.lanes:
            self.lanes[name] = Lane(self, "ln_" + name)
        return self.lanes[name]

    def _deps(self, op, reads, writes):
        evs = []
        for b in reads:
            if b.lastw is not None:
                evs.append(b.lastw)
        for b in writes:
            if b.lastw is not None:
                evs.append(b.lastw)
            evs.extend(b.readers.values())
        for ev in evs:
            if ev[0] == "c":
                src = ev[1]
                if src.eng == "pe" and op.eng == "pe":
                    continue
                if src is op:
                    continue
                src.flag = True
            op.waits.append(ev)

    def _post(self, ev, reads, writes):
        for b in writes:
            b.lastw = ev
            b.readers = {}
        key = ("c", ev[1].eng) if ev[0] == "c" else ("d", id(ev[1]))
        for b in reads:
            if b not in writes:
                b.readers[key] = ev

    def op(self, eng, fn, reads=(), writes=()):
        o = Op(eng, fn)
        self._deps(o, reads, writes)
        o.idx = len(self.q[eng])
        self.q[eng].append(o)
        self._post(("c", o), reads, writes)
        return o

    def dma(self, eng, fn, reads, writes, lane):
        ln = self.lane(lane) if isinstance(lane, str) else lane
        o = Op(eng, fn)
        self._deps(o, reads, writes)
        if ln.last is not None:
            o.waits.append(ln.last)
        ln.count += 16
        o.lane = ln
        o.laneval = ln.count
        ev = ("d", ln, ln.count)
        ln.last = ev
        self.q[eng].append(o)
        self._post(ev, reads, writes)
        return o

    def claim(self, old_bufs, new_bufs):
        merged = {}
        for b in old_bufs:
            evs = list(b.readers.values())
            if b.lastw is not None:
                evs.append(b.lastw)
            for ev in evs:
                key = ("c", ev[1].eng) if ev[0] == "c" else ("d", id(ev[1]))
                rank = ev[1].idx if ev[0] == "c" else ev[2]
                if key not in merged or merged[key][0] < rank:
                    merged[key] = (rank, ev)
        for nb_ in new_bufs:
            for key, (rank, ev) in merged.items():
                nb_.readers[key] = ev

    def finalize(self):
        nc = self.nc
        for e in ENGS:
            c = 0
            for o in self.q[e]:
                if o.flag:
                    c += 1
                    o.val = c
        engobj = {"pe": "tensor", "act": "scalar", "dve": "vector", "pool": "gpsimd", "sp": "sync"}
        final_lane_vals = [(ln.sem, ln.count) for ln in self.lanes.values() if ln.count > 0]
        final_eng_vals = {e: max([o.val for o in self.q[e] if o.flag] + [0]) for e in ENGS}
        prog = self

        def emit(e, eng):
            known = {}
            for o in prog.q[e]:
                need = {}
                for ev in o.waits:
                    if ev[0] == "c":
                        src = ev[1]
                        key = ("c", src.eng)
                        sem, val = prog.sem[src.eng], src.val
                    else:
                        key = ("d", id(ev[1]))
                        sem, val = ev[1].sem, ev[2]
                    if known.get(key, 0) >= val:
                        continue
                    if key not in need or need[key][1] < val:
                        need[key] = (sem, val)
                for key, (sem, val) in need.items():
                    eng.wait_ge(sem, val)
                    known[key] = val
                ins = o.fn(eng)
                if o.lane is not None:
                    ins.then_inc(o.lane.sem, 16)
                elif o.flag:
                    ins.then_inc(prog.sem[e], 1)
            if e == "sp":
                for sem, val in final_lane_vals:
                    eng.wait_ge(sem, val)
                for e2, v in final_eng_vals.items():
                    if v > 0:
                        eng.wait_ge(prog.sem[e2], v)

        with nc.Block() as block:
            @block.tensor
            def _(eng):
                emit("pe", eng)

            @block.scalar
            def _(eng):
                emit("act", eng)

            @block.vector
            def _(eng):
                emit("dve", eng)

            @block.gpsimd
            def _(eng):
                emit("pool", eng)

            @block.sync
            def _(eng):
                emit("sp", eng)
        self.es.close()


D = 2048
DC = D // 128
DFF = 5632
FC = DFF // 128
NT = 512
NORM_EPS = 1e-6
LN_EPS = 1e-5
SEQ = 4096
CASTDMA = True


class Ctx:
    def __init__(self, P):
        self.P = P
        nc = P.nc
        self.ps = [P.psum(f"ps{i}", [128, 512]) for i in range(8)]
        self.ps_b = [Buf(f"ps{i}") for i in range(8)]
        self.NS = 4
        self.wbf = [P.sbuf(f"wbf{i}", [128, 4096], BF16) for i in range(self.NS)]
        self.wbf_b = [Buf(f"wbf{i}") for i in range(self.NS)]
        self.plan = []
        self.issued = 0
        self.R1 = P.sbuf("R1", [128, 8192], F32)
        self.R2 = P.sbuf("R2", [128, 4096], F32)
        self.R3 = P.sbuf("R3", [128, 11264], F32)
        self.ones = P.sbuf("ones", [128, 128], BF16)
        self.ones_b = Buf("ones")
        self.tmpa = [P.sbuf(f"tmpa{i}", [128, 512], F32) for i in range(2)]
        self.tmpa_b = [Buf(f"tmpa{i}") for i in range(2)]
        self.sq = [P.sbuf(f"sq{i}", [128, 512], BF16) for i in range(2)]
        self.sq_b = [Buf(f"sq{i}") for i in range(2)]
        self.rstd = P.sbuf("rstd", [128, 512], F32)
        self.rstd_b = Buf("rstd")
        self.epsc = P.sbuf("epsc", [128, 1], F32)
        self.epsc_b = Buf("epsc")
        self.cnt = 0
        self.reg = {"R1": [], "R2": [], "R3": []}
        def sb(name, shape, dt):
            setattr(self, name, P.sbuf("s_" + name, shape, dt))
            setattr(self, name + "_b", Buf(name))
        sb("kio", [128, 512], BF16); sb("wio", [128, 64], F32)
        sb("gt1", [128, 512], F32); sb("gt2", [128, 512], F32)
        self.R4 = P.sbuf("R4", [128, 4096], F32)
        self.reg["R4"] = []
        self.bro = self.R4[:, 0:3072]; self.bro_b = Buf("bro")
        self.wsf = self.R4[:, 3072:4096]; self.wsf_b = Buf("wsf")
        sb("wsb", [128, 8, 128], BF16)
        sb("mT0", [128, SEQ], BF16); sb("mT1", [128, SEQ], BF16)
        sb("qib2", [128, 8, 128], BF16); sb("qbk2", [128, 8, 128], BF16); sb("wx2", [128, 16], F32)
        sb("wabs2", [128, 16], F32); sb("wsg2", [128, 16], F32)
        sb("dg0", [128, 16, 128], BF16)
        self.dg1, self.dg1_b = self.dg0, self.dg0_b
        for i in range(4):
            sb(f"rl16_{i}", [128, 512], BF16)
        self.rl16 = [getattr(self, f"rl16_{i}") for i in range(4)]; self.rl16_b = [getattr(self, f"rl16_{i}_b") for i in range(4)]
        sb("blo", [128, 1], F32); sb("bmid", [128, 1], F32); sb("bcnt", [128, 1], F32); sb("bfs", [128, 1], F32); sb("brng", [128, 1], F32)
        sb("bstep", [128, 32], F32); sb("pw2", [128, 32], F32)
        for k in range(32):
            P.op("pool", (lambda e, k=k: e.memset(self.pw2[:, k:k + 1], 2.0 ** -(k + 1))), writes=[self.pw2_b])
        self.ta2 = self.tmpa; self.ta2_b = self.tmpa_b
        sb("bst", [128, 12], F32); sb("bag", [128, 2], F32); sb("lrs", [128, 1], F32); sb("lneps", [128, 1], F32)
        sb("qib", [128, 8, 128], BF16); sb("qbk", [128, 8, 128], BF16); sb("wx", [128, 16], F32)
        sb("wabs", [128, 16], F32); sb("wsg", [128, 16], F32); sb("kis", [128, SEQ], BF16)
        sb("m8", [128, 8], F32); sb("thrneg", [128, 1], F32); sb("ident", [128, 128], BF16); sb("onec", [128, 2], BF16)
        sb("pm0", [128, 512], BF16); sb("pm1", [128, 512], BF16); sb("rc", [128, 1], F32); sb("yb", [128, 1024], BF16)
        sb("Fb", [128, 2048], BF16); sb("cb8", [128, 8], F32)
        self.pm = [self.pm0, self.pm1]; self.pm_b = [self.pm0_b, self.pm1_b]
        P.op("pool", lambda e: e.memset(self.lneps[:], LN_EPS), writes=[self.lneps_b])
        P.op("pool", lambda e: e.memset(self.ones[:], 1.0), writes=[self.ones_b])
        P.op("pool", lambda e: e.memset(self.epsc[:], NORM_EPS), writes=[self.epsc_b])

    def claim(self, rname, bufs):
        self.P.claim(self.reg[rname], bufs)
        self.reg[rname] = list(bufs)

    def plan_extend(self, specs):
        self.plan.extend(specs)

    def _issue(self, i):
        P = self.P
        pieces, R, W = self.plan[i]
        s = i % self.NS
        if CASTDMA:
            bt = self.wbf[s][:, 0:R * W].rearrange("p (r w) -> p r w", w=W)
            for pi, (c0, wd, src) in enumerate(pieces):
                P.dma("pool", (lambda e, o=bt[:, :, c0:c0 + wd], s_=src: e.dma_start(out=o, in_=s_)),
                      reads=[], writes=[self.wbf_b[s]], lane=f"w{s}_{pi}")
            return
        st = self.wst[s][:, 0:R * W].rearrange("p (r w) -> p r w", w=W)
        for pi, (c0, wd, src) in enumerate(pieces):
            P.dma("sp", (lambda e, o=st[:, :, c0:c0 + wd], s_=src: e.dma_start(out=o, in_=s_)),
                  reads=[], writes=[self.wst_b[s]], lane=f"w{s}_{pi}")
        P.op("pool", (lambda e, o=self.wbf[s][:, 0:R * W], i_=self.wst[s][:, 0:R * W]: e.tensor_copy(out=o, in_=i_)),
             reads=[self.wst_b[s]], writes=[self.wbf_b[s]])

    def wget(self, i, look=2):
        while self.issued <= min(i + look, len(self.plan) - 1):
            self._issue(self.issued)
            self.issued += 1
        pieces, R, W = self.plan[i]
        s = i % self.NS
        return self.wbf[s][:, 0:R * W].rearrange("p (r w) -> p r w", w=W), self.wbf_b[s]


_FILLREG = {}


def fillreg(e, v):
    key = (id(e), v)
    if key not in _FILLREG:
        _FILLREG[key] = e.to_reg(v)
    return _FILLREG[key]


def mm(P, o, l, r, st, sp, reads, writes):
    P.op("pe", (lambda e: e.matmul(o, l, r, start=st, stop=sp)), reads=reads, writes=writes)


def w_panel(w2d, r0, nr, cols):
    pieces = []
    off = 0
    for (c0, wd) in cols:
        src = w2d[r0 * 128:(r0 + nr) * 128, c0:c0 + wd].rearrange("(r p) w -> p r w", p=128)
        pieces.append((off, wd, src))
        off += wd
    return (pieces, nr, off)


def emit_rstd(C, chunk_ap, nch, dim, bank):
    P = C.P
    ps, psb = C.ps[bank], C.ps_b[bank]
    for c in range(nch):
        ap, b = chunk_ap(c)
        k = C.cnt % 2
        C.cnt += 1
        P.op("act", (lambda e, o=C.sq[k][:], i_=ap: e.activation(out=o, in_=i_, func=AF.Square)),
             reads=[b], writes=[C.sq_b[k]])
        P.op("pe", (lambda e, o=ps[:], l=C.ones[:], r=C.sq[k][:], st=(c == 0), sp=(c == nch - 1):
                    e.matmul(o, l, r, start=st, stop=sp)),
             reads=[C.ones_b, C.sq_b[k]], writes=[psb])
    P.op("act", (lambda e: e.activation(out=C.rstd[:], in_=ps[:], func=AF.Sqrt, bias=C.epsc[:], scale=1.0 / dim)),
         reads=[psb, C.epsc_b], writes=[C.rstd_b])
    P.op("dve", (lambda e: e.reciprocal(out=C.rstd[:], in_=C.rstd[:])), reads=[C.rstd_b], writes=[C.rstd_b])


def ffn_plan(w_in, w_out):
    specs = []
    for g in range(FC // 2):
        specs.append(w_panel(w_in, 0, DC, [(g * 256, 256)]))
        specs.append(w_panel(w_in, 0, DC, [(DFF + g * 256, 256)]))
    for c2 in range(DC // 2):
        for kp in range(3):
            r0 = kp * 16
            nr = min(16, FC - r0)
            specs.append(w_panel(w_out, r0, nr, [(c2 * 256, 256)]))
    return specs


def emit_ffn(C, wbase, x_dram, xo_dram, t0, gpre, gpost, cb):
    P = C.P
    (xd, xdb), (xo, xob) = x_dram, xo_dram
    xT = C.R1[:, :].rearrange("p (c t) -> p c t", t=NT)
    xTb = Buf("xT")
    hT = C.R2[:, :].bitcast(BF16).rearrange("p (c t) -> p c t", t=NT)
    hTb = Buf("hT")
    gT = C.R3[:, :].bitcast(BF16).rearrange("p (c t) -> p c t", t=NT)
    gTb = [Buf(f"gT{j}") for j in range(FC)]
    C.claim("R1", [xTb]); C.claim("R2", [hTb]); C.claim("R3", gTb)
    xsrc = xd[:, t0:t0 + NT].rearrange("(c p) t -> p c t", p=128)
    P.dma("sp", lambda e: e.dma_start(out=xT, in_=xsrc), reads=[xdb], writes=[xTb], lane="ld0")
    emit_rstd(C, lambda c: (xT[:, c, :], xTb), DC, D, 0)
    for c in range(DC):
        P.op("dve", (lambda e, c=c: e.scalar_tensor_tensor(out=hT[:, c, :], in0=xT[:, c, :], scalar=gpre[:, c:c + 1],
                                                          in1=C.rstd[:], op0=ALU.mult, op1=ALU.mult)),
             reads=[xTb, C.rstd_b, cb], writes=[hTb])
    wi = wbase
    for g in range(FC // 2):
        base = 4 * (g % 2)
        wa, wab = C.wget(wi); wi += 1
        for cc in range(2):
            bk = base + cc
            for k in range(DC):
                P.op("pe", (lambda e, o=C.ps[bk][:], l=wa[:, k, cc * 128:(cc + 1) * 128], r=hT[:, k, :], st=(k == 0), sp=(k == DC - 1):
                            e.matmul(o, l, r, start=st, stop=sp)),
                     reads=[wab, hTb], writes=[C.ps_b[bk]])
        wb_, wbb = C.wget(wi); wi += 1
        for cc in range(2):
            bk = base + 2 + cc
            for k in range(DC):
                P.op("pe", (lambda e, o=C.ps[bk][:], l=wb_[:, k, cc * 128:(cc + 1) * 128], r=hT[:, k, :], st=(k == 0), sp=(k == DC - 1):
                            e.matmul(o, l, r, start=st, stop=sp)),
                     reads=[wbb, hTb], writes=[C.ps_b[bk]])
        for cc in range(2):
            j = 2 * g + cc
            k2 = C.cnt % 2
            C.cnt += 1
            P.op("act", (lambda e, o=C.tmpa[k2][:], i_=C.ps[base + cc][:]: e.activation(out=o, in_=i_, func=AF.Silu)),
                 reads=[C.ps_b[base + cc]], writes=[C.tmpa_b[k2]])
            P.op("dve", (lambda e, o=gT[:, j, :], a=C.tmpa[k2][:], b=C.ps[base + 2 + cc][:]:
                         e.tensor_tensor(out=o, in0=a, in1=b, op=ALU.mult)),
                 reads=[C.tmpa_b[k2], C.ps_b[base + 2 + cc]], writes=[gTb[j]])
    wi = emit_outproj(C, wi, gT, lambda f: gTb[f], FC, (xd, xdb), (xo, xob), t0, gpost, 0.5, cb, xT, xTb, hTb)
    return wi


def outproj_plan(w_out, K):
    specs = []
    for c2 in range(DC // 2):
        for r0 in range(0, K, 16):
            specs.append(w_panel(w_out, r0, min(16, K - r0), [(c2 * 256, 256)]))
    return specs


def emit_outproj(C, wi, inT, inb, K, x_dram, xo_dram, t0, gpost, alpha, cb, fT, fTb, r2b):
    P = C.P
    (xd, xdb), (xo, xob) = x_dram, xo_dram
    SB = 6
    for c2 in range(DC // 2):
        base = 2 * (c2 % 2)
        for r0 in range(0, K, 16):
            nr = min(16, K - r0)
            wo, wob = C.wget(wi); wi += 1
            for cc in range(2):
                bk = base + cc
                for r in range(nr):
                    f = r0 + r
                    mm(P, C.ps[bk][:], wo[:, r, cc * 128:(cc + 1) * 128], inT[:, f, :], f == 0, f == K - 1, [wob, inb(f)], [C.ps_b[bk]])
        for cc in range(2):
            c = 2 * c2 + cc
            bk = base + cc
            P.op("act", (lambda e, o=fT[:, c, :], i_=C.ps[bk][:]: e.activation(out=o, in_=i_, func=AF.Copy)),
                 reads=[C.ps_b[bk]], writes=[fTb])
            k2 = C.cnt % 2
            C.cnt += 1
            P.op("dve", (lambda e, o=C.sq[k2][:], i_=C.ps[bk][:], f_=fT[:, c, :]: e.tensor_tensor(out=o, in0=i_, in1=f_, op=ALU.mult)),
                 reads=[C.ps_b[bk], fTb], writes=[C.sq_b[k2]])
            mm(P, C.ps[SB][:], C.ones[:], C.sq[k2][:], c == 0, c == DC - 1, [C.ones_b, C.sq_b[k2]], [C.ps_b[SB]])
    P.op("act", (lambda e: e.activation(out=C.rstd[:], in_=C.ps[SB][:], func=AF.Sqrt, bias=C.epsc[:], scale=1.0 / D)),
         reads=[C.ps_b[SB], C.epsc_b], writes=[C.rstd_b])
    P.op("dve", (lambda e: e.reciprocal(out=C.rstd[:], in_=C.rstd[:])), reads=[C.rstd_b], writes=[C.rstd_b])
    xr = C.R2[:, :].rearrange("p (c t) -> p c t", t=NT)
    for h in range(2):
        xs = xd[h * 1024:(h + 1) * 1024, t0:t0 + NT].rearrange("(c p) t -> p c t", p=128)
        P.dma("sp", (lambda e, s_=xs: e.dma_start(out=xr, in_=s_)), reads=[xdb], writes=[r2b], lane="ld1")
        for cl in range(8):
            c = h * 8 + cl
            P.op("dve", (lambda e, c=c: e.scalar_tensor_tensor(out=fT[:, c, :], in0=fT[:, c, :], scalar=gpost[:, c:c + 1],
                                                              in1=C.rstd[:], op0=ALU.mult, op1=ALU.mult)),
                 reads=[fTb, C.rstd_b, cb], writes=[fTb])
            P.op("dve", (lambda e, c=c, cl=cl: e.scalar_tensor_tensor(out=fT[:, c, :], in0=fT[:, c, :], scalar=alpha,
                                                                       in1=xr[:, cl, :], op0=ALU.mult, op1=ALU.add)),
                 reads=[fTb, r2b], writes=[fTb])
    xdst = xo[:, t0:t0 + NT].rearrange("(c p) t -> p c t", p=128)
    P.dma("sp", lambda e: e.dma_start(out=xdst, in_=fT), reads=[fTb], writes=[xob], lane="st0")
    return wi


AW = 1024
NEG = -1.0e30
GELU_C = 0.7978845608028654 * 2.0
LN_EPS = 1e-5
O_ZU, O_ZV, O_Q, O_K, O_V, O_QI, O_KI, O_WI = 0, 1024, 2048, 3072, 4096, 5120, 6144, 6208


def mm(P, o, l, r, st, sp, reads, writes):
    P.op("pe", (lambda e: e.matmul(o, l, r, start=st, stop=sp)), reads=reads, writes=writes)


def emit_gelu(C, out_ap, in_ap, inb, outb, shape):
    P = C.P
    t1 = C.gt1[:, 0:shape]
    t2 = C.gt2[:, 0:shape]
    P.op("act", (lambda e: e.activation(out=t1, in_=in_ap, func=AF.Square)), reads=[inb], writes=[C.gt1_b])
    P.op("dve", (lambda e: e.tensor_scalar(out=t1, in0=t1, scalar1=0.044715, scalar2=1.0, op0=ALU.mult, op1=ALU.add)),
         reads=[C.gt1_b], writes=[C.gt1_b])
    P.op("dve", (lambda e: e.tensor_tensor(out=t1, in0=t1, in1=in_ap, op=ALU.mult)), reads=[C.gt1_b, inb], writes=[C.gt1_b])
    P.op("act", (lambda e: e.activation(out=t2, in_=t1, func=AF.Sigmoid, scale=GELU_C)), reads=[C.gt1_b], writes=[C.gt2_b])
    P.op("dve", (lambda e: e.tensor_tensor(out=out_ap, in0=t2, in1=in_ap, op=ALU.mult)), reads=[C.gt2_b, inb], writes=[outb])


def mixa_plan(w_in, w_gate, w_ba):
    specs = []
    for seg in (O_ZU, O_Q, O_K, O_QI):
        for c2 in range(4):
            specs.append(w_panel(w_in, 0, DC, [(seg + c2 * 256, 256)]))
    specs.append(w_panel(w_in, 0, DC, [(O_KI, 64), (O_KI, 64)]))
    for seg in (O_ZV, O_V):
        for c2 in range(4):
            specs.append(w_panel(w_in, 0, DC, [(seg + c2 * 256, 256)]))
    specs.append(w_panel(w_in, 0, DC, [(O_WI, 16)]))
    for c2 in range(8):
        specs.append(w_panel(w_ba, 0, 8, [(c2 * 256, 256)]))
        specs.append(w_panel(w_gate, 0, DC, [(c2 * 256, 256)]))
    for c2 in range(8):
        specs.append(w_panel(w_gate, 0, DC, [(D + c2 * 256, 256)]))
    return specs


class MixRes:
    pass


def emit_mix_consts(C, bro_d, wsT_d, M):
    P = C.P
    C.bro_b = Buf("bro"); C.wsf_b = Buf("wsf")
    C.claim("R4", [C.bro_b, C.wsf_b])
    P.dma("sp", lambda e: e.dma_start(out=C.bro, in_=bro_d), reads=[], writes=[C.bro_b], lane="ldc")
    wv = C.wsf.rearrange("p (g t) -> p g t", g=8)
    P.dma("sp", lambda e: e.dma_start(out=wv, in_=wsT_d), reads=[], writes=[C.wsf_b], lane="ldc")
    P.op("pool", (lambda e: e.affine_select(out=wv, in_=wv, pattern=[[0, 8], [1, 128]], compare_op=ALU.is_ge,
                                             fill=fillreg(e, 0.0), base=0, channel_multiplier=-1)),
         reads=[C.wsf_b], writes=[C.wsf_b])
    P.op("pool", (lambda e: e.tensor_copy(out=C.wsb[:], in_=wv)), reads=[C.wsf_b], writes=[C.wsb_b])


def emit_mixa(C, wbase, x1, t0, S, cs, cb, dr):
    P = C.P
    xd, xdb = x1
    xT = C.R1[:, :].rearrange("p (c t) -> p c t", t=NT)
    xTb = Buf("xT")
    hT = C.R2[:, :].bitcast(BF16).rearrange("p (c t) -> p c t", t=NT)
    hTb = Buf("hT")
    r3 = C.R3[:, :]
    zv = r3[:, 0:4096].rearrange("p (b c) -> p b c", c=1024)
    zvb = Buf("zv")
    vln = r3[:, 4096:6144].bitcast(BF16).rearrange("p (b c) -> p b c", c=1024)
    vlnb = Buf("vln")
    guT = r3[:, 6144:8192].bitcast(BF16).rearrange("p (c t) -> p c t", t=NT)
    guTb = Buf("guT")
    yaT = r3[:, 8192:10240].bitcast(BF16).rearrange("p (c t) -> p c t", t=NT)
    yaTb = Buf("yaT")
    gpre = cs[:, 32:48]
    C.claim("R1", [xTb]); C.claim("R2", [hTb]); C.claim("R3", [zvb, vlnb, guTb, yaTb])
    xsrc = xd[:, t0:t0 + NT].rearrange("(c p) t -> p c t", p=128)
    P.dma("sp", lambda e: e.dma_start(out=xT, in_=xsrc), reads=[xdb], writes=[xTb], lane="ld0")
    emit_rstd(C, lambda c: (xT[:, c, :], xTb), DC, D, 0)
    for c in range(DC):
        P.op("dve", (lambda e, c=c: e.scalar_tensor_tensor(out=hT[:, c, :], in0=xT[:, c, :], scalar=gpre[:, c:c + 1],
                                                          in1=C.rstd[:], op0=ALU.mult, op1=ALU.mult)),
             reads=[xTb, C.rstd_b, cb], writes=[hTb])
    osb = [C.R1[:, i * 2048:(i + 1) * 2048].bitcast(BF16).rearrange("p (c t) -> p c t", t=NT) for i in range(2)]
    osbb = [Buf("osb0"), Buf("osb1")]
    och = [C.R1[:, 4096 + i * 512: 4096 + (i + 1) * 512] for i in range(8)]
    ochb = [Buf(f"och{i}") for i in range(8)]
    C.claim("R1", osbb + ochb)
    wi = wbase
    bankrr = [0]

    def nb():
        b = bankrr[0] % 8
        bankrr[0] += 1
        return b

    first = [True]

    def r1dep():
        return [xTb]

    dests = {O_Q: ("qT", 0), O_K: ("kT", 1), O_QI: ("qiT", 0)}
    for seg in (O_ZU, O_Q, O_K, O_QI):
        if seg != O_ZU:
            dn, oi = dests[seg]
            ob, obb = osb[oi], osbb[oi]
        for c2 in range(4):
            w, wb = C.wget(wi); wi += 1
            for cc in range(2):
                ch = 2 * c2 + cc
                bk = nb()
                for k in range(DC):
                    mm(P, C.ps[bk][:], w[:, k, cc * 128:(cc + 1) * 128], hT[:, k, :], k == 0, k == DC - 1, [wb, hTb], [C.ps_b[bk]])
                if seg == O_ZU:
                    emit_gelu(C, guT[:, ch, :], C.ps[bk][:], C.ps_b[bk], guTb, 512)
                else:
                    P.op("act", (lambda e, o=ob[:, ch, :], i_=C.ps[bk][:]: e.activation(out=o, in_=i_, func=AF.Copy)),
                         reads=[C.ps_b[bk]], writes=[obb])
        if seg != O_ZU:
            dd, ddb = dr[dn]
            dst = dd[:, t0:t0 + NT].rearrange("(c p) t -> p c t", p=128)
            P.dma("sp", (lambda e, d_=dst, s_=ob: e.dma_start(out=d_, in_=s_)), reads=[obb], writes=[ddb], lane="st1")
    w, wb = C.wget(wi); wi += 1
    bk = nb()
    for k in range(DC):
        mm(P, C.ps[bk][:], w[:, k, 0:128], hT[:, k, :], k == 0, k == DC - 1, [wb, hTb], [C.ps_b[bk]])
    P.op("act", (lambda e, o=C.kio[:], i_=C.ps[bk][:]: e.activation(out=o, in_=i_, func=AF.Copy)), reads=[C.ps_b[bk]], writes=[C.kio_b])
    dd, ddb = dr["kiT"]
    P.dma("sp", (lambda e, d_=dd[:, t0:t0 + NT]: e.dma_start(out=d_, in_=C.kio[:])), reads=[C.kio_b], writes=[ddb], lane="st2")
    vsb = osb[0].rearrange("p c t -> p (c t)").rearrange("p (b c) -> p b c", c=1024)
    for seg in (O_ZV, O_V):
        for c2 in range(4):
            w, wb = C.wget(wi); wi += 1
            for tb in range(4):
                bk = nb()
                for k in range(DC):
                    mm(P, C.ps[bk][:, 0:256], hT[:, k, tb * 128:(tb + 1) * 128], w[:, k, :], k == 0, k == DC - 1, [wb, hTb], [C.ps_b[bk]])
                if seg == O_ZV:
                    P.op("act", (lambda e, o=zv[:, tb, c2 * 256:(c2 + 1) * 256], i_=C.ps[bk][:, 0:256]: e.activation(out=o, in_=i_, func=AF.Copy)),
                         reads=[C.ps_b[bk]], writes=[zvb])
                else:
                    P.op("act", (lambda e, o=vsb[:, tb, c2 * 256:(c2 + 1) * 256], i_=C.ps[bk][:, 0:256]: e.activation(out=o, in_=i_, func=AF.Copy)),
                         reads=[C.ps_b[bk]], writes=[osbb[0]])
    dd, ddb = dr["v"]
    P.dma("sp", (lambda e, d_=dd[t0:t0 + NT, :].rearrange("(b p) c -> p b c", p=128): e.dma_start(out=d_, in_=vsb)),
          reads=[osbb[0]], writes=[ddb], lane="st1")
    w, wb = C.wget(wi); wi += 1
    bk = nb()
    for tb in range(4):
        for k in range(DC):
            mm(P, C.ps[bk][:, tb * 16:(tb + 1) * 16], hT[:, k, tb * 128:(tb + 1) * 128], w[:, k, :], k == 0, k == DC - 1, [wb, hTb], [C.ps_b[bk]])
    P.op("act", (lambda e, i_=C.ps[bk][:, 0:64]: e.activation(out=C.wio[:], in_=i_, func=AF.Copy)), reads=[C.ps_b[bk]], writes=[C.wio_b])
    dd, ddb = dr["widx"]
    P.dma("sp", (lambda e, d_=dd[t0:t0 + NT, :].rearrange("(b p) c -> p b c", p=128): e.dma_start(out=d_, in_=C.wio[:].rearrange("p (b c) -> p b c", c=16))),
          reads=[C.wio_b], writes=[ddb], lane="st2")
    lng = C.bro[:, 0:1024]
    lnb = C.bro[:, 1024:2048]
    for tb in range(4):
        for h2 in range(2):
            emit_gelu(C, zv[:, tb, h2 * 512:(h2 + 1) * 512], zv[:, tb, h2 * 512:(h2 + 1) * 512], zvb, zvb, 512)
        P.op("dve", (lambda e, i_=zv[:, tb, :]: e.bn_stats(out=C.bst[:, 0:6], in_=i_[:, 0:512])), reads=[zvb], writes=[C.bst_b])
        P.op("dve", (lambda e, i_=zv[:, tb, :]: e.bn_stats(out=C.bst[:, 6:12], in_=i_[:, 512:1024])), reads=[zvb], writes=[C.bst_b])
        P.op("dve", (lambda e: e.bn_aggr(out=C.bag[:], in_=C.bst[:].rearrange("p (a b) -> p a b", b=6))), reads=[C.bst_b], writes=[C.bag_b])
        P.op("act", (lambda e: e.activation(out=C.lrs[:], in_=C.bag[:, 1:2], func=AF.Sqrt, bias=C.lneps[:], scale=1.0)),
             reads=[C.bag_b, C.lneps_b], writes=[C.lrs_b])
        P.op("dve", (lambda e: e.reciprocal(out=C.lrs[:], in_=C.lrs[:])), reads=[C.lrs_b], writes=[C.lrs_b])
        P.op("dve", (lambda e, o=zv[:, tb, :]: e.tensor_scalar(out=o, in0=o, scalar1=C.bag[:, 0:1], scalar2=C.lrs[:], op0=ALU.subtract, op1=ALU.mult)),
             reads=[zvb, C.bag_b, C.lrs_b], writes=[zvb])
        P.op("dve", (lambda e, o=zv[:, tb, :]: e.tensor_tensor(out=o, in0=o, in1=lng, op=ALU.mult)), reads=[zvb, C.bro_b], writes=[zvb])
        P.op("dve", (lambda e, o=vln[:, tb, :], i_=zv[:, tb, :]: e.tensor_tensor(out=o, in0=i_, in1=lnb, op=ALU.add)), reads=[zvb, C.bro_b], writes=[vlnb])
    for g in range(8):
        bk = nb()
        for tb in range(4):
            mm(P, C.ps[bk][:, tb * 128:(tb + 1) * 128], vln[:, tb, g * 128:(g + 1) * 128], C.wsb[:, g, :], True, True, [vlnb, C.wsb_b], [C.ps_b[bk]])
        for tb in range(4):
            P.op("dve", (lambda e, o=C.gt1[:, tb * 128:(tb + 1) * 128], i_=C.ps[bk][:, tb * 128:(tb + 1) * 128], b_=C.bro[:, 2048 + g * 128:2048 + (g + 1) * 128]:
                         e.tensor_tensor(out=o, in0=i_, in1=b_, op=ALU.add)),
                 reads=[C.ps_b[bk], C.bro_b], writes=[C.gt1_b])
        P.op("dve", (lambda e, o=yaT[:, g, :], g_=guT[:, g, :]: e.tensor_tensor(out=o, in0=C.gt1[:], in1=g_, op=ALU.mult)),
             reads=[C.gt1_b, guTb], writes=[yaTb])
    if "dbg_ya" in dr:
        P.dma("sp", (lambda e, d_=dr["dbg_ya"][0][:, t0:t0 + NT].rearrange("(c p) t -> p c t", p=128): e.dma_start(out=d_, in_=yaT)), reads=[yaTb], writes=[dr["dbg_ya"][1]], lane="dbg")
        P.dma("sp", (lambda e, d_=dr["dbg_gu"][0][:, t0:t0 + NT].rearrange("(c p) t -> p c t", p=128): e.dma_start(out=d_, in_=guT)), reads=[guTb], writes=[dr["dbg_gu"][1]], lane="dbg")
        P.dma("sp", (lambda e, d_=dr["dbg_vln"][0][t0:t0 + NT, :].rearrange("(b p) c -> p b c", p=128): e.dma_start(out=d_, in_=vln)), reads=[vlnb], writes=[dr["dbg_vln"][1]], lane="dbg")
    ocnt = [0]
    for c2 in range(8):
        wa, wab = C.wget(wi); wi += 1
        wg, wgb = C.wget(wi); wi += 1
        for cc in range(2):
            ch = 2 * c2 + cc
            bka = nb()
            for k in range(8):
                mm(P, C.ps[bka][:], wa[:, k, cc * 128:(cc + 1) * 128], yaT[:, k, :], k == 0, k == 7, [wab, yaTb], [C.ps_b[bka]])
            bkg = nb()
            for k in range(DC):
                mm(P, C.ps[bkg][:], wg[:, k, cc * 128:(cc + 1) * 128], hT[:, k, :], k == 0, k == DC - 1, [wgb, hTb], [C.ps_b[bkg]])
            k2 = C.cnt % 2
            C.cnt += 1
            P.op("act", (lambda e, o=C.tmpa[k2][:], i_=C.ps[bkg][:]: e.activation(out=o, in_=i_, func=AF.Sigmoid)),
                 reads=[C.ps_b[bkg]], writes=[C.tmpa_b[k2]])
            oi = ocnt[0] % 8
            ocnt[0] += 1
            P.op("dve", (lambda e, o=och[oi], a=C.tmpa[k2][:], b=C.ps[bka][:]: e.tensor_tensor(out=o, in0=a, in1=b, op=ALU.mult)),
                 reads=[C.tmpa_b[k2], C.ps_b[bka]], writes=[ochb[oi]])
            dd, ddb = dr["apart"]
            P.dma("sp", (lambda e, d_=dd[ch * 128:(ch + 1) * 128, t0:t0 + NT], s_=och[oi]: e.dma_start(out=d_, in_=s_)),
                  reads=[ochb[oi]], writes=[ddb], lane=f"so{oi}")
    for c2 in range(8):
        wg, wgb = C.wget(wi); wi += 1
        for cc in range(2):
            ch = 2 * c2 + cc
            bkg = nb()
            for k in range(DC):
                mm(P, C.ps[bkg][:], wg[:, k, cc * 128:(cc + 1) * 128], hT[:, k, :], k == 0, k == DC - 1, [wgb, hTb], [C.ps_b[bkg]])
            oi = ocnt[0] % 8
            ocnt[0] += 1
            P.op("act", (lambda e, o=och[oi], i_=C.ps[bkg][:]: e.activation(out=o, in_=i_, func=AF.Sigmoid)),
                 reads=[C.ps_b[bkg]], writes=[ochb[oi]])
            dd, ddb = dr["gb"]
            P.dma("sp", (lambda e, d_=dd[ch * 128:(ch + 1) * 128, t0:t0 + NT], s_=och[oi]: e.dma_start(out=d_, in_=s_)),
                  reads=[ochb[oi]], writes=[ddb], lane=f"so{oi}")
    return wi


ATT_SCALE = 128 ** -0.5
TOPK = 256
BIS_IT = 28
BIS_MIN = 1024


def emit_attn_consts(C, toep_d, cb8_d):
    P = C.P
    tpf = C.R3[:, 0:2048].rearrange("p (h w) -> p h w", h=8)
    tb = Buf("tpf")
    C.claim("R3", [tb])
    P.dma("sp", lambda e: e.dma_start(out=C.cb8[:], in_=cb8_d), reads=[], writes=[C.cb8_b], lane="ldc")
    P.dma("sp", lambda e: e.dma_start(out=tpf, in_=toep_d.rearrange("p (h w) -> p h w", h=8)), reads=[], writes=[tb], lane="ldc")
    for h in range(8):
        P.op("dve", (lambda e, h=h: e.tensor_scalar(out=tpf[:, h, :], in0=tpf[:, h, :], scalar1=C.cb8[:, h:h + 1], scalar2=None, op0=ALU.subtract)),
             reads=[tb, C.cb8_b], writes=[tb])
    P.op("act", (lambda e: e.activation(out=C.Fb[:].rearrange("p (h w) -> p h w", h=8), in_=tpf, func=AF.Exp)), reads=[tb], writes=[C.Fb_b])
    P.op("pool", lambda e: e.memset(C.ident[:], 1.0), writes=[C.ident_b])
    P.op("pool", (lambda e: e.affine_select(out=C.ident[:], in_=C.ident[:], pattern=[[-1, 128]], compare_op=ALU.is_equal,
                                             fill=fillreg(e, 0.0), base=0, channel_multiplier=1)), reads=[C.ident_b], writes=[C.ident_b])
    P.op("pool", lambda e: e.memset(C.thrneg[:], -1.0e29), writes=[C.thrneg_b])
    P.op("pool", lambda e: e.memset(C.onec[:], 1.0), writes=[C.onec_b])


def mixb_tail_plan(w_bb, w_o):
    specs = []
    for c2 in range(8):
        specs.append(w_panel(w_bb, 0, 8, [(c2 * 256, 256)]))
    specs += outproj_plan(w_o, DC)
    return specs


def emit_mixb_tile(C, wbase, tt, S, x1, x2, cs, cb, dr):
    P = C.P
    t0 = tt * NT
    kh = [C.R1[:, i * 2048:(i + 1) * 2048].bitcast(BF16) for i in range(2)]
    khb = [Buf("kh0"), Buf("kh1")]
    ybT = C.R1[:, 4096:6144].bitcast(BF16).rearrange("p (c t) -> p c t", t=NT)
    ybTb = Buf("ybT")
    maskT = C.R1[:, 6144:8192].bitcast(BF16)
    maskTb = Buf("maskT")
    vh = [C.R2[:, i * 2048:(i + 1) * 2048].bitcast(BF16).rearrange("p (b c) -> p b c", c=128) for i in range(2)]
    vhb = [Buf("vh0"), Buf("vh1")]
    score = C.R3[:, 0:4096]
    scb = Buf("score")
    work = C.R3[:, 4096:8192]
    wkb = Buf("work")
    mask = C.R3[:, 8192:10240].bitcast(BF16)
    mkb = Buf("mask")
    C.claim("R1", khb + [ybTb, maskTb]); C.claim("R2", vhb); C.claim("R3", [scb, wkb, mkb])
    rr = [0]

    def nb():
        b = rr[0] % 6
        rr[0] += 1
        return b
    OB, TB = 6, 7
    psT = C.ps[TB][:].bitcast(BF16)
    kvc = [0]
    for qi_ in range(4):
        qb = tt * 4 + qi_
        q0 = qb * 128
        nkb = qb + 1
        Sc = nkb * 128
        ngrp = (nkb + 3) // 4
        P.dma("sp", (lambda e, s_=dr["qiT"][0][:, q0:q0 + 128].rearrange("(c p) t -> p c t", p=128): e.dma_start(out=C.qib[:], in_=s_)),
              reads=[dr["qiT"][1]], writes=[C.qib_b], lane="lq0")
        P.dma("sp", (lambda e, s_=dr["qT"][0][:, q0:q0 + 128].rearrange("(c p) t -> p c t", p=128): e.dma_start(out=C.qbk[:], in_=s_)),
              reads=[dr["qT"][1]], writes=[C.qbk_b], lane="lq1")
        P.dma("sp", (lambda e, s_=dr["widx"][0][q0:q0 + 128, :]: e.dma_start(out=C.wx[:], in_=s_)),
              reads=[dr["widx"][1]], writes=[C.wx_b], lane="lq2")
        P.op("act", (lambda e: e.activation(out=C.wabs[:], in_=C.wx[:], func=AF.Abs)), reads=[C.wx_b], writes=[C.wabs_b])
        P.op("act", (lambda e: e.activation(out=C.wsg[:], in_=C.wx[:], func=AF.Sign)), reads=[C.wx_b], writes=[C.wsg_b])
        for grp in range(ngrp):
            c0 = grp * 512
            n = min(Sc, c0 + 512) - c0
            for h in range(16):
                c, half = h // 2, h % 2
                rows = slice(half * 64, half * 64 + 64)
                bk = nb()
                mm(P, C.ps[bk][:, 0:n], C.qib[rows, c, :], C.kis[rows, c0:c0 + n], True, True, [C.qib_b, C.kis_b], [C.ps_b[bk]])
                k2 = C.cnt % 2
                C.cnt += 1
                P.op("act", (lambda e, o=C.tmpa[k2][:, 0:n], i_=C.ps[bk][:, 0:n], h=h: e.activation(out=o, in_=i_, func=AF.Relu, scale=C.wabs[:, h:h + 1])),
                     reads=[C.ps_b[bk], C.wabs_b], writes=[C.tmpa_b[k2]])
                if h == 0:
                    P.op("dve", (lambda e, o=score[:, c0:c0 + n], i_=C.tmpa[k2][:, 0:n]: e.tensor_scalar(out=o, in0=i_, scalar1=C.wsg[:, 0:1], scalar2=None, op0=ALU.mult)),
                         reads=[C.tmpa_b[k2], C.wsg_b], writes=[scb])
                else:
                    P.op("dve", (lambda e, o=score[:, c0:c0 + n], i_=C.tmpa[k2][:, 0:n], h=h: e.scalar_tensor_tensor(out=o, in0=i_, scalar=C.wsg[:, h:h + 1], in1=o, op0=ALU.mult, op1=ALU.add)),
                         reads=[C.tmpa_b[k2], C.wsg_b, scb], writes=[scb])
        dsl = score[:, (nkb - 1) * 128:nkb * 128]
        P.op("pool", (lambda e, d_=dsl: e.affine_select(out=d_, in_=d_, pattern=[[-1, 128]], compare_op=ALU.is_ge, fill=fillreg(e, NEG), base=0, channel_multiplier=1)),
             reads=[scb], writes=[scb])
        if Sc > TOPK:
            for r in range(TOPK // 8):
                src = score[:, 0:Sc] if r == 0 else work[:, 0:Sc]
                srcb = scb if r == 0 else wkb
                P.op("dve", (lambda e, s_=src: e.max(out=C.m8[:], in_=s_)), reads=[srcb], writes=[C.m8_b])
                if r < TOPK // 8 - 1:
                    P.op("dve", (lambda e, s_=src, w_=work[:, 0:Sc]: e.match_replace(out=w_, in_to_replace=C.m8[:], in_values=s_, imm_value=NEG)),
                         reads=[srcb, C.m8_b], writes=[wkb])
            thr, thrb = C.m8[:, 7:8], C.m8_b
        else:
            thr, thrb = C.thrneg[:], C.thrneg_b
        P.op("dve", (lambda e, t_=thr, m_=mask[:, 0:Sc], s_=score[:, 0:Sc]: e.tensor_scalar(out=m_, in0=s_, scalar1=t_, scalar2=None, op0=ALU.is_ge)),
             reads=[scb, thrb], writes=[mkb])
        for grp in range(ngrp):
            kbs = list(range(grp * 4, min(nkb, grp * 4 + 4)))
            for j, kb in enumerate(kbs):
                P.op("pe", (lambda e, o=psT[:, j * 128:(j + 1) * 128], i_=mask[:, kb * 128:(kb + 1) * 128]: e.transpose(o, i_, C.ident[:])),
                     reads=[mkb, C.ident_b], writes=[C.ps_b[TB]])
            n = len(kbs) * 128
            P.op("act", (lambda e, o=maskT[:, grp * 512:grp * 512 + n], i_=psT[:, 0:n]: e.activation(out=o, in_=i_, func=AF.Copy)),
                 reads=[C.ps_b[TB]], writes=[maskTb])
        for h in range(8):
            s2 = kvc[0] % 2
            kvc[0] += 1
            P.dma("sp", (lambda e, o=kh[s2][:, 0:Sc], s_=dr["kT"][0][h * 128:(h + 1) * 128, 0:Sc]: e.dma_start(out=o, in_=s_)),
                  reads=[dr["kT"][1]], writes=[khb[s2]], lane=f"lk{s2}")
            P.dma("sp", (lambda e, o=vh[s2][:, 0:nkb, :], s_=dr["v"][0][0:Sc, h * 128:(h + 1) * 128].rearrange("(b p) c -> p b c", p=128): e.dma_start(out=o, in_=s_)),
                  reads=[dr["v"][1]], writes=[vhb[s2]], lane=f"lv{s2}")
            for grp in range(ngrp):
                kbs = list(range(grp * 4, min(nkb, grp * 4 + 4)))
                n = len(kbs) * 128
                bk = nb()
                for j, kb in enumerate(kbs):
                    mm(P, C.ps[bk][:, j * 128:(j + 1) * 128], kh[s2][:, kb * 128:(kb + 1) * 128], C.qbk[:, h, :], True, True, [khb[s2], C.qbk_b], [C.ps_b[bk]])
                k2 = C.cnt % 2
                C.cnt += 1
                P.op("act", (lambda e, o=C.tmpa[k2][:, 0:n], i_=C.ps[bk][:, 0:n], h=h: e.activation(out=o, in_=i_, func=AF.Exp, bias=C.cb8[:, h:h + 1], scale=ATT_SCALE)),
                     reads=[C.ps_b[bk], C.cb8_b], writes=[C.tmpa_b[k2]])
                P.op("dve", (lambda e, o=C.pm[k2][:, 0:n], a=C.tmpa[k2][:, 0:n], b=maskT[:, grp * 512:grp * 512 + n]: e.tensor_tensor(out=o, in0=a, in1=b, op=ALU.mult)),
                     reads=[C.tmpa_b[k2], maskTb], writes=[C.pm_b[k2]])
                for j, kb in enumerate(kbs):
                    w_ = nkb - 1 - kb
                    if w_ <= 1:
                        P.op("dve", (lambda e, o=C.pm[k2][:, j * 128:(j + 1) * 128], f_=C.Fb[:, (h * 2 + w_) * 128:(h * 2 + w_ + 1) * 128]: e.tensor_tensor(out=o, in0=o, in1=f_, op=ALU.mult)),
                             reads=[C.pm_b[k2], C.Fb_b], writes=[C.pm_b[k2]])
                for j, kb in enumerate(kbs):
                    P.op("pe", (lambda e, o=C.ps[OB][:, 0:128], l=C.pm[k2][:, j * 128:(j + 1) * 128], r=vh[s2][:, kb, :], st=(kb == 0), sp=(kb == nkb - 1):
                                e.matmul(o, l, r, start=st, stop=sp, skip_group_check=True)),
                         reads=[C.pm_b[k2], vhb[s2]], writes=[C.ps_b[OB]])
                    P.op("pe", (lambda e, o=C.ps[OB][:, 128:129], l=C.pm[k2][:, j * 128:(j + 1) * 128], sp=(kb == nkb - 1):
                                e.matmul(o, l, C.onec[:, 0:1], start=False, stop=sp, skip_group_check=True)),
                         reads=[C.pm_b[k2], C.onec_b], writes=[C.ps_b[OB]])
            P.op("dve", (lambda e: e.reciprocal(out=C.rc[:], in_=C.ps[OB][:, 128:129])), reads=[C.ps_b[OB]], writes=[C.rc_b])
            P.op("dve", (lambda e, o=C.yb[:, h * 128:(h + 1) * 128]: e.tensor_scalar(out=o, in0=C.ps[OB][:, 0:128], scalar1=C.rc[:], scalar2=None, op0=ALU.mult)),
                 reads=[C.ps_b[OB], C.rc_b], writes=[C.yb_b])
        for half in range(2):
            for j in range(4):
                c = half * 4 + j
                P.op("pe", (lambda e, o=psT[:, j * 128:(j + 1) * 128], i_=C.yb[:, c * 128:(c + 1) * 128]: e.transpose(o, i_, C.ident[:])),
                     reads=[C.yb_b, C.ident_b], writes=[C.ps_b[TB]])
            P.op("act", (lambda e, o=ybT[:, half * 4:half * 4 + 4, qi_ * 128:(qi_ + 1) * 128], i_=psT[:, 0:512].rearrange("p (c t) -> p c t", t=128):
                         e.activation(out=o, in_=i_, func=AF.Copy)),
                 reads=[C.ps_b[TB]], writes=[ybTb])
    mT = C.R2[:, :].bitcast(BF16).rearrange("p (c t) -> p c t", t=NT)
    mTb = Buf("mT")
    C.claim("R2", [mTb])
    wi = wbase
    lc = [0]
    for c2 in range(8):
        w, wb = C.wget(wi); wi += 1
        for cc in range(2):
            ch = 2 * c2 + cc
            bk = nb()
            for k in range(8):
                mm(P, C.ps[bk][:], w[:, k, cc * 128:(cc + 1) * 128], ybT[:, k, :], k == 0, k == 7, [wb, ybTb], [C.ps_b[bk]])
            k2 = lc[0] % 2
            lc[0] += 1
            P.dma("sp", (lambda e, o=C.gt1[:] if k2 == 0 else C.gt2[:], s_=dr["gb"][0][ch * 128:(ch + 1) * 128, t0:t0 + NT]: e.dma_start(out=o, in_=s_)),
                  reads=[dr["gb"][1]], writes=[C.gt1_b if k2 == 0 else C.gt2_b], lane=f"lg{k2}")
            P.dma("sp", (lambda e, o=C.tmpa[k2][:], s_=dr["apart"][0][ch * 128:(ch + 1) * 128, t0:t0 + NT]: e.dma_start(out=o, in_=s_)),
                  reads=[dr["apart"][1]], writes=[C.tmpa_b[k2]], lane=f"la{k2}")
            gbt, gbb = (C.gt1, C.gt1_b) if k2 == 0 else (C.gt2, C.gt2_b)
            P.op("dve", (lambda e, g_=gbt[:], i_=C.ps[bk][:]: e.tensor_tensor(out=g_, in0=g_, in1=i_, op=ALU.mult)),
                 reads=[gbb, C.ps_b[bk]], writes=[gbb])
            P.op("dve", (lambda e, o=mT[:, ch, :], g_=gbt[:], a=C.tmpa[k2][:]: e.tensor_tensor(out=o, in0=g_, in1=a, op=ALU.add)),
                 reads=[gbb, C.tmpa_b[k2]], writes=[mTb])
    fT = C.R1[:, :].rearrange("p (c t) -> p c t", t=NT)
    fTb = Buf("fT")
    C.claim("R1", [fTb])
    r2b = Buf("r2x")
    wi = emit_outproj_claim(C, wi, mT, mTb, x1, x2, t0, cs[:, 48:64], 1.0, cb, fT, fTb, r2b)
    return wi


def emit_outproj_claim(C, wi, mT, mTb, x1, x2, t0, gpost, alpha, cb, fT, fTb, r2b):
    class _Lazy:
        pass
    P = C.P
    orig_dma = P.dma
    state = {"claimed": False}

    def dma_hook(eng, fn, reads, writes, lane):
        if (not state["claimed"]) and r2b in writes:
            C.claim("R2", [r2b])
            state["claimed"] = True
        return orig_dma(eng, fn, reads, writes, lane)
    P.dma = dma_hook
    try:
        wi = emit_outproj(C, wi, mT, lambda f: mTb, DC, x1, x2, t0, gpost, alpha, cb, fT, fTb, r2b)
    finally:
        P.dma = orig_dma
    return wi


def _interleave(ga, gb_):
    la, lb = list(ga), None
    return la


def run_interleaved(gens_a, gens_b):
    na, nb_ = len(gens_a), len(gens_b)
    ia = ib = 0
    while ia < na or ib < nb_:
        fa = ia / na if na else 1.0
        fb = ib / nb_ if nb_ else 1.0
        if ia < na and (fa <= fb or ib >= nb_):
            gens_a[ia](); ia += 1
        else:
            gens_b[ib](); ib += 1


def emit_mixb_layer(C, wbase, NTL, S, x1, x2, cs, cb, dr):
    P = C.P
    NBLK = NTL * 4
    score = [C.R3[:, 0:4096], C.R4[:, 0:4096]]
    scb = [Buf("score0"), Buf("score1")]
    work = C.R3[:, 4096:8192]
    wkb = Buf("work")
    mask = C.R3[:, 8192:10240].bitcast(BF16)
    mkb = Buf("mask")
    C.claim("R3", [scb[0], wkb, mkb])
    C.claim("R4", [scb[1]])
    maskT = [C.mT0[:], C.mT1[:]]
    maskTb = [C.mT0_b, C.mT1_b]
    qib = [C.qib, C.qib2]; qibb = [C.qib_b, C.qib2_b]
    qbk = [C.qbk, C.qbk2]; qbkb = [C.qbk_b, C.qbk2_b]
    wabs = [C.wabs, C.wabs2]; wabsb = [C.wabs_b, C.wabs2_b]
    wsg = [C.wsg, C.wsg2]; wsgb = [C.wsg_b, C.wsg2_b]
    wx = [C.wx, C.wx2]; wxb = [C.wx_b, C.wx2_b]
    rr = [0]

    def nb():
        b = rr[0] % 5
        rr[0] += 1
        return b
    OB, TB, SCB = 6, 7, 5
    dg = [C.dg0, C.dg1]; dgb = [C.dg0_b, C.dg1_b]
    rlc = [0]
    psT = C.ps[TB][:].bitcast(BF16)
    kvc = [0]
    st = {}

    def phase_ab(qb):
        th = []
        p = qb % 2
        q0 = qb * 128
        nkb = qb + 1
        Sc = nkb * 128
        ngrp = (nkb + 3) // 4
        sc, scbp = score[p], scb[p]

        def loads():
            P.dma("sp", (lambda e, s_=dr["qiT"][0][:, q0:q0 + 128].rearrange("(c p) t -> p c t", p=128), o=qib[p][:]: e.dma_start(out=o, in_=s_)),
                  reads=[dr["qiT"][1]], writes=[qibb[p]], lane=f"lq0{p}")
            P.dma("sp", (lambda e, s_=dr["qT"][0][:, q0:q0 + 128].rearrange("(c p) t -> p c t", p=128), o=qbk[p][:]: e.dma_start(out=o, in_=s_)),
                  reads=[dr["qT"][1]], writes=[qbkb[p]], lane=f"lq1{p}")
            P.dma("sp", (lambda e, s_=dr["widx"][0][q0:q0 + 128, :], o=wx[p][:]: e.dma_start(out=o, in_=s_)),
                  reads=[dr["widx"][1]], writes=[wxb[p]], lane=f"lq2{p}")
            P.op("act", (lambda e, o=wabs[p][:], i_=wx[p][:]: e.activation(out=o, in_=i_, func=AF.Abs)), reads=[wxb[p]], writes=[wabsb[p]])
            P.op("act", (lambda e, o=wsg[p][:], i_=wx[p][:]: e.activation(out=o, in_=i_, func=AF.Sign)), reads=[wxb[p]], writes=[wsgb[p]])
            for h in range(16):
                P.op("pool", (lambda e, o=dg[p][:, h, :], s_=wsg[p][:, h:h + 1]: e.tensor_scalar(out=o, in0=C.ident[:], scalar1=s_, scalar2=1.0, op0=ALU.mult, op1=ALU.mult)),
                     reads=[C.ident_b, wsgb[p]], writes=[dgb[p]])
        th.append(loads)
        for grp in range(ngrp):
            c0 = grp * 512
            n = min(Sc, c0 + 512) - c0
            for h in range(16):
                def idx(grp=grp, c0=c0, n=n, h=h):
                    c, half = h // 2, h % 2
                    rows = slice(half * 64, half * 64 + 64)
                    bk = nb()
                    mm(P, C.ps[bk][:, 0:n], qib[p][rows, c, :], C.kis[rows, c0:c0 + n], True, True, [qibb[p], C.kis_b], [C.ps_b[bk]])
                    k2 = rlc[0] % 4
                    rlc[0] += 1
                    P.op("act", (lambda e, o=C.rl16[k2][:, 0:n], i_=C.ps[bk][:, 0:n], s_=wabs[p][:, h:h + 1]: e.activation(out=o, in_=i_, func=AF.Relu, scale=s_)),
                         reads=[C.ps_b[bk], wabsb[p]], writes=[C.rl16_b[k2]])
                    mm(P, C.ps[SCB][:, 0:n], dg[p][:, h, :], C.rl16[k2][:, 0:n], h == 0, h == 15, [dgb[p], C.rl16_b[k2]], [C.ps_b[SCB]])
                    if h == 15:
                        P.op("act", (lambda e, o=sc[:, c0:c0 + n], i_=C.ps[SCB][:, 0:n]: e.activation(out=o, in_=i_, func=AF.Copy)),
                             reads=[C.ps_b[SCB]], writes=[scbp])
                th.append(idx)

        if Sc >= BIS_MIN:
            def binit():
                P.op("dve", (lambda e, s_=sc[:, 0:Sc]: e.max(out=C.m8[:], in_=s_)), reads=[scbp], writes=[C.m8_b])
                P.op("dve", (lambda e, s_=sc[:, 0:Sc]: e.tensor_reduce(out=C.blo[:], in_=s_, axis=mybir.AxisListType.X, op=ALU.min)), reads=[scbp], writes=[C.blo_b])
                P.op("dve", (lambda e: e.tensor_tensor(out=C.brng[:], in0=C.m8[:, 0:1], in1=C.blo[:], op=ALU.subtract)), reads=[C.m8_b, C.blo_b], writes=[C.brng_b])
                P.op("dve", (lambda e: e.tensor_scalar(out=C.bstep[:], in0=C.pw2[:], scalar1=C.brng[:], scalar2=None, op0=ALU.mult)), reads=[C.pw2_b, C.brng_b], writes=[C.bstep_b])
            th.append(binit)

        def causal():
            dsl = sc[:, (nkb - 1) * 128:nkb * 128]
            P.op("pool", (lambda e, d_=dsl: e.affine_select(out=d_, in_=d_, pattern=[[-1, 128]], compare_op=ALU.is_ge, fill=fillreg(e, NEG), base=0, channel_multiplier=1)),
                 reads=[scbp], writes=[scbp])
        th.append(causal)
        use_bis = Sc >= BIS_MIN
        if use_bis:
            def bis_init():
                pass
            for k in range(BIS_IT):
                def it(k=k):
                    P.op("dve", (lambda e: e.tensor_tensor(out=C.bmid[:], in0=C.blo[:], in1=C.bstep[:, k:k + 1], op=ALU.add)),
                         reads=[C.blo_b, C.bstep_b], writes=[C.bmid_b])
                    P.op("dve", (lambda e, w_=work[:, 0:Sc], s_=sc[:, 0:Sc]: e.tensor_scalar(out=w_, in0=s_, scalar1=C.bmid[:], scalar2=None, op0=ALU.is_ge, op1=ALU.add, accum_out=C.bcnt[:])),
                         reads=[scbp, C.bmid_b], writes=[wkb, C.bcnt_b])
                    P.op("dve", (lambda e: e.tensor_scalar(out=C.bfs[:], in0=C.bcnt[:], scalar1=float(TOPK) - 0.5, scalar2=C.bstep[:, k:k + 1], op0=ALU.is_ge, op1=ALU.mult)),
                         reads=[C.bcnt_b, C.bstep_b], writes=[C.bfs_b])
                    P.op("dve", (lambda e: e.tensor_tensor(out=C.blo[:], in0=C.blo[:], in1=C.bfs[:], op=ALU.add)),
                         reads=[C.blo_b, C.bfs_b], writes=[C.blo_b])
                th.append(it)
        elif Sc > TOPK:
            for r in range(TOPK // 8):
                def rnd(r=r):
                    src = sc[:, 0:Sc] if r == 0 else work[:, 0:Sc]
                    srcb = scbp if r == 0 else wkb
                    P.op("dve", (lambda e, s_=src: e.max(out=C.m8[:], in_=s_)), reads=[srcb], writes=[C.m8_b])
                    if r < TOPK // 8 - 1:
                        P.op("dve", (lambda e, s_=src, w_=work[:, 0:Sc]: e.match_replace(out=w_, in_to_replace=C.m8[:], in_values=s_, imm_value=NEG)),
                             reads=[srcb, C.m8_b], writes=[wkb])
                th.append(rnd)

        def mk():
            if Sc >= BIS_MIN:
                thr, thrb = C.blo[:], C.blo_b
            elif Sc > TOPK:
                thr, thrb = C.m8[:, 7:8], C.m8_b
            else:
                thr, thrb = C.thrneg[:], C.thrneg_b
            P.op("dve", (lambda e, t_=thr, m_=mask[:, 0:Sc], s_=sc[:, 0:Sc]: e.tensor_scalar(out=m_, in0=s_, scalar1=t_, scalar2=None, op0=ALU.is_ge)),
                 reads=[scbp, thrb], writes=[mkb])
        th.append(mk)
        for grp in range(ngrp):
            def tr(grp=grp):
                kbs = list(range(grp * 4, min(nkb, grp * 4 + 4)))
                for j, kb in enumerate(kbs):
                    P.op("pe", (lambda e, o=psT[:, j * 128:(j + 1) * 128], i_=mask[:, kb * 128:(kb + 1) * 128]: e.transpose(o, i_, C.ident[:])),
                         reads=[mkb, C.ident_b], writes=[C.ps_b[TB]])
                n = len(kbs) * 128
                P.op("act", (lambda e, o=maskT[p][:, grp * 512:grp * 512 + n], i_=psT[:, 0:n]: e.activation(out=o, in_=i_, func=AF.Copy)),
                     reads=[C.ps_b[TB]], writes=[maskTb[p]])
            th.append(tr)
        return th

    def phase_c(qb):
        th = []
        p = qb % 2
        qi_ = qb % 4
        nkb = qb + 1
        Sc = nkb * 128
        ngrp = (nkb + 3) // 4
        kh, khb, vh, vhb, ybT, ybTb = st["kh"], st["khb"], st["vh"], st["vhb"], st["ybT"], st["ybTb"]
        for h in range(8):
            hs = {}

            def ld(h=h, hs=hs):
                s2 = kvc[0] % 2
                kvc[0] += 1
                hs["s2"] = s2
                P.dma("sp", (lambda e, o=kh[s2][:, 0:Sc], s_=dr["kT"][0][h * 128:(h + 1) * 128, 0:Sc]: e.dma_start(out=o, in_=s_)),
                      reads=[dr["kT"][1]], writes=[khb[s2]], lane=f"lk{s2}")
                P.dma("sp", (lambda e, o=vh[s2][:, 0:nkb, :], s_=dr["v"][0][0:Sc, h * 128:(h + 1) * 128].rearrange("(b p) c -> p b c", p=128): e.dma_start(out=o, in_=s_)),
                      reads=[dr["v"][1]], writes=[vhb[s2]], lane=f"lv{s2}")
            th.append(ld)
            for grp in range(ngrp):
                def at(h=h, grp=grp, hs=hs):
                    s2 = hs["s2"]
                    kbs = list(range(grp * 4, min(nkb, grp * 4 + 4)))
                    n = len(kbs) * 128
                    bk = nb()
                    for j, kb in enumerate(kbs):
                        mm(P, C.ps[bk][:, j * 128:(j + 1) * 128], kh[s2][:, kb * 128:(kb + 1) * 128], qbk[p][:, h, :], True, True, [khb[s2], qbkb[p]], [C.ps_b[bk]])
                    k2 = C.cnt % 2
                    C.cnt += 1
                    P.op("act", (lambda e, o=C.tmpa[k2][:, 0:n], i_=C.ps[bk][:, 0:n]: e.activation(out=o, in_=i_, func=AF.Exp, bias=C.cb8[:, h:h + 1], scale=ATT_SCALE)),
                         reads=[C.ps_b[bk], C.cb8_b], writes=[C.tmpa_b[k2]])
                    P.op("dve", (lambda e, o=C.pm[k2][:, 0:n], a=C.tmpa[k2][:, 0:n], b=maskT[p][:, grp * 512:grp * 512 + n]: e.tensor_tensor(out=o, in0=a, in1=b, op=ALU.mult)),
                         reads=[C.tmpa_b[k2], maskTb[p]], writes=[C.pm_b[k2]])
                    for j, kb in enumerate(kbs):
                        w_ = nkb - 1 - kb
                        if w_ <= 1:
                            P.op("dve", (lambda e, o=C.pm[k2][:, j * 128:(j + 1) * 128], f_=C.Fb[:, (h * 2 + w_) * 128:(h * 2 + w_ + 1) * 128]: e.tensor_tensor(out=o, in0=o, in1=f_, op=ALU.mult)),
                                 reads=[C.pm_b[k2], C.Fb_b], writes=[C.pm_b[k2]])
                    for j, kb in enumerate(kbs):
                        P.op("pe", (lambda e, o=C.ps[OB][:, 0:128], l=C.pm[k2][:, j * 128:(j + 1) * 128], r=vh[s2][:, kb, :], st_=(kb == 0), sp=(kb == nkb - 1):
                                    e.matmul(o, l, r, start=st_, stop=sp, skip_group_check=True)),
                             reads=[C.pm_b[k2], vhb[s2]], writes=[C.ps_b[OB]])
                        P.op("pe", (lambda e, o=C.ps[OB][:, 128:129], l=C.pm[k2][:, j * 128:(j + 1) * 128], sp=(kb == nkb - 1):
                                    e.matmul(o, l, C.onec[:, 0:1], start=False, stop=sp, skip_group_check=True)),
                             reads=[C.pm_b[k2], C.onec_b], writes=[C.ps_b[OB]])
                th.append(at)

            def fin(h=h):
                P.op("dve", (lambda e: e.reciprocal(out=C.rc[:], in_=C.ps[OB][:, 128:129])), reads=[C.ps_b[OB]], writes=[C.rc_b])
                P.op("dve", (lambda e, o=C.yb[:, h * 128:(h + 1) * 128]: e.tensor_scalar(out=o, in0=C.ps[OB][:, 0:128], scalar1=C.rc[:], scalar2=None, op0=ALU.mult)),
                     reads=[C.ps_b[OB], C.rc_b], writes=[C.yb_b])
            th.append(fin)
        for half in range(2):
            def ytr(half=half):
                for j in range(4):
                    c = half * 4 + j
                    P.op("pe", (lambda e, o=psT[:, j * 128:(j + 1) * 128], i_=C.yb[:, c * 128:(c + 1) * 128]: e.transpose(o, i_, C.ident[:])),
                         reads=[C.yb_b, C.ident_b], writes=[C.ps_b[TB]])
                P.op("act", (lambda e, o=ybT[:, half * 4:half * 4 + 4, qi_ * 128:(qi_ + 1) * 128], i_=psT[:, 0:512].rearrange("p (c t) -> p c t", t=128):
                             e.activation(out=o, in_=i_, func=AF.Copy)),
                     reads=[C.ps_b[TB]], writes=[ybTb])
            th.append(ytr)
        return th

    def open_tile():
        kh = [C.R1[:, i * 2048:(i + 1) * 2048].bitcast(BF16) for i in range(2)]
        khb = [Buf("kh0"), Buf("kh1")]
        ybT = C.R1[:, 4096:6144].bitcast(BF16).rearrange("p (c t) -> p c t", t=NT)
        ybTb = Buf("ybT")
        vh = [C.R2[:, i * 2048:(i + 1) * 2048].bitcast(BF16).rearrange("p (b c) -> p b c", c=128) for i in range(2)]
        vhb = [Buf("vh0"), Buf("vh1")]
        C.claim("R1", khb + [ybTb]); C.claim("R2", vhb)
        st.update(kh=kh, khb=khb, ybT=ybT, ybTb=ybTb, vh=vh, vhb=vhb)

    def tail(tt, wi):
        t0 = tt * NT
        ybT, ybTb = st["ybT"], st["ybTb"]
        mT = C.R2[:, :].bitcast(BF16).rearrange("p (c t) -> p c t", t=NT)
        mTb = Buf("mT")
        C.claim("R2", [mTb])
        lc = [0]
        for c2 in range(8):
            w, wb = C.wget(wi); wi += 1
            for cc in range(2):
                ch = 2 * c2 + cc
                bk = nb()
                for k in range(8):
                    mm(P, C.ps[bk][:], w[:, k, cc * 128:(cc + 1) * 128], ybT[:, k, :], k == 0, k == 7, [wb, ybTb], [C.ps_b[bk]])
                k2 = lc[0] % 2
                lc[0] += 1
                gbt, gbb = (C.gt1, C.gt1_b) if k2 == 0 else (C.gt2, C.gt2_b)
                P.dma("sp", (lambda e, o=gbt[:], s_=dr["gb"][0][ch * 128:(ch + 1) * 128, t0:t0 + NT]: e.dma_start(out=o, in_=s_)),
                      reads=[dr["gb"][1]], writes=[gbb], lane=f"lg{k2}")
                P.dma("sp", (lambda e, o=C.ta2[k2][:], s_=dr["apart"][0][ch * 128:(ch + 1) * 128, t0:t0 + NT]: e.dma_start(out=o, in_=s_)),
                      reads=[dr["apart"][1]], writes=[C.ta2_b[k2]], lane=f"la{k2}")
                P.op("dve", (lambda e, g_=gbt[:], i_=C.ps[bk][:]: e.tensor_tensor(out=g_, in0=g_, in1=i_, op=ALU.mult)),
                     reads=[gbb, C.ps_b[bk]], writes=[gbb])
                P.op("dve", (lambda e, o=mT[:, ch, :], g_=gbt[:], a=C.ta2[k2][:]: e.tensor_tensor(out=o, in0=g_, in1=a, op=ALU.add)),
                     reads=[gbb, C.ta2_b[k2]], writes=[mTb])
        fT = C.R1[:, :].rearrange("p (c t) -> p c t", t=NT)
        fTb = Buf("fT")
        C.claim("R1", [fTb])
        r2b = Buf("r2x")
        wi = emit_outproj_claim(C, wi, mT, mTb, x1, x2, t0, cs[:, 48:64], 1.0, cb, fT, fTb, r2b)
        return wi

    wi = wbase
    for f in phase_ab(0):
        f()
    for qb in range(NBLK):
        if qb % 4 == 0:
            open_tile()
        ca = phase_ab(qb + 1) if qb + 1 < NBLK else []
        cc_ = phase_c(qb)
        run_interleaved(ca, cc_)
        if qb % 4 == 3:
            wi = tail(qb // 4, wi)
    return wi


L_ = 2
NUM_BUCKETS = 32
MAX_DISTANCE = 128
IN_COLS = 6224


def build_program(S, depth):
    nc = bass.Bass("TRN2", target_bir_lowering=False)
    NTL = S // NT

    def din(name, shape, dt=F32):
        return nc.dram_tensor(name, list(shape), dt, kind="ExternalInput").ap()

    def dscr(name, shape, dt=F32):
        return nc.dram_tensor(name, list(shape), dt, kind="Internal").ap()
    x = din("x", [D, S])
    y = nc.dram_tensor("y", [D, S], F32, kind="ExternalOutput").ap()
    W = {}
    for l in range(depth):
        W[l] = dict(
            f1i=din(f"f1i{l}", [D, 2 * DFF]), f1o=din(f"f1o{l}", [DFF, D]),
            f2i=din(f"f2i{l}", [D, 2 * DFF]), f2o=din(f"f2o{l}", [DFF, D]),
            win=din(f"win{l}", [D, IN_COLS]), wg=din(f"wg{l}", [D, 2 * D]),
            wba=din(f"wba{l}", [AW, D]), wbb=din(f"wbb{l}", [AW, D]), wo=din(f"wo{l}", [D, D]),
            cpk=din(f"cpk{l}", [128, 96]), bro=din(f"bro{l}", [128, 3072]), wsT=din(f"wsT{l}", [128, 8, 128]),
        )
    toep = din("toep", [128, 2048])
    cb8d = din("cb8", [128, 8])
    xa = (dscr("xa", [D, S]), Buf("xa"))
    xb = (dscr("xb", [D, S]), Buf("xb"))
    xc = (dscr("xc", [D, S]), Buf("xc"))
    dr = {
        "qT": (dscr("qT", [AW, S], BF16), Buf("qT")), "qiT": (dscr("qiT", [AW, S], BF16), Buf("qiT")),
        "kT": (dscr("kT", [AW, S], BF16), Buf("kT")), "v": (dscr("v", [S, AW], BF16), Buf("v")),
        "kiT": (dscr("kiT", [128, S], BF16), Buf("kiT")), "widx": (dscr("widx", [S, 16]), Buf("widx")),
        "apart": (dscr("apart", [D, S]), Buf("apart")), "gb": (dscr("gb", [D, S]), Buf("gb")),
    }
    P = Prog(nc)
    C = Ctx(P)
    cs = [P.sbuf(f"consts{l}", [128, 96], F32) for l in range(depth)]
    cb = [Buf(f"consts{l}") for l in range(depth)]
    for l in range(depth):
        P.dma("sp", (lambda e, l=l: e.dma_start(out=cs[l][:], in_=W[l]["cpk"])), reads=[], writes=[cb[l]], lane="ldc")
    emit_attn_consts(C, toep, cb8d)
    for l in range(depth):
        w = W[l]
        for t in range(NTL):
            C.plan_extend(ffn_plan(w["f1i"], w["f1o"]))
        for t in range(NTL):
            C.plan_extend(mixa_plan(w["win"], w["wg"], w["wba"]))
        for t in range(NTL):
            C.plan_extend(mixb_tail_plan(w["wbb"], w["wo"]))
        for t in range(NTL):
            C.plan_extend(ffn_plan(w["f2i"], w["f2o"]))
    wi = 0
    xin = (x, Buf("x"))
    for l in range(depth):
        w = W[l]
        xout = (y, Buf("y")) if l == depth - 1 else xc
        for t in range(NTL):
            wi = emit_ffn(C, wi, xin, xa, t * NT, cs[l][:, 0:16], cs[l][:, 16:32], cb[l])
        emit_mix_consts(C, w["bro"], w["wsT"], None)
        for t in range(NTL):
            wi = emit_mixa(C, wi, xa, t * NT, S, cs[l], cb[l], dr)
        P.dma("sp", (lambda e: e.dma_start(out=C.kis[:, 0:S], in_=dr["kiT"][0])), reads=[dr["kiT"][1]], writes=[C.kis_b], lane="ldk")
        wi = emit_mixb_layer(C, wi, NTL, S, xa, xb, cs[l], cb[l], dr)
        for t in range(NTL):
            wi = emit_ffn(C, wi, xb, xout, t * NT, cs[l][:, 64:80], cs[l][:, 80:96], cb[l])
        xin = xc
    assert wi == len(C.plan), (wi, len(C.plan))
    P.finalize()
    return nc


def t5_bucket_np(n):
    max_exact = NUM_BUCKETS // 2
    nf = np.maximum(n, 1).astype(np.float32)
    large = max_exact + (np.log(nf / max_exact) / np.log(MAX_DISTANCE / max_exact) * (NUM_BUCKETS - max_exact)).astype(np.int32)
    large = np.minimum(large, NUM_BUCKETS - 1)
    return np.where(n < max_exact, n, large)


def host_prep(inp, depth):
    f = lambda a: np.ascontiguousarray(np.asarray(a, dtype=np.float32))
    m = {}

    def pc(v):
        return np.asarray(v, dtype=np.float32).reshape(16, 128).T
    for l in range(depth):
        m[f"f1i{l}"] = f(inp["ffn1_w_in"][l]); m[f"f1o{l}"] = f(inp["ffn1_w_out"][l])
        m[f"f2i{l}"] = f(inp["ffn2_w_in"][l]); m[f"f2o{l}"] = f(inp["ffn2_w_out"][l])
        m[f"win{l}"] = f(inp["w_in"][l]); m[f"wg{l}"] = f(inp["w_gate"][l])
        m[f"wba{l}"] = f(inp["w_branch_a"][l]); m[f"wbb{l}"] = f(inp["w_branch_b"][l]); m[f"wo{l}"] = f(inp["w_out"][l])
        m[f"cpk{l}"] = f(np.concatenate([pc(inp[k][l]) for k in ("ffn1_norm_pre", "ffn1_norm_post", "mix_norm_pre", "mix_norm_post", "ffn2_norm_pre", "ffn2_norm_post")], axis=1))
        row = np.concatenate([np.asarray(inp["sgu_ln_g"][l], np.float32), np.asarray(inp["sgu_ln_b"][l], np.float32), np.asarray(inp["sgu_b"][l], np.float32).reshape(-1)])
        m[f"bro{l}"] = f(np.broadcast_to(row[None, :], (128, 3072)))
        m[f"wsT{l}"] = f(np.transpose(np.asarray(inp["sgu_w_s"][l], np.float32), (2, 0, 1)))
    rb = np.asarray(inp["rel_bias"], np.float32)
    s_ = np.arange(128)[:, None]
    t_ = np.arange(128)[None, :]
    toep = np.zeros((128, 8, 2, 128), np.float32)
    for w_ in range(2):
        dist = np.maximum(t_ - s_ + 128 * w_, 0)
        bk = t5_bucket_np(dist)
        toep[:, :, w_, :] = np.transpose(rb[bk], (0, 2, 1))
    m["toep"] = f(toep.reshape(128, 2048))
    m["cb8"] = f(np.broadcast_to(rb[NUM_BUCKETS - 1][None, :], (128, 8)))
    return m


BATCH = 4
DEPTH = 2
_NC_CACHE = {}


def kernel(**inputs):
    inp = {k: np.asarray(v) for k, v in inputs.items()}
    S = inp["x"].shape[1]
    if "nc" not in _NC_CACHE:
        _NC_CACHE["nc"] = build_program(S, DEPTH)
    nc = _NC_CACHE["nc"]
    shared = host_prep(inp, DEPTH)
    in_maps = []
    for b in range(BATCH):
        m = dict(shared)
        m["x"] = np.ascontiguousarray(inp["x"][b].T.astype(np.float32))
        in_maps.append(m)
    res = run_bass_kernel_spmd(nc, in_maps, core_ids=list(range(BATCH)))
    out = np.stack([np.asarray(res.results[b]["y"]).T for b in range(BATCH)], axis=0)
    return np.ascontiguousarray(out.astype(np.float32))
```

```python
import numpy as np
from concourse.bass_utils import run_bass_kernel_spmd
from contextlib import ExitStack
import numpy as np
import concourse.bass as bass
import concourse.mybir as mybir

F32 = mybir.dt.float32
BF16 = mybir.dt.bfloat16
AF = mybir.ActivationFunctionType
ALU = mybir.AluOpType

ENGS = ("pe", "act", "dve", "pool", "sp")


class Buf:
    __slots__ = ("name", "lastw", "readers")

    def __init__(self, name):
        self.name = name
        self.lastw = None
        self.readers = {}


class Lane:
    def __init__(self, prog, name):
        self.sem = prog.nc.alloc_semaphore(name=name)
        self.count = 0
        self.last = None


class Op:
    __slots__ = ("eng", "fn", "waits", "flag", "val", "lane", "laneval", "idx")

    def __init__(self, eng, fn):
        self.eng = eng
        self.fn = fn
        self.waits = []
        self.flag = False
        self.val = None
        self.lane = None
        self.laneval = None


class Prog:
    def __init__(self, nc):
        self.nc = nc
        self.q = {e: [] for e in ENGS}
        self.sem = {e: nc.alloc_semaphore(name="sem_" + e) for e in ENGS}
        self.es = ExitStack()
        self.lanes = {}
        self.n_sb = 0

    def sbuf(self, name, shape, dtype):
        return self.es.enter_context(self.nc.sbuf_tensor(name, list(shape), dtype))

    def psum(self, name, shape, dtype=F32):
        return self.es.enter_context(self.nc.psum_tensor(name, list(shape), dtype))

    def lane(self, name):
        if name not in self.lanes:
            self.lanes[name] = Lane(self, "ln_" + name)
        return self.lanes[name]

    def _deps(self, op, reads, writes):
        evs = []
        for b in reads:
            if b.lastw is not None:
                evs.append(b.lastw)
        for b in writes:
            if b.lastw is not None:
                evs.append(b.lastw)
            evs.extend(b.readers.values())
        for ev in evs:
            if ev[0] == "c":
                src = ev[1]
                if src.eng == "pe" and op.eng == "pe":
                    continue
                if src is op:
                    continue
                src.flag = True
            op.waits.append(ev)

    def _post(self, ev, reads, writes):
        for b in writes:
            b.lastw = ev
            b.readers = {}
        key = ("c", ev[1].eng) if ev[0] == "c" else ("d", id(ev[1]))
        for b in reads:
            if b not in writes:
                b.readers[key] = ev

    def op(self, eng, fn, reads=(), writes=()):
        o = Op(eng, fn)
        self._deps(o, reads, writes)
        o.idx = len(self.q[eng])
        self.q[eng].append(o)
        self._post(("c", o), reads, writes)
        return o

    def dma(self, eng, fn, reads, writes, lane):
        ln = self.lane(lane) if isinstance(lane, str) else lane
        o = Op(eng, fn)
        self._deps(o, reads, writes)
        if ln.last is not None:
            o.waits.append(ln.last)
        ln.count += 16
        o.lane = ln
        o.laneval = ln.count
        ev = ("d", ln, ln.count)
        ln.last = ev
        self.q[eng].append(o)
        self._post(ev, reads, writes)
        return o

    def claim(self, old_bufs, new_bufs):
        merged = {}
        for b in old_bufs:
            evs = list(b.readers.values())
            if b.lastw is not None:
                evs.append(b.lastw)
            for ev in evs:
                key = ("c", ev[1].eng) if ev[0] == "c" else ("d", id(ev[1]))
                rank = ev[1].idx if ev[0] == "c" else ev[2]
                if key not in merged or merged[key][0] < rank:
                    merged[key] = (rank, ev)
        for nb_ in new_bufs:
            for key, (rank, ev) in merged.items():
                nb_.readers[key] = ev

    def finalize(self):
        nc = self.nc
        for e in ENGS:
            c = 0
            for o in self.q[e]:
                if o.flag:
                    c += 1
                    o.val = c
        engobj = {"pe": "tensor", "act": "scalar", "dve": "vector", "pool": "gpsimd", "sp": "sync"}
        final_lane_vals = [(ln.sem, ln.count) for ln in self.lanes.values() if ln.count > 0]
        final_eng_vals = {e: max([o.val for o in self.q[e] if o.flag] + [0]) for e in ENGS}
        prog = self

        def emit(e, eng):
            known = {}
            for o in prog.q[e]:
                need = {}
                for ev in o.waits:
                    if ev[0] == "c":
                        src = ev[1]
                        key = ("c", src.eng)
                        sem, val = prog.sem[src.eng], src.val
                    else:
                        key = ("d", id(ev[1]))
                        sem, val = ev[1].sem, ev[2]
                    if known.get(key, 0) >= val:
                        continue
                    if key not in need or need[key][1] < val:
                        need[key] = (sem, val)
                for key, (sem, val) in need.items():
                    eng.wait_ge(sem, val)
                    known[key] = val
                ins = o.fn(eng)
                if o.lane is not None:
                    ins.then_inc(o.lane.sem, 16)
                elif o.flag:
                    ins.then_inc(prog.sem[e], 1)
            if e == "sp":
                for sem, val in final_lane_vals:
                    eng.wait_ge(sem, val)
                for e2, v in final_eng_vals.items():
                    if v > 0:
                        eng.wait_ge(prog.sem[e2], v)

        with nc.Block() as block:
            @block.tensor
            def _(eng):
                emit("pe", eng)

            @block.scalar
            def _(eng):
                emit("act", eng)

            @block.vector
            def _(eng):
                emit("dve", eng)

            @block.gpsimd
            def _(eng):
                emit("pool", eng)

            @block.sync
            def _(eng):
                emit("sp", eng)
        self.es.close()


D = 2048
DC = D // 128
DFF = 5632
FC = DFF // 128
NT = 512
NORM_EPS = 1e-6
ATTN_PE = 1
LN_EPS = 1e-5
SEQ = 4096
CASTDMA = True


class Ctx:
    def __init__(self, P):
        self.P = P
        nc = P.nc
        self.ps = [P.psum(f"ps{i}", [128, 512]) for i in range(8)]
        self.ps_b = [Buf(f"ps{i}") for i in range(8)]
        self.NS = 4
        self.wbf = [P.sbuf(f"wbf{i}", [128, 4096], BF16) for i in range(self.NS)]
        self.wbf_b = [Buf(f"wbf{i}") for i in range(self.NS)]
        self.plan = []
        self.issued = 0
        self.R1 = P.sbuf("R1", [128, 8192], F32)
        self.R2 = P.sbuf("R2", [128, 4096], F32)
        self.R3 = P.sbuf("R3", [128, 11264], F32)
        self.ones = P.sbuf("ones", [128, 128], BF16)
        self.ones_b = Buf("ones")
        self.tmpa = [P.sbuf(f"tmpa{i}", [128, 512], F32) for i in range(2)]
        self.tmpa_b = [Buf(f"tmpa{i}") for i in range(2)]
        self.sq = [P.sbuf(f"sq{i}", [128, 512], BF16) for i in range(2)]
        self.sq_b = [Buf(f"sq{i}") for i in range(2)]
        self.rstd = P.sbuf("rstd", [128, 512], F32)
        self.rstd_b = Buf("rstd")
        self.epsc = P.sbuf("epsc", [128, 1], F32)
        self.epsc_b = Buf("epsc")
        self.cnt = 0
        self.reg = {"R1": [], "R2": [], "R3": []}
        def sb(name, shape, dt):
            setattr(self, name, P.sbuf("s_" + name, shape, dt))
            setattr(self, name + "_b", Buf(name))
        sb("kio", [128, 512], BF16); sb("wio", [128, 64], F32)
        sb("gt1", [128, 512], F32); sb("gt2", [128, 512], F32)
        self.R4 = P.sbuf("R4", [128, 4096], F32)
        self.reg["R4"] = []
        self.bro = self.R4[:, 0:3072]; self.bro_b = Buf("bro")
        self.wsf = self.R4[:, 3072:4096]; self.wsf_b = Buf("wsf")
        sb("wsb", [128, 8, 128], BF16)
        sb("mT0", [128, SEQ], BF16); sb("mT1", [128, SEQ], BF16)
        sb("qib2", [128, 8, 128], BF16); sb("qbk2", [128, 8, 128], BF16); sb("wx2", [128, 16], F32)
        sb("wabs2", [128, 16], F32); sb("wsg2", [128, 16], F32)
        sb("dg0", [128, 16, 128], BF16); sb("negI", [128, 128], BF16)
        sb("rc2", [128, 1], F32)
        if not ATTN_PE:
            sb("Fe", [128, 2048], BF16)
        self.dg1, self.dg1_b = self.dg0, self.dg0_b
        for i in range(4):
            sb(f"rl16_{i}", [128, 512], BF16)
        self.rl16 = [getattr(self, f"rl16_{i}") for i in range(4)]; self.rl16_b = [getattr(self, f"rl16_{i}_b") for i in range(4)]
        sb("blo", [128, 1], F32); sb("bmid", [128, 1], F32); sb("bcnt", [128, 1], F32); sb("bfs", [128, 1], F32); sb("brng", [128, 1], F32)
        sb("bstep", [128, 32], F32); sb("pw2", [128, 32], F32)
        for k in range(32):
            P.op("pool", (lambda e, k=k: e.memset(self.pw2[:, k:k + 1], 2.0 ** -(k + 1))), writes=[self.pw2_b])
        self.ta2 = self.tmpa; self.ta2_b = self.tmpa_b
        sb("bst", [128, 12], F32); sb("bag", [128, 2], F32); sb("lrs", [128, 1], F32); sb("lneps", [128, 1], F32)
        sb("qib", [128, 8, 128], BF16); sb("qbk", [128, 8, 128], BF16); sb("wx", [128, 16], F32)
        sb("wabs", [128, 16], F32); sb("wsg", [128, 16], F32); sb("kis", [128, SEQ], BF16)
        sb("m8", [128, 8], F32); sb("thrneg", [128, 1], F32); sb("ident", [128, 128], BF16); sb("onec", [128, 2], BF16)
        sb("rc", [128, 1], F32); sb("yb", [128, 1024], BF16)
        sb("Fb", [128, 2048], BF16); sb("cb8", [128, 8], F32)
        P.op("pool", lambda e: e.memset(self.lneps[:], LN_EPS), writes=[self.lneps_b])
        P.op("pool", lambda e: e.memset(self.ones[:], 1.0), writes=[self.ones_b])
        P.op("pool", lambda e: e.memset(self.epsc[:], NORM_EPS), writes=[self.epsc_b])

    def claim(self, rname, bufs):
        self.P.claim(self.reg[rname], bufs)
        self.reg[rname] = list(bufs)

    def plan_extend(self, specs):
        self.plan.extend(specs)

    def _issue(self, i):
        P = self.P
        pieces, R, W = self.plan[i]
        s = i % self.NS
        if CASTDMA:
            bt = self.wbf[s][:, 0:R * W].rearrange("p (r w) -> p r w", w=W)
            for pi, (c0, wd, src) in enumerate(pieces):
                P.dma("pool", (lambda e, o=bt[:, :, c0:c0 + wd], s_=src: e.dma_start(out=o, in_=s_)),
                      reads=[], writes=[self.wbf_b[s]], lane=f"w{s}_{pi}")
            return
        st = self.wst[s][:, 0:R * W].rearrange("p (r w) -> p r w", w=W)
        for pi, (c0, wd, src) in enumerate(pieces):
            P.dma("sp", (lambda e, o=st[:, :, c0:c0 + wd], s_=src: e.dma_start(out=o, in_=s_)),
                  reads=[], writes=[self.wst_b[s]], lane=f"w{s}_{pi}")
        P.op("pool", (lambda e, o=self.wbf[s][:, 0:R * W], i_=self.wst[s][:, 0:R * W]: e.tensor_copy(out=o, in_=i_)),
             reads=[self.wst_b[s]], writes=[self.wbf_b[s]])

    def wget(self, i, look=2):
        while self.issued <= min(i + look, len(self.plan) - 1):
            self._issue(self.issued)
            self.issued += 1
        pieces, R, W = self.plan[i]
        s = i % self.NS
        return self.wbf[s][:, 0:R * W].rearrange("p (r w) -> p r w", w=W), self.wbf_b[s]


_FILLREG = {}


def fillreg(e, v):
    key = (id(e), v)
    if key not in _FILLREG:
        _FILLREG[key] = e.to_reg(v)
    return _FILLREG[key]


def mm(P, o, l, r, st, sp, reads, writes):
    P.op("pe", (lambda e: e.matmul(o, l, r, start=st, stop=sp)), reads=reads, writes=writes)


def w_panel(w2d, r0, nr, cols):
    pieces = []
    off = 0
    for (c0, wd) in cols:
        src = w2d[r0 * 128:(r0 + nr) * 128, c0:c0 + wd].rearrange("(r p) w -> p r w", p=128)
        pieces.append((off, wd, src))
        off += wd
    return (pieces, nr, off)


def emit_rstd(C, chunk_ap, nch, dim, bank):
    P = C.P
    ps, psb = C.ps[bank], C.ps_b[bank]
    for c in range(nch):
        ap, b = chunk_ap(c)
        k = C.cnt % 2
        C.cnt += 1
        P.op("act", (lambda e, o=C.sq[k][:], i_=ap: e.activation(out=o, in_=i_, func=AF.Square)),
             reads=[b], writes=[C.sq_b[k]])
        P.op("pe", (lambda e, o=ps[:], l=C.ones[:], r=C.sq[k][:], st=(c == 0), sp=(c == nch - 1):
                    e.matmul(o, l, r, start=st, stop=sp)),
             reads=[C.ones_b, C.sq_b[k]], writes=[psb])
    P.op("act", (lambda e: e.activation(out=C.rstd[:], in_=ps[:], func=AF.Sqrt, bias=C.epsc[:], scale=1.0 / dim)),
         reads=[psb, C.epsc_b], writes=[C.rstd_b])
    P.op("dve", (lambda e: e.reciprocal(out=C.rstd[:], in_=C.rstd[:])), reads=[C.rstd_b], writes=[C.rstd_b])


def ffn_plan(w_in, w_out):
    specs = []
    for g in range(FC // 2):
        specs.append(w_panel(w_in, 0, DC, [(g * 256, 256)]))
        specs.append(w_panel(w_in, 0, DC, [(DFF + g * 256, 256)]))
    for c2 in range(DC // 2):
        for kp in range(3):
            r0 = kp * 16
            nr = min(16, FC - r0)
            specs.append(w_panel(w_out, r0, nr, [(c2 * 256, 256)]))
    return specs


def emit_ffn(C, wbase, x_dram, xo_dram, t0, gpre, gpost, cb):
    P = C.P
    (xd, xdb), (xo, xob) = x_dram, xo_dram
    xT = C.R1[:, :].rearrange("p (c t) -> p c t", t=NT)
    xTb = Buf("xT")
    hT = C.R2[:, :].bitcast(BF16).rearrange("p (c t) -> p c t", t=NT)
    hTb = Buf("hT")
    gT = C.R3[:, :].bitcast(BF16).rearrange("p (c t) -> p c t", t=NT)
    gTb = [Buf(f"gT{j}") for j in range(FC)]
    C.claim("R1", [xTb]); C.claim("R2", [hTb]); C.claim("R3", gTb)
    xsrc = xd[:, t0:t0 + NT].rearrange("(c p) t -> p c t", p=128)
    P.dma("sp", lambda e: e.dma_start(out=xT, in_=xsrc), reads=[xdb], writes=[xTb], lane="ld0")
    emit_rstd(C, lambda c: (xT[:, c, :], xTb), DC, D, 0)
    for c in range(DC):
        P.op("dve", (lambda e, c=c: e.scalar_tensor_tensor(out=hT[:, c, :], in0=xT[:, c, :], scalar=gpre[:, c:c + 1],
                                                          in1=C.rstd[:], op0=ALU.mult, op1=ALU.mult)),
             reads=[xTb, C.rstd_b, cb], writes=[hTb])
    wi = wbase
    for g in range(FC // 2):
        base = 4 * (g % 2)
        wa, wab = C.wget(wi); wi += 1
        for cc in range(2):
            bk = base + cc
            for k in range(DC):
                P.op("pe", (lambda e, o=C.ps[bk][:], l=wa[:, k, cc * 128:(cc + 1) * 128], r=hT[:, k, :], st=(k == 0), sp=(k == DC - 1):
                            e.matmul(o, l, r, start=st, stop=sp)),
                     reads=[wab, hTb], writes=[C.ps_b[bk]])
        wb_, wbb = C.wget(wi); wi += 1
        for cc in range(2):
            bk = base + 2 + cc
            for k in range(DC):
                P.op("pe", (lambda e, o=C.ps[bk][:], l=wb_[:, k, cc * 128:(cc + 1) * 128], r=hT[:, k, :], st=(k == 0), sp=(k == DC - 1):
                            e.matmul(o, l, r, start=st, stop=sp)),
                     reads=[wbb, hTb], writes=[C.ps_b[bk]])
        for cc in range(2):
            j = 2 * g + cc
            k2 = C.cnt % 2
            C.cnt += 1
            P.op("act", (lambda e, o=C.tmpa[k2][:], i_=C.ps[base + cc][:]: e.activation(out=o, in_=i_, func=AF.Silu)),
                 reads=[C.ps_b[base + cc]], writes=[C.tmpa_b[k2]])
            P.op("dve", (lambda e, o=gT[:, j, :], a=C.tmpa[k2][:], b=C.ps[base + 2 + cc][:]:
                         e.tensor_tensor(out=o, in0=a, in1=b, op=ALU.mult)),
                 reads=[C.tmpa_b[k2], C.ps_b[base + 2 + cc]], writes=[gTb[j]])
    wi = emit_outproj(C, wi, gT, lambda f: gTb[f], FC, (xd, xdb), (xo, xob), t0, gpost, 0.5, cb, xT, xTb, hTb)
    return wi


def outproj_plan(w_out, K):
    specs = []
    for c2 in range(DC // 2):
        for r0 in range(0, K, 16):
            specs.append(w_panel(w_out, r0, min(16, K - r0), [(c2 * 256, 256)]))
    return specs


def emit_outproj(C, wi, inT, inb, K, x_dram, xo_dram, t0, gpost, alpha, cb, fT, fTb, r2b):
    P = C.P
    (xd, xdb), (xo, xob) = x_dram, xo_dram
    SB = 6
    for c2 in range(DC // 2):
        base = 2 * (c2 % 2)
        for r0 in range(0, K, 16):
            nr = min(16, K - r0)
            wo, wob = C.wget(wi); wi += 1
            for cc in range(2):
                bk = base + cc
                for r in range(nr):
                    f = r0 + r
                    mm(P, C.ps[bk][:], wo[:, r, cc * 128:(cc + 1) * 128], inT[:, f, :], f == 0, f == K - 1, [wob, inb(f)], [C.ps_b[bk]])
        for cc in range(2):
            c = 2 * c2 + cc
            bk = base + cc
            P.op("act", (lambda e, o=fT[:, c, :], i_=C.ps[bk][:]: e.activation(out=o, in_=i_, func=AF.Copy)),
                 reads=[C.ps_b[bk]], writes=[fTb])
            k2 = C.cnt % 2
            C.cnt += 1
            P.op("dve", (lambda e, o=C.sq[k2][:], i_=C.ps[bk][:], f_=fT[:, c, :]: e.tensor_tensor(out=o, in0=i_, in1=f_, op=ALU.mult)),
                 reads=[C.ps_b[bk], fTb], writes=[C.sq_b[k2]])
            mm(P, C.ps[SB][:], C.ones[:], C.sq[k2][:], c == 0, c == DC - 1, [C.ones_b, C.sq_b[k2]], [C.ps_b[SB]])
    P.op("act", (lambda e: e.activation(out=C.rstd[:], in_=C.ps[SB][:], func=AF.Sqrt, bias=C.epsc[:], scale=1.0 / D)),
         reads=[C.ps_b[SB], C.epsc_b], writes=[C.rstd_b])
    P.op("dve", (lambda e: e.reciprocal(out=C.rstd[:], in_=C.rstd[:])), reads=[C.rstd_b], writes=[C.rstd_b])
    xr = C.R2[:, :].rearrange("p (c t) -> p c t", t=NT)
    for h in range(2):
        xs = xd[h * 1024:(h + 1) * 1024, t0:t0 + NT].rearrange("(c p) t -> p c t", p=128)
        P.dma("sp", (lambda e, s_=xs: e.dma_start(out=xr, in_=s_)), reads=[xdb], writes=[r2b], lane="ld1")
        for cl in range(8):
            c = h * 8 + cl
            P.op("dve", (lambda e, c=c: e.scalar_tensor_tensor(out=fT[:, c, :], in0=fT[:, c, :], scalar=gpost[:, c:c + 1],
                                                              in1=C.rstd[:], op0=ALU.mult, op1=ALU.mult)),
                 reads=[fTb, C.rstd_b, cb], writes=[fTb])
            P.op("dve", (lambda e, c=c, cl=cl: e.scalar_tensor_tensor(out=fT[:, c, :], in0=fT[:, c, :], scalar=alpha,
                                                                       in1=xr[:, cl, :], op0=ALU.mult, op1=ALU.add)),
                 reads=[fTb, r2b], writes=[fTb])
    xdst = xo[:, t0:t0 + NT].rearrange("(c p) t -> p c t", p=128)
    P.dma("sp", lambda e: e.dma_start(out=xdst, in_=fT), reads=[fTb], writes=[xob], lane="st0")
    return wi


AW = 1024
NEG = -1.0e30
GELU_C = 0.7978845608028654 * 2.0
LN_EPS = 1e-5
O_ZU, O_ZV, O_Q, O_K, O_V, O_QI, O_KI, O_WI = 0, 1024, 2048, 3072, 4096, 5120, 6144, 6208


def mm(P, o, l, r, st, sp, reads, writes):
    P.op("pe", (lambda e: e.matmul(o, l, r, start=st, stop=sp)), reads=reads, writes=writes)


def emit_gelu(C, out_ap, in_ap, inb, outb, shape):
    P = C.P
    t1 = C.gt1[:, 0:shape]
    t2 = C.gt2[:, 0:shape]
    P.op("act", (lambda e: e.activation(out=t1, in_=in_ap, func=AF.Square)), reads=[inb], writes=[C.gt1_b])
    P.op("dve", (lambda e: e.tensor_scalar(out=t1, in0=t1, scalar1=0.044715, scalar2=1.0, op0=ALU.mult, op1=ALU.add)),
         reads=[C.gt1_b], writes=[C.gt1_b])
    P.op("dve", (lambda e: e.tensor_tensor(out=t1, in0=t1, in1=in_ap, op=ALU.mult)), reads=[C.gt1_b, inb], writes=[C.gt1_b])
    P.op("act", (lambda e: e.activation(out=t2, in_=t1, func=AF.Sigmoid, scale=GELU_C)), reads=[C.gt1_b], writes=[C.gt2_b])
    P.op("dve", (lambda e: e.tensor_tensor(out=out_ap, in0=t2, in1=in_ap, op=ALU.mult)), reads=[C.gt2_b, inb], writes=[outb])


def mixa_plan(w_in, w_gate, w_ba):
    specs = []
    for seg in (O_ZU, O_Q, O_K, O_QI):
        for c2 in range(4):
            specs.append(w_panel(w_in, 0, DC, [(seg + c2 * 256, 256)]))
    specs.append(w_panel(w_in, 0, DC, [(O_KI, 64), (O_KI, 64)]))
    for seg in (O_ZV, O_V):
        for c2 in range(4):
            specs.append(w_panel(w_in, 0, DC, [(seg + c2 * 256, 256)]))
    specs.append(w_panel(w_in, 0, DC, [(O_WI, 16)]))
    for c2 in range(8):
        specs.append(w_panel(w_ba, 0, 8, [(c2 * 256, 256)]))
        specs.append(w_panel(w_gate, 0, DC, [(c2 * 256, 256)]))
    for c2 in range(8):
        specs.append(w_panel(w_gate, 0, DC, [(D + c2 * 256, 256)]))
    return specs


class MixRes:
    pass


def emit_mix_consts(C, bro_d, wsT_d, M):
    P = C.P
    C.bro_b = Buf("bro"); C.wsf_b = Buf("wsf")
    C.claim("R4", [C.bro_b, C.wsf_b])
    P.dma("sp", lambda e: e.dma_start(out=C.bro, in_=bro_d), reads=[], writes=[C.bro_b], lane="ldc")
    wv = C.wsf.rearrange("p (g t) -> p g t", g=8)
    P.dma("sp", lambda e: e.dma_start(out=wv, in_=wsT_d), reads=[], writes=[C.wsf_b], lane="ldc")
    P.op("pool", (lambda e: e.affine_select(out=wv, in_=wv, pattern=[[0, 8], [1, 128]], compare_op=ALU.is_ge,
                                             fill=fillreg(e, 0.0), base=0, channel_multiplier=-1)),
         reads=[C.wsf_b], writes=[C.wsf_b])
    P.op("pool", (lambda e: e.tensor_copy(out=C.wsb[:], in_=wv)), reads=[C.wsf_b], writes=[C.wsb_b])


def emit_mixa(C, wbase, x1, t0, S, cs, cb, dr):
    P = C.P
    xd, xdb = x1
    xT = C.R1[:, :].rearrange("p (c t) -> p c t", t=NT)
    xTb = Buf("xT")
    hT = C.R2[:, :].bitcast(BF16).rearrange("p (c t) -> p c t", t=NT)
    hTb = Buf("hT")
    r3 = C.R3[:, :]
    zv = r3[:, 0:4096].rearrange("p (b c) -> p b c", c=1024)
    zvb = Buf("zv")
    vln = r3[:, 4096:6144].bitcast(BF16).rearrange("p (b c) -> p b c", c=1024)
    vlnb = Buf("vln")
    guT = r3[:, 6144:8192].bitcast(BF16).rearrange("p (c t) -> p c t", t=NT)
    guTb = Buf("guT")
    yaT = r3[:, 8192:10240].bitcast(BF16).rearrange("p (c t) -> p c t", t=NT)
    yaTb = Buf("yaT")
    gpre = cs[:, 32:48]
    C.claim("R1", [xTb]); C.claim("R2", [hTb]); C.claim("R3", [zvb, vlnb, guTb, yaTb])
    xsrc = xd[:, t0:t0 + NT].rearrange("(c p) t -> p c t", p=128)
    P.dma("sp", lambda e: e.dma_start(out=xT, in_=xsrc), reads=[xdb], writes=[xTb], lane="ld0")
    emit_rstd(C, lambda c: (xT[:, c, :], xTb), DC, D, 0)
    for c in range(DC):
        P.op("dve", (lambda e, c=c: e.scalar_tensor_tensor(out=hT[:, c, :], in0=xT[:, c, :], scalar=gpre[:, c:c + 1],
                                                          in1=C.rstd[:], op0=ALU.mult, op1=ALU.mult)),
             reads=[xTb, C.rstd_b, cb], writes=[hTb])
    osb = [C.R1[:, i * 2048:(i + 1) * 2048].bitcast(BF16).rearrange("p (c t) -> p c t", t=NT) for i in range(2)]
    osbb = [Buf("osb0"), Buf("osb1")]
    och = [C.R1[:, 4096 + i * 512: 4096 + (i + 1) * 512] for i in range(8)]
    ochb = [Buf(f"och{i}") for i in range(8)]
    C.claim("R1", osbb + ochb)
    wi = wbase
    bankrr = [0]

    def nb():
        b = bankrr[0] % 8
        bankrr[0] += 1
        return b

    first = [True]

    def r1dep():
        return [xTb]

    dests = {O_Q: ("qT", 0), O_K: ("kT", 1), O_QI: ("qiT", 0)}
    for seg in (O_ZU, O_Q, O_K, O_QI):
        if seg != O_ZU:
            dn, oi = dests[seg]
            ob, obb = osb[oi], osbb[oi]
        for c2 in range(4):
            w, wb = C.wget(wi); wi += 1
            for cc in range(2):
                ch = 2 * c2 + cc
                bk = nb()
                for k in range(DC):
                    mm(P, C.ps[bk][:], w[:, k, cc * 128:(cc + 1) * 128], hT[:, k, :], k == 0, k == DC - 1, [wb, hTb], [C.ps_b[bk]])
                if seg == O_ZU:
                    emit_gelu(C, guT[:, ch, :], C.ps[bk][:], C.ps_b[bk], guTb, 512)
                else:
                    P.op("act", (lambda e, o=ob[:, ch, :], i_=C.ps[bk][:]: e.activation(out=o, in_=i_, func=AF.Copy)),
                         reads=[C.ps_b[bk]], writes=[obb])
        if seg != O_ZU:
            dd, ddb = dr[dn]
            dst = dd[:, t0:t0 + NT].rearrange("(c p) t -> p c t", p=128)
            P.dma("sp", (lambda e, d_=dst, s_=ob: e.dma_start(out=d_, in_=s_)), reads=[obb], writes=[ddb], lane="st1")
    w, wb = C.wget(wi); wi += 1
    bk = nb()
    for k in range(DC):
        mm(P, C.ps[bk][:], w[:, k, 0:128], hT[:, k, :], k == 0, k == DC - 1, [wb, hTb], [C.ps_b[bk]])
    P.op("act", (lambda e, o=C.kio[:], i_=C.ps[bk][:]: e.activation(out=o, in_=i_, func=AF.Copy)), reads=[C.ps_b[bk]], writes=[C.kio_b])
    dd, ddb = dr["kiT"]
    P.dma("sp", (lambda e, d_=dd[:, t0:t0 + NT]: e.dma_start(out=d_, in_=C.kio[:])), reads=[C.kio_b], writes=[ddb], lane="st2")
    vsb = osb[0].rearrange("p c t -> p (c t)").rearrange("p (b c) -> p b c", c=1024)
    for seg in (O_ZV, O_V):
        for c2 in range(4):
            w, wb = C.wget(wi); wi += 1
            for tb in range(4):
                bk = nb()
                for k in range(DC):
                    mm(P, C.ps[bk][:, 0:256], hT[:, k, tb * 128:(tb + 1) * 128], w[:, k, :], k == 0, k == DC - 1, [wb, hTb], [C.ps_b[bk]])
                if seg == O_ZV:
                    P.op("act", (lambda e, o=zv[:, tb, c2 * 256:(c2 + 1) * 256], i_=C.ps[bk][:, 0:256]: e.activation(out=o, in_=i_, func=AF.Copy)),
                         reads=[C.ps_b[bk]], writes=[zvb])
                else:
                    P.op("act", (lambda e, o=vsb[:, tb, c2 * 256:(c2 + 1) * 256], i_=C.ps[bk][:, 0:256]: e.activation(out=o, in_=i_, func=AF.Copy)),
                         reads=[C.ps_b[bk]], writes=[osbb[0]])
    dd, ddb = dr["v"]
    P.dma("sp", (lambda e, d_=dd[t0:t0 + NT, :].rearrange("(b p) c -> p b c", p=128): e.dma_start(out=d_, in_=vsb)),
          reads=[osbb[0]], writes=[ddb], lane="st1")
    w, wb = C.wget(wi); wi += 1
    bk = nb()
    for tb in range(4):
        for k in range(DC):
            mm(P, C.ps[bk][:, tb * 16:(tb + 1) * 16], hT[:, k, tb * 128:(tb + 1) * 128], w[:, k, :], k == 0, k == DC - 1, [wb, hTb], [C.ps_b[bk]])
    P.op("act", (lambda e, i_=C.ps[bk][:, 0:64]: e.activation(out=C.wio[:], in_=i_, func=AF.Copy)), reads=[C.ps_b[bk]], writes=[C.wio_b])
    dd, ddb = dr["widx"]
    P.dma("sp", (lambda e, d_=dd[t0:t0 + NT, :].rearrange("(b p) c -> p b c", p=128): e.dma_start(out=d_, in_=C.wio[:].rearrange("p (b c) -> p b c", c=16))),
          reads=[C.wio_b], writes=[ddb], lane="st2")
    lng = C.bro[:, 0:1024]
    lnb = C.bro[:, 1024:2048]
    for tb in range(4):
        for h2 in range(2):
            emit_gelu(C, zv[:, tb, h2 * 512:(h2 + 1) * 512], zv[:, tb, h2 * 512:(h2 + 1) * 512], zvb, zvb, 512)
        P.op("dve", (lambda e, i_=zv[:, tb, :]: e.bn_stats(out=C.bst[:, 0:6], in_=i_[:, 0:512])), reads=[zvb], writes=[C.bst_b])
        P.op("dve", (lambda e, i_=zv[:, tb, :]: e.bn_stats(out=C.bst[:, 6:12], in_=i_[:, 512:1024])), reads=[zvb], writes=[C.bst_b])
        P.op("dve", (lambda e: e.bn_aggr(out=C.bag[:], in_=C.bst[:].rearrange("p (a b) -> p a b", b=6))), reads=[C.bst_b], writes=[C.bag_b])
        P.op("act", (lambda e: e.activation(out=C.lrs[:], in_=C.bag[:, 1:2], func=AF.Sqrt, bias=C.lneps[:], scale=1.0)),
             reads=[C.bag_b, C.lneps_b], writes=[C.lrs_b])
        P.op("dve", (lambda e: e.reciprocal(out=C.lrs[:], in_=C.lrs[:])), reads=[C.lrs_b], writes=[C.lrs_b])
        P.op("dve", (lambda e, o=zv[:, tb, :]: e.tensor_scalar(out=o, in0=o, scalar1=C.bag[:, 0:1], scalar2=C.lrs[:], op0=ALU.subtract, op1=ALU.mult)),
             reads=[zvb, C.bag_b, C.lrs_b], writes=[zvb])
        P.op("dve", (lambda e, o=zv[:, tb, :]: e.tensor_tensor(out=o, in0=o, in1=lng, op=ALU.mult)), reads=[zvb, C.bro_b], writes=[zvb])
        P.op("dve", (lambda e, o=vln[:, tb, :], i_=zv[:, tb, :]: e.tensor_tensor(out=o, in0=i_, in1=lnb, op=ALU.add)), reads=[zvb, C.bro_b], writes=[vlnb])
    for g in range(8):
        bk = nb()
        for tb in range(4):
            mm(P, C.ps[bk][:, tb * 128:(tb + 1) * 128], vln[:, tb, g * 128:(g + 1) * 128], C.wsb[:, g, :], True, True, [vlnb, C.wsb_b], [C.ps_b[bk]])
        for tb in range(4):
            P.op("dve", (lambda e, o=C.gt1[:, tb * 128:(tb + 1) * 128], i_=C.ps[bk][:, tb * 128:(tb + 1) * 128], b_=C.bro[:, 2048 + g * 128:2048 + (g + 1) * 128]:
                         e.tensor_tensor(out=o, in0=i_, in1=b_, op=ALU.add)),
                 reads=[C.ps_b[bk], C.bro_b], writes=[C.gt1_b])
        P.op("dve", (lambda e, o=yaT[:, g, :], g_=guT[:, g, :]: e.tensor_tensor(out=o, in0=C.gt1[:], in1=g_, op=ALU.mult)),
             reads=[C.gt1_b, guTb], writes=[yaTb])
    if "dbg_ya" in dr:
        P.dma("sp", (lambda e, d_=dr["dbg_ya"][0][:, t0:t0 + NT].rearrange("(c p) t -> p c t", p=128): e.dma_start(out=d_, in_=yaT)), reads=[yaTb], writes=[dr["dbg_ya"][1]], lane="dbg")
        P.dma("sp", (lambda e, d_=dr["dbg_gu"][0][:, t0:t0 + NT].rearrange("(c p) t -> p c t", p=128): e.dma_start(out=d_, in_=guT)), reads=[guTb], writes=[dr["dbg_gu"][1]], lane="dbg")
        P.dma("sp", (lambda e, d_=dr["dbg_vln"][0][t0:t0 + NT, :].rearrange("(b p) c -> p b c", p=128): e.dma_start(out=d_, in_=vln)), reads=[vlnb], writes=[dr["dbg_vln"][1]], lane="dbg")
    ocnt = [0]
    for c2 in range(8):
        wa, wab = C.wget(wi); wi += 1
        wg, wgb = C.wget(wi); wi += 1
        for cc in range(2):
            ch = 2 * c2 + cc
            bka = nb()
            for k in range(8):
                mm(P, C.ps[bka][:], wa[:, k, cc * 128:(cc + 1) * 128], yaT[:, k, :], k == 0, k == 7, [wab, yaTb], [C.ps_b[bka]])
            bkg = nb()
            for k in range(DC):
                mm(P, C.ps[bkg][:], wg[:, k, cc * 128:(cc + 1) * 128], hT[:, k, :], k == 0, k == DC - 1, [wgb, hTb], [C.ps_b[bkg]])
            k2 = C.cnt % 2
            C.cnt += 1
            P.op("act", (lambda e, o=C.tmpa[k2][:], i_=C.ps[bkg][:]: e.activation(out=o, in_=i_, func=AF.Sigmoid)),
                 reads=[C.ps_b[bkg]], writes=[C.tmpa_b[k2]])
            oi = ocnt[0] % 8
            ocnt[0] += 1
            P.op("dve", (lambda e, o=och[oi], a=C.tmpa[k2][:], b=C.ps[bka][:]: e.tensor_tensor(out=o, in0=a, in1=b, op=ALU.mult)),
                 reads=[C.tmpa_b[k2], C.ps_b[bka]], writes=[ochb[oi]])
            dd, ddb = dr["apart"]
            P.dma("sp", (lambda e, d_=dd[ch * 128:(ch + 1) * 128, t0:t0 + NT], s_=och[oi]: e.dma_start(out=d_, in_=s_)),
                  reads=[ochb[oi]], writes=[ddb], lane=f"so{oi}")
    for c2 in range(8):
        wg, wgb = C.wget(wi); wi += 1
        for cc in range(2):
            ch = 2 * c2 + cc
            bkg = nb()
            for k in range(DC):
                mm(P, C.ps[bkg][:], wg[:, k, cc * 128:(cc + 1) * 128], hT[:, k, :], k == 0, k == DC - 1, [wgb, hTb], [C.ps_b[bkg]])
            oi = ocnt[0] % 8
            ocnt[0] += 1
            P.op("act", (lambda e, o=och[oi], i_=C.ps[bkg][:]: e.activation(out=o, in_=i_, func=AF.Sigmoid)),
                 reads=[C.ps_b[bkg]], writes=[ochb[oi]])
            dd, ddb = dr["gb"]
            P.dma("sp", (lambda e, d_=dd[ch * 128:(ch + 1) * 128, t0:t0 + NT], s_=och[oi]: e.dma_start(out=d_, in_=s_)),
                  reads=[ochb[oi]], writes=[ddb], lane=f"so{oi}")
    return wi


ATT_SCALE = 128 ** -0.5
MASK_NEG = -30000.0
TOPK = 256
BIS_IT = 28
BIS_MIN = 1024


def emit_attn_consts(C, toep_d, cb8_d):
    P = C.P
    tpf = C.R3[:, 0:2048].rearrange("p (h w) -> p h w", h=8)
    tb = Buf("tpf")
    C.claim("R3", [tb])
    P.dma("sp", lambda e: e.dma_start(out=C.cb8[:], in_=cb8_d), reads=[], writes=[C.cb8_b], lane="ldc")
    P.dma("sp", lambda e: e.dma_start(out=tpf, in_=toep_d.rearrange("p (h w) -> p h w", h=8)), reads=[], writes=[tb], lane="ldc")
    for h in range(8):
        P.op("dve", (lambda e, h=h: e.tensor_scalar(out=tpf[:, h, :], in0=tpf[:, h, :], scalar1=C.cb8[:, h:h + 1], scalar2=None, op0=ALU.subtract)),
             reads=[tb, C.cb8_b], writes=[tb])
    if not ATTN_PE:
        P.op("act", (lambda e: e.activation(out=C.Fe[:].rearrange("p (h w) -> p h w", h=8), in_=tpf, func=AF.Exp)), reads=[tb], writes=[C.Fe_b])
    P.op("dve", (lambda e: e.tensor_scalar(out=C.Fb[:].rearrange("p (h w) -> p h w", h=8), in0=tpf, scalar1=1.0 / ATT_SCALE, scalar2=None, op0=ALU.mult)),
         reads=[tb], writes=[C.Fb_b])
    P.op("pool", lambda e: e.memset(C.ident[:], 1.0), writes=[C.ident_b])
    P.op("pool", (lambda e: e.affine_select(out=C.ident[:], in_=C.ident[:], pattern=[[-1, 128]], compare_op=ALU.is_equal,
                                             fill=fillreg(e, 0.0), base=0, channel_multiplier=1)), reads=[C.ident_b], writes=[C.ident_b])
    P.op("pool", (lambda e: e.tensor_scalar(out=C.negI[:], in0=C.ident[:], scalar1=MASK_NEG, scalar2=1.0, op0=ALU.mult, op1=ALU.mult)),
         reads=[C.ident_b], writes=[C.negI_b])
    P.op("pool", lambda e: e.memset(C.thrneg[:], -1.0e29), writes=[C.thrneg_b])
    P.op("pool", lambda e: e.memset(C.onec[:], 1.0), writes=[C.onec_b])


def mixb_tail_plan(w_bb, w_o):
    specs = []
    for c2 in range(8):
        specs.append(w_panel(w_bb, 0, 8, [(c2 * 256, 256)]))
    specs += outproj_plan(w_o, DC)
    return specs


def emit_mixb_tile(C, wbase, tt, S, x1, x2, cs, cb, dr):
    P = C.P
    t0 = tt * NT
    kh = [C.R1[:, i * 2048:(i + 1) * 2048].bitcast(BF16) for i in range(2)]
    khb = [Buf("kh0"), Buf("kh1")]
    ybT = C.R1[:, 4096:6144].bitcast(BF16).rearrange("p (c t) -> p c t", t=NT)
    ybTb = Buf("ybT")
    maskT = C.R1[:, 6144:8192].bitcast(BF16)
    maskTb = Buf("maskT")
    vh = [C.R2[:, i * 2048:(i + 1) * 2048].bitcast(BF16).rearrange("p (b c) -> p b c", c=128) for i in range(2)]
    vhb = [Buf("vh0"), Buf("vh1")]
    score = C.R3[:, 0:4096]
    scb = Buf("score")
    work = C.R3[:, 4096:8192]
    wkb = Buf("work")
    mask = C.R3[:, 8192:10240].bitcast(BF16)
    mkb = Buf("mask")
    C.claim("R1", khb + [ybTb, maskTb]); C.claim("R2", vhb); C.claim("R3", [scb, wkb, mkb])
    rr = [0]

    def nb():
        b = rr[0] % 6
        rr[0] += 1
        return b
    OB, TB = 6, 7
    psT = C.ps[TB][:].bitcast(BF16)
    kvc = [0]
    for qi_ in range(4):
        qb = tt * 4 + qi_
        q0 = qb * 128
        nkb = qb + 1
        Sc = nkb * 128
        ngrp = (nkb + 3) // 4
        P.dma("sp", (lambda e, s_=dr["qiT"][0][:, q0:q0 + 128].rearrange("(c p) t -> p c t", p=128): e.dma_start(out=C.qib[:], in_=s_)),
              reads=[dr["qiT"][1]], writes=[C.qib_b], lane="lq0")
        P.dma("sp", (lambda e, s_=dr["qT"][0][:, q0:q0 + 128].rearrange("(c p) t -> p c t", p=128): e.dma_start(out=C.qbk[:], in_=s_)),
              reads=[dr["qT"][1]], writes=[C.qbk_b], lane="lq1")
        P.dma("sp", (lambda e, s_=dr["widx"][0][q0:q0 + 128, :]: e.dma_start(out=C.wx[:], in_=s_)),
              reads=[dr["widx"][1]], writes=[C.wx_b], lane="lq2")
        P.op("act", (lambda e: e.activation(out=C.wabs[:], in_=C.wx[:], func=AF.Abs)), reads=[C.wx_b], writes=[C.wabs_b])
        P.op("act", (lambda e: e.activation(out=C.wsg[:], in_=C.wx[:], func=AF.Sign)), reads=[C.wx_b], writes=[C.wsg_b])
        for grp in range(ngrp):
            c0 = grp * 512
            n = min(Sc, c0 + 512) - c0
            for h in range(16):
                c, half = h // 2, h % 2
                rows = slice(half * 64, half * 64 + 64)
                bk = nb()
                mm(P, C.ps[bk][:, 0:n], C.qib[rows, c, :], C.kis[rows, c0:c0 + n], True, True, [C.qib_b, C.kis_b], [C.ps_b[bk]])
                k2 = C.cnt % 2
                C.cnt += 1
                P.op("act", (lambda e, o=C.tmpa[k2][:, 0:n], i_=C.ps[bk][:, 0:n], h=h: e.activation(out=o, in_=i_, func=AF.Relu, scale=C.wabs[:, h:h + 1])),
                     reads=[C.ps_b[bk], C.wabs_b], writes=[C.tmpa_b[k2]])
                if h == 0:
                    P.op("dve", (lambda e, o=score[:, c0:c0 + n], i_=C.tmpa[k2][:, 0:n]: e.tensor_scalar(out=o, in0=i_, scalar1=C.wsg[:, 0:1], scalar2=None, op0=ALU.mult)),
                         reads=[C.tmpa_b[k2], C.wsg_b], writes=[scb])
                else:
                    P.op("dve", (lambda e, o=score[:, c0:c0 + n], i_=C.tmpa[k2][:, 0:n], h=h: e.scalar_tensor_tensor(out=o, in0=i_, scalar=C.wsg[:, h:h + 1], in1=o, op0=ALU.mult, op1=ALU.add)),
                         reads=[C.tmpa_b[k2], C.wsg_b, scb], writes=[scb])
        dsl = score[:, (nkb - 1) * 128:nkb * 128]
        P.op("pool", (lambda e, d_=dsl: e.affine_select(out=d_, in_=d_, pattern=[[-1, 128]], compare_op=ALU.is_ge, fill=fillreg(e, NEG), base=0, channel_multiplier=1)),
             reads=[scb], writes=[scb])
        if Sc > TOPK:
            for r in range(TOPK // 8):
                src = score[:, 0:Sc] if r == 0 else work[:, 0:Sc]
                srcb = scb if r == 0 else wkb
                P.op("dve", (lambda e, s_=src: e.max(out=C.m8[:], in_=s_)), reads=[srcb], writes=[C.m8_b])
                if r < TOPK // 8 - 1:
                    P.op("dve", (lambda e, s_=src, w_=work[:, 0:Sc]: e.match_replace(out=w_, in_to_replace=C.m8[:], in_values=s_, imm_value=NEG)),
                         reads=[srcb, C.m8_b], writes=[wkb])
            thr, thrb = C.m8[:, 7:8], C.m8_b
        else:
            thr, thrb = C.thrneg[:], C.thrneg_b
        P.op("dve", (lambda e, t_=thr, m_=mask[:, 0:Sc], s_=score[:, 0:Sc]: e.tensor_scalar(out=m_, in0=s_, scalar1=t_, scalar2=None, op0=ALU.is_ge)),
             reads=[scb, thrb], writes=[mkb])
        for grp in range(ngrp):
            kbs = list(range(grp * 4, min(nkb, grp * 4 + 4)))
            for j, kb in enumerate(kbs):
                P.op("pe", (lambda e, o=psT[:, j * 128:(j + 1) * 128], i_=mask[:, kb * 128:(kb + 1) * 128]: e.transpose(o, i_, C.ident[:])),
                     reads=[mkb, C.ident_b], writes=[C.ps_b[TB]])
            n = len(kbs) * 128
            P.op("act", (lambda e, o=maskT[:, grp * 512:grp * 512 + n], i_=psT[:, 0:n]: e.activation(out=o, in_=i_, func=AF.Copy)),
                 reads=[C.ps_b[TB]], writes=[maskTb])
        for h in range(8):
            s2 = kvc[0] % 2
            kvc[0] += 1
            P.dma("sp", (lambda e, o=kh[s2][:, 0:Sc], s_=dr["kT"][0][h * 128:(h + 1) * 128, 0:Sc]: e.dma_start(out=o, in_=s_)),
                  reads=[dr["kT"][1]], writes=[khb[s2]], lane=f"lk{s2}")
            P.dma("sp", (lambda e, o=vh[s2][:, 0:nkb, :], s_=dr["v"][0][0:Sc, h * 128:(h + 1) * 128].rearrange("(b p) c -> p b c", p=128): e.dma_start(out=o, in_=s_)),
                  reads=[dr["v"][1]], writes=[vhb[s2]], lane=f"lv{s2}")
            for grp in range(ngrp):
                kbs = list(range(grp * 4, min(nkb, grp * 4 + 4)))
                n = len(kbs) * 128
                bk = nb()
                for j, kb in enumerate(kbs):
                    mm(P, C.ps[bk][:, j * 128:(j + 1) * 128], kh[s2][:, kb * 128:(kb + 1) * 128], C.qbk[:, h, :], True, True, [khb[s2], C.qbk_b], [C.ps_b[bk]])
                k2 = C.cnt % 2
                C.cnt += 1
                P.op("act", (lambda e, o=C.tmpa[k2][:, 0:n], i_=C.ps[bk][:, 0:n], h=h: e.activation(out=o, in_=i_, func=AF.Exp, bias=C.cb8[:, h:h + 1], scale=ATT_SCALE)),
                     reads=[C.ps_b[bk], C.cb8_b], writes=[C.tmpa_b[k2]])
                P.op("dve", (lambda e, o=C.pm[k2][:, 0:n], a=C.tmpa[k2][:, 0:n], b=maskT[:, grp * 512:grp * 512 + n]: e.tensor_tensor(out=o, in0=a, in1=b, op=ALU.mult)),
                     reads=[C.tmpa_b[k2], maskTb], writes=[C.pm_b[k2]])
                for j, kb in enumerate(kbs):
                    w_ = nkb - 1 - kb
                    if w_ <= 1:
                        P.op("dve", (lambda e, o=C.pm[k2][:, j * 128:(j + 1) * 128], f_=C.Fb[:, (h * 2 + w_) * 128:(h * 2 + w_ + 1) * 128]: e.tensor_tensor(out=o, in0=o, in1=f_, op=ALU.mult)),
                             reads=[C.pm_b[k2], C.Fb_b], writes=[C.pm_b[k2]])
                for j, kb in enumerate(kbs):
                    P.op("pe", (lambda e, o=C.ps[OB][:, 0:128], l=C.pm[k2][:, j * 128:(j + 1) * 128], r=vh[s2][:, kb, :], st=(kb == 0), sp=(kb == nkb - 1):
                                e.matmul(o, l, r, start=st, stop=sp, skip_group_check=True)),
                         reads=[C.pm_b[k2], vhb[s2]], writes=[C.ps_b[OB]])
                    P.op("pe", (lambda e, o=C.ps[OB][:, 128:129], l=C.pm[k2][:, j * 128:(j + 1) * 128], sp=(kb == nkb - 1):
                                e.matmul(o, l, C.onec[:, 0:1], start=False, stop=sp, skip_group_check=True)),
                         reads=[C.pm_b[k2], C.onec_b], writes=[C.ps_b[OB]])
            P.op("dve", (lambda e: e.reciprocal(out=C.rc[:], in_=C.ps[OB][:, 128:129])), reads=[C.ps_b[OB]], writes=[C.rc_b])
            P.op("dve", (lambda e, o=C.yb[:, h * 128:(h + 1) * 128]: e.tensor_scalar(out=o, in0=C.ps[OB][:, 0:128], scalar1=C.rc[:], scalar2=None, op0=ALU.mult)),
                 reads=[C.ps_b[OB], C.rc_b], writes=[C.yb_b])
        for half in range(2):
            for j in range(4):
                c = half * 4 + j
                P.op("pe", (lambda e, o=psT[:, j * 128:(j + 1) * 128], i_=C.yb[:, c * 128:(c + 1) * 128]: e.transpose(o, i_, C.ident[:])),
                     reads=[C.yb_b, C.ident_b], writes=[C.ps_b[TB]])
            P.op("act", (lambda e, o=ybT[:, half * 4:half * 4 + 4, qi_ * 128:(qi_ + 1) * 128], i_=psT[:, 0:512].rearrange("p (c t) -> p c t", t=128):
                         e.activation(out=o, in_=i_, func=AF.Copy)),
                 reads=[C.ps_b[TB]], writes=[ybTb])
    mT = C.R2[:, :].bitcast(BF16).rearrange("p (c t) -> p c t", t=NT)
    mTb = Buf("mT")
    C.claim("R2", [mTb])
    wi = wbase
    lc = [0]
    for c2 in range(8):
        w, wb = C.wget(wi); wi += 1
        for cc in range(2):
            ch = 2 * c2 + cc
            bk = nb()
            for k in range(8):
                mm(P, C.ps[bk][:], w[:, k, cc * 128:(cc + 1) * 128], ybT[:, k, :], k == 0, k == 7, [wb, ybTb], [C.ps_b[bk]])
            k2 = lc[0] % 2
            lc[0] += 1
            P.dma("sp", (lambda e, o=C.gt1[:] if k2 == 0 else C.gt2[:], s_=dr["gb"][0][ch * 128:(ch + 1) * 128, t0:t0 + NT]: e.dma_start(out=o, in_=s_)),
                  reads=[dr["gb"][1]], writes=[C.gt1_b if k2 == 0 else C.gt2_b], lane=f"lg{k2}")
            P.dma("sp", (lambda e, o=C.tmpa[k2][:], s_=dr["apart"][0][ch * 128:(ch + 1) * 128, t0:t0 + NT]: e.dma_start(out=o, in_=s_)),
                  reads=[dr["apart"][1]], writes=[C.tmpa_b[k2]], lane=f"la{k2}")
            gbt, gbb = (C.gt1, C.gt1_b) if k2 == 0 else (C.gt2, C.gt2_b)
            P.op("dve", (lambda e, g_=gbt[:], i_=C.ps[bk][:]: e.tensor_tensor(out=g_, in0=g_, in1=i_, op=ALU.mult)),
                 reads=[gbb, C.ps_b[bk]], writes=[gbb])
            P.op("dve", (lambda e, o=mT[:, ch, :], g_=gbt[:], a=C.tmpa[k2][:]: e.tensor_tensor(out=o, in0=g_, in1=a, op=ALU.add)),
                 reads=[gbb, C.tmpa_b[k2]], writes=[mTb])
    fT = C.R1[:, :].rearrange("p (c t) -> p c t", t=NT)
    fTb = Buf("fT")
    C.claim("R1", [fTb])
    r2b = Buf("r2x")
    wi = emit_outproj_claim(C, wi, mT, mTb, x1, x2, t0, cs[:, 48:64], 1.0, cb, fT, fTb, r2b)
    return wi


def emit_outproj_claim(C, wi, mT, mTb, x1, x2, t0, gpost, alpha, cb, fT, fTb, r2b):
    class _Lazy:
        pass
    P = C.P
    orig_dma = P.dma
    state = {"claimed": False}

    def dma_hook(eng, fn, reads, writes, lane):
        if (not state["claimed"]) and r2b in writes:
            C.claim("R2", [r2b])
            state["claimed"] = True
        return orig_dma(eng, fn, reads, writes, lane)
    P.dma = dma_hook
    try:
        wi = emit_outproj(C, wi, mT, lambda f: mTb, DC, x1, x2, t0, gpost, alpha, cb, fT, fTb, r2b)
    finally:
        P.dma = orig_dma
    return wi


def _interleave(ga, gb_):
    la, lb = list(ga), None
    return la


def run_interleaved(gens_a, gens_b):
    na, nb_ = len(gens_a), len(gens_b)
    ia = ib = 0
    while ia < na or ib < nb_:
        fa = ia / na if na else 1.0
        fb = ib / nb_ if nb_ else 1.0
        if ia < na and (fa <= fb or ib >= nb_):
            gens_a[ia](); ia += 1
        else:
            gens_b[ib](); ib += 1


def emit_mixb_layer(C, wbase, NTL, S, x1, x2, cs, cb, dr):
    P = C.P
    NBLK = NTL * 4
    score = [C.R3[:, 0:4096], C.R4[:, 0:4096]]
    scb = [Buf("score0"), Buf("score1")]
    work = C.R3[:, 4096:8192]
    wkb = Buf("work")
    mask = C.R3[:, 8192:10240].bitcast(BF16)
    mkb = Buf("mask")
    C.claim("R3", [scb[0], wkb, mkb])
    C.claim("R4", [scb[1]])
    maskT = [C.mT0[:], C.mT1[:]]
    maskTb = [C.mT0_b, C.mT1_b]
    qib = [C.qib, C.qib2]; qibb = [C.qib_b, C.qib2_b]
    qbk = [C.qbk, C.qbk2]; qbkb = [C.qbk_b, C.qbk2_b]
    wabs = [C.wabs, C.wabs2]; wabsb = [C.wabs_b, C.wabs2_b]
    wsg = [C.wsg, C.wsg2]; wsgb = [C.wsg_b, C.wsg2_b]
    wx = [C.wx, C.wx2]; wxb = [C.wx_b, C.wx2_b]
    rr = [0]

    def nb():
        b = rr[0] % 4
        rr[0] += 1
        return b
    OBS, TB, SCB = (4, 6), 7, 5
    rcs = [C.rc, C.rc2]; rcsb = [C.rc_b, C.rc2_b]
    pmc = [0]
    hdc = [0]
    dg = [C.dg0, C.dg1]; dgb = [C.dg0_b, C.dg1_b]
    rlc = [0]
    psT = C.ps[TB][:].bitcast(BF16)
    kvc = [0]
    st = {}

    def phase_ab(qb):
        th = []
        p = qb % 2
        q0 = qb * 128
        nkb = qb + 1
        Sc = nkb * 128
        ngrp = (nkb + 3) // 4
        sc, scbp = score[p], scb[p]

        def loads():
            P.dma("sp", (lambda e, s_=dr["qiT"][0][:, q0:q0 + 128].rearrange("(c p) t -> p c t", p=128), o=qib[p][:]: e.dma_start(out=o, in_=s_)),
                  reads=[dr["qiT"][1]], writes=[qibb[p]], lane=f"lq0{p}")
            P.dma("sp", (lambda e, s_=dr["qT"][0][:, q0:q0 + 128].rearrange("(c p) t -> p c t", p=128), o=qbk[p][:]: e.dma_start(out=o, in_=s_)),
                  reads=[dr["qT"][1]], writes=[qbkb[p]], lane=f"lq1{p}")
            P.dma("sp", (lambda e, s_=dr["widx"][0][q0:q0 + 128, :], o=wx[p][:]: e.dma_start(out=o, in_=s_)),
                  reads=[dr["widx"][1]], writes=[wxb[p]], lane=f"lq2{p}")
            P.op("act", (lambda e, o=wabs[p][:], i_=wx[p][:]: e.activation(out=o, in_=i_, func=AF.Abs)), reads=[wxb[p]], writes=[wabsb[p]])
            P.op("act", (lambda e, o=wsg[p][:], i_=wx[p][:]: e.activation(out=o, in_=i_, func=AF.Sign)), reads=[wxb[p]], writes=[wsgb[p]])
            for h in range(16):
                P.op("pool", (lambda e, o=dg[p][:, h, :], s_=wsg[p][:, h:h + 1]: e.tensor_scalar(out=o, in0=C.ident[:], scalar1=s_, scalar2=1.0, op0=ALU.mult, op1=ALU.mult)),
                     reads=[C.ident_b, wsgb[p]], writes=[dgb[p]])
        th.append(loads)
        for grp in range(ngrp):
            c0 = grp * 512
            n = min(Sc, c0 + 512) - c0
            for h in range(16):
                def idx(grp=grp, c0=c0, n=n, h=h):
                    c, half = h // 2, h % 2
                    rows = slice(half * 64, half * 64 + 64)
                    bk = nb()
                    mm(P, C.ps[bk][:, 0:n], qib[p][rows, c, :], C.kis[rows, c0:c0 + n], True, True, [qibb[p], C.kis_b], [C.ps_b[bk]])
                    k2 = rlc[0] % 4
                    rlc[0] += 1
                    P.op("act", (lambda e, o=C.rl16[k2][:, 0:n], i_=C.ps[bk][:, 0:n], s_=wabs[p][:, h:h + 1]: e.activation(out=o, in_=i_, func=AF.Relu, scale=s_)),
                         reads=[C.ps_b[bk], wabsb[p]], writes=[C.rl16_b[k2]])
                    mm(P, C.ps[SCB][:, 0:n], dg[p][:, h, :], C.rl16[k2][:, 0:n], h == 0, h == 15, [dgb[p], C.rl16_b[k2]], [C.ps_b[SCB]])
                    if h == 15:
                        P.op("act", (lambda e, o=sc[:, c0:c0 + n], i_=C.ps[SCB][:, 0:n]: e.activation(out=o, in_=i_, func=AF.Copy)),
                             reads=[C.ps_b[SCB]], writes=[scbp])
                th.append(idx)

        if Sc >= BIS_MIN:
            def binit():
                P.op("dve", (lambda e, s_=sc[:, 0:Sc]: e.max(out=C.m8[:], in_=s_)), reads=[scbp], writes=[C.m8_b])
                P.op("dve", (lambda e, s_=sc[:, 0:Sc]: e.tensor_reduce(out=C.blo[:], in_=s_, axis=mybir.AxisListType.X, op=ALU.min)), reads=[scbp], writes=[C.blo_b])
                P.op("dve", (lambda e: e.tensor_tensor(out=C.brng[:], in0=C.m8[:, 0:1], in1=C.blo[:], op=ALU.subtract)), reads=[C.m8_b, C.blo_b], writes=[C.brng_b])
                P.op("dve", (lambda e: e.tensor_scalar(out=C.bstep[:], in0=C.pw2[:], scalar1=C.brng[:], scalar2=None, op0=ALU.mult)), reads=[C.pw2_b, C.brng_b], writes=[C.bstep_b])
            th.append(binit)

        def causal():
            dsl = sc[:, (nkb - 1) * 128:nkb * 128]
            P.op("pool", (lambda e, d_=dsl: e.affine_select(out=d_, in_=d_, pattern=[[-1, 128]], compare_op=ALU.is_ge, fill=fillreg(e, NEG), base=0, channel_multiplier=1)),
                 reads=[scbp], writes=[scbp])
        th.append(causal)
        use_bis = Sc >= BIS_MIN
        if use_bis:
            def bis_init():
                pass
            for k in range(BIS_IT):
                def it(k=k):
                    P.op("dve", (lambda e: e.tensor_tensor(out=C.bmid[:], in0=C.blo[:], in1=C.bstep[:, k:k + 1], op=ALU.add)),
                         reads=[C.blo_b, C.bstep_b], writes=[C.bmid_b])
                    P.op("dve", (lambda e, w_=work[:, 0:Sc], s_=sc[:, 0:Sc]: e.tensor_scalar(out=w_, in0=s_, scalar1=C.bmid[:], scalar2=None, op0=ALU.is_ge, op1=ALU.add, accum_out=C.bcnt[:])),
                         reads=[scbp, C.bmid_b], writes=[wkb, C.bcnt_b])
                    P.op("dve", (lambda e: e.tensor_scalar(out=C.bfs[:], in0=C.bcnt[:], scalar1=float(TOPK) - 0.5, scalar2=C.bstep[:, k:k + 1], op0=ALU.is_ge, op1=ALU.mult)),
                         reads=[C.bcnt_b, C.bstep_b], writes=[C.bfs_b])
                    P.op("dve", (lambda e: e.tensor_tensor(out=C.blo[:], in0=C.blo[:], in1=C.bfs[:], op=ALU.add)),
                         reads=[C.blo_b, C.bfs_b], writes=[C.blo_b])
                th.append(it)
        elif Sc > TOPK:
            for r in range(TOPK // 8):
                def rnd(r=r):
                    src = sc[:, 0:Sc] if r == 0 else work[:, 0:Sc]
                    srcb = scbp if r == 0 else wkb
                    P.op("dve", (lambda e, s_=src: e.max(out=C.m8[:], in_=s_)), reads=[srcb], writes=[C.m8_b])
                    if r < TOPK // 8 - 1:
                        P.op("dve", (lambda e, s_=src, w_=work[:, 0:Sc]: e.match_replace(out=w_, in_to_replace=C.m8[:], in_values=s_, imm_value=NEG)),
                             reads=[srcb, C.m8_b], writes=[wkb])
                th.append(rnd)

        def mk():
            if Sc >= BIS_MIN:
                thr, thrb = C.blo[:], C.blo_b
            elif Sc > TOPK:
                thr, thrb = C.m8[:, 7:8], C.m8_b
            else:
                thr, thrb = C.thrneg[:], C.thrneg_b
            P.op("dve", (lambda e, t_=thr, m_=mask[:, 0:Sc], s_=sc[:, 0:Sc]: e.tensor_scalar(out=m_, in0=s_, scalar1=t_, scalar2=None, op0=(ALU.is_lt if ATTN_PE else ALU.is_ge))),
                 reads=[scbp, thrb], writes=[mkb])
        th.append(mk)
        for grp in range(ngrp):
            def tr(grp=grp):
                kbs = list(range(grp * 4, min(nkb, grp * 4 + 4)))
                for j, kb in enumerate(kbs):
                    P.op("pe", (lambda e, o=psT[:, j * 128:(j + 1) * 128], i_=mask[:, kb * 128:(kb + 1) * 128]: e.transpose(o, i_, C.ident[:])),
                         reads=[mkb, C.ident_b], writes=[C.ps_b[TB]])
                n = len(kbs) * 128
                P.op("act", (lambda e, o=maskT[p][:, grp * 512:grp * 512 + n], i_=psT[:, 0:n]: e.activation(out=o, in_=i_, func=AF.Copy)),
                     reads=[C.ps_b[TB]], writes=[maskTb[p]])
            th.append(tr)
        return th

    def phase_c(qb):
        th = []
        p = qb % 2
        qi_ = qb % 4
        nkb = qb + 1
        Sc = nkb * 128
        ngrp = (nkb + 3) // 4
        kh, khb, vh, vhb, ybT, ybTb = st["kh"], st["khb"], st["vh"], st["vhb"], st["ybT"], st["ybTb"]
        pms, pmsb = st["pms"], st["pmsb"]
        for h in range(8):
            hs = {}

            def ld(h=h, hs=hs):
                s2 = kvc[0] % 2
                kvc[0] += 1
                hs["s2"] = s2
                hs["ob"] = OBS[hdc[0] % 2]
                hs["rc"] = hdc[0] % 2
                hdc[0] += 1
                P.dma("sp", (lambda e, o=kh[s2][:, 0:Sc], s_=dr["kT"][0][h * 128:(h + 1) * 128, 0:Sc]: e.dma_start(out=o, in_=s_)),
                      reads=[dr["kT"][1]], writes=[khb[s2]], lane=f"lk{s2}")
                P.dma("sp", (lambda e, o=vh[s2][:, 0:nkb, :], s_=dr["v"][0][0:Sc, h * 128:(h + 1) * 128].rearrange("(b p) c -> p b c", p=128): e.dma_start(out=o, in_=s_)),
                      reads=[dr["v"][1]], writes=[vhb[s2]], lane=f"lv{s2}")
            th.append(ld)
            for grp in range(ngrp):
                def at(h=h, grp=grp, hs=hs):
                    s2, OB = hs["s2"], hs["ob"]
                    kbs = list(range(grp * 4, min(nkb, grp * 4 + 4)))
                    n = len(kbs) * 128
                    if ATTN_PE:
                        bk = nb()
                        for j, kb in enumerate(kbs):
                            w_ = nkb - 1 - kb
                            near = w_ <= 1
                            o_ = C.ps[bk][:, j * 128:(j + 1) * 128]
                            P.op("pe", (lambda e, o=o_, l=kh[s2][:, kb * 128:(kb + 1) * 128], r=qbk[p][:, h, :]: e.matmul(o, l, r, start=True, stop=False, skip_group_check=True)),
                                 reads=[khb[s2], qbkb[p]], writes=[C.ps_b[bk]])
                            P.op("pe", (lambda e, o=o_, r=maskT[p][:, kb * 128:(kb + 1) * 128], sp=(not near): e.matmul(o, C.negI[:], r, start=False, stop=sp, skip_group_check=True)),
                                 reads=[C.negI_b, maskTb[p]], writes=[C.ps_b[bk]])
                            if near:
                                P.op("pe", (lambda e, o=o_, r=C.Fb[:, (h * 2 + w_) * 128:(h * 2 + w_ + 1) * 128]: e.matmul(o, C.ident[:], r, start=False, stop=True, skip_group_check=True)),
                                     reads=[C.ident_b, C.Fb_b], writes=[C.ps_b[bk]])
                        k2 = pmc[0] % 4
                        pmc[0] += 1
                        P.op("act", (lambda e, o=pms[k2][:, 0:n], i_=C.ps[bk][:, 0:n]: e.activation(out=o, in_=i_, func=AF.Exp, bias=C.cb8[:, h:h + 1], scale=ATT_SCALE)),
                             reads=[C.ps_b[bk], C.cb8_b], writes=[pmsb[k2]])
                    else:
                        bk = nb()
                        for j, kb in enumerate(kbs):
                            mm(P, C.ps[bk][:, j * 128:(j + 1) * 128], kh[s2][:, kb * 128:(kb + 1) * 128], qbk[p][:, h, :], True, True, [khb[s2], qbkb[p]], [C.ps_b[bk]])
                        k3 = C.cnt % 2
                        C.cnt += 1
                        P.op("act", (lambda e, o=C.tmpa[k3][:, 0:n], i_=C.ps[bk][:, 0:n]: e.activation(out=o, in_=i_, func=AF.Exp, bias=C.cb8[:, h:h + 1], scale=ATT_SCALE)),
                             reads=[C.ps_b[bk], C.cb8_b], writes=[C.tmpa_b[k3]])
                        k2 = pmc[0] % 4
                        pmc[0] += 1
                        P.op("dve", (lambda e, o=pms[k2][:, 0:n], a=C.tmpa[k3][:, 0:n], b=maskT[p][:, grp * 512:grp * 512 + n]: e.tensor_tensor(out=o, in0=a, in1=b, op=ALU.mult)),
                             reads=[C.tmpa_b[k3], maskTb[p]], writes=[pmsb[k2]])
                        for j, kb in enumerate(kbs):
                            w_ = nkb - 1 - kb
                            if w_ <= 1:
                                P.op("dve", (lambda e, o=pms[k2][:, j * 128:(j + 1) * 128], f_=C.Fe[:, (h * 2 + w_) * 128:(h * 2 + w_ + 1) * 128]: e.tensor_tensor(out=o, in0=o, in1=f_, op=ALU.mult)),
                                     reads=[pmsb[k2], C.Fe_b], writes=[pmsb[k2]])
                    for j, kb in enumerate(kbs):
                        P.op("pe", (lambda e, o=C.ps[OB][:, 0:128], l=pms[k2][:, j * 128:(j + 1) * 128], r=vh[s2][:, kb, :], st_=(kb == 0), sp=(kb == nkb - 1):
                                    e.matmul(o, l, r, start=st_, stop=sp, skip_group_check=True)),
                             reads=[pmsb[k2], vhb[s2]], writes=[C.ps_b[OB]])
                        P.op("pe", (lambda e, o=C.ps[OB][:, 128:129], l=pms[k2][:, j * 128:(j + 1) * 128], sp=(kb == nkb - 1):
                                    e.matmul(o, l, C.onec[:, 0:1], start=False, stop=sp, skip_group_check=True)),
                             reads=[pmsb[k2], C.onec_b], writes=[C.ps_b[OB]])
                th.append(at)

            def fin(h=h, hs=hs):
                OB, ri = hs["ob"], hs["rc"]
                P.op("dve", (lambda e, o=rcs[ri][:], i_=C.ps[OB][:, 128:129]: e.reciprocal(out=o, in_=i_)), reads=[C.ps_b[OB]], writes=[rcsb[ri]])
                P.op("dve", (lambda e, o=C.yb[:, h * 128:(h + 1) * 128], i_=C.ps[OB][:, 0:128], s_=rcs[ri][:]: e.tensor_scalar(out=o, in0=i_, scalar1=s_, scalar2=None, op0=ALU.mult)),
                     reads=[C.ps_b[OB], rcsb[ri]], writes=[C.yb_b])
            th.append(fin)
        for half in range(2):
            def ytr(half=half):
                for j in range(4):
                    c = half * 4 + j
                    P.op("pe", (lambda e, o=psT[:, j * 128:(j + 1) * 128], i_=C.yb[:, c * 128:(c + 1) * 128]: e.transpose(o, i_, C.ident[:])),
                         reads=[C.yb_b, C.ident_b], writes=[C.ps_b[TB]])
                P.op("act", (lambda e, o=ybT[:, half * 4:half * 4 + 4, qi_ * 128:(qi_ + 1) * 128], i_=psT[:, 0:512].rearrange("p (c t) -> p c t", t=128):
                             e.activation(out=o, in_=i_, func=AF.Copy)),
                     reads=[C.ps_b[TB]], writes=[ybTb])
            th.append(ytr)
        return th

    def open_tile():
        kh = [C.R1[:, i * 2048:(i + 1) * 2048].bitcast(BF16) for i in range(2)]
        khb = [Buf("kh0"), Buf("kh1")]
        ybT = C.R1[:, 4096:6144].bitcast(BF16).rearrange("p (c t) -> p c t", t=NT)
        ybTb = Buf("ybT")
        vh = [C.R2[:, i * 2048:(i + 1) * 2048].bitcast(BF16).rearrange("p (b c) -> p b c", c=128) for i in range(2)]
        vhb = [Buf("vh0"), Buf("vh1")]
        pms = [C.R1[:, 6144 + i * 256:6144 + (i + 1) * 256].bitcast(BF16) for i in range(4)]
        pmsb = [Buf(f"pm{i}") for i in range(4)]
        C.claim("R1", khb + [ybTb] + pmsb); C.claim("R2", vhb)
        st.update(kh=kh, khb=khb, ybT=ybT, ybTb=ybTb, vh=vh, vhb=vhb, pms=pms, pmsb=pmsb)

    def tail(tt, wi):
        t0 = tt * NT
        ybT, ybTb = st["ybT"], st["ybTb"]
        mT = C.R2[:, :].bitcast(BF16).rearrange("p (c t) -> p c t", t=NT)
        mTb = Buf("mT")
        C.claim("R2", [mTb])
        lc = [0]
        for c2 in range(8):
            w, wb = C.wget(wi); wi += 1
            for cc in range(2):
                ch = 2 * c2 + cc
                bk = nb()
                for k in range(8):
                    mm(P, C.ps[bk][:], w[:, k, cc * 128:(cc + 1) * 128], ybT[:, k, :], k == 0, k == 7, [wb, ybTb], [C.ps_b[bk]])
                k2 = lc[0] % 2
                lc[0] += 1
                gbt, gbb = (C.gt1, C.gt1_b) if k2 == 0 else (C.gt2, C.gt2_b)
                P.dma("sp", (lambda e, o=gbt[:], s_=dr["gb"][0][ch * 128:(ch + 1) * 128, t0:t0 + NT]: e.dma_start(out=o, in_=s_)),
                      reads=[dr["gb"][1]], writes=[gbb], lane=f"lg{k2}")
                P.dma("sp", (lambda e, o=C.ta2[k2][:], s_=dr["apart"][0][ch * 128:(ch + 1) * 128, t0:t0 + NT]: e.dma_start(out=o, in_=s_)),
                      reads=[dr["apart"][1]], writes=[C.ta2_b[k2]], lane=f"la{k2}")
                P.op("dve", (lambda e, g_=gbt[:], i_=C.ps[bk][:]: e.tensor_tensor(out=g_, in0=g_, in1=i_, op=ALU.mult)),
                     reads=[gbb, C.ps_b[bk]], writes=[gbb])
                P.op("dve", (lambda e, o=mT[:, ch, :], g_=gbt[:], a=C.ta2[k2][:]: e.tensor_tensor(out=o, in0=g_, in1=a, op=ALU.add)),
                     reads=[gbb, C.ta2_b[k2]], writes=[mTb])
        fT = C.R1[:, :].rearrange("p (c t) -> p c t", t=NT)
        fTb = Buf("fT")
        C.claim("R1", [fTb])
        r2b = Buf("r2x")
        wi = emit_outproj_claim(C, wi, mT, mTb, x1, x2, t0, cs[:, 48:64], 1.0, cb, fT, fTb, r2b)
        return wi

    wi = wbase
    for f in phase_ab(0):
        f()
    for qb in range(NBLK):
        if qb % 4 == 0:
            open_tile()
        ca = phase_ab(qb + 1) if qb + 1 < NBLK else []
        cc_ = phase_c(qb)
        run_interleaved(ca, cc_)
        if qb % 4 == 3:
            wi = tail(qb // 4, wi)
    return wi


L_ = 2
NUM_BUCKETS = 32
MAX_DISTANCE = 128
IN_COLS = 6224


def build_program(S, depth):
    nc = bass.Bass("TRN2", target_bir_lowering=False)
    NTL = S // NT

    def din(name, shape, dt=F32):
        return nc.dram_tensor(name, list(shape), dt, kind="ExternalInput").ap()

    def dscr(name, shape, dt=F32):
        return nc.dram_tensor(name, list(shape), dt, kind="Internal").ap()
    x = din("x", [D, S])
    y = nc.dram_tensor("y", [D, S], F32, kind="ExternalOutput").ap()
    W = {}
    for l in range(depth):
        W[l] = dict(
            f1i=din(f"f1i{l}", [D, 2 * DFF]), f1o=din(f"f1o{l}", [DFF, D]),
            f2i=din(f"f2i{l}", [D, 2 * DFF]), f2o=din(f"f2o{l}", [DFF, D]),
            win=din(f"win{l}", [D, IN_COLS]), wg=din(f"wg{l}", [D, 2 * D]),
            wba=din(f"wba{l}", [AW, D]), wbb=din(f"wbb{l}", [AW, D]), wo=din(f"wo{l}", [D, D]),
            cpk=din(f"cpk{l}", [128, 96]), bro=din(f"bro{l}", [128, 3072]), wsT=din(f"wsT{l}", [128, 8, 128]),
        )
    toep = din("toep", [128, 2048])
    cb8d = din("cb8", [128, 8])
    xa = (dscr("xa", [D, S]), Buf("xa"))
    xb = (dscr("xb", [D, S]), Buf("xb"))
    xc = (dscr("xc", [D, S]), Buf("xc"))
    dr = {
        "qT": (dscr("qT", [AW, S], BF16), Buf("qT")), "qiT": (dscr("qiT", [AW, S], BF16), Buf("qiT")),
        "kT": (dscr("kT", [AW, S], BF16), Buf("kT")), "v": (dscr("v", [S, AW], BF16), Buf("v")),
        "kiT": (dscr("kiT", [128, S], BF16), Buf("kiT")), "widx": (dscr("widx", [S, 16]), Buf("widx")),
        "apart": (dscr("apart", [D, S]), Buf("apart")), "gb": (dscr("gb", [D, S]), Buf("gb")),
    }
    P = Prog(nc)
    C = Ctx(P)
    cs = [P.sbuf(f"consts{l}", [128, 96], F32) for l in range(depth)]
    cb = [Buf(f"consts{l}") for l in range(depth)]
    for l in range(depth):
        P.dma("sp", (lambda e, l=l: e.dma_start(out=cs[l][:], in_=W[l]["cpk"])), reads=[], writes=[cb[l]], lane="ldc")
    emit_attn_consts(C, toep, cb8d)
    for l in range(depth):
        w = W[l]
        for t in range(NTL):
            C.plan_extend(ffn_plan(w["f1i"], w["f1o"]))
        for t in range(NTL):
            C.plan_extend(mixa_plan(w["win"], w["wg"], w["wba"]))
        for t in range(NTL):
            C.plan_extend(mixb_tail_plan(w["wbb"], w["wo"]))
        for t in range(NTL):
            C.plan_extend(ffn_plan(w["f2i"], w["f2o"]))
    wi = 0
    xin = (x, Buf("x"))
    for l in range(depth):
        w = W[l]
        xout = (y, Buf("y")) if l == depth - 1 else xc
        for t in range(NTL):
            wi = emit_ffn(C, wi, xin, xa, t * NT, cs[l][:, 0:16], cs[l][:, 16:32], cb[l])
        emit_mix_consts(C, w["bro"], w["wsT"], None)
        for t in range(NTL):
            wi = emit_mixa(C, wi, xa, t * NT, S, cs[l], cb[l], dr)
        P.dma("sp", (lambda e: e.dma_start(out=C.kis[:, 0:S], in_=dr["kiT"][0])), reads=[dr["kiT"][1]], writes=[C.kis_b], lane="ldk")
        wi = emit_mixb_layer(C, wi, NTL, S, xa, xb, cs[l], cb[l], dr)
        for t in range(NTL):
            wi = emit_ffn(C, wi, xb, xout, t * NT, cs[l][:, 64:80], cs[l][:, 80:96], cb[l])
        xin = xc
    assert wi == len(C.plan), (wi, len(C.plan))
    P.finalize()
    return nc


def t5_bucket_np(n):
    max_exact = NUM_BUCKETS // 2
    nf = np.maximum(n, 1).astype(np.float32)
    large = max_exact + (np.log(nf / max_exact) / np.log(MAX_DISTANCE / max_exact) * (NUM_BUCKETS - max_exact)).astype(np.int32)
    large = np.minimum(large, NUM_BUCKETS - 1)
    return np.where(n < max_exact, n, large)


def host_prep(inp, depth):
    f = lambda a: np.ascontiguousarray(np.asarray(a, dtype=np.float32))
    m = {}

    def pc(v):
        return np.asarray(v, dtype=np.float32).reshape(16, 128).T
    for l in range(depth):
        m[f"f1i{l}"] = f(inp["ffn1_w_in"][l]); m[f"f1o{l}"] = f(inp["ffn1_w_out"][l])
        m[f"f2i{l}"] = f(inp["ffn2_w_in"][l]); m[f"f2o{l}"] = f(inp["ffn2_w_out"][l])
        m[f"win{l}"] = f(inp["w_in"][l]); m[f"wg{l}"] = f(inp["w_gate"][l])
        m[f"wba{l}"] = f(inp["w_branch_a"][l]); m[f"wbb{l}"] = f(inp["w_branch_b"][l]); m[f"wo{l}"] = f(inp["w_out"][l])
        m[f"cpk{l}"] = f(np.concatenate([pc(inp[k][l]) for k in ("ffn1_norm_pre", "ffn1_norm_post", "mix_norm_pre", "mix_norm_post", "ffn2_norm_pre", "ffn2_norm_post")], axis=1))
        row = np.concatenate([np.asarray(inp["sgu_ln_g"][l], np.float32), np.asarray(inp["sgu_ln_b"][l], np.float32), np.asarray(inp["sgu_b"][l], np.float32).reshape(-1)])
        m[f"bro{l}"] = f(np.broadcast_to(row[None, :], (128, 3072)))
        m[f"wsT{l}"] = f(np.transpose(np.asarray(inp["sgu_w_s"][l], np.float32), (2, 0, 1)))
    rb = np.asarray(inp["rel_bias"], np.float32)
    s_ = np.arange(128)[:, None]
    t_ = np.arange(128)[None, :]
    toep = np.zeros((128, 8, 2, 128), np.float32)
    for w_ in range(2):
        dist = np.maximum(t_ - s_ + 128 * w_, 0)
        bk = t5_bucket_np(dist)
        toep[:, :, w_, :] = np.transpose(rb[bk], (0, 2, 1))
    m["toep"] = f(toep.reshape(128, 2048))
    m["cb8"] = f(np.broadcast_to(rb[NUM_BUCKETS - 1][None, :], (128, 8)))
    return m


BATCH = 4
DEPTH = 2
_NC_CACHE = {}


def kernel(**inputs):
    inp = {k: np.asarray(v) for k, v in inputs.items()}
    S = inp["x"].shape[1]
    if "nc" not in _NC_CACHE:
        _NC_CACHE["nc"] = build_program(S, DEPTH)
    nc = _NC_CACHE["nc"]
    shared = host_prep(inp, DEPTH)
    in_maps = []
    for b in range(BATCH):
        m = dict(shared)
        m["x"] = np.ascontiguousarray(inp["x"][b].T.astype(np.float32))
        in_maps.append(m)
    res = run_bass_kernel_spmd(nc, in_maps, core_ids=list(range(BATCH)))
    out = np.stack([np.asarray(res.results[b]["y"]).T for b in range(BATCH)], axis=0)
    return np.ascontiguousarray(out.astype(np.float32))
```

```python
import numpy as np
from concourse.bass_utils import run_bass_kernel_spmd
from contextlib import ExitStack
import numpy as np
import concourse.bass as bass
import concourse.mybir as mybir

F32 = mybir.dt.float32
BF16 = mybir.dt.bfloat16
AF = mybir.ActivationFunctionType
ALU = mybir.AluOpType

ENGS = ("pe", "act", "dve", "pool", "sp")


class Buf:
    __slots__ = ("name", "lastw", "readers")

    def __init__(self, name):
        self.name = name
        self.lastw = None
        self.readers = {}


class Lane:
    def __init__(self, prog, name):
        self.sem = prog.nc.alloc_semaphore(name=name)
        self.count = 0
        self.last = None


class Op:
    __slots__ = ("eng", "fn", "waits", "flag", "val", "lane", "laneval", "idx")

    def __init__(self, eng, fn):
        self.eng = eng
        self.fn = fn
        self.waits = []
        self.flag = False
        self.val = None
        self.lane = None
        self.laneval = None


class Prog:
    def __init__(self, nc):
        self.nc = nc
        self.q = {e: [] for e in ENGS}
        self.sem = {e: nc.alloc_semaphore(name="sem_" + e) for e in ENGS}
        self.es = ExitStack()
        self.lanes = {}
        self.n_sb = 0

    def sbuf(self, name, shape, dtype):
        return self.es.enter_context(self.nc.sbuf_tensor(name, list(shape), dtype))

    def psum(self, name, shape, dtype=F32):
        return self.es.enter_context(self.nc.psum_tensor(name, list(shape), dtype))

    def lane(self, name):
        if name not in self.lanes:
            self.lanes[name] = Lane(self, "ln_" + name)
        return self.lanes[name]

    def _deps(self, op, reads, writes):
        evs = []
        for b in reads:
            if b.lastw is not None:
                evs.append(b.lastw)
        for b in writes:
            if b.lastw is not None:
                evs.append(b.lastw)
            evs.extend(b.readers.values())
        for ev in evs:
            if ev[0] == "c":
                src = ev[1]
                if src.eng == "pe" and op.eng == "pe":
                    continue
                if src is op:
                    continue
                src.flag = True
            op.waits.append(ev)

    def _post(self, ev, reads, writes):
        for b in writes:
            b.lastw = ev
            b.readers = {}
        key = ("c", ev[1].eng) if ev[0] == "c" else ("d", id(ev[1]))
        for b in reads:
            if b not in writes:
                b.readers[key] = ev

    def op(self, eng, fn, reads=(), writes=()):
        o = Op(eng, fn)
        self._deps(o, reads, writes)
        o.idx = len(self.q[eng])
        self.q[eng].append(o)
        self._post(("c", o), reads, writes)
        return o

    def dma(self, eng, fn, reads, writes, lane):
        ln = self.lane(lane) if isinstance(lane, str) else lane
        o = Op(eng, fn)
        self._deps(o, reads, writes)
        if ln.last is not None:
            o.waits.append(ln.last)
        ln.count += 16
        o.lane = ln
        o.laneval = ln.count
        ev = ("d", ln, ln.count)
        ln.last = ev
        self.q[eng].append(o)
        self._post(ev, reads, writes)
        return o

    def claim(self, old_bufs, new_bufs):
        merged = {}
        for b in old_bufs:
            evs = list(b.readers.values())
            if b.lastw is not None:
                evs.append(b.lastw)
            for ev in evs:
                key = ("c", ev[1].eng) if ev[0] == "c" else ("d", id(ev[1]))
                rank = ev[1].idx if ev[0] == "c" else ev[2]
                if key not in merged or merged[key][0] < rank:
                    merged[key] = (rank, ev)
        for nb_ in new_bufs:
            for key, (rank, ev) in merged.items():
                nb_.readers[key] = ev

    def finalize(self):
        nc = self.nc
        for e in ENGS:
            c = 0
            for o in self.q[e]:
                if o.flag:
                    c += 1
                    o.val = c
        engobj = {"pe": "tensor", "act": "scalar", "dve": "vector", "pool": "gpsimd", "sp": "sync"}
        final_lane_vals = [(ln.sem, ln.count) for ln in self.lanes.values() if ln.count > 0]
        final_eng_vals = {e: max([o.val for o in self.q[e] if o.flag] + [0]) for e in ENGS}
        prog = self

        def emit(e, eng):
            known = {}
            for o in prog.q[e]:
                need = {}
                for ev in o.waits:
                    if ev[0] == "c":
                        src = ev[1]
                        key = ("c", src.eng)
                        sem, val = prog.sem[src.eng], src.val
                    else:
                        key = ("d", id(ev[1]))
                        sem, val = ev[1].sem, ev[2]
                    if known.get(key, 0) >= val:
                        continue
                    if key not in need or need[key][1] < val:
                        need[key] = (sem, val)
                for key, (sem, val) in need.items():
                    eng.wait_ge(sem, val)
                    known[key] = val
                ins = o.fn(eng)
                if o.lane is not None:
                    ins.then_inc(o.lane.sem, 16)
                elif o.flag:
                    ins.then_inc(prog.sem[e], 1)
            if e == "sp":
                for sem, val in final_lane_vals:
                    eng.wait_ge(sem, val)
                for e2, v in final_eng_vals.items():
                    if v > 0:
                        eng.wait_ge(prog.sem[e2], v)

        with nc.Block() as block:
            @block.tensor
            def _(eng):
                emit("pe", eng)

            @block.scalar
            def _(eng):
                emit("act", eng)

            @block.vector
            def _(eng):
                emit("dve", eng)

            @block.gpsimd
            def _(eng):
                emit("pool", eng)

            @block.sync
            def _(eng):
                emit("sp", eng)
        self.es.close()


D = 2048
DC = D // 128
DFF = 5632
FC = DFF // 128
NT = 512
NORM_EPS = 1e-6
ATTN_PE = 1
LN_EPS = 1e-5
SEQ = 4096
CASTDMA = True


class Ctx:
    def __init__(self, P):
        self.P = P
        nc = P.nc
        self.ps = [P.psum(f"ps{i}", [128, 512]) for i in range(8)]
        self.ps_b = [Buf(f"ps{i}") for i in range(8)]
        self.NS = 4
        self.wbf = [P.sbuf(f"wbf{i}", [128, 4096], BF16) for i in range(self.NS)]
        self.wbf_b = [Buf(f"wbf{i}") for i in range(self.NS)]
        self.plan = []
        self.issued = 0
        self.R1 = P.sbuf("R1", [128, 8192], F32)
        self.R2 = P.sbuf("R2", [128, 4096], F32)
        self.R3 = P.sbuf("R3", [128, 11264], F32)
        self.ones = P.sbuf("ones", [128, 128], BF16)
        self.ones_b = Buf("ones")
        self.tmpa = [P.sbuf(f"tmpa{i}", [128, 512], F32) for i in range(2)]
        self.tmpa_b = [Buf(f"tmpa{i}") for i in range(2)]
        self.sq = [P.sbuf(f"sq{i}", [128, 512], BF16) for i in range(2)]
        self.sq_b = [Buf(f"sq{i}") for i in range(2)]
        self.rstd = P.sbuf("rstd", [128, 512], F32)
        self.rstd_b = Buf("rstd")
        self.epsc = P.sbuf("epsc", [128, 1], F32)
        self.epsc_b = Buf("epsc")
        self.cnt = 0
        self.reg = {"R1": [], "R2": [], "R3": []}
        def sb(name, shape, dt):
            setattr(self, name, P.sbuf("s_" + name, shape, dt))
            setattr(self, name + "_b", Buf(name))
        sb("kio", [128, 512], BF16); sb("wio", [128, 64], F32)
        sb("gt1", [128, 512], F32); sb("gt2", [128, 512], F32)
        self.R4 = P.sbuf("R4", [128, 4096], F32)
        self.reg["R4"] = []
        self.bro = self.R4[:, 0:3072]; self.bro_b = Buf("bro")
        self.wsf = self.R4[:, 3072:4096]; self.wsf_b = Buf("wsf")
        sb("wsb", [128, 8, 128], BF16)
        sb("mT0", [128, SEQ], BF16); sb("mT1", [128, SEQ], BF16)
        sb("qib2", [128, 8, 128], BF16); sb("qbk2", [128, 8, 128], BF16); sb("wx2", [128, 16], F32)
        sb("wabs2", [128, 16], F32); sb("wsg2", [128, 16], F32)
        sb("dg0", [128, 16, 128], BF16); sb("negI", [128, 128], BF16)
        sb("rc2", [128, 1], F32)
        if not ATTN_PE:
            sb("Fe", [128, 2048], BF16)
        self.dg1, self.dg1_b = self.dg0, self.dg0_b
        for i in range(4):
            sb(f"rl16_{i}", [128, 512], BF16)
        self.rl16 = [getattr(self, f"rl16_{i}") for i in range(4)]; self.rl16_b = [getattr(self, f"rl16_{i}_b") for i in range(4)]
        sb("blo", [128, 1], F32); sb("bmid", [128, 1], F32); sb("bcnt", [128, 1], F32); sb("bfs", [128, 1], F32); sb("brng", [128, 1], F32)
        sb("bstep", [128, 32], F32); sb("pw2", [128, 32], F32)
        for k in range(32):
            P.op("pool", (lambda e, k=k: e.memset(self.pw2[:, k:k + 1], 2.0 ** -(k + 1))), writes=[self.pw2_b])
        self.ta2 = self.tmpa; self.ta2_b = self.tmpa_b
        sb("bst", [128, 12], F32); sb("bag", [128, 2], F32); sb("lrs", [128, 1], F32); sb("lneps", [128, 1], F32)
        sb("qib", [128, 8, 128], BF16); sb("qbk", [128, 8, 128], BF16); sb("wx", [128, 16], F32)
        sb("wabs", [128, 16], F32); sb("wsg", [128, 16], F32); sb("kis", [128, SEQ], BF16)
        sb("m8", [128, 8], F32); sb("thrneg", [128, 1], F32); sb("ident", [128, 128], BF16); sb("onec", [128, 2], BF16)
        sb("rc", [128, 1], F32); sb("yb", [128, 1024], BF16)
        sb("Fb", [128, 2048], BF16); sb("cb8", [128, 8], F32)
        P.op("pool", lambda e: e.memset(self.lneps[:], LN_EPS), writes=[self.lneps_b])
        P.op("pool", lambda e: e.memset(self.ones[:], 1.0), writes=[self.ones_b])
        P.op("pool", lambda e: e.memset(self.epsc[:], NORM_EPS), writes=[self.epsc_b])

    def claim(self, rname, bufs):
        self.P.claim(self.reg[rname], bufs)
        self.reg[rname] = list(bufs)

    def plan_extend(self, specs):
        self.plan.extend(specs)

    def _issue(self, i):
        P = self.P
        pieces, R, W = self.plan[i]
        s = i % self.NS
        if CASTDMA:
            bt = self.wbf[s][:, 0:R * W].rearrange("p (r w) -> p r w", w=W)
            for pi, (c0, wd, src) in enumerate(pieces):
                P.dma("pool", (lambda e, o=bt[:, :, c0:c0 + wd], s_=src: e.dma_start(out=o, in_=s_)),
                      reads=[], writes=[self.wbf_b[s]], lane=f"w{s}_{pi}")
            return
        st = self.wst[s][:, 0:R * W].rearrange("p (r w) -> p r w", w=W)
        for pi, (c0, wd, src) in enumerate(pieces):
            P.dma("sp", (lambda e, o=st[:, :, c0:c0 + wd], s_=src: e.dma_start(out=o, in_=s_)),
                  reads=[], writes=[self.wst_b[s]], lane=f"w{s}_{pi}")
        P.op("pool", (lambda e, o=self.wbf[s][:, 0:R * W], i_=self.wst[s][:, 0:R * W]: e.tensor_copy(out=o, in_=i_)),
             reads=[self.wst_b[s]], writes=[self.wbf_b[s]])

    def wget(self, i, look=2):
        while self.issued <= min(i + look, len(self.plan) - 1):
            self._issue(self.issued)
            self.issued += 1
        pieces, R, W = self.plan[i]
        s = i % self.NS
        return self.wbf[s][:, 0:R * W].rearrange("p (r w) -> p r w", w=W), self.wbf_b[s]


_FILLREG = {}


def fillreg(e, v):
    key = (id(e), v)
    if key not in _FILLREG:
        _FILLREG[key] = e.to_reg(v)
    return _FILLREG[key]


def mm(P, o, l, r, st, sp, reads, writes):
    P.op("pe", (lambda e: e.matmul(o, l, r, start=st, stop=sp)), reads=reads, writes=writes)


def w_panel(w2d, r0, nr, cols):
    pieces = []
    off = 0
    for (c0, wd) in cols:
        src = w2d[r0 * 128:(r0 + nr) * 128, c0:c0 + wd].rearrange("(r p) w -> p r w", p=128)
        pieces.append((off, wd, src))
        off += wd
    return (pieces, nr, off)


def emit_rstd(C, chunk_ap, nch, dim, bank):
    P = C.P
    ps, psb = C.ps[bank], C.ps_b[bank]
    for c in range(nch):
        ap, b = chunk_ap(c)
        k = C.cnt % 2
        C.cnt += 1
        P.op("act", (lambda e, o=C.sq[k][:], i_=ap: e.activation(out=o, in_=i_, func=AF.Square)),
             reads=[b], writes=[C.sq_b[k]])
        P.op("pe", (lambda e, o=ps[:], l=C.ones[:], r=C.sq[k][:], st=(c == 0), sp=(c == nch - 1):
                    e.matmul(o, l, r, start=st, stop=sp)),
             reads=[C.ones_b, C.sq_b[k]], writes=[psb])
    P.op("act", (lambda e: e.activation(out=C.rstd[:], in_=ps[:], func=AF.Sqrt, bias=C.epsc[:], scale=1.0 / dim)),
         reads=[psb, C.epsc_b], writes=[C.rstd_b])
    P.op("dve", (lambda e: e.reciprocal(out=C.rstd[:], in_=C.rstd[:])), reads=[C.rstd_b], writes=[C.rstd_b])


def ffn_plan(w_in, w_out):
    specs = []
    for g in range(FC // 2):
        specs.append(w_panel(w_in, 0, DC, [(g * 256, 256)]))
        specs.append(w_panel(w_in, 0, DC, [(DFF + g * 256, 256)]))
    for c2 in range(DC // 2):
        for kp in range(3):
            r0 = kp * 16
            nr = min(16, FC - r0)
            specs.append(w_panel(w_out, r0, nr, [(c2 * 256, 256)]))
    return specs


def emit_ffn(C, wbase, x_dram, xo_dram, t0, gpre, gpost, cb):
    P = C.P
    (xd, xdb), (xo, xob) = x_dram, xo_dram
    xT = C.R1[:, :].rearrange("p (c t) -> p c t", t=NT)
    xTb = Buf("xT")
    hT = C.R2[:, :].bitcast(BF16).rearrange("p (c t) -> p c t", t=NT)
    hTb = Buf("hT")
    gT = C.R3[:, :].bitcast(BF16).rearrange("p (c t) -> p c t", t=NT)
    gTb = [Buf(f"gT{j}") for j in range(FC)]
    C.claim("R1", [xTb]); C.claim("R2", [hTb]); C.claim("R3", gTb)
    xsrc = xd[:, t0:t0 + NT].rearrange("(c p) t -> p c t", p=128)
    P.dma("sp", lambda e: e.dma_start(out=xT, in_=xsrc), reads=[xdb], writes=[xTb], lane="ld0")
    emit_rstd(C, lambda c: (xT[:, c, :], xTb), DC, D, 0)
    for c in range(DC):
        P.op("dve", (lambda e, c=c: e.scalar_tensor_tensor(out=hT[:, c, :], in0=xT[:, c, :], scalar=gpre[:, c:c + 1],
                                                          in1=C.rstd[:], op0=ALU.mult, op1=ALU.mult)),
             reads=[xTb, C.rstd_b, cb], writes=[hTb])
    wi = wbase
    for g in range(FC // 2):
        base = 4 * (g % 2)
        wa, wab = C.wget(wi); wi += 1
        for cc in range(2):
            bk = base + cc
            for k in range(DC):
                P.op("pe", (lambda e, o=C.ps[bk][:], l=wa[:, k, cc * 128:(cc + 1) * 128], r=hT[:, k, :], st=(k == 0), sp=(k == DC - 1):
                            e.matmul(o, l, r, start=st, stop=sp)),
                     reads=[wab, hTb], writes=[C.ps_b[bk]])
        wb_, wbb = C.wget(wi); wi += 1
        for cc in range(2):
            bk = base + 2 + cc
            for k in range(DC):
                P.op("pe", (lambda e, o=C.ps[bk][:], l=wb_[:, k, cc * 128:(cc + 1) * 128], r=hT[:, k, :], st=(k == 0), sp=(k == DC - 1):
                            e.matmul(o, l, r, start=st, stop=sp)),
                     reads=[wbb, hTb], writes=[C.ps_b[bk]])
        for cc in range(2):
            j = 2 * g + cc
            k2 = C.cnt % 2
            C.cnt += 1
            P.op("act", (lambda e, o=C.tmpa[k2][:], i_=C.ps[base + cc][:]: e.activation(out=o, in_=i_, func=AF.Silu)),
                 reads=[C.ps_b[base + cc]], writes=[C.tmpa_b[k2]])
            P.op("dve", (lambda e, o=gT[:, j, :], a=C.tmpa[k2][:], b=C.ps[base + 2 + cc][:]:
                         e.tensor_tensor(out=o, in0=a, in1=b, op=ALU.mult)),
                 reads=[C.tmpa_b[k2], C.ps_b[base + 2 + cc]], writes=[gTb[j]])
    wi = emit_outproj(C, wi, gT, lambda f: gTb[f], FC, (xd, xdb), (xo, xob), t0, gpost, 0.5, cb, xT, xTb, hTb)
    return wi


def outproj_plan(w_out, K):
    specs = []
    for c2 in range(DC // 2):
        for r0 in range(0, K, 16):
            specs.append(w_panel(w_out, r0, min(16, K - r0), [(c2 * 256, 256)]))
    return specs


def emit_outproj(C, wi, inT, inb, K, x_dram, xo_dram, t0, gpost, alpha, cb, fT, fTb, r2b):
    P = C.P
    (xd, xdb), (xo, xob) = x_dram, xo_dram
    SB = 6
    for c2 in range(DC // 2):
        base = 2 * (c2 % 2)
        for r0 in range(0, K, 16):
            nr = min(16, K - r0)
            wo, wob = C.wget(wi); wi += 1
            for cc in range(2):
                bk = base + cc
                for r in range(nr):
                    f = r0 + r
                    mm(P, C.ps[bk][:], wo[:, r, cc * 128:(cc + 1) * 128], inT[:, f, :], f == 0, f == K - 1, [wob, inb(f)], [C.ps_b[bk]])
        for cc in range(2):
            c = 2 * c2 + cc
            bk = base + cc
            P.op("act", (lambda e, o=fT[:, c, :], i_=C.ps[bk][:]: e.activation(out=o, in_=i_, func=AF.Copy)),
                 reads=[C.ps_b[bk]], writes=[fTb])
            k2 = C.cnt % 2
            C.cnt += 1
            P.op("dve", (lambda e, o=C.sq[k2][:], i_=C.ps[bk][:], f_=fT[:, c, :]: e.tensor_tensor(out=o, in0=i_, in1=f_, op=ALU.mult)),
                 reads=[C.ps_b[bk], fTb], writes=[C.sq_b[k2]])
            mm(P, C.ps[SB][:], C.ones[:], C.sq[k2][:], c == 0, c == DC - 1, [C.ones_b, C.sq_b[k2]], [C.ps_b[SB]])
    P.op("act", (lambda e: e.activation(out=C.rstd[:], in_=C.ps[SB][:], func=AF.Sqrt, bias=C.epsc[:], scale=1.0 / D)),
         reads=[C.ps_b[SB], C.epsc_b], writes=[C.rstd_b])
    P.op("dve", (lambda e: e.reciprocal(out=C.rstd[:], in_=C.rstd[:])), reads=[C.rstd_b], writes=[C.rstd_b])
    xr = C.R2[:, :].rearrange("p (c t) -> p c t", t=NT)
    for h in range(2):
        xs = xd[h * 1024:(h + 1) * 1024, t0:t0 + NT].rearrange("(c p) t -> p c t", p=128)
        P.dma("sp", (lambda e, s_=xs: e.dma_start(out=xr, in_=s_)), reads=[xdb], writes=[r2b], lane="ld1")
        for cl in range(8):
            c = h * 8 + cl
            P.op("dve", (lambda e, c=c: e.scalar_tensor_tensor(out=fT[:, c, :], in0=fT[:, c, :], scalar=gpost[:, c:c + 1],
                                                              in1=C.rstd[:], op0=ALU.mult, op1=ALU.mult)),
                 reads=[fTb, C.rstd_b, cb], writes=[fTb])
            P.op("dve", (lambda e, c=c, cl=cl: e.scalar_tensor_tensor(out=fT[:, c, :], in0=fT[:, c, :], scalar=alpha,
                                                                       in1=xr[:, cl, :], op0=ALU.mult, op1=ALU.add)),
                 reads=[fTb, r2b], writes=[fTb])
    xdst = xo[:, t0:t0 + NT].rearrange("(c p) t -> p c t", p=128)
    P.dma("sp", lambda e: e.dma_start(out=xdst, in_=fT), reads=[fTb], writes=[xob], lane="st0")
    return wi


AW = 1024
NEG = -1.0e30
GELU_C = 0.7978845608028654 * 2.0
LN_EPS = 1e-5
O_ZU, O_ZV, O_Q, O_K, O_V, O_QI, O_KI, O_WI = 0, 1024, 2048, 3072, 4096, 5120, 6144, 6208


def mm(P, o, l, r, st, sp, reads, writes):
    P.op("pe", (lambda e: e.matmul(o, l, r, start=st, stop=sp)), reads=reads, writes=writes)


def emit_gelu(C, out_ap, in_ap, inb, outb, shape):
    P = C.P
    t1 = C.gt1[:, 0:shape]
    t2 = C.gt2[:, 0:shape]
    P.op("act", (lambda e: e.activation(out=t1, in_=in_ap, func=AF.Square)), reads=[inb], writes=[C.gt1_b])
    P.op("dve", (lambda e: e.tensor_scalar(out=t1, in0=t1, scalar1=0.044715, scalar2=1.0, op0=ALU.mult, op1=ALU.add)),
         reads=[C.gt1_b], writes=[C.gt1_b])
    P.op("dve", (lambda e: e.tensor_tensor(out=t1, in0=t1, in1=in_ap, op=ALU.mult)), reads=[C.gt1_b, inb], writes=[C.gt1_b])
    P.op("act", (lambda e: e.activation(out=t2, in_=t1, func=AF.Sigmoid, scale=GELU_C)), reads=[C.gt1_b], writes=[C.gt2_b])
    P.op("dve", (lambda e: e.tensor_tensor(out=out_ap, in0=t2, in1=in_ap, op=ALU.mult)), reads=[C.gt2_b, inb], writes=[outb])


def mixa_plan(w_in, w_gate, w_ba):
    specs = []
    for seg in (O_ZU, O_Q, O_K, O_QI):
        for c2 in range(4):
            specs.append(w_panel(w_in, 0, DC, [(seg + c2 * 256, 256)]))
    specs.append(w_panel(w_in, 0, DC, [(O_KI, 64), (O_KI, 64)]))
    for seg in (O_ZV, O_V):
        for c2 in range(4):
            specs.append(w_panel(w_in, 0, DC, [(seg + c2 * 256, 256)]))
    specs.append(w_panel(w_in, 0, DC, [(O_WI, 16)]))
    for c2 in range(8):
        specs.append(w_panel(w_ba, 0, 8, [(c2 * 256, 256)]))
        specs.append(w_panel(w_gate, 0, DC, [(c2 * 256, 256)]))
    for c2 in range(8):
        specs.append(w_panel(w_gate, 0, DC, [(D + c2 * 256, 256)]))
    return specs


class MixRes:
    pass


def emit_mix_consts(C, bro_d, wsT_d, M):
    P = C.P
    C.bro_b = Buf("bro"); C.wsf_b = Buf("wsf")
    C.claim("R4", [C.bro_b, C.wsf_b])
    P.dma("sp", lambda e: e.dma_start(out=C.bro, in_=bro_d), reads=[], writes=[C.bro_b], lane="ldc")
    wv = C.wsf.rearrange("p (g t) -> p g t", g=8)
    P.dma("sp", lambda e: e.dma_start(out=wv, in_=wsT_d), reads=[], writes=[C.wsf_b], lane="ldc")
    P.op("pool", (lambda e: e.affine_select(out=wv, in_=wv, pattern=[[0, 8], [1, 128]], compare_op=ALU.is_ge,
                                             fill=fillreg(e, 0.0), base=0, channel_multiplier=-1)),
         reads=[C.wsf_b], writes=[C.wsf_b])
    P.op("pool", (lambda e: e.tensor_copy(out=C.wsb[:], in_=wv)), reads=[C.wsf_b], writes=[C.wsb_b])


def emit_mixa(C, wbase, x1, t0, S, cs, cb, dr):
    P = C.P
    xd, xdb = x1
    xT = C.R1[:, :].rearrange("p (c t) -> p c t", t=NT)
    xTb = Buf("xT")
    hT = C.R2[:, :].bitcast(BF16).rearrange("p (c t) -> p c t", t=NT)
    hTb = Buf("hT")
    r3 = C.R3[:, :]
    zv = r3[:, 0:4096].rearrange("p (b c) -> p b c", c=1024)
    zvb = Buf("zv")
    vln = r3[:, 4096:6144].bitcast(BF16).rearrange("p (b c) -> p b c", c=1024)
    vlnb = Buf("vln")
    guT = r3[:, 6144:8192].bitcast(BF16).rearrange("p (c t) -> p c t", t=NT)
    guTb = Buf("guT")
    yaT = r3[:, 8192:10240].bitcast(BF16).rearrange("p (c t) -> p c t", t=NT)
    yaTb = Buf("yaT")
    gpre = cs[:, 32:48]
    C.claim("R1", [xTb]); C.claim("R2", [hTb]); C.claim("R3", [zvb, vlnb, guTb, yaTb])
    xsrc = xd[:, t0:t0 + NT].rearrange("(c p) t -> p c t", p=128)
    P.dma("sp", lambda e: e.dma_start(out=xT, in_=xsrc), reads=[xdb], writes=[xTb], lane="ld0")
    emit_rstd(C, lambda c: (xT[:, c, :], xTb), DC, D, 0)
    for c in range(DC):
        P.op("dve", (lambda e, c=c: e.scalar_tensor_tensor(out=hT[:, c, :], in0=xT[:, c, :], scalar=gpre[:, c:c + 1],
                                                          in1=C.rstd[:], op0=ALU.mult, op1=ALU.mult)),
             reads=[xTb, C.rstd_b, cb], writes=[hTb])
    osb = [C.R1[:, i * 2048:(i + 1) * 2048].bitcast(BF16).rearrange("p (c t) -> p c t", t=NT) for i in range(2)]
    osbb = [Buf("osb0"), Buf("osb1")]
    och = [C.R1[:, 4096 + i * 512: 4096 + (i + 1) * 512] for i in range(8)]
    ochb = [Buf(f"och{i}") for i in range(8)]
    C.claim("R1", osbb + ochb)
    wi = wbase
    bankrr = [0]

    def nb():
        b = bankrr[0] % 8
        bankrr[0] += 1
        return b

    first = [True]

    def r1dep():
        return [xTb]

    dests = {O_Q: ("qT", 0), O_K: ("kT", 1), O_QI: ("qiT", 0)}
    for seg in (O_ZU, O_Q, O_K, O_QI):
        if seg != O_ZU:
            dn, oi = dests[seg]
            ob, obb = osb[oi], osbb[oi]
        for c2 in range(4):
            w, wb = C.wget(wi); wi += 1
            for cc in range(2):
                ch = 2 * c2 + cc
                bk = nb()
                for k in range(DC):
                    mm(P, C.ps[bk][:], w[:, k, cc * 128:(cc + 1) * 128], hT[:, k, :], k == 0, k == DC - 1, [wb, hTb], [C.ps_b[bk]])
                if seg == O_ZU:
                    emit_gelu(C, guT[:, ch, :], C.ps[bk][:], C.ps_b[bk], guTb, 512)
                else:
                    P.op("act", (lambda e, o=ob[:, ch, :], i_=C.ps[bk][:]: e.activation(out=o, in_=i_, func=AF.Copy)),
                         reads=[C.ps_b[bk]], writes=[obb])
        if seg != O_ZU:
            dd, ddb = dr[dn]
            dst = dd[:, t0:t0 + NT].rearrange("(c p) t -> p c t", p=128)
            P.dma("sp", (lambda e, d_=dst, s_=ob: e.dma_start(out=d_, in_=s_)), reads=[obb], writes=[ddb], lane="st1")
    w, wb = C.wget(wi); wi += 1
    bk = nb()
    for k in range(DC):
        mm(P, C.ps[bk][:], w[:, k, 0:128], hT[:, k, :], k == 0, k == DC - 1, [wb, hTb], [C.ps_b[bk]])
    P.op("act", (lambda e, o=C.kio[:], i_=C.ps[bk][:]: e.activation(out=o, in_=i_, func=AF.Copy)), reads=[C.ps_b[bk]], writes=[C.kio_b])
    dd, ddb = dr["kiT"]
    P.dma("sp", (lambda e, d_=dd[:, t0:t0 + NT]: e.dma_start(out=d_, in_=C.kio[:])), reads=[C.kio_b], writes=[ddb], lane="st2")
    vsb = osb[0].rearrange("p c t -> p (c t)").rearrange("p (b c) -> p b c", c=1024)
    for seg in (O_ZV, O_V):
        for c2 in range(4):
            w, wb = C.wget(wi); wi += 1
            for tb in range(4):
                bk = nb()
                for k in range(DC):
                    mm(P, C.ps[bk][:, 0:256], hT[:, k, tb * 128:(tb + 1) * 128], w[:, k, :], k == 0, k == DC - 1, [wb, hTb], [C.ps_b[bk]])
                if seg == O_ZV:
                    P.op("act", (lambda e, o=zv[:, tb, c2 * 256:(c2 + 1) * 256], i_=C.ps[bk][:, 0:256]: e.activation(out=o, in_=i_, func=AF.Copy)),
                         reads=[C.ps_b[bk]], writes=[zvb])
                else:
                    P.op("act", (lambda e, o=vsb[:, tb, c2 * 256:(c2 + 1) * 256], i_=C.ps[bk][:, 0:256]: e.activation(out=o, in_=i_, func=AF.Copy)),
                         reads=[C.ps_b[bk]], writes=[osbb[0]])
    dd, ddb = dr["v"]
    P.dma("sp", (lambda e, d_=dd[t0:t0 + NT, :].rearrange("(b p) c -> p b c", p=128): e.dma_start(out=d_, in_=vsb)),
          reads=[osbb[0]], writes=[ddb], lane="st1")
    w, wb = C.wget(wi); wi += 1
    bk = nb()
    for tb in range(4):
        for k in range(DC):
            mm(P, C.ps[bk][:, tb * 16:(tb + 1) * 16], hT[:, k, tb * 128:(tb + 1) * 128], w[:, k, :], k == 0, k == DC - 1, [wb, hTb], [C.ps_b[bk]])
    P.op("act", (lambda e, i_=C.ps[bk][:, 0:64]: e.activation(out=C.wio[:], in_=i_, func=AF.Copy)), reads=[C.ps_b[bk]], writes=[C.wio_b])
    dd, ddb = dr["widx"]
    P.dma("sp", (lambda e, d_=dd[t0:t0 + NT, :].rearrange("(b p) c -> p b c", p=128): e.dma_start(out=d_, in_=C.wio[:].rearrange("p (b c) -> p b c", c=16))),
          reads=[C.wio_b], writes=[ddb], lane="st2")
    lng = C.bro[:, 0:1024]
    lnb = C.bro[:, 1024:2048]
    for tb in range(4):
        for h2 in range(2):
            emit_gelu(C, zv[:, tb, h2 * 512:(h2 + 1) * 512], zv[:, tb, h2 * 512:(h2 + 1) * 512], zvb, zvb, 512)
        P.op("dve", (lambda e, i_=zv[:, tb, :]: e.bn_stats(out=C.bst[:, 0:6], in_=i_[:, 0:512])), reads=[zvb], writes=[C.bst_b])
        P.op("dve", (lambda e, i_=zv[:, tb, :]: e.bn_stats(out=C.bst[:, 6:12], in_=i_[:, 512:1024])), reads=[zvb], writes=[C.bst_b])
        P.op("dve", (lambda e: e.bn_aggr(out=C.bag[:], in_=C.bst[:].rearrange("p (a b) -> p a b", b=6))), reads=[C.bst_b], writes=[C.bag_b])
        P.op("act", (lambda e: e.activation(out=C.lrs[:], in_=C.bag[:, 1:2], func=AF.Sqrt, bias=C.lneps[:], scale=1.0)),
             reads=[C.bag_b, C.lneps_b], writes=[C.lrs_b])
        P.op("dve", (lambda e: e.reciprocal(out=C.lrs[:], in_=C.lrs[:])), reads=[C.lrs_b], writes=[C.lrs_b])
        P.op("dve", (lambda e, o=zv[:, tb, :]: e.tensor_scalar(out=o, in0=o, scalar1=C.bag[:, 0:1], scalar2=C.lrs[:], op0=ALU.subtract, op1=ALU.mult)),
             reads=[zvb, C.bag_b, C.lrs_b], writes=[zvb])
        P.op("dve", (lambda e, o=zv[:, tb, :]: e.tensor_tensor(out=o, in0=o, in1=lng, op=ALU.mult)), reads=[zvb, C.bro_b], writes=[zvb])
        P.op("dve", (lambda e, o=vln[:, tb, :], i_=zv[:, tb, :]: e.tensor_tensor(out=o, in0=i_, in1=lnb, op=ALU.add)), reads=[zvb, C.bro_b], writes=[vlnb])
    for g in range(8):
        bk = nb()
        for tb in range(4):
            mm(P, C.ps[bk][:, tb * 128:(tb + 1) * 128], vln[:, tb, g * 128:(g + 1) * 128], C.wsb[:, g, :], True, True, [vlnb, C.wsb_b], [C.ps_b[bk]])
        for tb in range(4):
            P.op("dve", (lambda e, o=C.gt1[:, tb * 128:(tb + 1) * 128], i_=C.ps[bk][:, tb * 128:(tb + 1) * 128], b_=C.bro[:, 2048 + g * 128:2048 + (g + 1) * 128]:
                         e.tensor_tensor(out=o, in0=i_, in1=b_, op=ALU.add)),
                 reads=[C.ps_b[bk], C.bro_b], writes=[C.gt1_b])
        P.op("dve", (lambda e, o=yaT[:, g, :], g_=guT[:, g, :]: e.tensor_tensor(out=o, in0=C.gt1[:], in1=g_, op=ALU.mult)),
             reads=[C.gt1_b, guTb], writes=[yaTb])
    if "dbg_ya" in dr:
        P.dma("sp", (lambda e, d_=dr["dbg_ya"][0][:, t0:t0 + NT].rearrange("(c p) t -> p c t", p=128): e.dma_start(out=d_, in_=yaT)), reads=[yaTb], writes=[dr["dbg_ya"][1]], lane="dbg")
        P.dma("sp", (lambda e, d_=dr["dbg_gu"][0][:, t0:t0 + NT].rearrange("(c p) t -> p c t", p=128): e.dma_start(out=d_, in_=guT)), reads=[guTb], writes=[dr["dbg_gu"][1]], lane="dbg")
        P.dma("sp", (lambda e, d_=dr["dbg_vln"][0][t0:t0 + NT, :].rearrange("(b p) c -> p b c", p=128): e.dma_start(out=d_, in_=vln)), reads=[vlnb], writes=[dr["dbg_vln"][1]], lane="dbg")
    ocnt = [0]
    for c2 in range(8):
        wa, wab = C.wget(wi); wi += 1
        wg, wgb = C.wget(wi); wi += 1
        for cc in range(2):
            ch = 2 * c2 + cc
            bka = nb()
            for k in range(8):
                mm(P, C.ps[bka][:], wa[:, k, cc * 128:(cc + 1) * 128], yaT[:, k, :], k == 0, k == 7, [wab, yaTb], [C.ps_b[bka]])
            bkg = nb()
            for k in range(DC):
                mm(P, C.ps[bkg][:], wg[:, k, cc * 128:(cc + 1) * 128], hT[:, k, :], k == 0, k == DC - 1, [wgb, hTb], [C.ps_b[bkg]])
            k2 = C.cnt % 2
            C.cnt += 1
            P.op("act", (lambda e, o=C.tmpa[k2][:], i_=C.ps[bkg][:]: e.activation(out=o, in_=i_, func=AF.Sigmoid)),
                 reads=[C.ps_b[bkg]], writes=[C.tmpa_b[k2]])
            oi = ocnt[0] % 8
            ocnt[0] += 1
            P.op("dve", (lambda e, o=och[oi], a=C.tmpa[k2][:], b=C.ps[bka][:]: e.tensor_tensor(out=o, in0=a, in1=b, op=ALU.mult)),
                 reads=[C.tmpa_b[k2], C.ps_b[bka]], writes=[ochb[oi]])
            dd, ddb = dr["apart"]
            P.dma("sp", (lambda e, d_=dd[ch * 128:(ch + 1) * 128, t0:t0 + NT], s_=och[oi]: e.dma_start(out=d_, in_=s_)),
                  reads=[ochb[oi]], writes=[ddb], lane=f"so{oi}")
    for c2 in range(8):
        wg, wgb = C.wget(wi); wi += 1
        for cc in range(2):
            ch = 2 * c2 + cc
            bkg = nb()
            for k in range(DC):
                mm(P, C.ps[bkg][:], wg[:, k, cc * 128:(cc + 1) * 128], hT[:, k, :], k == 0, k == DC - 1, [wgb, hTb], [C.ps_b[bkg]])
            oi = ocnt[0] % 8
            ocnt[0] += 1
            P.op("act", (lambda e, o=och[oi], i_=C.ps[bkg][:]: e.activation(out=o, in_=i_, func=AF.Sigmoid)),
                 reads=[C.ps_b[bkg]], writes=[ochb[oi]])
            dd, ddb = dr["gb"]
            P.dma("sp", (lambda e, d_=dd[ch * 128:(ch + 1) * 128, t0:t0 + NT], s_=och[oi]: e.dma_start(out=d_, in_=s_)),
                  reads=[ochb[oi]], writes=[ddb], lane=f"so{oi}")
    return wi


ATT_SCALE = 128 ** -0.5
MASK_NEG = -30000.0
TOPK = 256
BIS_IT = 28
BIS_MIN = 1024


def emit_attn_consts(C, toep_d, cb8_d):
    P = C.P
    tpf = C.R3[:, 0:2048].rearrange("p (h w) -> p h w", h=8)
    tb = Buf("tpf")
    C.claim("R3", [tb])
    P.dma("sp", lambda e: e.dma_start(out=C.cb8[:], in_=cb8_d), reads=[], writes=[C.cb8_b], lane="ldc")
    P.dma("sp", lambda e: e.dma_start(out=tpf, in_=toep_d.rearrange("p (h w) -> p h w", h=8)), reads=[], writes=[tb], lane="ldc")
    for h in range(8):
        P.op("dve", (lambda e, h=h: e.tensor_scalar(out=tpf[:, h, :], in0=tpf[:, h, :], scalar1=C.cb8[:, h:h + 1], scalar2=None, op0=ALU.subtract)),
             reads=[tb, C.cb8_b], writes=[tb])
    if not ATTN_PE:
        P.op("act", (lambda e: e.activation(out=C.Fe[:].rearrange("p (h w) -> p h w", h=8), in_=tpf, func=AF.Exp)), reads=[tb], writes=[C.Fe_b])
    P.op("dve", (lambda e: e.tensor_scalar(out=C.Fb[:].rearrange("p (h w) -> p h w", h=8), in0=tpf, scalar1=1.0 / ATT_SCALE, scalar2=None, op0=ALU.mult)),
         reads=[tb], writes=[C.Fb_b])
    P.op("pool", lambda e: e.memset(C.ident[:], 1.0), writes=[C.ident_b])
    P.op("pool", (lambda e: e.affine_select(out=C.ident[:], in_=C.ident[:], pattern=[[-1, 128]], compare_op=ALU.is_equal,
                                             fill=fillreg(e, 0.0), base=0, channel_multiplier=1)), reads=[C.ident_b], writes=[C.ident_b])
    P.op("pool", (lambda e: e.tensor_scalar(out=C.negI[:], in0=C.ident[:], scalar1=MASK_NEG, scalar2=1.0, op0=ALU.mult, op1=ALU.mult)),
         reads=[C.ident_b], writes=[C.negI_b])
    P.op("pool", lambda e: e.memset(C.thrneg[:], -1.0e29), writes=[C.thrneg_b])
    P.op("pool", lambda e: e.memset(C.onec[:], 1.0), writes=[C.onec_b])


def mixb_tail_plan(w_bb, w_o):
    specs = []
    for c2 in range(8):
        specs.append(w_panel(w_bb, 0, 8, [(c2 * 256, 256)]))
    specs += outproj_plan(w_o, DC)
    return specs


def emit_mixb_tile(C, wbase, tt, S, x1, x2, cs, cb, dr):
    P = C.P
    t0 = tt * NT
    kh = [C.R1[:, i * 2048:(i + 1) * 2048].bitcast(BF16) for i in range(2)]
    khb = [Buf("kh0"), Buf("kh1")]
    ybT = C.R1[:, 4096:6144].bitcast(BF16).rearrange("p (c t) -> p c t", t=NT)
    ybTb = Buf("ybT")
    maskT = C.R1[:, 6144:8192].bitcast(BF16)
    maskTb = Buf("maskT")
    vh = [C.R2[:, i * 2048:(i + 1) * 2048].bitcast(BF16).rearrange("p (b c) -> p b c", c=128) for i in range(2)]
    vhb = [Buf("vh0"), Buf("vh1")]
    score = C.R3[:, 0:4096]
    scb = Buf("score")
    work = C.R3[:, 4096:8192]
    wkb = Buf("work")
    mask = C.R3[:, 8192:10240].bitcast(BF16)
    mkb = Buf("mask")
    C.claim("R1", khb + [ybTb, maskTb]); C.claim("R2", vhb); C.claim("R3", [scb, wkb, mkb])
    rr = [0]

    def nb():
        b = rr[0] % 6
        rr[0] += 1
        return b
    OB, TB = 6, 7
    psT = C.ps[TB][:].bitcast(BF16)
    kvc = [0]
    for qi_ in range(4):
        qb = tt * 4 + qi_
        q0 = qb * 128
        nkb = qb + 1
        Sc = nkb * 128
        ngrp = (nkb + 3) // 4
        P.dma("sp", (lambda e, s_=dr["qiT"][0][:, q0:q0 + 128].rearrange("(c p) t -> p c t", p=128): e.dma_start(out=C.qib[:], in_=s_)),
              reads=[dr["qiT"][1]], writes=[C.qib_b], lane="lq0")
        P.dma("sp", (lambda e, s_=dr["qT"][0][:, q0:q0 + 128].rearrange("(c p) t -> p c t", p=128): e.dma_start(out=C.qbk[:], in_=s_)),
              reads=[dr["qT"][1]], writes=[C.qbk_b], lane="lq1")
        P.dma("sp", (lambda e, s_=dr["widx"][0][q0:q0 + 128, :]: e.dma_start(out=C.wx[:], in_=s_)),
              reads=[dr["widx"][1]], writes=[C.wx_b], lane="lq2")
        P.op("act", (lambda e: e.activation(out=C.wabs[:], in_=C.wx[:], func=AF.Abs)), reads=[C.wx_b], writes=[C.wabs_b])
        P.op("act", (lambda e: e.activation(out=C.wsg[:], in_=C.wx[:], func=AF.Sign)), reads=[C.wx_b], writes=[C.wsg_b])
        for grp in range(ngrp):
            c0 = grp * 512
            n = min(Sc, c0 + 512) - c0
            for h in range(16):
                c, half = h // 2, h % 2
                rows = slice(half * 64, half * 64 + 64)
                bk = nb()
                mm(P, C.ps[bk][:, 0:n], C.qib[rows, c, :], C.kis[rows, c0:c0 + n], True, True, [C.qib_b, C.kis_b], [C.ps_b[bk]])
                k2 = C.cnt % 2
                C.cnt += 1
                P.op("act", (lambda e, o=C.tmpa[k2][:, 0:n], i_=C.ps[bk][:, 0:n], h=h: e.activation(out=o, in_=i_, func=AF.Relu, scale=C.wabs[:, h:h + 1])),
                     reads=[C.ps_b[bk], C.wabs_b], writes=[C.tmpa_b[k2]])
                if h == 0:
                    P.op("dve", (lambda e, o=score[:, c0:c0 + n], i_=C.tmpa[k2][:, 0:n]: e.tensor_scalar(out=o, in0=i_, scalar1=C.wsg[:, 0:1], scalar2=None, op0=ALU.mult)),
                         reads=[C.tmpa_b[k2], C.wsg_b], writes=[scb])
                else:
                    P.op("dve", (lambda e, o=score[:, c0:c0 + n], i_=C.tmpa[k2][:, 0:n], h=h: e.scalar_tensor_tensor(out=o, in0=i_, scalar=C.wsg[:, h:h + 1], in1=o, op0=ALU.mult, op1=ALU.add)),
                         reads=[C.tmpa_b[k2], C.wsg_b, scb], writes=[scb])
        dsl = score[:, (nkb - 1) * 128:nkb * 128]
        P.op("pool", (lambda e, d_=dsl: e.affine_select(out=d_, in_=d_, pattern=[[-1, 128]], compare_op=ALU.is_ge, fill=fillreg(e, NEG), base=0, channel_multiplier=1)),
             reads=[scb], writes=[scb])
        if Sc > TOPK:
            for r in range(TOPK // 8):
                src = score[:, 0:Sc] if r == 0 else work[:, 0:Sc]
                srcb = scb if r == 0 else wkb
                P.op("dve", (lambda e, s_=src: e.max(out=C.m8[:], in_=s_)), reads=[srcb], writes=[C.m8_b])
                if r < TOPK // 8 - 1:
                    P.op("dve", (lambda e, s_=src, w_=work[:, 0:Sc]: e.match_replace(out=w_, in_to_replace=C.m8[:], in_values=s_, imm_value=NEG)),
                         reads=[srcb, C.m8_b], writes=[wkb])
            thr, thrb = C.m8[:, 7:8], C.m8_b
        else:
            thr, thrb = C.thrneg[:], C.thrneg_b
        P.op("dve", (lambda e, t_=thr, m_=mask[:, 0:Sc], s_=score[:, 0:Sc]: e.tensor_scalar(out=m_, in0=s_, scalar1=t_, scalar2=None, op0=ALU.is_ge)),
             reads=[scb, thrb], writes=[mkb])
        for grp in range(ngrp):
            kbs = list(range(grp * 4, min(nkb, grp * 4 + 4)))
            for j, kb in enumerate(kbs):
                P.op("pe", (lambda e, o=psT[:, j * 128:(j + 1) * 128], i_=mask[:, kb * 128:(kb + 1) * 128]: e.transpose(o, i_, C.ident[:])),
                     reads=[mkb, C.ident_b], writes=[C.ps_b[TB]])
            n = len(kbs) * 128
            P.op("act", (lambda e, o=maskT[:, grp * 512:grp * 512 + n], i_=psT[:, 0:n]: e.activation(out=o, in_=i_, func=AF.Copy)),
                 reads=[C.ps_b[TB]], writes=[maskTb])
        for h in range(8):
            s2 = kvc[0] % 2
            kvc[0] += 1
            P.dma("sp", (lambda e, o=kh[s2][:, 0:Sc], s_=dr["kT"][0][h * 128:(h + 1) * 128, 0:Sc]: e.dma_start(out=o, in_=s_)),
                  reads=[dr["kT"][1]], writes=[khb[s2]], lane=f"lk{s2}")
            P.dma("sp", (lambda e, o=vh[s2][:, 0:nkb, :], s_=dr["v"][0][0:Sc, h * 128:(h + 1) * 128].rearrange("(b p) c -> p b c", p=128): e.dma_start(out=o, in_=s_)),
                  reads=[dr["v"][1]], writes=[vhb[s2]], lane=f"lv{s2}")
            for grp in range(ngrp):
                kbs = list(range(grp * 4, min(nkb, grp * 4 + 4)))
                n = len(kbs) * 128
                bk = nb()
                for j, kb in enumerate(kbs):
                    mm(P, C.ps[bk][:, j * 128:(j + 1) * 128], kh[s2][:, kb * 128:(kb + 1) * 128], C.qbk[:, h, :], True, True, [khb[s2], C.qbk_b], [C.ps_b[bk]])
                k2 = C.cnt % 2
                C.cnt += 1
                P.op("act", (lambda e, o=C.tmpa[k2][:, 0:n], i_=C.ps[bk][:, 0:n], h=h: e.activation(out=o, in_=i_, func=AF.Exp, bias=C.cb8[:, h:h + 1], scale=ATT_SCALE)),
                     reads=[C.ps_b[bk], C.cb8_b], writes=[C.tmpa_b[k2]])
                P.op("dve", (lambda e, o=C.pm[k2][:, 0:n], a=C.tmpa[k2][:, 0:n], b=maskT[:, grp * 512:grp * 512 + n]: e.tensor_tensor(out=o, in0=a, in1=b, op=ALU.mult)),
                     reads=[C.tmpa_b[k2], maskTb], writes=[C.pm_b[k2]])
                for j, kb in enumerate(kbs):
                    w_ = nkb - 1 - kb
                    if w_ <= 1:
                        P.op("dve", (lambda e, o=C.pm[k2][:, j * 128:(j + 1) * 128], f_=C.Fb[:, (h * 2 + w_) * 128:(h * 2 + w_ + 1) * 128]: e.tensor_tensor(out=o, in0=o, in1=f_, op=ALU.mult)),
                             reads=[C.pm_b[k2], C.Fb_b], writes=[C.pm_b[k2]])
                for j, kb in enumerate(kbs):
                    P.op("pe", (lambda e, o=C.ps[OB][:, 0:128], l=C.pm[k2][:, j * 128:(j + 1) * 128], r=vh[s2][:, kb, :], st=(kb == 0), sp=(kb == nkb - 1):
                                e.matmul(o, l, r, start=st, stop=sp, skip_group_check=True)),
                         reads=[C.pm_b[k2], vhb[s2]], writes=[C.ps_b[OB]])
                    P.op("pe", (lambda e, o=C.ps[OB][:, 128:129], l=C.pm[k2][:, j * 128:(j + 1) * 128], sp=(kb == nkb - 1):
                                e.matmul(o, l, C.onec[:, 0:1], start=False, stop=sp, skip_group_check=True)),
                         reads=[C.pm_b[k2], C.onec_b], writes=[C.ps_b[OB]])
            P.op("dve", (lambda e: e.reciprocal(out=C.rc[:], in_=C.ps[OB][:, 128:129])), reads=[C.ps_b[OB]], writes=[C.rc_b])
            P.op("dve", (lambda e, o=C.yb[:, h * 128:(h + 1) * 128]: e.tensor_scalar(out=o, in0=C.ps[OB][:, 0:128], scalar1=C.rc[:], scalar2=None, op0=ALU.mult)),
                 reads=[C.ps_b[OB], C.rc_b], writes=[C.yb_b])
        for half in range(2):
            for j in range(4):
                c = half * 4 + j
                P.op("pe", (lambda e, o=psT[:, j * 128:(j + 1) * 128], i_=C.yb[:, c * 128:(c + 1) * 128]: e.transpose(o, i_, C.ident[:])),
                     reads=[C.yb_b, C.ident_b], writes=[C.ps_b[TB]])
            P.op("act", (lambda e, o=ybT[:, half * 4:half * 4 + 4, qi_ * 128:(qi_ + 1) * 128], i_=psT[:, 0:512].rearrange("p (c t) -> p c t", t=128):
                         e.activation(out=o, in_=i_, func=AF.Copy)),
                 reads=[C.ps_b[TB]], writes=[ybTb])
    mT = C.R2[:, :].bitcast(BF16).rearrange("p (c t) -> p c t", t=NT)
    mTb = Buf("mT")
    C.claim("R2", [mTb])
    wi = wbase
    lc = [0]
    for c2 in range(8):
        w, wb = C.wget(wi); wi += 1
        for cc in range(2):
            ch = 2 * c2 + cc
            bk = nb()
            for k in range(8):
                mm(P, C.ps[bk][:], w[:, k, cc * 128:(cc + 1) * 128], ybT[:, k, :], k == 0, k == 7, [wb, ybTb], [C.ps_b[bk]])
            k2 = lc[0] % 2
            lc[0] += 1
            P.dma("sp", (lambda e, o=C.gt1[:] if k2 == 0 else C.gt2[:], s_=dr["gb"][0][ch * 128:(ch + 1) * 128, t0:t0 + NT]: e.dma_start(out=o, in_=s_)),
                  reads=[dr["gb"][1]], writes=[C.gt1_b if k2 == 0 else C.gt2_b], lane=f"lg{k2}")
            P.dma("sp", (lambda e, o=C.tmpa[k2][:], s_=dr["apart"][0][ch * 128:(ch + 1) * 128, t0:t0 + NT]: e.dma_start(out=o, in_=s_)),
                  reads=[dr["apart"][1]], writes=[C.tmpa_b[k2]], lane=f"la{k2}")
            gbt, gbb = (C.gt1, C.gt1_b) if k2 == 0 else (C.gt2, C.gt2_b)
            P.op("dve", (lambda e, g_=gbt[:], i_=C.ps[bk][:]: e.tensor_tensor(out=g_, in0=g_, in1=i_, op=ALU.mult)),
                 reads=[gbb, C.ps_b[bk]], writes=[gbb])
            P.op("dve", (lambda e, o=mT[:, ch, :], g_=gbt[:], a=C.tmpa[k2][:]: e.tensor_tensor(out=o, in0=g_, in1=a, op=ALU.add)),
                 reads=[gbb, C.tmpa_b[k2]], writes=[mTb])
    fT = C.R1[:, :].rearrange("p (c t) -> p c t", t=NT)
    fTb = Buf("fT")
    C.claim("R1", [fTb])
    r2b = Buf("r2x")
    wi = emit_outproj_claim(C, wi, mT, mTb, x1, x2, t0, cs[:, 48:64], 1.0, cb, fT, fTb, r2b)
    return wi


def emit_outproj_claim(C, wi, mT, mTb, x1, x2, t0, gpost, alpha, cb, fT, fTb, r2b):
    class _Lazy:
        pass
    P = C.P
    orig_dma = P.dma
    state = {"claimed": False}

    def dma_hook(eng, fn, reads, writes, lane):
        if (not state["claimed"]) and r2b in writes:
            C.claim("R2", [r2b])
            state["claimed"] = True
        return orig_dma(eng, fn, reads, writes, lane)
    P.dma = dma_hook
    try:
        wi = emit_outproj(C, wi, mT, lambda f: mTb, DC, x1, x2, t0, gpost, alpha, cb, fT, fTb, r2b)
    finally:
        P.dma = orig_dma
    return wi


def _interleave(ga, gb_):
    la, lb = list(ga), None
    return la


def run_interleaved(gens_a, gens_b):
    na, nb_ = len(gens_a), len(gens_b)
    ia = ib = 0
    while ia < na or ib < nb_:
        fa = ia / na if na else 1.0
        fb = ib / nb_ if nb_ else 1.0
        if ia < na and (fa <= fb or ib >= nb_):
            gens_a[ia](); ia += 1
        else:
            gens_b[ib](); ib += 1


def emit_mixb_layer(C, wbase, NTL, S, x1, x2, cs, cb, dr):
    P = C.P
    NBLK = NTL * 4
    score = [C.R3[:, 0:4096], C.R4[:, 0:4096]]
    scb = [Buf("score0"), Buf("score1")]
    work = C.R3[:, 4096:8192]
    wkb = Buf("work")
    mask = C.R3[:, 8192:10240].bitcast(BF16)
    mkb = Buf("mask")
    C.claim("R3", [scb[0], wkb, mkb])
    C.claim("R4", [scb[1]])
    maskT = [C.mT0[:], C.mT1[:]]
    maskTb = [C.mT0_b, C.mT1_b]
    qib = [C.qib, C.qib2]; qibb = [C.qib_b, C.qib2_b]
    qbk = [C.qbk, C.qbk2]; qbkb = [C.qbk_b, C.qbk2_b]
    wabs = [C.wabs, C.wabs2]; wabsb = [C.wabs_b, C.wabs2_b]
    wsg = [C.wsg, C.wsg2]; wsgb = [C.wsg_b, C.wsg2_b]
    wx = [C.wx, C.wx2]; wxb = [C.wx_b, C.wx2_b]
    rr = [0]

    def nb():
        b = rr[0] % 4
        rr[0] += 1
        return b
    OBS, TB, SCB = (4, 6), 7, 5
    rcs = [C.rc, C.rc2]; rcsb = [C.rc_b, C.rc2_b]
    pmc = [0]
    hdc = [0]
    dg = [C.dg0, C.dg1]; dgb = [C.dg0_b, C.dg1_b]
    rlc = [0]
    psT = C.ps[TB][:].bitcast(BF16)
    kvc = [0]
    st = {}

    def phase_ab(qb):
        th = []
        p = qb % 2
        q0 = qb * 128
        nkb = qb + 1
        Sc = nkb * 128
        ngrp = (nkb + 3) // 4
        sc, scbp = score[p], scb[p]

        def loads():
            P.dma("sp", (lambda e, s_=dr["qiT"][0][:, q0:q0 + 128].rearrange("(c p) t -> p c t", p=128), o=qib[p][:]: e.dma_start(out=o, in_=s_)),
                  reads=[dr["qiT"][1]], writes=[qibb[p]], lane=f"lq0{p}")
            P.dma("sp", (lambda e, s_=dr["qT"][0][:, q0:q0 + 128].rearrange("(c p) t -> p c t", p=128), o=qbk[p][:]: e.dma_start(out=o, in_=s_)),
                  reads=[dr["qT"][1]], writes=[qbkb[p]], lane=f"lq1{p}")
            P.dma("sp", (lambda e, s_=dr["widx"][0][q0:q0 + 128, :], o=wx[p][:]: e.dma_start(out=o, in_=s_)),
                  reads=[dr["widx"][1]], writes=[wxb[p]], lane=f"lq2{p}")
            P.op("act", (lambda e, o=wabs[p][:], i_=wx[p][:]: e.activation(out=o, in_=i_, func=AF.Abs)), reads=[wxb[p]], writes=[wabsb[p]])
            P.op("act", (lambda e, o=wsg[p][:], i_=wx[p][:]: e.activation(out=o, in_=i_, func=AF.Sign)), reads=[wxb[p]], writes=[wsgb[p]])
            for h in range(16):
                P.op("pool", (lambda e, o=dg[p][:, h, :], s_=wsg[p][:, h:h + 1]: e.tensor_scalar(out=o, in0=C.ident[:], scalar1=s_, scalar2=1.0, op0=ALU.mult, op1=ALU.mult)),
                     reads=[C.ident_b, wsgb[p]], writes=[dgb[p]])
        th.append(loads)
        for grp in range(ngrp):
            c0 = grp * 512
            n = min(Sc, c0 + 512) - c0
            for h in range(16):
                def idx(grp=grp, c0=c0, n=n, h=h):
                    c, half = h // 2, h % 2
                    rows = slice(half * 64, half * 64 + 64)
                    bk = nb()
                    mm(P, C.ps[bk][:, 0:n], qib[p][rows, c, :], C.kis[rows, c0:c0 + n], True, True, [qibb[p], C.kis_b], [C.ps_b[bk]])
                    k2 = rlc[0] % 4
                    rlc[0] += 1
                    P.op("act", (lambda e, o=C.rl16[k2][:, 0:n], i_=C.ps[bk][:, 0:n], s_=wabs[p][:, h:h + 1]: e.activation(out=o, in_=i_, func=AF.Relu, scale=s_)),
                         reads=[C.ps_b[bk], wabsb[p]], writes=[C.rl16_b[k2]])
                    mm(P, C.ps[SCB][:, 0:n], dg[p][:, h, :], C.rl16[k2][:, 0:n], h == 0, h == 15, [dgb[p], C.rl16_b[k2]], [C.ps_b[SCB]])
                    if h == 15:
                        P.op("act", (lambda e, o=sc[:, c0:c0 + n], i_=C.ps[SCB][:, 0:n]: e.activation(out=o, in_=i_, func=AF.Copy)),
                             reads=[C.ps_b[SCB]], writes=[scbp])
                th.append(idx)

        if Sc >= BIS_MIN:
            def binit():
                P.op("dve", (lambda e, s_=sc[:, 0:Sc]: e.max(out=C.m8[:], in_=s_)), reads=[scbp], writes=[C.m8_b])
                P.op("dve", (lambda e, s_=sc[:, 0:Sc]: e.tensor_reduce(out=C.blo[:], in_=s_, axis=mybir.AxisListType.X, op=ALU.min)), reads=[scbp], writes=[C.blo_b])
                P.op("dve", (lambda e: e.tensor_tensor(out=C.brng[:], in0=C.m8[:, 0:1], in1=C.blo[:], op=ALU.subtract)), reads=[C.m8_b, C.blo_b], writes=[C.brng_b])
                P.op("dve", (lambda e: e.tensor_scalar(out=C.bstep[:], in0=C.pw2[:], scalar1=C.brng[:], scalar2=None, op0=ALU.mult)), reads=[C.pw2_b, C.brng_b], writes=[C.bstep_b])
            th.append(binit)

        def causal():
            dsl = sc[:, (nkb - 1) * 128:nkb * 128]
            P.op("pool", (lambda e, d_=dsl: e.affine_select(out=d_, in_=d_, pattern=[[-1, 128]], compare_op=ALU.is_ge, fill=fillreg(e, NEG), base=0, channel_multiplier=1)),
                 reads=[scbp], writes=[scbp])
        th.append(causal)
        use_bis = Sc >= BIS_MIN
        if use_bis:
            def bis_init():
                pass
            for k in range(BIS_IT):
                def it(k=k):
                    P.op("dve", (lambda e: e.tensor_tensor(out=C.bmid[:], in0=C.blo[:], in1=C.bstep[:, k:k + 1], op=ALU.add)),
                         reads=[C.blo_b, C.bstep_b], writes=[C.bmid_b])
                    P.op("dve", (lambda e, w_=work[:, 0:Sc], s_=sc[:, 0:Sc]: e.tensor_scalar(out=w_, in0=s_, scalar1=C.bmid[:], scalar2=None, op0=ALU.is_ge, op1=ALU.add, accum_out=C.bcnt[:])),
                         reads=[scbp, C.bmid_b], writes=[wkb, C.bcnt_b])
                    P.op("dve", (lambda e: e.tensor_scalar(out=C.bfs[:], in0=C.bcnt[:], scalar1=float(TOPK) - 0.5, scalar2=C.bstep[:, k:k + 1], op0=ALU.is_ge, op1=ALU.mult)),
                         reads=[C.bcnt_b, C.bstep_b], writes=[C.bfs_b])
                    P.op("dve", (lambda e: e.tensor_tensor(out=C.blo[:], in0=C.blo[:], in1=C.bfs[:], op=ALU.add)),
                         reads=[C.blo_b, C.bfs_b], writes=[C.blo_b])
                th.append(it)
        elif Sc > TOPK:
            for r in range(TOPK // 8):
                def rnd(r=r):
                    src = sc[:, 0:Sc] if r == 0 else work[:, 0:Sc]
                    srcb = scbp if r == 0 else wkb
                    P.op("dve", (lambda e, s_=src: e.max(out=C.m8[:], in_=s_)), reads=[srcb], writes=[C.m8_b])
                    if r < TOPK // 8 - 1:
                        P.op("dve", (lambda e, s_=src, w_=work[:, 0:Sc]: e.match_replace(out=w_, in_to_replace=C.m8[:], in_values=s_, imm_value=NEG)),
                             reads=[srcb, C.m8_b], writes=[wkb])
                th.append(rnd)

        def mk():
            if Sc >= BIS_MIN:
                thr, thrb = C.blo[:], C.blo_b
            elif Sc > TOPK:
                thr, thrb = C.m8[:, 7:8], C.m8_b
            else:
                thr, thrb = C.thrneg[:], C.thrneg_b
            P.op("dve", (lambda e, t_=thr, m_=mask[:, 0:Sc], s_=sc[:, 0:Sc]: e.tensor_scalar(out=m_, in0=s_, scalar1=t_, scalar2=None, op0=(ALU.is_lt if ATTN_PE else ALU.is_ge))),
                 reads=[scbp, thrb], writes=[mkb])
        th.append(mk)
        for grp in range(ngrp):
            def tr(grp=grp):
                kbs = list(range(grp * 4, min(nkb, grp * 4 + 4)))
                for j, kb in enumerate(kbs):
                    P.op("pe", (lambda e, o=psT[:, j * 128:(j + 1) * 128], i_=mask[:, kb * 128:(kb + 1) * 128]: e.transpose(o, i_, C.ident[:])),
                         reads=[mkb, C.ident_b], writes=[C.ps_b[TB]])
                n = len(kbs) * 128
                P.op("act", (lambda e, o=maskT[p][:, grp * 512:grp * 512 + n], i_=psT[:, 0:n]: e.activation(out=o, in_=i_, func=AF.Copy)),
                     reads=[C.ps_b[TB]], writes=[maskTb[p]])
            th.append(tr)
        return th

    def phase_c(qb):
        th = []
        p = qb % 2
        qi_ = qb % 4
        nkb = qb + 1
        Sc = nkb * 128
        ngrp = (nkb + 3) // 4
        kh, khb, vh, vhb, ybT, ybTb = st["kh"], st["khb"], st["vh"], st["vhb"], st["ybT"], st["ybTb"]
        pms, pmsb = st["pms"], st["pmsb"]
        for h in range(8):
            hs = {}

            def ld(h=h, hs=hs):
                s2 = kvc[0] % 2
                kvc[0] += 1
                hs["s2"] = s2
                hs["ob"] = OBS[hdc[0] % 2]
                hs["rc"] = hdc[0] % 2
                hdc[0] += 1
                P.dma("sp", (lambda e, o=kh[s2][:, 0:Sc], s_=dr["kT"][0][h * 128:(h + 1) * 128, 0:Sc]: e.dma_start(out=o, in_=s_)),
                      reads=[dr["kT"][1]], writes=[khb[s2]], lane=f"lk{s2}")
                P.dma("sp", (lambda e, o=vh[s2][:, 0:nkb, :], s_=dr["v"][0][0:Sc, h * 128:(h + 1) * 128].rearrange("(b p) c -> p b c", p=128): e.dma_start(out=o, in_=s_)),
                      reads=[dr["v"][1]], writes=[vhb[s2]], lane=f"lv{s2}")
            th.append(ld)
            def mk_qk(grp, h=h, hs=hs):
                def qk():
                    s2 = hs["s2"]
                    kbs = list(range(grp * 4, min(nkb, grp * 4 + 4)))
                    n = len(kbs) * 128
                    if ATTN_PE:
                        bk = nb()
                        for j, kb in enumerate(kbs):
                            w_ = nkb - 1 - kb
                            near = w_ <= 1
                            o_ = C.ps[bk][:, j * 128:(j + 1) * 128]
                            P.op("pe", (lambda e, o=o_, l=kh[s2][:, kb * 128:(kb + 1) * 128], r=qbk[p][:, h, :]: e.matmul(o, l, r, start=True, stop=False, skip_group_check=True)),
                                 reads=[khb[s2], qbkb[p]], writes=[C.ps_b[bk]])
                            P.op("pe", (lambda e, o=o_, r=maskT[p][:, kb * 128:(kb + 1) * 128], sp=(not near): e.matmul(o, C.negI[:], r, start=False, stop=sp, skip_group_check=True)),
                                 reads=[C.negI_b, maskTb[p]], writes=[C.ps_b[bk]])
                            if near:
                                P.op("pe", (lambda e, o=o_, r=C.Fb[:, (h * 2 + w_) * 128:(h * 2 + w_ + 1) * 128]: e.matmul(o, C.ident[:], r, start=False, stop=True, skip_group_check=True)),
                                     reads=[C.ident_b, C.Fb_b], writes=[C.ps_b[bk]])
                        k2 = pmc[0] % 4
                        pmc[0] += 1
                        P.op("act", (lambda e, o=pms[k2][:, 0:n], i_=C.ps[bk][:, 0:n]: e.activation(out=o, in_=i_, func=AF.Exp, bias=C.cb8[:, h:h + 1], scale=ATT_SCALE)),
                             reads=[C.ps_b[bk], C.cb8_b], writes=[pmsb[k2]])
                    else:
                        bk = nb()
                        for j, kb in enumerate(kbs):
                            mm(P, C.ps[bk][:, j * 128:(j + 1) * 128], kh[s2][:, kb * 128:(kb + 1) * 128], qbk[p][:, h, :], True, True, [khb[s2], qbkb[p]], [C.ps_b[bk]])
                        k3 = C.cnt % 2
                        C.cnt += 1
                        P.op("act", (lambda e, o=C.tmpa[k3][:, 0:n], i_=C.ps[bk][:, 0:n]: e.activation(out=o, in_=i_, func=AF.Exp, bias=C.cb8[:, h:h + 1], scale=ATT_SCALE)),
                             reads=[C.ps_b[bk], C.cb8_b], writes=[C.tmpa_b[k3]])
                        k2 = pmc[0] % 4
                        pmc[0] += 1
                        P.op("dve", (lambda e, o=pms[k2][:, 0:n], a=C.tmpa[k3][:, 0:n], b=maskT[p][:, grp * 512:grp * 512 + n]: e.tensor_tensor(out=o, in0=a, in1=b, op=ALU.mult)),
                             reads=[C.tmpa_b[k3], maskTb[p]], writes=[pmsb[k2]])
                        for j, kb in enumerate(kbs):
                            w_ = nkb - 1 - kb
                            if w_ <= 1:
                                P.op("dve", (lambda e, o=pms[k2][:, j * 128:(j + 1) * 128], f_=C.Fe[:, (h * 2 + w_) * 128:(h * 2 + w_ + 1) * 128]: e.tensor_tensor(out=o, in0=o, in1=f_, op=ALU.mult)),
                                     reads=[pmsb[k2], C.Fe_b], writes=[pmsb[k2]])
                    hs[("k2", grp)] = k2
                return qk

            def mk_pv(grp, h=h, hs=hs):
                def pv():
                    s2, OB = hs["s2"], hs["ob"]
                    k2 = hs[("k2", grp)]
                    kbs = list(range(grp * 4, min(nkb, grp * 4 + 4)))
                    for j, kb in enumerate(kbs):
                        P.op("pe", (lambda e, o=C.ps[OB][:, 0:128], l=pms[k2][:, j * 128:(j + 1) * 128], r=vh[s2][:, kb, :], st_=(kb == 0), sp=(kb == nkb - 1):
                                    e.matmul(o, l, r, start=st_, stop=sp, skip_group_check=True)),
                             reads=[pmsb[k2], vhb[s2]], writes=[C.ps_b[OB]])
                        P.op("pe", (lambda e, o=C.ps[OB][:, 128:129], l=pms[k2][:, j * 128:(j + 1) * 128], sp=(kb == nkb - 1):
                                    e.matmul(o, l, C.onec[:, 0:1], start=False, stop=sp, skip_group_check=True)),
                             reads=[pmsb[k2], C.onec_b], writes=[C.ps_b[OB]])

                return pv
            th.append(mk_qk(0))
            for grp in range(ngrp):
                if grp + 1 < ngrp:
                    th.append(mk_qk(grp + 1))
                th.append(mk_pv(grp))

            def fin(h=h, hs=hs):
                OB, ri = hs["ob"], hs["rc"]
                P.op("dve", (lambda e, o=rcs[ri][:], i_=C.ps[OB][:, 128:129]: e.reciprocal(out=o, in_=i_)), reads=[C.ps_b[OB]], writes=[rcsb[ri]])
                P.op("dve", (lambda e, o=C.yb[:, h * 128:(h + 1) * 128], i_=C.ps[OB][:, 0:128], s_=rcs[ri][:]: e.tensor_scalar(out=o, in0=i_, scalar1=s_, scalar2=None, op0=ALU.mult)),
                     reads=[C.ps_b[OB], rcsb[ri]], writes=[C.yb_b])
            th.append(fin)
        for half in range(2):
            def ytr(half=half):
                for j in range(4):
                    c = half * 4 + j
                    P.op("pe", (lambda e, o=psT[:, j * 128:(j + 1) * 128], i_=C.yb[:, c * 128:(c + 1) * 128]: e.transpose(o, i_, C.ident[:])),
                         reads=[C.yb_b, C.ident_b], writes=[C.ps_b[TB]])
                P.op("act", (lambda e, o=ybT[:, half * 4:half * 4 + 4, qi_ * 128:(qi_ + 1) * 128], i_=psT[:, 0:512].rearrange("p (c t) -> p c t", t=128):
                             e.activation(out=o, in_=i_, func=AF.Copy)),
                     reads=[C.ps_b[TB]], writes=[ybTb])
            th.append(ytr)
        return th

    def open_tile():
        kh = [C.R1[:, i * 2048:(i + 1) * 2048].bitcast(BF16) for i in range(2)]
        khb = [Buf("kh0"), Buf("kh1")]
        ybT = C.R1[:, 4096:6144].bitcast(BF16).rearrange("p (c t) -> p c t", t=NT)
        ybTb = Buf("ybT")
        vh = [C.R2[:, i * 2048:(i + 1) * 2048].bitcast(BF16).rearrange("p (b c) -> p b c", c=128) for i in range(2)]
        vhb = [Buf("vh0"), Buf("vh1")]
        pms = [C.R1[:, 6144 + i * 256:6144 + (i + 1) * 256].bitcast(BF16) for i in range(4)]
        pmsb = [Buf(f"pm{i}") for i in range(4)]
        C.claim("R1", khb + [ybTb] + pmsb); C.claim("R2", vhb)
        st.update(kh=kh, khb=khb, ybT=ybT, ybTb=ybTb, vh=vh, vhb=vhb, pms=pms, pmsb=pmsb)

    def tail(tt, wi):
        t0 = tt * NT
        ybT, ybTb = st["ybT"], st["ybTb"]
        mT = C.R2[:, :].bitcast(BF16).rearrange("p (c t) -> p c t", t=NT)
        mTb = Buf("mT")
        C.claim("R2", [mTb])
        lc = [0]
        for c2 in range(8):
            w, wb = C.wget(wi); wi += 1
            for cc in range(2):
                ch = 2 * c2 + cc
                bk = nb()
                for k in range(8):
                    mm(P, C.ps[bk][:], w[:, k, cc * 128:(cc + 1) * 128], ybT[:, k, :], k == 0, k == 7, [wb, ybTb], [C.ps_b[bk]])
                k2 = lc[0] % 2
                lc[0] += 1
                gbt, gbb = (C.gt1, C.gt1_b) if k2 == 0 else (C.gt2, C.gt2_b)
                P.dma("sp", (lambda e, o=gbt[:], s_=dr["gb"][0][ch * 128:(ch + 1) * 128, t0:t0 + NT]: e.dma_start(out=o, in_=s_)),
                      reads=[dr["gb"][1]], writes=[gbb], lane=f"lg{k2}")
                P.dma("sp", (lambda e, o=C.ta2[k2][:], s_=dr["apart"][0][ch * 128:(ch + 1) * 128, t0:t0 + NT]: e.dma_start(out=o, in_=s_)),
                      reads=[dr["apart"][1]], writes=[C.ta2_b[k2]], lane=f"la{k2}")
                P.op("dve", (lambda e, g_=gbt[:], i_=C.ps[bk][:]: e.tensor_tensor(out=g_, in0=g_, in1=i_, op=ALU.mult)),
                     reads=[gbb, C.ps_b[bk]], writes=[gbb])
                P.op("dve", (lambda e, o=mT[:, ch, :], g_=gbt[:], a=C.ta2[k2][:]: e.tensor_tensor(out=o, in0=g_, in1=a, op=ALU.add)),
                     reads=[gbb, C.ta2_b[k2]], writes=[mTb])
        fT = C.R1[:, :].rearrange("p (c t) -> p c t", t=NT)
        fTb = Buf("fT")
        C.claim("R1", [fTb])
        r2b = Buf("r2x")
        wi = emit_outproj_claim(C, wi, mT, mTb, x1, x2, t0, cs[:, 48:64], 1.0, cb, fT, fTb, r2b)
        return wi

    wi = wbase
    for f in phase_ab(0):
        f()
    for qb in range(NBLK):
        if qb % 4 == 0:
            open_tile()
        ca = phase_ab(qb + 1) if qb + 1 < NBLK else []
        cc_ = phase_c(qb)
        run_interleaved(ca, cc_)
        if qb % 4 == 3:
            wi = tail(qb // 4, wi)
    return wi


L_ = 2
NUM_BUCKETS = 32
MAX_DISTANCE = 128
IN_COLS = 6224


def build_program(S, depth):
    nc = bass.Bass("TRN2", target_bir_lowering=False)
    NTL = S // NT

    def din(name, shape, dt=F32):
        return nc.dram_tensor(name, list(shape), dt, kind="ExternalInput").ap()

    def dscr(name, shape, dt=F32):
        return nc.dram_tensor(name, list(shape), dt, kind="Internal").ap()
    x = din("x", [D, S])
    y = nc.dram_tensor("y", [D, S], F32, kind="ExternalOutput").ap()
    W = {}
    for l in range(depth):
        W[l] = dict(
            f1i=din(f"f1i{l}", [D, 2 * DFF]), f1o=din(f"f1o{l}", [DFF, D]),
            f2i=din(f"f2i{l}", [D, 2 * DFF]), f2o=din(f"f2o{l}", [DFF, D]),
            win=din(f"win{l}", [D, IN_COLS]), wg=din(f"wg{l}", [D, 2 * D]),
            wba=din(f"wba{l}", [AW, D]), wbb=din(f"wbb{l}", [AW, D]), wo=din(f"wo{l}", [D, D]),
            cpk=din(f"cpk{l}", [128, 96]), bro=din(f"bro{l}", [128, 3072]), wsT=din(f"wsT{l}", [128, 8, 128]),
        )
    toep = din("toep", [128, 2048])
    cb8d = din("cb8", [128, 8])
    xa = (dscr("xa", [D, S]), Buf("xa"))
    xb = (dscr("xb", [D, S]), Buf("xb"))
    xc = (dscr("xc", [D, S]), Buf("xc"))
    dr = {
        "qT": (dscr("qT", [AW, S], BF16), Buf("qT")), "qiT": (dscr("qiT", [AW, S], BF16), Buf("qiT")),
        "kT": (dscr("kT", [AW, S], BF16), Buf("kT")), "v": (dscr("v", [S, AW], BF16), Buf("v")),
        "kiT": (dscr("kiT", [128, S], BF16), Buf("kiT")), "widx": (dscr("widx", [S, 16]), Buf("widx")),
        "apart": (dscr("apart", [D, S]), Buf("apart")), "gb": (dscr("gb", [D, S]), Buf("gb")),
    }
    P = Prog(nc)
    C = Ctx(P)
    cs = [P.sbuf(f"consts{l}", [128, 96], F32) for l in range(depth)]
    cb = [Buf(f"consts{l}") for l in range(depth)]
    for l in range(depth):
        P.dma("sp", (lambda e, l=l: e.dma_start(out=cs[l][:], in_=W[l]["cpk"])), reads=[], writes=[cb[l]], lane="ldc")
    emit_attn_consts(C, toep, cb8d)
    for l in range(depth):
        w = W[l]
        for t in range(NTL):
            C.plan_extend(ffn_plan(w["f1i"], w["f1o"]))
        for t in range(NTL):
            C.plan_extend(mixa_plan(w["win"], w["wg"], w["wba"]))
        for t in range(NTL):
            C.plan_extend(mixb_tail_plan(w["wbb"], w["wo"]))
        for t in range(NTL):
            C.plan_extend(ffn_plan(w["f2i"], w["f2o"]))
    wi = 0
    xin = (x, Buf("x"))
    for l in range(depth):
        w = W[l]
        xout = (y, Buf("y")) if l == depth - 1 else xc
        for t in range(NTL):
            wi = emit_ffn(C, wi, xin, xa, t * NT, cs[l][:, 0:16], cs[l][:, 16:32], cb[l])
        emit_mix_consts(C, w["bro"], w["wsT"], None)
        for t in range(NTL):
            wi = emit_mixa(C, wi, xa, t * NT, S, cs[l], cb[l], dr)
        P.dma("sp", (lambda e: e.dma_start(out=C.kis[:, 0:S], in_=dr["kiT"][0])), reads=[dr["kiT"][1]], writes=[C.kis_b], lane="ldk")
        wi = emit_mixb_layer(C, wi, NTL, S, xa, xb, cs[l], cb[l], dr)
        for t in range(NTL):
            wi = emit_ffn(C, wi, xb, xout, t * NT, cs[l][:, 64:80], cs[l][:, 80:96], cb[l])
        xin = xc
    assert wi == len(C.plan), (wi, len(C.plan))
    P.finalize()
    return nc


def t5_bucket_np(n):
    max_exact = NUM_BUCKETS // 2
    nf = np.maximum(n, 1).astype(np.float32)
    large = max_exact + (np.log(nf / max_exact) / np.log(MAX_DISTANCE / max_exact) * (NUM_BUCKETS - max_exact)).astype(np.int32)
    large = np.minimum(large, NUM_BUCKETS - 1)
    return np.where(n < max_exact, n, large)


def host_prep(inp, depth):
    f = lambda a: np.ascontiguousarray(np.asarray(a, dtype=np.float32))
    m = {}

    def pc(v):
        return np.asarray(v, dtype=np.float32).reshape(16, 128).T
    for l in range(depth):
        m[f"f1i{l}"] = f(inp["ffn1_w_in"][l]); m[f"f1o{l}"] = f(inp["ffn1_w_out"][l])
        m[f"f2i{l}"] = f(inp["ffn2_w_in"][l]); m[f"f2o{l}"] = f(inp["ffn2_w_out"][l])
        m[f"win{l}"] = f(inp["w_in"][l]); m[f"wg{l}"] = f(inp["w_gate"][l])
        m[f"wba{l}"] = f(inp["w_branch_a"][l]); m[f"wbb{l}"] = f(inp["w_branch_b"][l]); m[f"wo{l}"] = f(inp["w_out"][l])
        m[f"cpk{l}"] = f(np.concatenate([pc(inp[k][l]) for k in ("ffn1_norm_pre", "ffn1_norm_post", "mix_norm_pre", "mix_norm_post", "ffn2_norm_pre", "ffn2_norm_post")], axis=1))
        row = np.concatenate([np.asarray(inp["sgu_ln_g"][l], np.float32), np.asarray(inp["sgu_ln_b"][l], np.float32), np.asarray(inp["sgu_b"][l], np.float32).reshape(-1)])
        m[f"bro{l}"] = f(np.broadcast_to(row[None, :], (128, 3072)))
        m[f"wsT{l}"] = f(np.transpose(np.asarray(inp["sgu_w_s"][l], np.float32), (2, 0, 1)))
    rb = np.asarray(inp["rel_bias"], np.float32)
    s_ = np.arange(128)[:, None]
    t_ = np.arange(128)[None, :]
    toep = np.zeros((128, 8, 2, 128), np.float32)
    for w_ in range(2):
        dist = np.maximum(t_ - s_ + 128 * w_, 0)
        bk = t5_bucket_np(dist)
        toep[:, :, w_, :] = np.transpose(rb[bk], (0, 2, 1))
    m["toep"] = f(toep.reshape(128, 2048))
    m["cb8"] = f(np.broadcast_to(rb[NUM_BUCKETS - 1][None, :], (128, 8)))
    return m


BATCH = 4
DEPTH = 2
_NC_CACHE = {}


def kernel(**inputs):
    inp = {k: np.asarray(v) for k, v in inputs.items()}
    S = inp["x"].shape[1]
    if "nc" not in _NC_CACHE:
        _NC_CACHE["nc"] = build_program(S, DEPTH)
    nc = _NC_CACHE["nc"]
    shared = host_prep(inp, DEPTH)
    in_maps = []
    for b in range(BATCH):
        m = dict(shared)
        m["x"] = np.ascontiguousarray(inp["x"][b].T.astype(np.float32))
        in_maps.append(m)
    res = run_bass_kernel_spmd(nc, in_maps, core_ids=list(range(BATCH)))
    out = np.stack([np.asarray(res.results[b]["y"]).T for b in range(BATCH)], axis=0)
    return np.ascontiguousarray(out.astype(np.float32))
```

```python
import numpy as np
from concourse.bass_utils import run_bass_kernel_spmd
from contextlib import ExitStack
import numpy as np
import concourse.bass as bass
import concourse.mybir as mybir

F32 = mybir.dt.float32
BF16 = mybir.dt.bfloat16
AF = mybir.ActivationFunctionType
ALU = mybir.AluOpType

ENGS = ("pe", "act", "dve", "pool", "sp")


class Buf:
    __slots__ = ("name", "lastw", "readers")

    def __init__(self, name):
        self.name = name
        self.lastw = None
        self.readers = {}


class Lane:
    def __init__(self, prog, name):
        self.sem = prog.nc.alloc_semaphore(name=name)
        self.count = 0
        self.last = None


class Op:
    __slots__ = ("eng", "fn", "waits", "flag", "val", "lane", "laneval", "idx")

    def __init__(self, eng, fn):
        self.eng = eng
        self.fn = fn
        self.waits = []
        self.flag = False
        self.val = None
        self.lane = None
        self.laneval = None


class Prog:
    def __init__(self, nc):
        self.nc = nc
        self.q = {e: [] for e in ENGS}
        self.sem = {e: nc.alloc_semaphore(name="sem_" + e) for e in ENGS}
        self.es = ExitStack()
        self.lanes = {}
        self.n_sb = 0

    def sbuf(self, name, shape, dtype):
        return self.es.enter_context(self.nc.sbuf_tensor(name, list(shape), dtype))

    def psum(self, name, shape, dtype=F32):
        return self.es.enter_context(self.nc.psum_tensor(name, list(shape), dtype))

    def lane(self, name):
        if name not in self.lanes:
            self.lanes[name] = Lane(self, "ln_" + name)
        return self.lanes[name]

    def _deps(self, op, reads, writes):
        evs = []
        for b in reads:
            if b.lastw is not None:
                evs.append(b.lastw)
        for b in writes:
            if b.lastw is not None:
                evs.append(b.lastw)
            evs.extend(b.readers.values())
        for ev in evs:
            if ev[0] == "c":
                src = ev[1]
                if src.eng == "pe" and op.eng == "pe":
                    continue
                if src is op:
                    continue
                src.flag = True
            op.waits.append(ev)

    def _post(self, ev, reads, writes):
        for b in writes:
            b.lastw = ev
            b.readers = {}
        key = ("c", ev[1].eng) if ev[0] == "c" else ("d", id(ev[1]))
        for b in reads:
            if b not in writes:
                b.readers[key] = ev

    def op(self, eng, fn, reads=(), writes=()):
        o = Op(eng, fn)
        self._deps(o, reads, writes)
        o.idx = len(self.q[eng])
        self.q[eng].append(o)
        self._post(("c", o), reads, writes)
        return o

    def dma(self, eng, fn, reads, writes, lane):
        ln = self.lane(lane) if isinstance(lane, str) else lane
        o = Op(eng, fn)
        self._deps(o, reads, writes)
        if ln.last is not None:
            o.waits.append(ln.last)
        ln.count += 16
        o.lane = ln
        o.laneval = ln.count
        ev = ("d", ln, ln.count)
        ln.last = ev
        self.q[eng].append(o)
        self._post(ev, reads, writes)
        return o

    def claim(self, old_bufs, new_bufs):
        merged = {}
        for b in old_bufs:
            evs = list(b.readers.values())
            if b.lastw is not None:
                evs.append(b.lastw)
            for ev in evs:
                key = ("c", ev[1].eng) if ev[0] == "c" else ("d", id(ev[1]))
                rank = ev[1].idx if ev[0] == "c" else ev[2]
                if key not in merged or merged[key][0] < rank:
                    merged[key] = (rank, ev)
        for nb_ in new_bufs:
            for key, (rank, ev) in merged.items():
                nb_.readers[key] = ev

    def finalize(self):
        nc = self.nc
        for e in ENGS:
            c = 0
            for o in self.q[e]:
                if o.flag:
                    c += 1
                    o.val = c
        engobj = {"pe": "tensor", "act": "scalar", "dve": "vector", "pool": "gpsimd", "sp": "sync"}
        final_lane_vals = [(ln.sem, ln.count) for ln in self.lanes.values() if ln.count > 0]
        final_eng_vals = {e: max([o.val for o in self.q[e] if o.flag] + [0]) for e in ENGS}
        prog = self

        def emit(e, eng):
            known = {}
            for o in prog.q[e]:
                need = {}
                for ev in o.waits:
                    if ev[0] == "c":
                        src = ev[1]
                        key = ("c", src.eng)
                        sem, val = prog.sem[src.eng], src.val
                    else:
                        key = ("d", id(ev[1]))
                        sem, val = ev[1].sem, ev[2]
                    if known.get(key, 0) >= val:
                        continue
                    if key not in need or need[key][1] < val:
                        need[key] = (sem, val)
                for key, (sem, val) in need.items():
                    eng.wait_ge(sem, val)
                    known[key] = val
                ins = o.fn(eng)
                if o.lane is not None:
                    ins.then_inc(o.lane.sem, 16)
                elif o.flag:
                    ins.then_inc(prog.sem[e], 1)
            if e == "sp":
                for sem, val in final_lane_vals:
                    eng.wait_ge(sem, val)
                for e2, v in final_eng_vals.items():
                    if v > 0:
                        eng.wait_ge(prog.sem[e2], v)

        with nc.Block() as block:
            @block.tensor
            def _(eng):
                emit("pe", eng)

            @block.scalar
            def _(eng):
                emit("act", eng)

            @block.vector
            def _(eng):
                emit("dve", eng)

            @block.gpsimd
            def _(eng):
                emit("pool", eng)

            @block.sync
            def _(eng):
                emit("sp", eng)
        self.es.close()


D = 2048
DC = D // 128
DFF = 5632
FC = DFF // 128
NT = 512
NORM_EPS = 1e-6
ATTN_PE = 1
LN_EPS = 1e-5
SEQ = 4096
CASTDMA = True


class Ctx:
    def __init__(self, P):
        self.P = P
        nc = P.nc
        self.ps = [P.psum(f"ps{i}", [128, 512]) for i in range(8)]
        self.ps_b = [Buf(f"ps{i}") for i in range(8)]
        self.NS = 4
        self.wbf = [P.sbuf(f"wbf{i}", [128, 4096], BF16) for i in range(self.NS)]
        self.wbf_b = [Buf(f"wbf{i}") for i in range(self.NS)]
        self.plan = []
        self.issued = 0
        self.R1 = P.sbuf("R1", [128, 8192], F32)
        self.R2 = P.sbuf("R2", [128, 4096], F32)
        self.R3 = P.sbuf("R3", [128, 11264], F32)
        self.ones = P.sbuf("ones", [128, 128], BF16)
        self.ones_b = Buf("ones")
        self.tmpa = [P.sbuf(f"tmpa{i}", [128, 512], F32) for i in range(2)]
        self.tmpa_b = [Buf(f"tmpa{i}") for i in range(2)]
        self.sq = [P.sbuf(f"sq{i}", [128, 512], BF16) for i in range(2)]
        self.sq_b = [Buf(f"sq{i}") for i in range(2)]
        self.rstd = P.sbuf("rstd", [128, 512], F32)
        self.rstd_b = Buf("rstd")
        self.epsc = P.sbuf("epsc", [128, 1], F32)
        self.epsc_b = Buf("epsc")
        self.cnt = 0
        self.reg = {"R1": [], "R2": [], "R3": []}
        def sb(name, shape, dt):
            setattr(self, name, P.sbuf("s_" + name, shape, dt))
            setattr(self, name + "_b", Buf(name))
        sb("kio", [128, 512], BF16); sb("wio", [128, 64], F32)
        sb("gt1", [128, 512], F32); sb("gt2", [128, 512], F32)
        self.R4 = P.sbuf("R4", [128, 4096], F32)
        self.reg["R4"] = []
        self.bro = self.R4[:, 0:3072]; self.bro_b = Buf("bro")
        self.wsf = self.R4[:, 3072:4096]; self.wsf_b = Buf("wsf")
        sb("wsb", [128, 8, 128], BF16)
        sb("mT0", [128, SEQ], BF16); sb("mT1", [128, SEQ], BF16)
        sb("qib2", [128, 8, 128], BF16); sb("qbk2", [128, 8, 128], BF16); sb("wx2", [128, 16], F32)
        sb("wabs2", [128, 16], F32); sb("wsg2", [128, 16], F32)
        sb("dg0", [128, 16, 128], BF16); sb("negI", [128, 128], BF16)
        sb("rc2", [128, 1], F32)
        if not ATTN_PE:
            sb("Fe", [128, 2048], BF16)
        self.dg1, self.dg1_b = self.dg0, self.dg0_b
        for i in range(4):
            sb(f"rl16_{i}", [128, 512], BF16)
        self.rl16 = [getattr(self, f"rl16_{i}") for i in range(4)]; self.rl16_b = [getattr(self, f"rl16_{i}_b") for i in range(4)]
        sb("blo", [128, 1], F32); sb("bmid", [128, 1], F32); sb("bcnt", [128, 1], F32); sb("bfs", [128, 1], F32); sb("brng", [128, 1], F32)
        sb("bstep", [128, 32], F32); sb("pw2", [128, 32], F32)
        for k in range(32):
            P.op("pool", (lambda e, k=k: e.memset(self.pw2[:, k:k + 1], 2.0 ** -(k + 1))), writes=[self.pw2_b])
        self.ta2 = self.tmpa; self.ta2_b = self.tmpa_b
        sb("bst", [128, 12], F32); sb("bag", [128, 2], F32); sb("lrs", [128, 1], F32); sb("lneps", [128, 1], F32)
        sb("qib", [128, 8, 128], BF16); sb("qbk", [128, 8, 128], BF16); sb("wx", [128, 16], F32)
        sb("wabs", [128, 16], F32); sb("wsg", [128, 16], F32); sb("kis", [128, SEQ], BF16)
        sb("m8", [128, 8], F32); sb("thrneg", [128, 1], F32); sb("ident", [128, 128], BF16); sb("onec", [128, 2], BF16)
        sb("rc", [128, 1], F32); sb("yb", [128, 1024], BF16)
        sb("Fb", [128, 2048], BF16); sb("cb8", [128, 8], F32)
        P.op("pool", lambda e: e.memset(self.lneps[:], LN_EPS), writes=[self.lneps_b])
        P.op("pool", lambda e: e.memset(self.ones[:], 1.0), writes=[self.ones_b])
        P.op("pool", lambda e: e.memset(self.epsc[:], NORM_EPS), writes=[self.epsc_b])

    def claim(self, rname, bufs):
        self.P.claim(self.reg[rname], bufs)
        self.reg[rname] = list(bufs)

    def plan_extend(self, specs):
        self.plan.extend(specs)

    def _issue(self, i):
        P = self.P
        pieces, R, W = self.plan[i]
        s = i % self.NS
        if CASTDMA:
            bt = self.wbf[s][:, 0:R * W].rearrange("p (r w) -> p r w", w=W)
            for pi, (c0, wd, src) in enumerate(pieces):
                P.dma("pool", (lambda e, o=bt[:, :, c0:c0 + wd], s_=src: e.dma_start(out=o, in_=s_)),
                      reads=[], writes=[self.wbf_b[s]], lane=f"w{s}_{pi}")
            return
        st = self.wst[s][:, 0:R * W].rearrange("p (r w) -> p r w", w=W)
        for pi, (c0, wd, src) in enumerate(pieces):
            P.dma("sp", (lambda e, o=st[:, :, c0:c0 + wd], s_=src: e.dma_start(out=o, in_=s_)),
                  reads=[], writes=[self.wst_b[s]], lane=f"w{s}_{pi}")
        P.op("pool", (lambda e, o=self.wbf[s][:, 0:R * W], i_=self.wst[s][:, 0:R * W]: e.tensor_copy(out=o, in_=i_)),
             reads=[self.wst_b[s]], writes=[self.wbf_b[s]])

    def wget(self, i, look=2):
        while self.issued <= min(i + look, len(self.plan) - 1):
            self._issue(self.issued)
            self.issued += 1
        pieces, R, W = self.plan[i]
        s = i % self.NS
        return self.wbf[s][:, 0:R * W].rearrange("p (r w) -> p r w", w=W), self.wbf_b[s]


_FILLREG = {}


def fillreg(e, v):
    key = (id(e), v)
    if key not in _FILLREG:
        _FILLREG[key] = e.to_reg(v)
    return _FILLREG[key]


def mm(P, o, l, r, st, sp, reads, writes):
    P.op("pe", (lambda e: e.matmul(o, l, r, start=st, stop=sp)), reads=reads, writes=writes)


def w_panel(w2d, r0, nr, cols):
    pieces = []
    off = 0
    for (c0, wd) in cols:
        src = w2d[r0 * 128:(r0 + nr) * 128, c0:c0 + wd].rearrange("(r p) w -> p r w", p=128)
        pieces.append((off, wd, src))
        off += wd
    return (pieces, nr, off)


def emit_rstd(C, chunk_ap, nch, dim, bank):
    P = C.P
    ps, psb = C.ps[bank], C.ps_b[bank]
    for c in range(nch):
        ap, b = chunk_ap(c)
        k = C.cnt % 2
        C.cnt += 1
        P.op("act", (lambda e, o=C.sq[k][:], i_=ap: e.activation(out=o, in_=i_, func=AF.Square)),
             reads=[b], writes=[C.sq_b[k]])
        P.op("pe", (lambda e, o=ps[:], l=C.ones[:], r=C.sq[k][:], st=(c == 0), sp=(c == nch - 1):
                    e.matmul(o, l, r, start=st, stop=sp)),
             reads=[C.ones_b, C.sq_b[k]], writes=[psb])
    P.op("act", (lambda e: e.activation(out=C.rstd[:], in_=ps[:], func=AF.Sqrt, bias=C.epsc[:], scale=1.0 / dim)),
         reads=[psb, C.epsc_b], writes=[C.rstd_b])
    P.op("dve", (lambda e: e.reciprocal(out=C.rstd[:], in_=C.rstd[:])), reads=[C.rstd_b], writes=[C.rstd_b])


def ffn_plan(w_in, w_out):
    specs = []
    for g in range(FC // 2):
        specs.append(w_panel(w_in, 0, DC, [(g * 256, 256)]))
        specs.append(w_panel(w_in, 0, DC, [(DFF + g * 256, 256)]))
    for c2 in range(DC // 2):
        for kp in range(3):
            r0 = kp * 16
            nr = min(16, FC - r0)
            specs.append(w_panel(w_out, r0, nr, [(c2 * 256, 256)]))
    return specs


def emit_ffn(C, wbase, x_dram, xo_dram, t0, gpre, gpost, cb):
    P = C.P
    (xd, xdb), (xo, xob) = x_dram, xo_dram
    xT = C.R1[:, :].rearrange("p (c t) -> p c t", t=NT)
    xTb = Buf("xT")
    hT = C.R2[:, :].bitcast(BF16).rearrange("p (c t) -> p c t", t=NT)
    hTb = Buf("hT")
    gT = C.R3[:, :].bitcast(BF16).rearrange("p (c t) -> p c t", t=NT)
    gTb = [Buf(f"gT{j}") for j in range(FC)]
    C.claim("R1", [xTb]); C.claim("R2", [hTb]); C.claim("R3", gTb)
    xsrc = xd[:, t0:t0 + NT].rearrange("(c p) t -> p c t", p=128)
    P.dma("sp", lambda e: e.dma_start(out=xT, in_=xsrc), reads=[xdb], writes=[xTb], lane="ld0")
    emit_rstd(C, lambda c: (xT[:, c, :], xTb), DC, D, 0)
    for c in range(DC):
        P.op("dve", (lambda e, c=c: e.scalar_tensor_tensor(out=hT[:, c, :], in0=xT[:, c, :], scalar=gpre[:, c:c + 1],
                                                          in1=C.rstd[:], op0=ALU.mult, op1=ALU.mult)),
             reads=[xTb, C.rstd_b, cb], writes=[hTb])
    wi = wbase
    for g in range(FC // 2):
        base = 4 * (g % 2)
        wa, wab = C.wget(wi); wi += 1
        for cc in range(2):
            bk = base + cc
            for k in range(DC):
                P.op("pe", (lambda e, o=C.ps[bk][:], l=wa[:, k, cc * 128:(cc + 1) * 128], r=hT[:, k, :], st=(k == 0), sp=(k == DC - 1):
                            e.matmul(o, l, r, start=st, stop=sp)),
                     reads=[wab, hTb], writes=[C.ps_b[bk]])
        wb_, wbb = C.wget(wi); wi += 1
        for cc in range(2):
            bk = base + 2 + cc
            for k in range(DC):
                P.op("pe", (lambda e, o=C.ps[bk][:], l=wb_[:, k, cc * 128:(cc + 1) * 128], r=hT[:, k, :], st=(k == 0), sp=(k == DC - 1):
                            e.matmul(o, l, r, start=st, stop=sp)),
                     reads=[wbb, hTb], writes=[C.ps_b[bk]])
        for cc in range(2):
            j = 2 * g + cc
            k2 = C.cnt % 2
            C.cnt += 1
            P.op("act", (lambda e, o=C.tmpa[k2][:], i_=C.ps[base + cc][:]: e.activation(out=o, in_=i_, func=AF.Silu)),
                 reads=[C.ps_b[base + cc]], writes=[C.tmpa_b[k2]])
            P.op("dve", (lambda e, o=gT[:, j, :], a=C.tmpa[k2][:], b=C.ps[base + 2 + cc][:]:
                         e.tensor_tensor(out=o, in0=a, in1=b, op=ALU.mult)),
                 reads=[C.tmpa_b[k2], C.ps_b[base + 2 + cc]], writes=[gTb[j]])
    wi = emit_outproj(C, wi, gT, lambda f: gTb[f], FC, (xd, xdb), (xo, xob), t0, gpost, 0.5, cb, xT, xTb, hTb)
    return wi


def outproj_plan(w_out, K):
    specs = []
    for c2 in range(DC // 2):
        for r0 in range(0, K, 16):
            specs.append(w_panel(w_out, r0, min(16, K - r0), [(c2 * 256, 256)]))
    return specs


def emit_outproj(C, wi, inT, inb, K, x_dram, xo_dram, t0, gpost, alpha, cb, fT, fTb, r2b):
    P = C.P
    (xd, xdb), (xo, xob) = x_dram, xo_dram
    SB = 6
    for c2 in range(DC // 2):
        base = 2 * (c2 % 2)
        for r0 in range(0, K, 16):
            nr = min(16, K - r0)
            wo, wob = C.wget(wi); wi += 1
            for cc in range(2):
                bk = base + cc
                for r in range(nr):
                    f = r0 + r
                    mm(P, C.ps[bk][:], wo[:, r, cc * 128:(cc + 1) * 128], inT[:, f, :], f == 0, f == K - 1, [wob, inb(f)], [C.ps_b[bk]])
        for cc in range(2):
            c = 2 * c2 + cc
            bk = base + cc
            P.op("act", (lambda e, o=fT[:, c, :], i_=C.ps[bk][:]: e.activation(out=o, in_=i_, func=AF.Copy)),
                 reads=[C.ps_b[bk]], writes=[fTb])
            k2 = C.cnt % 2
            C.cnt += 1
            P.op("dve", (lambda e, o=C.sq[k2][:], i_=C.ps[bk][:], f_=fT[:, c, :]: e.tensor_tensor(out=o, in0=i_, in1=f_, op=ALU.mult)),
                 reads=[C.ps_b[bk], fTb], writes=[C.sq_b[k2]])
            mm(P, C.ps[SB][:], C.ones[:], C.sq[k2][:], c == 0, c == DC - 1, [C.ones_b, C.sq_b[k2]], [C.ps_b[SB]])
    P.op("act", (lambda e: e.activation(out=C.rstd[:], in_=C.ps[SB][:], func=AF.Sqrt, bias=C.epsc[:], scale=1.0 / D)),
         reads=[C.ps_b[SB], C.epsc_b], writes=[C.rstd_b])
    P.op("dve", (lambda e: e.reciprocal(out=C.rstd[:], in_=C.rstd[:])), reads=[C.rstd_b], writes=[C.rstd_b])
    xr = C.R2[:, :].rearrange("p (c t) -> p c t", t=NT)
    for h in range(2):
        xs = xd[h * 1024:(h + 1) * 1024, t0:t0 + NT].rearrange("(c p) t -> p c t", p=128)
        P.dma("sp", (lambda e, s_=xs: e.dma_start(out=xr, in_=s_)), reads=[xdb], writes=[r2b], lane="ld1")
        for cl in range(8):
            c = h * 8 + cl
            P.op("dve", (lambda e, c=c: e.scalar_tensor_tensor(out=fT[:, c, :], in0=fT[:, c, :], scalar=gpost[:, c:c + 1],
                                                              in1=C.rstd[:], op0=ALU.mult, op1=ALU.mult)),
                 reads=[fTb, C.rstd_b, cb], writes=[fTb])
            P.op("dve", (lambda e, c=c, cl=cl: e.scalar_tensor_tensor(out=fT[:, c, :], in0=fT[:, c, :], scalar=alpha,
                                                                       in1=xr[:, cl, :], op0=ALU.mult, op1=ALU.add)),
                 reads=[fTb, r2b], writes=[fTb])
    xdst = xo[:, t0:t0 + NT].rearrange("(c p) t -> p c t", p=128)
    P.dma("sp", lambda e: e.dma_start(out=xdst, in_=fT), reads=[fTb], writes=[xob], lane="st0")
    return wi


AW = 1024
NEG = -1.0e30
GELU_C = 0.7978845608028654 * 2.0
LN_EPS = 1e-5
O_ZU, O_ZV, O_Q, O_K, O_V, O_QI, O_KI, O_WI = 0, 1024, 2048, 3072, 4096, 5120, 6144, 6208


def mm(P, o, l, r, st, sp, reads, writes):
    P.op("pe", (lambda e: e.matmul(o, l, r, start=st, stop=sp)), reads=reads, writes=writes)


def emit_gelu(C, out_ap, in_ap, inb, outb, shape):
    P = C.P
    t1 = C.gt1[:, 0:shape]
    t2 = C.gt2[:, 0:shape]
    P.op("act", (lambda e: e.activation(out=t1, in_=in_ap, func=AF.Square)), reads=[inb], writes=[C.gt1_b])
    P.op("dve", (lambda e: e.tensor_scalar(out=t1, in0=t1, scalar1=0.044715, scalar2=1.0, op0=ALU.mult, op1=ALU.add)),
         reads=[C.gt1_b], writes=[C.gt1_b])
    P.op("dve", (lambda e: e.tensor_tensor(out=t1, in0=t1, in1=in_ap, op=ALU.mult)), reads=[C.gt1_b, inb], writes=[C.gt1_b])
    P.op("act", (lambda e: e.activation(out=t2, in_=t1, func=AF.Sigmoid, scale=GELU_C)), reads=[C.gt1_b], writes=[C.gt2_b])
    P.op("dve", (lambda e: e.tensor_tensor(out=out_ap, in0=t2, in1=in_ap, op=ALU.mult)), reads=[C.gt2_b, inb], writes=[outb])


def mixa_plan(w_in, w_gate, w_ba):
    specs = []
    for seg in (O_ZU, O_Q, O_K, O_QI):
        for c2 in range(4):
            specs.append(w_panel(w_in, 0, DC, [(seg + c2 * 256, 256)]))
    specs.append(w_panel(w_in, 0, DC, [(O_KI, 64), (O_KI, 64)]))
    for seg in (O_ZV, O_V):
        for c2 in range(4):
            specs.append(w_panel(w_in, 0, DC, [(seg + c2 * 256, 256)]))
    specs.append(w_panel(w_in, 0, DC, [(O_WI, 16)]))
    for c2 in range(8):
        specs.append(w_panel(w_ba, 0, 8, [(c2 * 256, 256)]))
        specs.append(w_panel(w_gate, 0, DC, [(c2 * 256, 256)]))
    for c2 in range(8):
        specs.append(w_panel(w_gate, 0, DC, [(D + c2 * 256, 256)]))
    return specs


class MixRes:
    pass


def emit_mix_consts(C, bro_d, wsT_d, M):
    P = C.P
    C.bro_b = Buf("bro"); C.wsf_b = Buf("wsf")
    C.claim("R4", [C.bro_b, C.wsf_b])
    P.dma("sp", lambda e: e.dma_start(out=C.bro, in_=bro_d), reads=[], writes=[C.bro_b], lane="ldc")
    wv = C.wsf.rearrange("p (g t) -> p g t", g=8)
    P.dma("sp", lambda e: e.dma_start(out=wv, in_=wsT_d), reads=[], writes=[C.wsf_b], lane="ldc")
    P.op("pool", (lambda e: e.affine_select(out=wv, in_=wv, pattern=[[0, 8], [1, 128]], compare_op=ALU.is_ge,
                                             fill=fillreg(e, 0.0), base=0, channel_multiplier=-1)),
         reads=[C.wsf_b], writes=[C.wsf_b])
    P.op("pool", (lambda e: e.tensor_copy(out=C.wsb[:], in_=wv)), reads=[C.wsf_b], writes=[C.wsb_b])


def emit_mixa(C, wbase, x1, t0, S, cs, cb, dr):
    P = C.P
    xd, xdb = x1
    xT = C.R1[:, :].rearrange("p (c t) -> p c t", t=NT)
    xTb = Buf("xT")
    hT = C.R2[:, :].bitcast(BF16).rearrange("p (c t) -> p c t", t=NT)
    hTb = Buf("hT")
    r3 = C.R3[:, :]
    zv = r3[:, 0:4096].rearrange("p (b c) -> p b c", c=1024)
    zvb = Buf("zv")
    vln = r3[:, 4096:6144].bitcast(BF16).rearrange("p (b c) -> p b c", c=1024)
    vlnb = Buf("vln")
    guT = r3[:, 6144:8192].bitcast(BF16).rearrange("p (c t) -> p c t", t=NT)
    guTb = Buf("guT")
    yaT = r3[:, 8192:10240].bitcast(BF16).rearrange("p (c t) -> p c t", t=NT)
    yaTb = Buf("yaT")
    gpre = cs[:, 32:48]
    C.claim("R1", [xTb]); C.claim("R2", [hTb]); C.claim("R3", [zvb, vlnb, guTb, yaTb])
    xsrc = xd[:, t0:t0 + NT].rearrange("(c p) t -> p c t", p=128)
    P.dma("sp", lambda e: e.dma_start(out=xT, in_=xsrc), reads=[xdb], writes=[xTb], lane="ld0")
    emit_rstd(C, lambda c: (xT[:, c, :], xTb), DC, D, 0)
    for c in range(DC):
        P.op("dve", (lambda e, c=c: e.scalar_tensor_tensor(out=hT[:, c, :], in0=xT[:, c, :], scalar=gpre[:, c:c + 1],
                                                          in1=C.rstd[:], op0=ALU.mult, op1=ALU.mult)),
             reads=[xTb, C.rstd_b, cb], writes=[hTb])
    osb = [C.R1[:, i * 2048:(i + 1) * 2048].bitcast(BF16).rearrange("p (c t) -> p c t", t=NT) for i in range(2)]
    osbb = [Buf("osb0"), Buf("osb1")]
    och = [C.R1[:, 4096 + i * 512: 4096 + (i + 1) * 512] for i in range(8)]
    ochb = [Buf(f"och{i}") for i in range(8)]
    C.claim("R1", osbb + ochb)
    wi = wbase
    bankrr = [0]

    def nb():
        b = bankrr[0] % 8
        bankrr[0] += 1
        return b

    first = [True]

    def r1dep():
        return [xTb]

    dests = {O_Q: ("qT", 0), O_K: ("kT", 1), O_QI: ("qiT", 0)}
    for seg in (O_ZU, O_Q, O_K, O_QI):
        if seg != O_ZU:
            dn, oi = dests[seg]
            ob, obb = osb[oi], osbb[oi]
        for c2 in range(4):
            w, wb = C.wget(wi); wi += 1
            for cc in range(2):
                ch = 2 * c2 + cc
                bk = nb()
                for k in range(DC):
                    mm(P, C.ps[bk][:], w[:, k, cc * 128:(cc + 1) * 128], hT[:, k, :], k == 0, k == DC - 1, [wb, hTb], [C.ps_b[bk]])
                if seg == O_ZU:
                    emit_gelu(C, guT[:, ch, :], C.ps[bk][:], C.ps_b[bk], guTb, 512)
                else:
                    P.op("act", (lambda e, o=ob[:, ch, :], i_=C.ps[bk][:]: e.activation(out=o, in_=i_, func=AF.Copy)),
                         reads=[C.ps_b[bk]], writes=[obb])
        if seg != O_ZU:
            dd, ddb = dr[dn]
            dst = dd[:, t0:t0 + NT].rearrange("(c p) t -> p c t", p=128)
            P.dma("sp", (lambda e, d_=dst, s_=ob: e.dma_start(out=d_, in_=s_)), reads=[obb], writes=[ddb], lane="st1")
    w, wb = C.wget(wi); wi += 1
    bk = nb()
    for k in range(DC):
        mm(P, C.ps[bk][:], w[:, k, 0:128], hT[:, k, :], k == 0, k == DC - 1, [wb, hTb], [C.ps_b[bk]])
    P.op("act", (lambda e, o=C.kio[:], i_=C.ps[bk][:]: e.activation(out=o, in_=i_, func=AF.Copy)), reads=[C.ps_b[bk]], writes=[C.kio_b])
    dd, ddb = dr["kiT"]
    P.dma("sp", (lambda e, d_=dd[:, t0:t0 + NT]: e.dma_start(out=d_, in_=C.kio[:])), reads=[C.kio_b], writes=[ddb], lane="st2")
    vsb = osb[0].rearrange("p c t -> p (c t)").rearrange("p (b c) -> p b c", c=1024)
    for seg in (O_ZV, O_V):
        for c2 in range(4):
            w, wb = C.wget(wi); wi += 1
            for tb in range(4):
                bk = nb()
                for k in range(DC):
                    mm(P, C.ps[bk][:, 0:256], hT[:, k, tb * 128:(tb + 1) * 128], w[:, k, :], k == 0, k == DC - 1, [wb, hTb], [C.ps_b[bk]])
                if seg == O_ZV:
                    P.op("act", (lambda e, o=zv[:, tb, c2 * 256:(c2 + 1) * 256], i_=C.ps[bk][:, 0:256]: e.activation(out=o, in_=i_, func=AF.Copy)),
                         reads=[C.ps_b[bk]], writes=[zvb])
                else:
                    P.op("act", (lambda e, o=vsb[:, tb, c2 * 256:(c2 + 1) * 256], i_=C.ps[bk][:, 0:256]: e.activation(out=o, in_=i_, func=AF.Copy)),
                         reads=[C.ps_b[bk]], writes=[osbb[0]])
    dd, ddb = dr["v"]
    P.dma("sp", (lambda e, d_=dd[t0:t0 + NT, :].rearrange("(b p) c -> p b c", p=128): e.dma_start(out=d_, in_=vsb)),
          reads=[osbb[0]], writes=[ddb], lane="st1")
    w, wb = C.wget(wi); wi += 1
    bk = nb()
    for tb in range(4):
        for k in range(DC):
            mm(P, C.ps[bk][:, tb * 16:(tb + 1) * 16], hT[:, k, tb * 128:(tb + 1) * 128], w[:, k, :], k == 0, k == DC - 1, [wb, hTb], [C.ps_b[bk]])
    P.op("act", (lambda e, i_=C.ps[bk][:, 0:64]: e.activation(out=C.wio[:], in_=i_, func=AF.Copy)), reads=[C.ps_b[bk]], writes=[C.wio_b])
    dd, ddb = dr["widx"]
    P.dma("sp", (lambda e, d_=dd[t0:t0 + NT, :].rearrange("(b p) c -> p b c", p=128): e.dma_start(out=d_, in_=C.wio[:].rearrange("p (b c) -> p b c", c=16))),
          reads=[C.wio_b], writes=[ddb], lane="st2")
    lng = C.bro[:, 0:1024]
    lnb = C.bro[:, 1024:2048]
    for tb in range(4):
        for h2 in range(2):
            emit_gelu(C, zv[:, tb, h2 * 512:(h2 + 1) * 512], zv[:, tb, h2 * 512:(h2 + 1) * 512], zvb, zvb, 512)
        P.op("dve", (lambda e, i_=zv[:, tb, :]: e.bn_stats(out=C.bst[:, 0:6], in_=i_[:, 0:512])), reads=[zvb], writes=[C.bst_b])
        P.op("dve", (lambda e, i_=zv[:, tb, :]: e.bn_stats(out=C.bst[:, 6:12], in_=i_[:, 512:1024])), reads=[zvb], writes=[C.bst_b])
        P.op("dve", (lambda e: e.bn_aggr(out=C.bag[:], in_=C.bst[:].rearrange("p (a b) -> p a b", b=6))), reads=[C.bst_b], writes=[C.bag_b])
        P.op("act", (lambda e: e.activation(out=C.lrs[:], in_=C.bag[:, 1:2], func=AF.Sqrt, bias=C.lneps[:], scale=1.0)),
             reads=[C.bag_b, C.lneps_b], writes=[C.lrs_b])
        P.op("dve", (lambda e: e.reciprocal(out=C.lrs[:], in_=C.lrs[:])), reads=[C.lrs_b], writes=[C.lrs_b])
        P.op("dve", (lambda e, o=zv[:, tb, :]: e.tensor_scalar(out=o, in0=o, scalar1=C.bag[:, 0:1], scalar2=C.lrs[:], op0=ALU.subtract, op1=ALU.mult)),
             reads=[zvb, C.bag_b, C.lrs_b], writes=[zvb])
        P.op("dve", (lambda e, o=zv[:, tb, :]: e.tensor_tensor(out=o, in0=o, in1=lng, op=ALU.mult)), reads=[zvb, C.bro_b], writes=[zvb])
        P.op("dve", (lambda e, o=vln[:, tb, :], i_=zv[:, tb, :]: e.tensor_tensor(out=o, in0=i_, in1=lnb, op=ALU.add)), reads=[zvb, C.bro_b], writes=[vlnb])
    for g in range(8):
        bk = nb()
        for tb in range(4):
            mm(P, C.ps[bk][:, tb * 128:(tb + 1) * 128], vln[:, tb, g * 128:(g + 1) * 128], C.wsb[:, g, :], True, True, [vlnb, C.wsb_b], [C.ps_b[bk]])
        for tb in range(4):
            P.op("dve", (lambda e, o=C.gt1[:, tb * 128:(tb + 1) * 128], i_=C.ps[bk][:, tb * 128:(tb + 1) * 128], b_=C.bro[:, 2048 + g * 128:2048 + (g + 1) * 128]:
                         e.tensor_tensor(out=o, in0=i_, in1=b_, op=ALU.add)),
                 reads=[C.ps_b[bk], C.bro_b], writes=[C.gt1_b])
        P.op("dve", (lambda e, o=yaT[:, g, :], g_=guT[:, g, :]: e.tensor_tensor(out=o, in0=C.gt1[:], in1=g_, op=ALU.mult)),
             reads=[C.gt1_b, guTb], writes=[yaTb])
    if "dbg_ya" in dr:
        P.dma("sp", (lambda e, d_=dr["dbg_ya"][0][:, t0:t0 + NT].rearrange("(c p) t -> p c t", p=128): e.dma_start(out=d_, in_=yaT)), reads=[yaTb], writes=[dr["dbg_ya"][1]], lane="dbg")
        P.dma("sp", (lambda e, d_=dr["dbg_gu"][0][:, t0:t0 + NT].rearrange("(c p) t -> p c t", p=128): e.dma_start(out=d_, in_=guT)), reads=[guTb], writes=[dr["dbg_gu"][1]], lane="dbg")
        P.dma("sp", (lambda e, d_=dr["dbg_vln"][0][t0:t0 + NT, :].rearrange("(b p) c -> p b c", p=128): e.dma_start(out=d_, in_=vln)), reads=[vlnb], writes=[dr["dbg_vln"][1]], lane="dbg")
    ocnt = [0]
    for c2 in range(8):
        wa, wab = C.wget(wi); wi += 1
        wg, wgb = C.wget(wi); wi += 1
        for cc in range(2):
            ch = 2 * c2 + cc
            bka = nb()
            for k in range(8):
                mm(P, C.ps[bka][:], wa[:, k, cc * 128:(cc + 1) * 128], yaT[:, k, :], k == 0, k == 7, [wab, yaTb], [C.ps_b[bka]])
            bkg = nb()
            for k in range(DC):
                mm(P, C.ps[bkg][:], wg[:, k, cc * 128:(cc + 1) * 128], hT[:, k, :], k == 0, k == DC - 1, [wgb, hTb], [C.ps_b[bkg]])
            k2 = C.cnt % 2
            C.cnt += 1
            P.op("act", (lambda e, o=C.tmpa[k2][:], i_=C.ps[bkg][:]: e.activation(out=o, in_=i_, func=AF.Sigmoid)),
                 reads=[C.ps_b[bkg]], writes=[C.tmpa_b[k2]])
            oi = ocnt[0] % 8
            ocnt[0] += 1
            P.op("dve", (lambda e, o=och[oi], a=C.tmpa[k2][:], b=C.ps[bka][:]: e.tensor_tensor(out=o, in0=a, in1=b, op=ALU.mult)),
                 reads=[C.tmpa_b[k2], C.ps_b[bka]], writes=[ochb[oi]])
            dd, ddb = dr["apart"]
            P.dma("sp", (lambda e, d_=dd[ch * 128:(ch + 1) * 128, t0:t0 + NT], s_=och[oi]: e.dma_start(out=d_, in_=s_)),
                  reads=[ochb[oi]], writes=[ddb], lane=f"so{oi}")
    for c2 in range(8):
        wg, wgb = C.wget(wi); wi += 1
        for cc in range(2):
            ch = 2 * c2 + cc
            bkg = nb()
            for k in range(DC):
                mm(P, C.ps[bkg][:], wg[:, k, cc * 128:(cc + 1) * 128], hT[:, k, :], k == 0, k == DC - 1, [wgb, hTb], [C.ps_b[bkg]])
            oi = ocnt[0] % 8
            ocnt[0] += 1
            P.op("act", (lambda e, o=och[oi], i_=C.ps[bkg][:]: e.activation(out=o, in_=i_, func=AF.Sigmoid)),
                 reads=[C.ps_b[bkg]], writes=[ochb[oi]])
            dd, ddb = dr["gb"]
            P.dma("sp", (lambda e, d_=dd[ch * 128:(ch + 1) * 128, t0:t0 + NT], s_=och[oi]: e.dma_start(out=d_, in_=s_)),
                  reads=[ochb[oi]], writes=[ddb], lane=f"so{oi}")
    return wi


ATT_SCALE = 128 ** -0.5
MASK_NEG = -30000.0
TOPK = 256
BIS_IT = 28
BIS_MIN = 1024


def emit_attn_consts(C, toep_d, cb8_d):
    P = C.P
    tpf = C.R3[:, 0:2048].rearrange("p (h w) -> p h w", h=8)
    tb = Buf("tpf")
    C.claim("R3", [tb])
    P.dma("sp", lambda e: e.dma_start(out=C.cb8[:], in_=cb8_d), reads=[], writes=[C.cb8_b], lane="ldc")
    P.dma("sp", lambda e: e.dma_start(out=tpf, in_=toep_d.rearrange("p (h w) -> p h w", h=8)), reads=[], writes=[tb], lane="ldc")
    for h in range(8):
        P.op("dve", (lambda e, h=h: e.tensor_scalar(out=tpf[:, h, :], in0=tpf[:, h, :], scalar1=C.cb8[:, h:h + 1], scalar2=None, op0=ALU.subtract)),
             reads=[tb, C.cb8_b], writes=[tb])
    if not ATTN_PE:
        P.op("act", (lambda e: e.activation(out=C.Fe[:].rearrange("p (h w) -> p h w", h=8), in_=tpf, func=AF.Exp)), reads=[tb], writes=[C.Fe_b])
    P.op("dve", (lambda e: e.tensor_scalar(out=C.Fb[:].rearrange("p (h w) -> p h w", h=8), in0=tpf, scalar1=1.0 / ATT_SCALE, scalar2=None, op0=ALU.mult)),
         reads=[tb], writes=[C.Fb_b])
    P.op("pool", lambda e: e.memset(C.ident[:], 1.0), writes=[C.ident_b])
    P.op("pool", (lambda e: e.affine_select(out=C.ident[:], in_=C.ident[:], pattern=[[-1, 128]], compare_op=ALU.is_equal,
                                             fill=fillreg(e, 0.0), base=0, channel_multiplier=1)), reads=[C.ident_b], writes=[C.ident_b])
    P.op("pool", (lambda e: e.tensor_scalar(out=C.negI[:], in0=C.ident[:], scalar1=MASK_NEG, scalar2=1.0, op0=ALU.mult, op1=ALU.mult)),
         reads=[C.ident_b], writes=[C.negI_b])
    P.op("pool", lambda e: e.memset(C.thrneg[:], -1.0e29), writes=[C.thrneg_b])
    P.op("pool", lambda e: e.memset(C.onec[:], 1.0), writes=[C.onec_b])


def mixb_tail_plan(w_bb, w_o):
    specs = []
    for c2 in range(8):
        specs.append(w_panel(w_bb, 0, 8, [(c2 * 256, 256)]))
    specs += outproj_plan(w_o, DC)
    return specs


def emit_mixb_tile(C, wbase, tt, S, x1, x2, cs, cb, dr):
    P = C.P
    t0 = tt * NT
    kh = [C.R1[:, i * 2048:(i + 1) * 2048].bitcast(BF16) for i in range(2)]
    khb = [Buf("kh0"), Buf("kh1")]
    ybT = C.R1[:, 4096:6144].bitcast(BF16).rearrange("p (c t) -> p c t", t=NT)
    ybTb = Buf("ybT")
    maskT = C.R1[:, 6144:8192].bitcast(BF16)
    maskTb = Buf("maskT")
    vh = [C.R2[:, i * 2048:(i + 1) * 2048].bitcast(BF16).rearrange("p (b c) -> p b c", c=128) for i in range(2)]
    vhb = [Buf("vh0"), Buf("vh1")]
    score = C.R3[:, 0:4096]
    scb = Buf("score")
    work = C.R3[:, 4096:8192]
    wkb = Buf("work")
    mask = C.R3[:, 8192:10240].bitcast(BF16)
    mkb = Buf("mask")
    C.claim("R1", khb + [ybTb, maskTb]); C.claim("R2", vhb); C.claim("R3", [scb, wkb, mkb])
    rr = [0]

    def nb():
        b = rr[0] % 6
        rr[0] += 1
        return b
    OB, TB = 6, 7
    psT = C.ps[TB][:].bitcast(BF16)
    kvc = [0]
    for qi_ in range(4):
        qb = tt * 4 + qi_
        q0 = qb * 128
        nkb = qb + 1
        Sc = nkb * 128
        ngrp = (nkb + 3) // 4
        P.dma("sp", (lambda e, s_=dr["qiT"][0][:, q0:q0 + 128].rearrange("(c p) t -> p c t", p=128): e.dma_start(out=C.qib[:], in_=s_)),
              reads=[dr["qiT"][1]], writes=[C.qib_b], lane="lq0")
        P.dma("sp", (lambda e, s_=dr["qT"][0][:, q0:q0 + 128].rearrange("(c p) t -> p c t", p=128): e.dma_start(out=C.qbk[:], in_=s_)),
              reads=[dr["qT"][1]], writes=[C.qbk_b], lane="lq1")
        P.dma("sp", (lambda e, s_=dr["widx"][0][q0:q0 + 128, :]: e.dma_start(out=C.wx[:], in_=s_)),
              reads=[dr["widx"][1]], writes=[C.wx_b], lane="lq2")
        P.op("act", (lambda e: e.activation(out=C.wabs[:], in_=C.wx[:], func=AF.Abs)), reads=[C.wx_b], writes=[C.wabs_b])
        P.op("act", (lambda e: e.activation(out=C.wsg[:], in_=C.wx[:], func=AF.Sign)), reads=[C.wx_b], writes=[C.wsg_b])
        for grp in range(ngrp):
            c0 = grp * 512
            n = min(Sc, c0 + 512) - c0
            for h in range(16):
                c, half = h // 2, h % 2
                rows = slice(half * 64, half * 64 + 64)
                bk = nb()
                mm(P, C.ps[bk][:, 0:n], C.qib[rows, c, :], C.kis[rows, c0:c0 + n], True, True, [C.qib_b, C.kis_b], [C.ps_b[bk]])
                k2 = C.cnt % 2
                C.cnt += 1
                P.op("act", (lambda e, o=C.tmpa[k2][:, 0:n], i_=C.ps[bk][:, 0:n], h=h: e.activation(out=o, in_=i_, func=AF.Relu, scale=C.wabs[:, h:h + 1])),
                     reads=[C.ps_b[bk], C.wabs_b], writes=[C.tmpa_b[k2]])
                if h == 0:
                    P.op("dve", (lambda e, o=score[:, c0:c0 + n], i_=C.tmpa[k2][:, 0:n]: e.tensor_scalar(out=o, in0=i_, scalar1=C.wsg[:, 0:1], scalar2=None, op0=ALU.mult)),
                         reads=[C.tmpa_b[k2], C.wsg_b], writes=[scb])
                else:
                    P.op("dve", (lambda e, o=score[:, c0:c0 + n], i_=C.tmpa[k2][:, 0:n], h=h: e.scalar_tensor_tensor(out=o, in0=i_, scalar=C.wsg[:, h:h + 1], in1=o, op0=ALU.mult, op1=ALU.add)),
                         reads=[C.tmpa_b[k2], C.wsg_b, scb], writes=[scb])
        dsl = score[:, (nkb - 1) * 128:nkb * 128]
        P.op("pool", (lambda e, d_=dsl: e.affine_select(out=d_, in_=d_, pattern=[[-1, 128]], compare_op=ALU.is_ge, fill=fillreg(e, NEG), base=0, channel_multiplier=1)),
             reads=[scb], writes=[scb])
        if Sc > TOPK:
            for r in range(TOPK // 8):
                src = score[:, 0:Sc] if r == 0 else work[:, 0:Sc]
                srcb = scb if r == 0 else wkb
                P.op("dve", (lambda e, s_=src: e.max(out=C.m8[:], in_=s_)), reads=[srcb], writes=[C.m8_b])
                if r < TOPK // 8 - 1:
                    P.op("dve", (lambda e, s_=src, w_=work[:, 0:Sc]: e.match_replace(out=w_, in_to_replace=C.m8[:], in_values=s_, imm_value=NEG)),
                         reads=[srcb, C.m8_b], writes=[wkb])
            thr, thrb = C.m8[:, 7:8], C.m8_b
        else:
            thr, thrb = C.thrneg[:], C.thrneg_b
        P.op("dve", (lambda e, t_=thr, m_=mask[:, 0:Sc], s_=score[:, 0:Sc]: e.tensor_scalar(out=m_, in0=s_, scalar1=t_, scalar2=None, op0=ALU.is_ge)),
             reads=[scb, thrb], writes=[mkb])
        for grp in range(ngrp):
            kbs = list(range(grp * 4, min(nkb, grp * 4 + 4)))
            for j, kb in enumerate(kbs):
                P.op("pe", (lambda e, o=psT[:, j * 128:(j + 1) * 128], i_=mask[:, kb * 128:(kb + 1) * 128]: e.transpose(o, i_, C.ident[:])),
                     reads=[mkb, C.ident_b], writes=[C.ps_b[TB]])
            n = len(kbs) * 128
            P.op("act", (lambda e, o=maskT[:, grp * 512:grp * 512 + n], i_=psT[:, 0:n]: e.activation(out=o, in_=i_, func=AF.Copy)),
                 reads=[C.ps_b[TB]], writes=[maskTb])
        for h in range(8):
            s2 = kvc[0] % 2
            kvc[0] += 1
            P.dma("sp", (lambda e, o=kh[s2][:, 0:Sc], s_=dr["kT"][0][h * 128:(h + 1) * 128, 0:Sc]: e.dma_start(out=o, in_=s_)),
                  reads=[dr["kT"][1]], writes=[khb[s2]], lane=f"lk{s2}")
            P.dma("sp", (lambda e, o=vh[s2][:, 0:nkb, :], s_=dr["v"][0][0:Sc, h * 128:(h + 1) * 128].rearrange("(b p) c -> p b c", p=128): e.dma_start(out=o, in_=s_)),
                  reads=[dr["v"][1]], writes=[vhb[s2]], lane=f"lv{s2}")
            for grp in range(ngrp):
                kbs = list(range(grp * 4, min(nkb, grp * 4 + 4)))
                n = len(kbs) * 128
                bk = nb()
                for j, kb in enumerate(kbs):
                    mm(P, C.ps[bk][:, j * 128:(j + 1) * 128], kh[s2][:, kb * 128:(kb + 1) * 128], C.qbk[:, h, :], True, True, [khb[s2], C.qbk_b], [C.ps_b[bk]])
                k2 = C.cnt % 2
                C.cnt += 1
                P.op("act", (lambda e, o=C.tmpa[k2][:, 0:n], i_=C.ps[bk][:, 0:n], h=h: e.activation(out=o, in_=i_, func=AF.Exp, bias=C.cb8[:, h:h + 1], scale=ATT_SCALE)),
                     reads=[C.ps_b[bk], C.cb8_b], writes=[C.tmpa_b[k2]])
                P.op("dve", (lambda e, o=C.pm[k2][:, 0:n], a=C.tmpa[k2][:, 0:n], b=maskT[:, grp * 512:grp * 512 + n]: e.tensor_tensor(out=o, in0=a, in1=b, op=ALU.mult)),
                     reads=[C.tmpa_b[k2], maskTb], writes=[C.pm_b[k2]])
                for j, kb in enumerate(kbs):
                    w_ = nkb - 1 - kb
                    if w_ <= 1:
                        P.op("dve", (lambda e, o=C.pm[k2][:, j * 128:(j + 1) * 128], f_=C.Fb[:, (h * 2 + w_) * 128:(h * 2 + w_ + 1) * 128]: e.tensor_tensor(out=o, in0=o, in1=f_, op=ALU.mult)),
                             reads=[C.pm_b[k2], C.Fb_b], writes=[C.pm_b[k2]])
                for j, kb in enumerate(kbs):
                    P.op("pe", (lambda e, o=C.ps[OB][:, 0:128], l=C.pm[k2][:, j * 128:(j + 1) * 128], r=vh[s2][:, kb, :], st=(kb == 0), sp=(kb == nkb - 1):
                                e.matmul(o, l, r, start=st, stop=sp, skip_group_check=True)),
                         reads=[C.pm_b[k2], vhb[s2]], writes=[C.ps_b[OB]])
                    P.op("pe", (lambda e, o=C.ps[OB][:, 128:129], l=C.pm[k2][:, j * 128:(j + 1) * 128], sp=(kb == nkb - 1):
                                e.matmul(o, l, C.onec[:, 0:1], start=False, stop=sp, skip_group_check=True)),
                         reads=[C.pm_b[k2], C.onec_b], writes=[C.ps_b[OB]])
            P.op("dve", (lambda e: e.reciprocal(out=C.rc[:], in_=C.ps[OB][:, 128:129])), reads=[C.ps_b[OB]], writes=[C.rc_b])
            P.op("dve", (lambda e, o=C.yb[:, h * 128:(h + 1) * 128]: e.tensor_scalar(out=o, in0=C.ps[OB][:, 0:128], scalar1=C.rc[:], scalar2=None, op0=ALU.mult)),
                 reads=[C.ps_b[OB], C.rc_b], writes=[C.yb_b])
        for half in range(2):
            for j in range(4):
                c = half * 4 + j
                P.op("pe", (lambda e, o=psT[:, j * 128:(j + 1) * 128], i_=C.yb[:, c * 128:(c + 1) * 128]: e.transpose(o, i_, C.ident[:])),
                     reads=[C.yb_b, C.ident_b], writes=[C.ps_b[TB]])
            P.op("act", (lambda e, o=ybT[:, half * 4:half * 4 + 4, qi_ * 128:(qi_ + 1) * 128], i_=psT[:, 0:512].rearrange("p (c t) -> p c t", t=128):
                         e.activation(out=o, in_=i_, func=AF.Copy)),
                 reads=[C.ps_b[TB]], writes=[ybTb])
    mT = C.R2[:, :].bitcast(BF16).rearrange("p (c t) -> p c t", t=NT)
    mTb = Buf("mT")
    C.claim("R2", [mTb])
    wi = wbase
    lc = [0]
    for c2 in range(8):
        w, wb = C.wget(wi); wi += 1
        for cc in range(2):
            ch = 2 * c2 + cc
            bk = nb()
            for k in range(8):
                mm(P, C.ps[bk][:], w[:, k, cc * 128:(cc + 1) * 128], ybT[:, k, :], k == 0, k == 7, [wb, ybTb], [C.ps_b[bk]])
            k2 = lc[0] % 2
            lc[0] += 1
            P.dma("sp", (lambda e, o=C.gt1[:] if k2 == 0 else C.gt2[:], s_=dr["gb"][0][ch * 128:(ch + 1) * 128, t0:t0 + NT]: e.dma_start(out=o, in_=s_)),
                  reads=[dr["gb"][1]], writes=[C.gt1_b if k2 == 0 else C.gt2_b], lane=f"lg{k2}")
            P.dma("sp", (lambda e, o=C.tmpa[k2][:], s_=dr["apart"][0][ch * 128:(ch + 1) * 128, t0:t0 + NT]: e.dma_start(out=o, in_=s_)),
                  reads=[dr["apart"][1]], writes=[C.tmpa_b[k2]], lane=f"la{k2}")
            gbt, gbb = (C.gt1, C.gt1_b) if k2 == 0 else (C.gt2, C.gt2_b)
            P.op("dve", (lambda e, g_=gbt[:], i_=C.ps[bk][:]: e.tensor_tensor(out=g_, in0=g_, in1=i_, op=ALU.mult)),
                 reads=[gbb, C.ps_b[bk]], writes=[gbb])
            P.op("dve", (lambda e, o=mT[:, ch, :], g_=gbt[:], a=C.tmpa[k2][:]: e.tensor_tensor(out=o, in0=g_, in1=a, op=ALU.add)),
                 reads=[gbb, C.tmpa_b[k2]], writes=[mTb])
    fT = C.R1[:, :].rearrange("p (c t) -> p c t", t=NT)
    fTb = Buf("fT")
    C.claim("R1", [fTb])
    r2b = Buf("r2x")
    wi = emit_outproj_claim(C, wi, mT, mTb, x1, x2, t0, cs[:, 48:64], 1.0, cb, fT, fTb, r2b)
    return wi


def emit_outproj_claim(C, wi, mT, mTb, x1, x2, t0, gpost, alpha, cb, fT, fTb, r2b):
    class _Lazy:
        pass
    P = C.P
    orig_dma = P.dma
    state = {"claimed": False}

    def dma_hook(eng, fn, reads, writes, lane):
        if (not state["claimed"]) and r2b in writes:
            C.claim("R2", [r2b])
            state["claimed"] = True
        return orig_dma(eng, fn, reads, writes, lane)
    P.dma = dma_hook
    try:
        wi = emit_outproj(C, wi, mT, lambda f: mTb, DC, x1, x2, t0, gpost, alpha, cb, fT, fTb, r2b)
    finally:
        P.dma = orig_dma
    return wi


def _interleave(ga, gb_):
    la, lb = list(ga), None
    return la


def run_interleaved(gens_a, gens_b):
    na, nb_ = len(gens_a), len(gens_b)
    ia = ib = 0
    while ia < na or ib < nb_:
        fa = ia / na if na else 1.0
        fb = ib / nb_ if nb_ else 1.0
        if ia < na and (fa <= fb or ib >= nb_):
            gens_a[ia](); ia += 1
        else:
            gens_b[ib](); ib += 1


def emit_mixb_layer(C, wbase, NTL, S, x1, x2, cs, cb, dr):
    P = C.P
    NBLK = NTL * 4
    score = [C.R3[:, 0:4096], C.R4[:, 0:4096]]
    scb = [Buf("score0"), Buf("score1")]
    work = C.R3[:, 4096:8192]
    wkb = Buf("work")
    mask = C.R3[:, 8192:10240].bitcast(BF16)
    mkb = Buf("mask")
    C.claim("R3", [scb[0], wkb, mkb])
    C.claim("R4", [scb[1]])
    maskT = [C.mT0[:], C.mT1[:]]
    maskTb = [C.mT0_b, C.mT1_b]
    qib = [C.qib, C.qib2]; qibb = [C.qib_b, C.qib2_b]
    qbk = [C.qbk, C.qbk2]; qbkb = [C.qbk_b, C.qbk2_b]
    wabs = [C.wabs, C.wabs2]; wabsb = [C.wabs_b, C.wabs2_b]
    wsg = [C.wsg, C.wsg2]; wsgb = [C.wsg_b, C.wsg2_b]
    wx = [C.wx, C.wx2]; wxb = [C.wx_b, C.wx2_b]
    rr = [0]

    def nb():
        b = rr[0] % 4
        rr[0] += 1
        return b
    OBS, TB, SCB = (4, 6), 7, 5
    rcs = [C.rc, C.rc2]; rcsb = [C.rc_b, C.rc2_b]
    pmc = [0]
    hdc = [0]
    dg = [C.dg0, C.dg1]; dgb = [C.dg0_b, C.dg1_b]
    rlc = [0]
    psT = C.ps[TB][:].bitcast(BF16)
    kvc = [0]
    st = {}

    def phase_ab(qb):
        th = []
        p = qb % 2
        q0 = qb * 128
        nkb = qb + 1
        Sc = nkb * 128
        ngrp = (nkb + 3) // 4
        sc, scbp = score[p], scb[p]

        def loads():
            P.dma("sp", (lambda e, s_=dr["qiT"][0][:, q0:q0 + 128].rearrange("(c p) t -> p c t", p=128), o=qib[p][:]: e.dma_start(out=o, in_=s_)),
                  reads=[dr["qiT"][1]], writes=[qibb[p]], lane=f"lq0{p}")
            P.dma("sp", (lambda e, s_=dr["qT"][0][:, q0:q0 + 128].rearrange("(c p) t -> p c t", p=128), o=qbk[p][:]: e.dma_start(out=o, in_=s_)),
                  reads=[dr["qT"][1]], writes=[qbkb[p]], lane=f"lq1{p}")
            P.dma("sp", (lambda e, s_=dr["widx"][0][q0:q0 + 128, :], o=wx[p][:]: e.dma_start(out=o, in_=s_)),
                  reads=[dr["widx"][1]], writes=[wxb[p]], lane=f"lq2{p}")
            P.op("act", (lambda e, o=wabs[p][:], i_=wx[p][:]: e.activation(out=o, in_=i_, func=AF.Abs)), reads=[wxb[p]], writes=[wabsb[p]])
            P.op("act", (lambda e, o=wsg[p][:], i_=wx[p][:]: e.activation(out=o, in_=i_, func=AF.Sign)), reads=[wxb[p]], writes=[wsgb[p]])
            for h in range(16):
                P.op("pool", (lambda e, o=dg[p][:, h, :], s_=wsg[p][:, h:h + 1]: e.tensor_scalar(out=o, in0=C.ident[:], scalar1=s_, scalar2=1.0, op0=ALU.mult, op1=ALU.mult)),
                     reads=[C.ident_b, wsgb[p]], writes=[dgb[p]])
        th.append(loads)
        for grp in range(ngrp):
            c0 = grp * 512
            n = min(Sc, c0 + 512) - c0
            gs = {}

            def mk_dots(h, grp=grp, c0=c0, n=n, gs=gs):
                def dots():
                    c, half = h // 2, h % 2
                    rows = slice(half * 64, half * 64 + 64)
                    bk = nb()
                    mm(P, C.ps[bk][:, 0:n], qib[p][rows, c, :], C.kis[rows, c0:c0 + n], True, True, [qibb[p], C.kis_b], [C.ps_b[bk]])
                    k2 = rlc[0] % 4
                    rlc[0] += 1
                    gs[h] = k2
                    P.op("act", (lambda e, o=C.rl16[k2][:, 0:n], i_=C.ps[bk][:, 0:n], s_=wabs[p][:, h:h + 1]: e.activation(out=o, in_=i_, func=AF.Relu, scale=s_)),
                         reads=[C.ps_b[bk], wabsb[p]], writes=[C.rl16_b[k2]])
                return dots

            def mk_hsum(h, grp=grp, c0=c0, n=n, gs=gs):
                def hsum():
                    k2 = gs[h]
                    mm(P, C.ps[SCB][:, 0:n], dg[p][:, h, :], C.rl16[k2][:, 0:n], h == 0, h == 15, [dgb[p], C.rl16_b[k2]], [C.ps_b[SCB]])
                    if h == 15:
                        P.op("act", (lambda e, o=sc[:, c0:c0 + n], i_=C.ps[SCB][:, 0:n]: e.activation(out=o, in_=i_, func=AF.Copy)),
                             reads=[C.ps_b[SCB]], writes=[scbp])
                return hsum
            th.append(mk_dots(0))
            for h in range(16):
                if h + 1 < 16:
                    th.append(mk_dots(h + 1))
                th.append(mk_hsum(h))

        if Sc >= BIS_MIN:
            def binit():
                P.op("dve", (lambda e, s_=sc[:, 0:Sc]: e.max(out=C.m8[:], in_=s_)), reads=[scbp], writes=[C.m8_b])
                P.op("dve", (lambda e, s_=sc[:, 0:Sc]: e.tensor_reduce(out=C.blo[:], in_=s_, axis=mybir.AxisListType.X, op=ALU.min)), reads=[scbp], writes=[C.blo_b])
                P.op("dve", (lambda e: e.tensor_tensor(out=C.brng[:], in0=C.m8[:, 0:1], in1=C.blo[:], op=ALU.subtract)), reads=[C.m8_b, C.blo_b], writes=[C.brng_b])
                P.op("dve", (lambda e: e.tensor_scalar(out=C.bstep[:], in0=C.pw2[:], scalar1=C.brng[:], scalar2=None, op0=ALU.mult)), reads=[C.pw2_b, C.brng_b], writes=[C.bstep_b])
            th.append(binit)

        def causal():
            dsl = sc[:, (nkb - 1) * 128:nkb * 128]
            P.op("pool", (lambda e, d_=dsl: e.affine_select(out=d_, in_=d_, pattern=[[-1, 128]], compare_op=ALU.is_ge, fill=fillreg(e, NEG), base=0, channel_multiplier=1)),
                 reads=[scbp], writes=[scbp])
        th.append(causal)
        use_bis = Sc >= BIS_MIN
        if use_bis:
            def bis_init():
                pass
            for k in range(BIS_IT):
                def it(k=k):
                    P.op("dve", (lambda e: e.tensor_tensor(out=C.bmid[:], in0=C.blo[:], in1=C.bstep[:, k:k + 1], op=ALU.add)),
                         reads=[C.blo_b, C.bstep_b], writes=[C.bmid_b])
                    P.op("dve", (lambda e, w_=work[:, 0:Sc], s_=sc[:, 0:Sc]: e.tensor_scalar(out=w_, in0=s_, scalar1=C.bmid[:], scalar2=None, op0=ALU.is_ge, op1=ALU.add, accum_out=C.bcnt[:])),
                         reads=[scbp, C.bmid_b], writes=[wkb, C.bcnt_b])
                    P.op("dve", (lambda e: e.tensor_scalar(out=C.bfs[:], in0=C.bcnt[:], scalar1=float(TOPK) - 0.5, scalar2=C.bstep[:, k:k + 1], op0=ALU.is_ge, op1=ALU.mult)),
                         reads=[C.bcnt_b, C.bstep_b], writes=[C.bfs_b])
                    P.op("dve", (lambda e: e.tensor_tensor(out=C.blo[:], in0=C.blo[:], in1=C.bfs[:], op=ALU.add)),
                         reads=[C.blo_b, C.bfs_b], writes=[C.blo_b])
                th.append(it)
        elif Sc > TOPK:
            for r in range(TOPK // 8):
                def rnd(r=r):
                    src = sc[:, 0:Sc] if r == 0 else work[:, 0:Sc]
                    srcb = scbp if r == 0 else wkb
                    P.op("dve", (lambda e, s_=src: e.max(out=C.m8[:], in_=s_)), reads=[srcb], writes=[C.m8_b])
                    if r < TOPK // 8 - 1:
                        P.op("dve", (lambda e, s_=src, w_=work[:, 0:Sc]: e.match_replace(out=w_, in_to_replace=C.m8[:], in_values=s_, imm_value=NEG)),
                             reads=[srcb, C.m8_b], writes=[wkb])
                th.append(rnd)

        def mk():
            if Sc >= BIS_MIN:
                thr, thrb = C.blo[:], C.blo_b
            elif Sc > TOPK:
                thr, thrb = C.m8[:, 7:8], C.m8_b
            else:
                thr, thrb = C.thrneg[:], C.thrneg_b
            P.op("dve", (lambda e, t_=thr, m_=mask[:, 0:Sc], s_=sc[:, 0:Sc]: e.tensor_scalar(out=m_, in0=s_, scalar1=t_, scalar2=None, op0=(ALU.is_lt if ATTN_PE else ALU.is_ge))),
                 reads=[scbp, thrb], writes=[mkb])
        th.append(mk)
        for grp in range(ngrp):
            def tr(grp=grp):
                kbs = list(range(grp * 4, min(nkb, grp * 4 + 4)))
                for j, kb in enumerate(kbs):
                    P.op("pe", (lambda e, o=psT[:, j * 128:(j + 1) * 128], i_=mask[:, kb * 128:(kb + 1) * 128]: e.transpose(o, i_, C.ident[:])),
                         reads=[mkb, C.ident_b], writes=[C.ps_b[TB]])
                n = len(kbs) * 128
                P.op("act", (lambda e, o=maskT[p][:, grp * 512:grp * 512 + n], i_=psT[:, 0:n]: e.activation(out=o, in_=i_, func=AF.Copy)),
                     reads=[C.ps_b[TB]], writes=[maskTb[p]])
            th.append(tr)
        return th

    def phase_c(qb):
        th = []
        p = qb % 2
        qi_ = qb % 4
        nkb = qb + 1
        Sc = nkb * 128
        ngrp = (nkb + 3) // 4
        kh, khb, vh, vhb, ybT, ybTb = st["kh"], st["khb"], st["vh"], st["vhb"], st["ybT"], st["ybTb"]
        pms, pmsb = st["pms"], st["pmsb"]
        for h in range(8):
            hs = {}

            def ld(h=h, hs=hs):
                s2 = kvc[0] % 2
                kvc[0] += 1
                hs["s2"] = s2
                hs["ob"] = OBS[hdc[0] % 2]
                hs["rc"] = hdc[0] % 2
                hdc[0] += 1
                P.dma("sp", (lambda e, o=kh[s2][:, 0:Sc], s_=dr["kT"][0][h * 128:(h + 1) * 128, 0:Sc]: e.dma_start(out=o, in_=s_)),
                      reads=[dr["kT"][1]], writes=[khb[s2]], lane=f"lk{s2}")
                P.dma("sp", (lambda e, o=vh[s2][:, 0:nkb, :], s_=dr["v"][0][0:Sc, h * 128:(h + 1) * 128].rearrange("(b p) c -> p b c", p=128): e.dma_start(out=o, in_=s_)),
                      reads=[dr["v"][1]], writes=[vhb[s2]], lane=f"lv{s2}")
            th.append(ld)
            def mk_qk(grp, h=h, hs=hs):
                def qk():
                    s2 = hs["s2"]
                    kbs = list(range(grp * 4, min(nkb, grp * 4 + 4)))
                    n = len(kbs) * 128
                    if ATTN_PE:
                        bk = nb()
                        for j, kb in enumerate(kbs):
                            w_ = nkb - 1 - kb
                            near = w_ <= 1
                            o_ = C.ps[bk][:, j * 128:(j + 1) * 128]
                            P.op("pe", (lambda e, o=o_, l=kh[s2][:, kb * 128:(kb + 1) * 128], r=qbk[p][:, h, :]: e.matmul(o, l, r, start=True, stop=False, skip_group_check=True)),
                                 reads=[khb[s2], qbkb[p]], writes=[C.ps_b[bk]])
                            P.op("pe", (lambda e, o=o_, r=maskT[p][:, kb * 128:(kb + 1) * 128], sp=(not near): e.matmul(o, C.negI[:], r, start=False, stop=sp, skip_group_check=True)),
                                 reads=[C.negI_b, maskTb[p]], writes=[C.ps_b[bk]])
                            if near:
                                P.op("pe", (lambda e, o=o_, r=C.Fb[:, (h * 2 + w_) * 128:(h * 2 + w_ + 1) * 128]: e.matmul(o, C.ident[:], r, start=False, stop=True, skip_group_check=True)),
                                     reads=[C.ident_b, C.Fb_b], writes=[C.ps_b[bk]])
                        k2 = pmc[0] % 4
                        pmc[0] += 1
                        P.op("act", (lambda e, o=pms[k2][:, 0:n], i_=C.ps[bk][:, 0:n]: e.activation(out=o, in_=i_, func=AF.Exp, bias=C.cb8[:, h:h + 1], scale=ATT_SCALE)),
                             reads=[C.ps_b[bk], C.cb8_b], writes=[pmsb[k2]])
                    else:
                        bk = nb()
                        for j, kb in enumerate(kbs):
                            mm(P, C.ps[bk][:, j * 128:(j + 1) * 128], kh[s2][:, kb * 128:(kb + 1) * 128], qbk[p][:, h, :], True, True, [khb[s2], qbkb[p]], [C.ps_b[bk]])
                        k3 = C.cnt % 2
                        C.cnt += 1
                        P.op("act", (lambda e, o=C.tmpa[k3][:, 0:n], i_=C.ps[bk][:, 0:n]: e.activation(out=o, in_=i_, func=AF.Exp, bias=C.cb8[:, h:h + 1], scale=ATT_SCALE)),
                             reads=[C.ps_b[bk], C.cb8_b], writes=[C.tmpa_b[k3]])
                        k2 = pmc[0] % 4
                        pmc[0] += 1
                        P.op("dve", (lambda e, o=pms[k2][:, 0:n], a=C.tmpa[k3][:, 0:n], b=maskT[p][:, grp * 512:grp * 512 + n]: e.tensor_tensor(out=o, in0=a, in1=b, op=ALU.mult)),
                             reads=[C.tmpa_b[k3], maskTb[p]], writes=[pmsb[k2]])
                        for j, kb in enumerate(kbs):
                            w_ = nkb - 1 - kb
                            if w_ <= 1:
                                P.op("dve", (lambda e, o=pms[k2][:, j * 128:(j + 1) * 128], f_=C.Fe[:, (h * 2 + w_) * 128:(h * 2 + w_ + 1) * 128]: e.tensor_tensor(out=o, in0=o, in1=f_, op=ALU.mult)),
                                     reads=[pmsb[k2], C.Fe_b], writes=[pmsb[k2]])
                    hs[("k2", grp)] = k2
                return qk

            def mk_pv(grp, h=h, hs=hs):
                def pv():
                    s2, OB = hs["s2"], hs["ob"]
                    k2 = hs[("k2", grp)]
                    kbs = list(range(grp * 4, min(nkb, grp * 4 + 4)))
                    for j, kb in enumerate(kbs):
                        P.op("pe", (lambda e, o=C.ps[OB][:, 0:128], l=pms[k2][:, j * 128:(j + 1) * 128], r=vh[s2][:, kb, :], st_=(kb == 0), sp=(kb == nkb - 1):
                                    e.matmul(o, l, r, start=st_, stop=sp, skip_group_check=True)),
                             reads=[pmsb[k2], vhb[s2]], writes=[C.ps_b[OB]])
                        P.op("pe", (lambda e, o=C.ps[OB][:, 128:129], l=pms[k2][:, j * 128:(j + 1) * 128], sp=(kb == nkb - 1):
                                    e.matmul(o, l, C.onec[:, 0:1], start=False, stop=sp, skip_group_check=True)),
                             reads=[pmsb[k2], C.onec_b], writes=[C.ps_b[OB]])

                return pv
            th.append(mk_qk(0))
            for grp in range(ngrp):
                if grp + 1 < ngrp:
                    th.append(mk_qk(grp + 1))
                th.append(mk_pv(grp))

            def fin(h=h, hs=hs):
                OB, ri = hs["ob"], hs["rc"]
                P.op("dve", (lambda e, o=rcs[ri][:], i_=C.ps[OB][:, 128:129]: e.reciprocal(out=o, in_=i_)), reads=[C.ps_b[OB]], writes=[rcsb[ri]])
                P.op("dve", (lambda e, o=C.yb[:, h * 128:(h + 1) * 128], i_=C.ps[OB][:, 0:128], s_=rcs[ri][:]: e.tensor_scalar(out=o, in0=i_, scalar1=s_, scalar2=None, op0=ALU.mult)),
                     reads=[C.ps_b[OB], rcsb[ri]], writes=[C.yb_b])
            th.append(fin)
        for half in range(2):
            def ytr(half=half):
                for j in range(4):
                    c = half * 4 + j
                    P.op("pe", (lambda e, o=psT[:, j * 128:(j + 1) * 128], i_=C.yb[:, c * 128:(c + 1) * 128]: e.transpose(o, i_, C.ident[:])),
                         reads=[C.yb_b, C.ident_b], writes=[C.ps_b[TB]])
                P.op("act", (lambda e, o=ybT[:, half * 4:half * 4 + 4, qi_ * 128:(qi_ + 1) * 128], i_=psT[:, 0:512].rearrange("p (c t) -> p c t", t=128):
                             e.activation(out=o, in_=i_, func=AF.Copy)),
                     reads=[C.ps_b[TB]], writes=[ybTb])
            th.append(ytr)
        return th

    def open_tile():
        kh = [C.R1[:, i * 2048:(i + 1) * 2048].bitcast(BF16) for i in range(2)]
        khb = [Buf("kh0"), Buf("kh1")]
        ybT = C.R1[:, 4096:6144].bitcast(BF16).rearrange("p (c t) -> p c t", t=NT)
        ybTb = Buf("ybT")
        vh = [C.R2[:, i * 2048:(i + 1) * 2048].bitcast(BF16).rearrange("p (b c) -> p b c", c=128) for i in range(2)]
        vhb = [Buf("vh0"), Buf("vh1")]
        pms = [C.R1[:, 6144 + i * 256:6144 + (i + 1) * 256].bitcast(BF16) for i in range(4)]
        pmsb = [Buf(f"pm{i}") for i in range(4)]
        C.claim("R1", khb + [ybTb] + pmsb); C.claim("R2", vhb)
        st.update(kh=kh, khb=khb, ybT=ybT, ybTb=ybTb, vh=vh, vhb=vhb, pms=pms, pmsb=pmsb)

    def tail(tt, wi):
        t0 = tt * NT
        ybT, ybTb = st["ybT"], st["ybTb"]
        mT = C.R2[:, :].bitcast(BF16).rearrange("p (c t) -> p c t", t=NT)
        mTb = Buf("mT")
        C.claim("R2", [mTb])
        lc = [0]
        for c2 in range(8):
            w, wb = C.wget(wi); wi += 1
            for cc in range(2):
                ch = 2 * c2 + cc
                bk = nb()
                for k in range(8):
                    mm(P, C.ps[bk][:], w[:, k, cc * 128:(cc + 1) * 128], ybT[:, k, :], k == 0, k == 7, [wb, ybTb], [C.ps_b[bk]])
                k2 = lc[0] % 2
                lc[0] += 1
                gbt, gbb = (C.gt1, C.gt1_b) if k2 == 0 else (C.gt2, C.gt2_b)
                P.dma("sp", (lambda e, o=gbt[:], s_=dr["gb"][0][ch * 128:(ch + 1) * 128, t0:t0 + NT]: e.dma_start(out=o, in_=s_)),
                      reads=[dr["gb"][1]], writes=[gbb], lane=f"lg{k2}")
                P.dma("sp", (lambda e, o=C.ta2[k2][:], s_=dr["apart"][0][ch * 128:(ch + 1) * 128, t0:t0 + NT]: e.dma_start(out=o, in_=s_)),
                      reads=[dr["apart"][1]], writes=[C.ta2_b[k2]], lane=f"la{k2}")
                P.op("dve", (lambda e, g_=gbt[:], i_=C.ps[bk][:]: e.tensor_tensor(out=g_, in0=g_, in1=i_, op=ALU.mult)),
                     reads=[gbb, C.ps_b[bk]], writes=[gbb])
                P.op("dve", (lambda e, o=mT[:, ch, :], g_=gbt[:], a=C.ta2[k2][:]: e.tensor_tensor(out=o, in0=g_, in1=a, op=ALU.add)),
                     reads=[gbb, C.ta2_b[k2]], writes=[mTb])
        fT = C.R1[:, :].rearrange("p (c t) -> p c t", t=NT)
        fTb = Buf("fT")
        C.claim("R1", [fTb])
        r2b = Buf("r2x")
        wi = emit_outproj_claim(C, wi, mT, mTb, x1, x2, t0, cs[:, 48:64], 1.0, cb, fT, fTb, r2b)
        return wi

    wi = wbase
    for f in phase_ab(0):
        f()
    for qb in range(NBLK):
        if qb % 4 == 0:
            open_tile()
        ca = phase_ab(qb + 1) if qb + 1 < NBLK else []
        cc_ = phase_c(qb)
        run_interleaved(ca, cc_)
        if qb % 4 == 3:
            wi = tail(qb // 4, wi)
    return wi


L_ = 2
NUM_BUCKETS = 32
MAX_DISTANCE = 128
IN_COLS = 6224


def build_program(S, depth):
    nc = bass.Bass("TRN2", target_bir_lowering=False)
    NTL = S // NT

    def din(name, shape, dt=F32):
        return nc.dram_tensor(name, list(shape), dt, kind="ExternalInput").ap()

    def dscr(name, shape, dt=F32):
        return nc.dram_tensor(name, list(shape), dt, kind="Internal").ap()
    x = din("x", [D, S])
    y = nc.dram_tensor("y", [D, S], F32, kind="ExternalOutput").ap()
    W = {}
    for l in range(depth):
        W[l] = dict(
            f1i=din(f"f1i{l}", [D, 2 * DFF]), f1o=din(f"f1o{l}", [DFF, D]),
            f2i=din(f"f2i{l}", [D, 2 * DFF]), f2o=din(f"f2o{l}", [DFF, D]),
            win=din(f"win{l}", [D, IN_COLS]), wg=din(f"wg{l}", [D, 2 * D]),
            wba=din(f"wba{l}", [AW, D]), wbb=din(f"wbb{l}", [AW, D]), wo=din(f"wo{l}", [D, D]),
            cpk=din(f"cpk{l}", [128, 96]), bro=din(f"bro{l}", [128, 3072]), wsT=din(f"wsT{l}", [128, 8, 128]),
        )
    toep = din("toep", [128, 2048])
    cb8d = din("cb8", [128, 8])
    xa = (dscr("xa", [D, S]), Buf("xa"))
    xb = (dscr("xb", [D, S]), Buf("xb"))
    xc = (dscr("xc", [D, S]), Buf("xc"))
    dr = {
        "qT": (dscr("qT", [AW, S], BF16), Buf("qT")), "qiT": (dscr("qiT", [AW, S], BF16), Buf("qiT")),
        "kT": (dscr("kT", [AW, S], BF16), Buf("kT")), "v": (dscr("v", [S, AW], BF16), Buf("v")),
        "kiT": (dscr("kiT", [128, S], BF16), Buf("kiT")), "widx": (dscr("widx", [S, 16]), Buf("widx")),
        "apart": (dscr("apart", [D, S]), Buf("apart")), "gb": (dscr("gb", [D, S]), Buf("gb")),
    }
    P = Prog(nc)
    C = Ctx(P)
    cs = [P.sbuf(f"consts{l}", [128, 96], F32) for l in range(depth)]
    cb = [Buf(f"consts{l}") for l in range(depth)]
    for l in range(depth):
        P.dma("sp", (lambda e, l=l: e.dma_start(out=cs[l][:], in_=W[l]["cpk"])), reads=[], writes=[cb[l]], lane="ldc")
    emit_attn_consts(C, toep, cb8d)
    for l in range(depth):
        w = W[l]
        for t in range(NTL):
            C.plan_extend(ffn_plan(w["f1i"], w["f1o"]))
        for t in range(NTL):
            C.plan_extend(mixa_plan(w["win"], w["wg"], w["wba"]))
        for t in range(NTL):
            C.plan_extend(mixb_tail_plan(w["wbb"], w["wo"]))
        for t in range(NTL):
            C.plan_extend(ffn_plan(w["f2i"], w["f2o"]))
    wi = 0
    xin = (x, Buf("x"))
    for l in range(depth):
        w = W[l]
        xout = (y, Buf("y")) if l == depth - 1 else xc
        for t in range(NTL):
            wi = emit_ffn(C, wi, xin, xa, t * NT, cs[l][:, 0:16], cs[l][:, 16:32], cb[l])
        emit_mix_consts(C, w["bro"], w["wsT"], None)
        for t in range(NTL):
            wi = emit_mixa(C, wi, xa, t * NT, S, cs[l], cb[l], dr)
        P.dma("sp", (lambda e: e.dma_start(out=C.kis[:, 0:S], in_=dr["kiT"][0])), reads=[dr["kiT"][1]], writes=[C.kis_b], lane="ldk")
        wi = emit_mixb_layer(C, wi, NTL, S, xa, xb, cs[l], cb[l], dr)
        for t in range(NTL):
            wi = emit_ffn(C, wi, xb, xout, t * NT, cs[l][:, 64:80], cs[l][:, 80:96], cb[l])
        xin = xc
    assert wi == len(C.plan), (wi, len(C.plan))
    P.finalize()
    return nc


def t5_bucket_np(n):
    max_exact = NUM_BUCKETS // 2
    nf = np.maximum(n, 1).astype(np.float32)
    large = max_exact + (np.log(nf / max_exact) / np.log(MAX_DISTANCE / max_exact) * (NUM_BUCKETS - max_exact)).astype(np.int32)
    large = np.minimum(large, NUM_BUCKETS - 1)
    return np.where(n < max_exact, n, large)


def host_prep(inp, depth):
    f = lambda a: np.ascontiguousarray(np.asarray(a, dtype=np.float32))
    m = {}

    def pc(v):
        return np.asarray(v, dtype=np.float32).reshape(16, 128).T
    for l in range(depth):
        m[f"f1i{l}"] = f(inp["ffn1_w_in"][l]); m[f"f1o{l}"] = f(inp["ffn1_w_out"][l])
        m[f"f2i{l}"] = f(inp["ffn2_w_in"][l]); m[f"f2o{l}"] = f(inp["ffn2_w_out"][l])
        m[f"win{l}"] = f(inp["w_in"][l]); m[f"wg{l}"] = f(inp["w_gate"][l])
        m[f"wba{l}"] = f(inp["w_branch_a"][l]); m[f"wbb{l}"] = f(inp["w_branch_b"][l]); m[f"wo{l}"] = f(inp["w_out"][l])
        m[f"cpk{l}"] = f(np.concatenate([pc(inp[k][l]) for k in ("ffn1_norm_pre", "ffn1_norm_post", "mix_norm_pre", "mix_norm_post", "ffn2_norm_pre", "ffn2_norm_post")], axis=1))
        row = np.concatenate([np.asarray(inp["sgu_ln_g"][l], np.float32), np.asarray(inp["sgu_ln_b"][l], np.float32), np.asarray(inp["sgu_b"][l], np.float32).reshape(-1)])
        m[f"bro{l}"] = f(np.broadcast_to(row[None, :], (128, 3072)))
        m[f"wsT{l}"] = f(np.transpose(np.asarray(inp["sgu_w_s"][l], np.float32), (2, 0, 1)))
    rb = np.asarray(inp["rel_bias"], np.float32)
    s_ = np.arange(128)[:, None]
    t_ = np.arange(128)[None, :]
    toep = np.zeros((128, 8, 2, 128), np.float32)
    for w_ in range(2):
        dist = np.maximum(t_ - s_ + 128 * w_, 0)
        bk = t5_bucket_np(dist)
        toep[:, :, w_, :] = np.transpose(rb[bk], (0, 2, 1))
    m["toep"] = f(toep.reshape(128, 2048))
    m["cb8"] = f(np.broadcast_to(rb[NUM_BUCKETS - 1][None, :], (128, 8)))
    return m


BATCH = 4
DEPTH = 2
_NC_CACHE = {}


def kernel(**inputs):
    inp = {k: np.asarray(v) for k, v in inputs.items()}
    S = inp["x"].shape[1]
    if "nc" not in _NC_CACHE:
        _NC_CACHE["nc"] = build_program(S, DEPTH)
    nc = _NC_CACHE["nc"]
    shared = host_prep(inp, DEPTH)
    in_maps = []
    for b in range(BATCH):
        m = dict(shared)
        m["x"] = np.ascontiguousarray(inp["x"][b].T.astype(np.float32))
        in_maps.append(m)
    res = run_bass_kernel_spmd(nc, in_maps, core_ids=list(range(BATCH)))
    out = np.stack([np.asarray(res.results[b]["y"]).T for b in range(BATCH)], axis=0)
    return np.ascontiguousarray(out.astype(np.float32))
```

```python
import numpy as np
from concourse.bass_utils import run_bass_kernel_spmd
from contextlib import ExitStack
import numpy as np
import concourse.bass as bass
import concourse.mybir as mybir

F32 = mybir.dt.float32
BF16 = mybir.dt.bfloat16
AF = mybir.ActivationFunctionType
ALU = mybir.AluOpType

ENGS = ("pe", "act", "dve", "pool", "sp")


class Buf:
    __slots__ = ("name", "lastw", "readers")

    def __init__(self, name):
        self.name = name
        self.lastw = None
        self.readers = {}


class Lane:
    def __init__(self, prog, name):
        self.sem = prog.nc.alloc_semaphore(name=name)
        self.count = 0
        self.last = None


class Op:
    __slots__ = ("eng", "fn", "waits", "flag", "val", "lane", "laneval", "idx")

    def __init__(self, eng, fn):
        self.eng = eng
        self.fn = fn
        self.waits = []
        self.flag = False
        self.val = None
        self.lane = None
        self.laneval = None


class Prog:
    def __init__(self, nc):
        self.nc = nc
        self.q = {e: [] for e in ENGS}
        self.sem = {e: nc.alloc_semaphore(name="sem_" + e) for e in ENGS}
        self.es = ExitStack()
        self.lanes = {}
        self.n_sb = 0

    def sbuf(self, name, shape, dtype):
        return self.es.enter_context(self.nc.sbuf_tensor(name, list(shape), dtype))

    def psum(self, name, shape, dtype=F32):
        return self.es.enter_context(self.nc.psum_tensor(name, list(shape), dtype))

    def lane(self, name):
        if name not in self.lanes:
            self.lanes[name] = Lane(self, "ln_" + name)
        return self.lanes[name]

    def _deps(self, op, reads, writes):
        evs = []
        for b in reads:
            if b.lastw is not None:
                evs.append(b.lastw)
        for b in writes:
            if b.lastw is not None:
                evs.append(b.lastw)
            evs.extend(b.readers.values())
        for ev in evs:
            if ev[0] == "c":
                src = ev[1]
                if src.eng == "pe" and op.eng == "pe":
                    continue
                if src is op:
                    continue
                src.flag = True
            op.waits.append(ev)

    def _post(self, ev, reads, writes):
        for b in writes:
            b.lastw = ev
            b.readers = {}
        key = ("c", ev[1].eng) if ev[0] == "c" else ("d", id(ev[1]))
        for b in reads:
            if b not in writes:
                b.readers[key] = ev

    def op(self, eng, fn, reads=(), writes=()):
        o = Op(eng, fn)
        self._deps(o, reads, writes)
        o.idx = len(self.q[eng])
        self.q[eng].append(o)
        self._post(("c", o), reads, writes)
        return o

    def dma(self, eng, fn, reads, writes, lane):
        ln = self.lane(lane) if isinstance(lane, str) else lane
        o = Op(eng, fn)
        self._deps(o, reads, writes)
        if ln.last is not None:
            o.waits.append(ln.last)
        ln.count += 16
        o.lane = ln
        o.laneval = ln.count
        ev = ("d", ln, ln.count)
        ln.last = ev
        self.q[eng].append(o)
        self._post(ev, reads, writes)
        return o

    def claim(self, old_bufs, new_bufs):
        merged = {}
        for b in old_bufs:
            evs = list(b.readers.values())
            if b.lastw is not None:
                evs.append(b.lastw)
            for ev in evs:
                key = ("c", ev[1].eng) if ev[0] == "c" else ("d", id(ev[1]))
                rank = ev[1].idx if ev[0] == "c" else ev[2]
                if key not in merged or merged[key][0] < rank:
                    merged[key] = (rank, ev)
        for nb_ in new_bufs:
            for key, (rank, ev) in merged.items():
                nb_.readers[key] = ev

    def finalize(self):
        nc = self.nc
        for e in ENGS:
            c = 0
            for o in self.q[e]:
                if o.flag:
                    c += 1
                    o.val = c
        engobj = {"pe": "tensor", "act": "scalar", "dve": "vector", "pool": "gpsimd", "sp": "sync"}
        final_lane_vals = [(ln.sem, ln.count) for ln in self.lanes.values() if ln.count > 0]
        final_eng_vals = {e: max([o.val for o in self.q[e] if o.flag] + [0]) for e in ENGS}
        prog = self

        def emit(e, eng):
            known = {}
            for o in prog.q[e]:
                need = {}
                for ev in o.waits:
                    if ev[0] == "c":
                        src = ev[1]
                        key = ("c", src.eng)
                        sem, val = prog.sem[src.eng], src.val
                    else:
                        key = ("d", id(ev[1]))
                        sem, val = ev[1].sem, ev[2]
                    if known.get(key, 0) >= val:
                        continue
                    if key not in need or need[key][1] < val:
                        need[key] = (sem, val)
                for key, (sem, val) in need.items():
                    eng.wait_ge(sem, val)
                    known[key] = val
                ins = o.fn(eng)
                if o.lane is not None:
                    ins.then_inc(o.lane.sem, 16)
                elif o.flag:
                    ins.then_inc(prog.sem[e], 1)
            if e == "sp":
                for sem, val in final_lane_vals:
                    eng.wait_ge(sem, val)
                for e2, v in final_eng_vals.items():
                    if v > 0:
                        eng.wait_ge(prog.sem[e2], v)

        with nc.Block() as block:
            @block.tensor
            def _(eng):
                emit("pe", eng)

            @block.scalar
            def _(eng):
                emit("act", eng)

            @block.vector
            def _(eng):
                emit("dve", eng)

            @block.gpsimd
            def _(eng):
                emit("pool", eng)

            @block.sync
            def _(eng):
                emit("sp", eng)
        self.es.close()


D = 2048
DC = D // 128
DFF = 5632
FC = DFF // 128
NT = 512
NORM_EPS = 1e-6
ATTN_PE = 1
LN_EPS = 1e-5
SEQ = 4096
CASTDMA = True


class Ctx:
    def __init__(self, P):
        self.P = P
        nc = P.nc
        self.ps = [P.psum(f"ps{i}", [128, 512]) for i in range(8)]
        self.ps_b = [Buf(f"ps{i}") for i in range(8)]
        self.NS = 4
        self.wbf = [P.sbuf(f"wbf{i}", [128, 4096], BF16) for i in range(self.NS)]
        self.wbf_b = [Buf(f"wbf{i}") for i in range(self.NS)]
        self.plan = []
        self.issued = 0
        self.R1 = P.sbuf("R1", [128, 8192], F32)
        self.R2 = P.sbuf("R2", [128, 4096], F32)
        self.R3 = P.sbuf("R3", [128, 11264], F32)
        self.ones = P.sbuf("ones", [128, 128], BF16)
        self.ones_b = Buf("ones")
        self.tmpa = [P.sbuf(f"tmpa{i}", [128, 512], F32) for i in range(2)]
        self.tmpa_b = [Buf(f"tmpa{i}") for i in range(2)]
        self.sq = [P.sbuf(f"sq{i}", [128, 512], BF16) for i in range(2)]
        self.sq_b = [Buf(f"sq{i}") for i in range(2)]
        self.rstd = P.sbuf("rstd", [128, 512], F32)
        self.rstd_b = Buf("rstd")
        self.epsc = P.sbuf("epsc", [128, 1], F32)
        self.epsc_b = Buf("epsc")
        self.cnt = 0
        self.reg = {"R1": [], "R2": [], "R3": []}
        def sb(name, shape, dt):
            setattr(self, name, P.sbuf("s_" + name, shape, dt))
            setattr(self, name + "_b", Buf(name))
        sb("kio", [128, 512], BF16); sb("wio", [128, 64], F32)
        sb("gt1", [128, 512], F32); sb("gt2", [128, 512], F32)
        self.R4 = P.sbuf("R4", [128, 4096], F32)
        self.reg["R4"] = []
        self.bro = self.R4[:, 0:3072]; self.bro_b = Buf("bro")
        self.wsf = self.R4[:, 3072:4096]; self.wsf_b = Buf("wsf")
        sb("wsb", [128, 8, 128], BF16)
        sb("mT0", [128, SEQ], BF16); sb("mT1", [128, SEQ], BF16)
        sb("qib2", [128, 8, 128], BF16); sb("qbk2", [128, 8, 128], BF16); sb("wx2", [128, 16], F32)
        sb("wabs2", [128, 16], F32); sb("wsg2", [128, 16], F32)
        sb("dg0", [128, 16, 128], BF16); sb("negI", [128, 128], BF16)
        sb("rc2", [128, 1], F32)
        if not ATTN_PE:
            sb("Fe", [128, 2048], BF16)
        self.dg1, self.dg1_b = self.dg0, self.dg0_b
        for i in range(4):
            sb(f"rl16_{i}", [128, 512], BF16)
        self.rl16 = [getattr(self, f"rl16_{i}") for i in range(4)]; self.rl16_b = [getattr(self, f"rl16_{i}_b") for i in range(4)]
        sb("blo", [128, 1], F32); sb("bmid", [128, 1], F32); sb("bcnt", [128, 1], F32); sb("bfs", [128, 1], F32); sb("brng", [128, 1], F32)
        sb("bstep", [128, 32], F32); sb("pw2", [128, 32], F32)
        for k in range(32):
            P.op("pool", (lambda e, k=k: e.memset(self.pw2[:, k:k + 1], 2.0 ** -(k + 1))), writes=[self.pw2_b])
        self.ta2 = self.tmpa; self.ta2_b = self.tmpa_b
        sb("bst", [128, 12], F32); sb("bag", [128, 2], F32); sb("lrs", [128, 1], F32); sb("lneps", [128, 1], F32)
        sb("qib", [128, 8, 128], BF16); sb("qbk", [128, 8, 128], BF16); sb("wx", [128, 16], F32)
        sb("wabs", [128, 16], F32); sb("wsg", [128, 16], F32); sb("kis", [128, SEQ], BF16)
        sb("m8", [128, 8], F32); sb("thrneg", [128, 1], F32); sb("ident", [128, 128], BF16); sb("onec", [128, 2], BF16)
        sb("rc", [128, 1], F32); sb("yb", [128, 1024], BF16)
        sb("Fb", [128, 2048], BF16); sb("cb8", [128, 8], F32)
        P.op("pool", lambda e: e.memset(self.lneps[:], LN_EPS), writes=[self.lneps_b])
        P.op("pool", lambda e: e.memset(self.ones[:], 1.0), writes=[self.ones_b])
        P.op("pool", lambda e: e.memset(self.epsc[:], NORM_EPS), writes=[self.epsc_b])

    def claim(self, rname, bufs):
        self.P.claim(self.reg[rname], bufs)
        self.reg[rname] = list(bufs)

    def plan_extend(self, specs):
        self.plan.extend(specs)

    def _issue(self, i):
        P = self.P
        pieces, R, W = self.plan[i]
        s = i % self.NS
        if CASTDMA:
            bt = self.wbf[s][:, 0:R * W].rearrange("p (r w) -> p r w", w=W)
            for pi, (c0, wd, src) in enumerate(pieces):
                P.dma("pool", (lambda e, o=bt[:, :, c0:c0 + wd], s_=src: e.dma_start(out=o, in_=s_)),
                      reads=[], writes=[self.wbf_b[s]], lane=f"w{s}_{pi}")
            return
        st = self.wst[s][:, 0:R * W].rearrange("p (r w) -> p r w", w=W)
        for pi, (c0, wd, src) in enumerate(pieces):
            P.dma("sp", (lambda e, o=st[:, :, c0:c0 + wd], s_=src: e.dma_start(out=o, in_=s_)),
                  reads=[], writes=[self.wst_b[s]], lane=f"w{s}_{pi}")
        P.op("pool", (lambda e, o=self.wbf[s][:, 0:R * W], i_=self.wst[s][:, 0:R * W]: e.tensor_copy(out=o, in_=i_)),
             reads=[self.wst_b[s]], writes=[self.wbf_b[s]])

    def wget(self, i, look=2):
        while self.issued <= min(i + look, len(self.plan) - 1):
            self._issue(self.issued)
            self.issued += 1
        pieces, R, W = self.plan[i]
        s = i % self.NS
        return self.wbf[s][:, 0:R * W].rearrange("p (r w) -> p r w", w=W), self.wbf_b[s]


_FILLREG = {}


def fillreg(e, v):
    key = (id(e), v)
    if key not in _FILLREG:
        _FILLREG[key] = e.to_reg(v)
    return _FILLREG[key]


def mm(P, o, l, r, st, sp, reads, writes):
    P.op("pe", (lambda e: e.matmul(o, l, r, start=st, stop=sp)), reads=reads, writes=writes)


def w_panel(w2d, r0, nr, cols):
    pieces = []
    off = 0
    for (c0, wd) in cols:
        src = w2d[r0 * 128:(r0 + nr) * 128, c0:c0 + wd].rearrange("(r p) w -> p r w", p=128)
        pieces.append((off, wd, src))
        off += wd
    return (pieces, nr, off)


def emit_rstd(C, chunk_ap, nch, dim, bank):
    P = C.P
    ps, psb = C.ps[bank], C.ps_b[bank]
    for c in range(nch):
        ap, b = chunk_ap(c)
        k = C.cnt % 2
        C.cnt += 1
        P.op("act", (lambda e, o=C.sq[k][:], i_=ap: e.activation(out=o, in_=i_, func=AF.Square)),
             reads=[b], writes=[C.sq_b[k]])
        P.op("pe", (lambda e, o=ps[:], l=C.ones[:], r=C.sq[k][:], st=(c == 0), sp=(c == nch - 1):
                    e.matmul(o, l, r, start=st, stop=sp)),
             reads=[C.ones_b, C.sq_b[k]], writes=[psb])
    P.op("act", (lambda e: e.activation(out=C.rstd[:], in_=ps[:], func=AF.Sqrt, bias=C.epsc[:], scale=1.0 / dim)),
         reads=[psb, C.epsc_b], writes=[C.rstd_b])
    P.op("dve", (lambda e: e.reciprocal(out=C.rstd[:], in_=C.rstd[:])), reads=[C.rstd_b], writes=[C.rstd_b])


def ffn_plan(w_in, w_out):
    specs = []
    for g in range(FC // 2):
        specs.append(w_panel(w_in, 0, DC, [(g * 256, 256)]))
        specs.append(w_panel(w_in, 0, DC, [(DFF + g * 256, 256)]))
    for c2 in range(DC // 2):
        for kp in range(3):
            r0 = kp * 16
            nr = min(16, FC - r0)
            specs.append(w_panel(w_out, r0, nr, [(c2 * 256, 256)]))
    return specs


def emit_ffn(C, wbase, x_dram, xo_dram, t0, gpre, gpost, cb):
    P = C.P
    (xd, xdb), (xo, xob) = x_dram, xo_dram
    xT = C.R1[:, :].rearrange("p (c t) -> p c t", t=NT)
    xTb = Buf("xT")
    hT = C.R2[:, :].bitcast(BF16).rearrange("p (c t) -> p c t", t=NT)
    hTb = Buf("hT")
    gT = C.R3[:, :].bitcast(BF16).rearrange("p (c t) -> p c t", t=NT)
    gTb = [Buf(f"gT{j}") for j in range(FC)]
    C.claim("R1", [xTb]); C.claim("R2", [hTb]); C.claim("R3", gTb)
    xsrc = xd[:, t0:t0 + NT].rearrange("(c p) t -> p c t", p=128)
    P.dma("sp", lambda e: e.dma_start(out=xT, in_=xsrc), reads=[xdb], writes=[xTb], lane="ld0")
    emit_rstd(C, lambda c: (xT[:, c, :], xTb), DC, D, 0)
    for c in range(DC):
        P.op("dve", (lambda e, c=c: e.scalar_tensor_tensor(out=hT[:, c, :], in0=xT[:, c, :], scalar=gpre[:, c:c + 1],
                                                          in1=C.rstd[:], op0=ALU.mult, op1=ALU.mult)),
             reads=[xTb, C.rstd_b, cb], writes=[hTb])
    wi = wbase
    for g in range(FC // 2):
        base = 4 * (g % 2)
        wa, wab = C.wget(wi); wi += 1
        for cc in range(2):
            bk = base + cc
            for k in range(DC):
                P.op("pe", (lambda e, o=C.ps[bk][:], l=wa[:, k, cc * 128:(cc + 1) * 128], r=hT[:, k, :], st=(k == 0), sp=(k == DC - 1):
                            e.matmul(o, l, r, start=st, stop=sp)),
                     reads=[wab, hTb], writes=[C.ps_b[bk]])
        wb_, wbb = C.wget(wi); wi += 1
        for cc in range(2):
            bk = base + 2 + cc
            for k in range(DC):
                P.op("pe", (lambda e, o=C.ps[bk][:], l=wb_[:, k, cc * 128:(cc + 1) * 128], r=hT[:, k, :], st=(k == 0), sp=(k == DC - 1):
                            e.matmul(o, l, r, start=st, stop=sp)),
                     reads=[wbb, hTb], writes=[C.ps_b[bk]])
        for cc in range(2):
            j = 2 * g + cc
            k2 = C.cnt % 2
            C.cnt += 1
            P.op("act", (lambda e, o=C.tmpa[k2][:], i_=C.ps[base + cc][:]: e.activation(out=o, in_=i_, func=AF.Silu)),
                 reads=[C.ps_b[base + cc]], writes=[C.tmpa_b[k2]])
            P.op("dve", (lambda e, o=gT[:, j, :], a=C.tmpa[k2][:], b=C.ps[base + 2 + cc][:]:
                         e.tensor_tensor(out=o, in0=a, in1=b, op=ALU.mult)),
                 reads=[C.tmpa_b[k2], C.ps_b[base + 2 + cc]], writes=[gTb[j]])
    wi = emit_outproj(C, wi, gT, lambda f: gTb[f], FC, (xd, xdb), (xo, xob), t0, gpost, 0.5, cb, xT, xTb, hTb)
    return wi


def outproj_plan(w_out, K):
    specs = []
    for c2 in range(DC // 2):
        for r0 in range(0, K, 16):
            specs.append(w_panel(w_out, r0, min(16, K - r0), [(c2 * 256, 256)]))
    return specs


def emit_outproj(C, wi, inT, inb, K, x_dram, xo_dram, t0, gpost, alpha, cb, fT, fTb, r2b):
    P = C.P
    (xd, xdb), (xo, xob) = x_dram, xo_dram
    SB = 6
    for c2 in range(DC // 2):
        base = 2 * (c2 % 2)
        for r0 in range(0, K, 16):
            nr = min(16, K - r0)
            wo, wob = C.wget(wi); wi += 1
            for cc in range(2):
                bk = base + cc
                for r in range(nr):
                    f = r0 + r
                    mm(P, C.ps[bk][:], wo[:, r, cc * 128:(cc + 1) * 128], inT[:, f, :], f == 0, f == K - 1, [wob, inb(f)], [C.ps_b[bk]])
        for cc in range(2):
            c = 2 * c2 + cc
            bk = base + cc
            P.op("act", (lambda e, o=fT[:, c, :], i_=C.ps[bk][:]: e.activation(out=o, in_=i_, func=AF.Copy)),
                 reads=[C.ps_b[bk]], writes=[fTb])
            k2 = C.cnt % 2
            C.cnt += 1
            P.op("dve", (lambda e, o=C.sq[k2][:], i_=C.ps[bk][:], f_=fT[:, c, :]: e.tensor_tensor(out=o, in0=i_, in1=f_, op=ALU.mult)),
                 reads=[C.ps_b[bk], fTb], writes=[C.sq_b[k2]])
            mm(P, C.ps[SB][:], C.ones[:], C.sq[k2][:], c == 0, c == DC - 1, [C.ones_b, C.sq_b[k2]], [C.ps_b[SB]])
    P.op("act", (lambda e: e.activation(out=C.rstd[:], in_=C.ps[SB][:], func=AF.Sqrt, bias=C.epsc[:], scale=1.0 / D)),
         reads=[C.ps_b[SB], C.epsc_b], writes=[C.rstd_b])
    P.op("dve", (lambda e: e.reciprocal(out=C.rstd[:], in_=C.rstd[:])), reads=[C.rstd_b], writes=[C.rstd_b])
    xr = C.R2[:, :].rearrange("p (c t) -> p c t", t=NT)
    for h in range(2):
        xs = xd[h * 1024:(h + 1) * 1024, t0:t0 + NT].rearrange("(c p) t -> p c t", p=128)
        P.dma("sp", (lambda e, s_=xs: e.dma_start(out=xr, in_=s_)), reads=[xdb], writes=[r2b], lane="ld1")
        for cl in range(8):
            c = h * 8 + cl
            P.op("dve", (lambda e, c=c: e.scalar_tensor_tensor(out=fT[:, c, :], in0=fT[:, c, :], scalar=gpost[:, c:c + 1],
                                                              in1=C.rstd[:], op0=ALU.mult, op1=ALU.mult)),
                 reads=[fTb, C.rstd_b, cb], writes=[fTb])
            P.op("dve", (lambda e, c=c, cl=cl: e.scalar_tensor_tensor(out=fT[:, c, :], in0=fT[:, c, :], scalar=alpha,
                                                                       in1=xr[:, cl, :], op0=ALU.mult, op1=ALU.add)),
                 reads=[fTb, r2b], writes=[fTb])
    xdst = xo[:, t0:t0 + NT].rearrange("(c p) t -> p c t", p=128)
    P.dma("sp", lambda e: e.dma_start(out=xdst, in_=fT), reads=[fTb], writes=[xob], lane="st0")
    return wi


AW = 1024
NEG = -1.0e30
GELU_C = 0.7978845608028654 * 2.0
LN_EPS = 1e-5
O_ZU, O_ZV, O_Q, O_K, O_V, O_QI, O_KI, O_WI = 0, 1024, 2048, 3072, 4096, 5120, 6144, 6208


def mm(P, o, l, r, st, sp, reads, writes):
    P.op("pe", (lambda e: e.matmul(o, l, r, start=st, stop=sp)), reads=reads, writes=writes)


def emit_gelu(C, out_ap, in_ap, inb, outb, shape):
    P = C.P
    t1 = C.gt1[:, 0:shape]
    t2 = C.gt2[:, 0:shape]
    P.op("act", (lambda e: e.activation(out=t1, in_=in_ap, func=AF.Square)), reads=[inb], writes=[C.gt1_b])
    P.op("dve", (lambda e: e.tensor_scalar(out=t1, in0=t1, scalar1=0.044715, scalar2=1.0, op0=ALU.mult, op1=ALU.add)),
         reads=[C.gt1_b], writes=[C.gt1_b])
    P.op("dve", (lambda e: e.tensor_tensor(out=t1, in0=t1, in1=in_ap, op=ALU.mult)), reads=[C.gt1_b, inb], writes=[C.gt1_b])
    P.op("act", (lambda e: e.activation(out=t2, in_=t1, func=AF.Sigmoid, scale=GELU_C)), reads=[C.gt1_b], writes=[C.gt2_b])
    P.op("dve", (lambda e: e.tensor_tensor(out=out_ap, in0=t2, in1=in_ap, op=ALU.mult)), reads=[C.gt2_b, inb], writes=[outb])


def mixa_plan(w_in, w_gate, w_ba):
    specs = []
    for seg in (O_ZU, O_Q, O_K, O_QI):
        for c2 in range(4):
            specs.append(w_panel(w_in, 0, DC, [(seg + c2 * 256, 256)]))
    specs.append(w_panel(w_in, 0, DC, [(O_KI, 64), (O_KI, 64)]))
    for seg in (O_ZV, O_V):
        for c2 in range(4):
            specs.append(w_panel(w_in, 0, DC, [(seg + c2 * 256, 256)]))
    specs.append(w_panel(w_in, 0, DC, [(O_WI, 16)]))
    for c2 in range(8):
        specs.append(w_panel(w_ba, 0, 8, [(c2 * 256, 256)]))
        specs.append(w_panel(w_gate, 0, DC, [(c2 * 256, 256)]))
    for c2 in range(8):
        specs.append(w_panel(w_gate, 0, DC, [(D + c2 * 256, 256)]))
    return specs


class MixRes:
    pass


def emit_mix_consts(C, bro_d, wsT_d, M):
    P = C.P
    C.bro_b = Buf("bro"); C.wsf_b = Buf("wsf")
    C.claim("R4", [C.bro_b, C.wsf_b])
    P.dma("sp", lambda e: e.dma_start(out=C.bro, in_=bro_d), reads=[], writes=[C.bro_b], lane="ldc")
    wv = C.wsf.rearrange("p (g t) -> p g t", g=8)
    P.dma("sp", lambda e: e.dma_start(out=wv, in_=wsT_d), reads=[], writes=[C.wsf_b], lane="ldc")
    P.op("pool", (lambda e: e.affine_select(out=wv, in_=wv, pattern=[[0, 8], [1, 128]], compare_op=ALU.is_ge,
                                             fill=fillreg(e, 0.0), base=0, channel_multiplier=-1)),
         reads=[C.wsf_b], writes=[C.wsf_b])
    P.op("pool", (lambda e: e.tensor_copy(out=C.wsb[:], in_=wv)), reads=[C.wsf_b], writes=[C.wsb_b])


def emit_mixa(C, wbase, x1, t0, S, cs, cb, dr):
    P = C.P
    xd, xdb = x1
    xT = C.R1[:, :].rearrange("p (c t) -> p c t", t=NT)
    xTb = Buf("xT")
    hT = C.R2[:, :].bitcast(BF16).rearrange("p (c t) -> p c t", t=NT)
    hTb = Buf("hT")
    r3 = C.R3[:, :]
    zv = r3[:, 0:4096].rearrange("p (b c) -> p b c", c=1024)
    zvb = Buf("zv")
    vln = r3[:, 4096:6144].bitcast(BF16).rearrange("p (b c) -> p b c", c=1024)
    vlnb = Buf("vln")
    guT = r3[:, 6144:8192].bitcast(BF16).rearrange("p (c t) -> p c t", t=NT)
    guTb = Buf("guT")
    yaT = r3[:, 8192:10240].bitcast(BF16).rearrange("p (c t) -> p c t", t=NT)
    yaTb = Buf("yaT")
    gpre = cs[:, 32:48]
    C.claim("R1", [xTb]); C.claim("R2", [hTb]); C.claim("R3", [zvb, vlnb, guTb, yaTb])
    xsrc = xd[:, t0:t0 + NT].rearrange("(c p) t -> p c t", p=128)
    P.dma("sp", lambda e: e.dma_start(out=xT, in_=xsrc), reads=[xdb], writes=[xTb], lane="ld0")
    emit_rstd(C, lambda c: (xT[:, c, :], xTb), DC, D, 0)
    for c in range(DC):
        P.op("dve", (lambda e, c=c: e.scalar_tensor_tensor(out=hT[:, c, :], in0=xT[:, c, :], scalar=gpre[:, c:c + 1],
                                                          in1=C.rstd[:], op0=ALU.mult, op1=ALU.mult)),
             reads=[xTb, C.rstd_b, cb], writes=[hTb])
    osb = [C.R1[:, i * 2048:(i + 1) * 2048].bitcast(BF16).rearrange("p (c t) -> p c t", t=NT) for i in range(2)]
    osbb = [Buf("osb0"), Buf("osb1")]
    och = [C.R1[:, 4096 + i * 512: 4096 + (i + 1) * 512] for i in range(8)]
    ochb = [Buf(f"och{i}") for i in range(8)]
    C.claim("R1", osbb + ochb)
    wi = wbase
    bankrr = [0]

    def nb():
        b = bankrr[0] % 8
        bankrr[0] += 1
        return b

    first = [True]

    def r1dep():
        return [xTb]

    dests = {O_Q: ("qT", 0), O_K: ("kT", 1), O_QI: ("qiT", 0)}
    for seg in (O_ZU, O_Q, O_K, O_QI):
        if seg != O_ZU:
            dn, oi = dests[seg]
            ob, obb = osb[oi], osbb[oi]
        for c2 in range(4):
            w, wb = C.wget(wi); wi += 1
            for cc in range(2):
                ch = 2 * c2 + cc
                bk = nb()
                for k in range(DC):
                    mm(P, C.ps[bk][:], w[:, k, cc * 128:(cc + 1) * 128], hT[:, k, :], k == 0, k == DC - 1, [wb, hTb], [C.ps_b[bk]])
                if seg == O_ZU:
                    emit_gelu(C, guT[:, ch, :], C.ps[bk][:], C.ps_b[bk], guTb, 512)
                else:
                    P.op("act", (lambda e, o=ob[:, ch, :], i_=C.ps[bk][:]: e.activation(out=o, in_=i_, func=AF.Copy)),
                         reads=[C.ps_b[bk]], writes=[obb])
        if seg != O_ZU:
            dd, ddb = dr[dn]
            dst = dd[:, t0:t0 + NT].rearrange("(c p) t -> p c t", p=128)
            P.dma("sp", (lambda e, d_=dst, s_=ob: e.dma_start(out=d_, in_=s_)), reads=[obb], writes=[ddb], lane="st1")
    w, wb = C.wget(wi); wi += 1
    bk = nb()
    for k in range(DC):
        mm(P, C.ps[bk][:], w[:, k, 0:128], hT[:, k, :], k == 0, k == DC - 1, [wb, hTb], [C.ps_b[bk]])
    P.op("act", (lambda e, o=C.kio[:], i_=C.ps[bk][:]: e.activation(out=o, in_=i_, func=AF.Copy)), reads=[C.ps_b[bk]], writes=[C.kio_b])
    dd, ddb = dr["kiT"]
    P.dma("sp", (lambda e, d_=dd[:, t0:t0 + NT]: e.dma_start(out=d_, in_=C.kio[:])), reads=[C.kio_b], writes=[ddb], lane="st2")
    vsb = osb[0].rearrange("p c t -> p (c t)").rearrange("p (b c) -> p b c", c=1024)
    for seg in (O_ZV, O_V):
        for c2 in range(4):
            w, wb = C.wget(wi); wi += 1
            for tb in range(4):
                bk = nb()
                for k in range(DC):
                    mm(P, C.ps[bk][:, 0:256], hT[:, k, tb * 128:(tb + 1) * 128], w[:, k, :], k == 0, k == DC - 1, [wb, hTb], [C.ps_b[bk]])
                if seg == O_ZV:
                    P.op("act", (lambda e, o=zv[:, tb, c2 * 256:(c2 + 1) * 256], i_=C.ps[bk][:, 0:256]: e.activation(out=o, in_=i_, func=AF.Copy)),
                         reads=[C.ps_b[bk]], writes=[zvb])
                else:
                    P.op("act", (lambda e, o=vsb[:, tb, c2 * 256:(c2 + 1) * 256], i_=C.ps[bk][:, 0:256]: e.activation(out=o, in_=i_, func=AF.Copy)),
                         reads=[C.ps_b[bk]], writes=[osbb[0]])
    dd, ddb = dr["v"]
    P.dma("sp", (lambda e, d_=dd[t0:t0 + NT, :].rearrange("(b p) c -> p b c", p=128): e.dma_start(out=d_, in_=vsb)),
          reads=[osbb[0]], writes=[ddb], lane="st1")
    w, wb = C.wget(wi); wi += 1
    bk = nb()
    for tb in range(4):
        for k in range(DC):
            mm(P, C.ps[bk][:, tb * 16:(tb + 1) * 16], hT[:, k, tb * 128:(tb + 1) * 128], w[:, k, :], k == 0, k == DC - 1, [wb, hTb], [C.ps_b[bk]])
    P.op("act", (lambda e, i_=C.ps[bk][:, 0:64]: e.activation(out=C.wio[:], in_=i_, func=AF.Copy)), reads=[C.ps_b[bk]], writes=[C.wio_b])
    dd, ddb = dr["widx"]
    P.dma("sp", (lambda e, d_=dd[t0:t0 + NT, :].rearrange("(b p) c -> p b c", p=128): e.dma_start(out=d_, in_=C.wio[:].rearrange("p (b c) -> p b c", c=16))),
          reads=[C.wio_b], writes=[ddb], lane="st2")
    lng = C.bro[:, 0:1024]
    lnb = C.bro[:, 1024:2048]
    for tb in range(4):
        for h2 in range(2):
            emit_gelu(C, zv[:, tb, h2 * 512:(h2 + 1) * 512], zv[:, tb, h2 * 512:(h2 + 1) * 512], zvb, zvb, 512)
        P.op("dve", (lambda e, i_=zv[:, tb, :]: e.bn_stats(out=C.bst[:, 0:6], in_=i_[:, 0:512])), reads=[zvb], writes=[C.bst_b])
        P.op("dve", (lambda e, i_=zv[:, tb, :]: e.bn_stats(out=C.bst[:, 6:12], in_=i_[:, 512:1024])), reads=[zvb], writes=[C.bst_b])
        P.op("dve", (lambda e: e.bn_aggr(out=C.bag[:], in_=C.bst[:].rearrange("p (a b) -> p a b", b=6))), reads=[C.bst_b], writes=[C.bag_b])
        P.op("act", (lambda e: e.activation(out=C.lrs[:], in_=C.bag[:, 1:2], func=AF.Sqrt, bias=C.lneps[:], scale=1.0)),
             reads=[C.bag_b, C.lneps_b], writes=[C.lrs_b])
        P.op("dve", (lambda e: e.reciprocal(out=C.lrs[:], in_=C.lrs[:])), reads=[C.lrs_b], writes=[C.lrs_b])
        P.op("dve", (lambda e, o=zv[:, tb, :]: e.tensor_scalar(out=o, in0=o, scalar1=C.bag[:, 0:1], scalar2=C.lrs[:], op0=ALU.subtract, op1=ALU.mult)),
             reads=[zvb, C.bag_b, C.lrs_b], writes=[zvb])
        P.op("dve", (lambda e, o=zv[:, tb, :]: e.tensor_tensor(out=o, in0=o, in1=lng, op=ALU.mult)), reads=[zvb, C.bro_b], writes=[zvb])
        P.op("dve", (lambda e, o=vln[:, tb, :], i_=zv[:, tb, :]: e.tensor_tensor(out=o, in0=i_, in1=lnb, op=ALU.add)), reads=[zvb, C.bro_b], writes=[vlnb])
    for g in range(8):
        bk = nb()
        for tb in range(4):
            mm(P, C.ps[bk][:, tb * 128:(tb + 1) * 128], vln[:, tb, g * 128:(g + 1) * 128], C.wsb[:, g, :], True, True, [vlnb, C.wsb_b], [C.ps_b[bk]])
        for tb in range(4):
            P.op("dve", (lambda e, o=C.gt1[:, tb * 128:(tb + 1) * 128], i_=C.ps[bk][:, tb * 128:(tb + 1) * 128], b_=C.bro[:, 2048 + g * 128:2048 + (g + 1) * 128]:
                         e.tensor_tensor(out=o, in0=i_, in1=b_, op=ALU.add)),
                 reads=[C.ps_b[bk], C.bro_b], writes=[C.gt1_b])
        P.op("dve", (lambda e, o=yaT[:, g, :], g_=guT[:, g, :]: e.tensor_tensor(out=o, in0=C.gt1[:], in1=g_, op=ALU.mult)),
             reads=[C.gt1_b, guTb], writes=[yaTb])
    if "dbg_ya" in dr:
        P.dma("sp", (lambda e, d_=dr["dbg_ya"][0][:, t0:t0 + NT].rearrange("(c p) t -> p c t", p=128): e.dma_start(out=d_, in_=yaT)), reads=[yaTb], writes=[dr["dbg_ya"][1]], lane="dbg")
        P.dma("sp", (lambda e, d_=dr["dbg_gu"][0][:, t0:t0 + NT].rearrange("(c p) t -> p c t", p=128): e.dma_start(out=d_, in_=guT)), reads=[guTb], writes=[dr["dbg_gu"][1]], lane="dbg")
        P.dma("sp", (lambda e, d_=dr["dbg_vln"][0][t0:t0 + NT, :].rearrange("(b p) c -> p b c", p=128): e.dma_start(out=d_, in_=vln)), reads=[vlnb], writes=[dr["dbg_vln"][1]], lane="dbg")
    ocnt = [0]
    for c2 in range(8):
        wa, wab = C.wget(wi); wi += 1
        wg, wgb = C.wget(wi); wi += 1
        for cc in range(2):
            ch = 2 * c2 + cc
            bka = nb()
            for k in range(8):
                mm(P, C.ps[bka][:], wa[:, k, cc * 128:(cc + 1) * 128], yaT[:, k, :], k == 0, k == 7, [wab, yaTb], [C.ps_b[bka]])
            bkg = nb()
            for k in range(DC):
                mm(P, C.ps[bkg][:], wg[:, k, cc * 128:(cc + 1) * 128], hT[:, k, :], k == 0, k == DC - 1, [wgb, hTb], [C.ps_b[bkg]])
            k2 = C.cnt % 2
            C.cnt += 1
            P.op("act", (lambda e, o=C.tmpa[k2][:], i_=C.ps[bkg][:]: e.activation(out=o, in_=i_, func=AF.Sigmoid)),
                 reads=[C.ps_b[bkg]], writes=[C.tmpa_b[k2]])
            oi = ocnt[0] % 8
            ocnt[0] += 1
            P.op("dve", (lambda e, o=och[oi], a=C.tmpa[k2][:], b=C.ps[bka][:]: e.tensor_tensor(out=o, in0=a, in1=b, op=ALU.mult)),
                 reads=[C.tmpa_b[k2], C.ps_b[bka]], writes=[ochb[oi]])
            dd, ddb = dr["apart"]
            P.dma("sp", (lambda e, d_=dd[ch * 128:(ch + 1) * 128, t0:t0 + NT], s_=och[oi]: e.dma_start(out=d_, in_=s_)),
                  reads=[ochb[oi]], writes=[ddb], lane=f"so{oi}")
    for c2 in range(8):
        wg, wgb = C.wget(wi); wi += 1
        for cc in range(2):
            ch = 2 * c2 + cc
            bkg = nb()
            for k in range(DC):
                mm(P, C.ps[bkg][:], wg[:, k, cc * 128:(cc + 1) * 128], hT[:, k, :], k == 0, k == DC - 1, [wgb, hTb], [C.ps_b[bkg]])
            oi = ocnt[0] % 8
            ocnt[0] += 1
            P.op("act", (lambda e, o=och[oi], i_=C.ps[bkg][:]: e.activation(out=o, in_=i_, func=AF.Sigmoid)),
                 reads=[C.ps_b[bkg]], writes=[ochb[oi]])
            dd, ddb = dr["gb"]
            P.dma("sp", (lambda e, d_=dd[ch * 128:(ch + 1) * 128, t0:t0 + NT], s_=och[oi]: e.dma_start(out=d_, in_=s_)),
                  reads=[ochb[oi]], writes=[ddb], lane=f"so{oi}")
    return wi


ATT_SCALE = 128 ** -0.5
MASK_NEG = -30000.0
TOPK = 256
BIS_IT = 28
BIS_MIN = 1024


def emit_attn_consts(C, toep_d, cb8_d):
    P = C.P
    tpf = C.R3[:, 0:2048].rearrange("p (h w) -> p h w", h=8)
    tb = Buf("tpf")
    C.claim("R3", [tb])
    P.dma("sp", lambda e: e.dma_start(out=C.cb8[:], in_=cb8_d), reads=[], writes=[C.cb8_b], lane="ldc")
    P.dma("sp", lambda e: e.dma_start(out=tpf, in_=toep_d.rearrange("p (h w) -> p h w", h=8)), reads=[], writes=[tb], lane="ldc")
    for h in range(8):
        P.op("dve", (lambda e, h=h: e.tensor_scalar(out=tpf[:, h, :], in0=tpf[:, h, :], scalar1=C.cb8[:, h:h + 1], scalar2=None, op0=ALU.subtract)),
             reads=[tb, C.cb8_b], writes=[tb])
    if not ATTN_PE:
        P.op("act", (lambda e: e.activation(out=C.Fe[:].rearrange("p (h w) -> p h w", h=8), in_=tpf, func=AF.Exp)), reads=[tb], writes=[C.Fe_b])
    P.op("dve", (lambda e: e.tensor_scalar(out=C.Fb[:].rearrange("p (h w) -> p h w", h=8), in0=tpf, scalar1=1.0 / ATT_SCALE, scalar2=None, op0=ALU.mult)),
         reads=[tb], writes=[C.Fb_b])
    P.op("pool", lambda e: e.memset(C.ident[:], 1.0), writes=[C.ident_b])
    P.op("pool", (lambda e: e.affine_select(out=C.ident[:], in_=C.ident[:], pattern=[[-1, 128]], compare_op=ALU.is_equal,
                                             fill=fillreg(e, 0.0), base=0, channel_multiplier=1)), reads=[C.ident_b], writes=[C.ident_b])
    P.op("pool", (lambda e: e.tensor_scalar(out=C.negI[:], in0=C.ident[:], scalar1=MASK_NEG, scalar2=1.0, op0=ALU.mult, op1=ALU.mult)),
         reads=[C.ident_b], writes=[C.negI_b])
    P.op("pool", lambda e: e.memset(C.thrneg[:], -1.0e29), writes=[C.thrneg_b])
    P.op("pool", lambda e: e.memset(C.onec[:], 1.0), writes=[C.onec_b])


def mixb_tail_plan(w_bb, w_o):
    specs = []
    for c2 in range(8):
        specs.append(w_panel(w_bb, 0, 8, [(c2 * 256, 256)]))
    specs += outproj_plan(w_o, DC)
    return specs


def emit_mixb_tile(C, wbase, tt, S, x1, x2, cs, cb, dr):
    P = C.P
    t0 = tt * NT
    kh = [C.R1[:, i * 2048:(i + 1) * 2048].bitcast(BF16) for i in range(2)]
    khb = [Buf("kh0"), Buf("kh1")]
    ybT = C.R1[:, 4096:6144].bitcast(BF16).rearrange("p (c t) -> p c t", t=NT)
    ybTb = Buf("ybT")
    maskT = C.R1[:, 6144:8192].bitcast(BF16)
    maskTb = Buf("maskT")
    vh = [C.R2[:, i * 2048:(i + 1) * 2048].bitcast(BF16).rearrange("p (b c) -> p b c", c=128) for i in range(2)]
    vhb = [Buf("vh0"), Buf("vh1")]
    score = C.R3[:, 0:4096]
    scb = Buf("score")
    work = C.R3[:, 4096:8192]
    wkb = Buf("work")
    mask = C.R3[:, 8192:10240].bitcast(BF16)
    mkb = Buf("mask")
    C.claim("R1", khb + [ybTb, maskTb]); C.claim("R2", vhb); C.claim("R3", [scb, wkb, mkb])
    rr = [0]

    def nb():
        b = rr[0] % 6
        rr[0] += 1
        return b
    OB, TB = 6, 7
    psT = C.ps[TB][:].bitcast(BF16)
    kvc = [0]
    for qi_ in range(4):
        qb = tt * 4 + qi_
        q0 = qb * 128
        nkb = qb + 1
        Sc = nkb * 128
        ngrp = (nkb + 3) // 4
        P.dma("sp", (lambda e, s_=dr["qiT"][0][:, q0:q0 + 128].rearrange("(c p) t -> p c t", p=128): e.dma_start(out=C.qib[:], in_=s_)),
              reads=[dr["qiT"][1]], writes=[C.qib_b], lane="lq0")
        P.dma("sp", (lambda e, s_=dr["qT"][0][:, q0:q0 + 128].rearrange("(c p) t -> p c t", p=128): e.dma_start(out=C.qbk[:], in_=s_)),
              reads=[dr["qT"][1]], writes=[C.qbk_b], lane="lq1")
        P.dma("sp", (lambda e, s_=dr["widx"][0][q0:q0 + 128, :]: e.dma_start(out=C.wx[:], in_=s_)),
              reads=[dr["widx"][1]], writes=[C.wx_b], lane="lq2")
        P.op("act", (lambda e: e.activation(out=C.wabs[:], in_=C.wx[:], func=AF.Abs)), reads=[C.wx_b], writes=[C.wabs_b])
        P.op("act", (lambda e: e.activation(out=C.wsg[:], in_=C.wx[:], func=AF.Sign)), reads=[C.wx_b], writes=[C.wsg_b])
        for grp in range(ngrp):
            c0 = grp * 512
            n = min(Sc, c0 + 512) - c0
            for h in range(16):
                c, half = h // 2, h % 2
                rows = slice(half * 64, half * 64 + 64)
                bk = nb()
                mm(P, C.ps[bk][:, 0:n], C.qib[rows, c, :], C.kis[rows, c0:c0 + n], True, True, [C.qib_b, C.kis_b], [C.ps_b[bk]])
                k2 = C.cnt % 2
                C.cnt += 1
                P.op("act", (lambda e, o=C.tmpa[k2][:, 0:n], i_=C.ps[bk][:, 0:n], h=h: e.activation(out=o, in_=i_, func=AF.Relu, scale=C.wabs[:, h:h + 1])),
                     reads=[C.ps_b[bk], C.wabs_b], writes=[C.tmpa_b[k2]])
                if h == 0:
                    P.op("dve", (lambda e, o=score[:, c0:c0 + n], i_=C.tmpa[k2][:, 0:n]: e.tensor_scalar(out=o, in0=i_, scalar1=C.wsg[:, 0:1], scalar2=None, op0=ALU.mult)),
                         reads=[C.tmpa_b[k2], C.wsg_b], writes=[scb])
                else:
                    P.op("dve", (lambda e, o=score[:, c0:c0 + n], i_=C.tmpa[k2][:, 0:n], h=h: e.scalar_tensor_tensor(out=o, in0=i_, scalar=C.wsg[:, h:h + 1], in1=o, op0=ALU.mult, op1=ALU.add)),
                         reads=[C.tmpa_b[k2], C.wsg_b, scb], writes=[scb])
        dsl = score[:, (nkb - 1) * 128:nkb * 128]
        P.op("pool", (lambda e, d_=dsl: e.affine_select(out=d_, in_=d_, pattern=[[-1, 128]], compare_op=ALU.is_ge, fill=fillreg(e, NEG), base=0, channel_multiplier=1)),
             reads=[scb], writes=[scb])
        if Sc > TOPK:
            for r in range(TOPK // 8):
                src = score[:, 0:Sc] if r == 0 else work[:, 0:Sc]
                srcb = scb if r == 0 else wkb
                P.op("dve", (lambda e, s_=src: e.max(out=C.m8[:], in_=s_)), reads=[srcb], writes=[C.m8_b])
                if r < TOPK // 8 - 1:
                    P.op("dve", (lambda e, s_=src, w_=work[:, 0:Sc]: e.match_replace(out=w_, in_to_replace=C.m8[:], in_values=s_, imm_value=NEG)),
                         reads=[srcb, C.m8_b], writes=[wkb])
            thr, thrb = C.m8[:, 7:8], C.m8_b
        else:
            thr, thrb = C.thrneg[:], C.thrneg_b
        P.op("dve", (lambda e, t_=thr, m_=mask[:, 0:Sc], s_=score[:, 0:Sc]: e.tensor_scalar(out=m_, in0=s_, scalar1=t_, scalar2=None, op0=ALU.is_ge)),
             reads=[scb, thrb], writes=[mkb])
        for grp in range(ngrp):
            kbs = list(range(grp * 4, min(nkb, grp * 4 + 4)))
            for j, kb in enumerate(kbs):
                P.op("pe", (lambda e, o=psT[:, j * 128:(j + 1) * 128], i_=mask[:, kb * 128:(kb + 1) * 128]: e.transpose(o, i_, C.ident[:])),
                     reads=[mkb, C.ident_b], writes=[C.ps_b[TB]])
            n = len(kbs) * 128
            P.op("act", (lambda e, o=maskT[:, grp * 512:grp * 512 + n], i_=psT[:, 0:n]: e.activation(out=o, in_=i_, func=AF.Copy)),
                 reads=[C.ps_b[TB]], writes=[maskTb])
        for h in range(8):
            s2 = kvc[0] % 2
            kvc[0] += 1
            P.dma("sp", (lambda e, o=kh[s2][:, 0:Sc], s_=dr["kT"][0][h * 128:(h + 1) * 128, 0:Sc]: e.dma_start(out=o, in_=s_)),
                  reads=[dr["kT"][1]], writes=[khb[s2]], lane=f"lk{s2}")
            P.dma("sp", (lambda e, o=vh[s2][:, 0:nkb, :], s_=dr["v"][0][0:Sc, h * 128:(h + 1) * 128].rearrange("(b p) c -> p b c", p=128): e.dma_start(out=o, in_=s_)),
                  reads=[dr["v"][1]], writes=[vhb[s2]], lane=f"lv{s2}")
            for grp in range(ngrp):
                kbs = list(range(grp * 4, min(nkb, grp * 4 + 4)))
                n = len(kbs) * 128
                bk = nb()
                for j, kb in enumerate(kbs):
                    mm(P, C.ps[bk][:, j * 128:(j + 1) * 128], kh[s2][:, kb * 128:(kb + 1) * 128], C.qbk[:, h, :], True, True, [khb[s2], C.qbk_b], [C.ps_b[bk]])
                k2 = C.cnt % 2
                C.cnt += 1
                P.op("act", (lambda e, o=C.tmpa[k2][:, 0:n], i_=C.ps[bk][:, 0:n], h=h: e.activation(out=o, in_=i_, func=AF.Exp, bias=C.cb8[:, h:h + 1], scale=ATT_SCALE)),
                     reads=[C.ps_b[bk], C.cb8_b], writes=[C.tmpa_b[k2]])
                P.op("dve", (lambda e, o=C.pm[k2][:, 0:n], a=C.tmpa[k2][:, 0:n], b=maskT[:, grp * 512:grp * 512 + n]: e.tensor_tensor(out=o, in0=a, in1=b, op=ALU.mult)),
                     reads=[C.tmpa_b[k2], maskTb], writes=[C.pm_b[k2]])
                for j, kb in enumerate(kbs):
                    w_ = nkb - 1 - kb
                    if w_ <= 1:
                        P.op("dve", (lambda e, o=C.pm[k2][:, j * 128:(j + 1) * 128], f_=C.Fb[:, (h * 2 + w_) * 128:(h * 2 + w_ + 1) * 128]: e.tensor_tensor(out=o, in0=o, in1=f_, op=ALU.mult)),
                             reads=[C.pm_b[k2], C.Fb_b], writes=[C.pm_b[k2]])
                for j, kb in enumerate(kbs):
                    P.op("pe", (lambda e, o=C.ps[OB][:, 0:128], l=C.pm[k2][:, j * 128:(j + 1) * 128], r=vh[s2][:, kb, :], st=(kb == 0), sp=(kb == nkb - 1):
                                e.matmul(o, l, r, start=st, stop=sp, skip_group_check=True)),
                         reads=[C.pm_b[k2], vhb[s2]], writes=[C.ps_b[OB]])
                    P.op("pe", (lambda e, o=C.ps[OB][:, 128:129], l=C.pm[k2][:, j * 128:(j + 1) * 128], sp=(kb == nkb - 1):
                                e.matmul(o, l, C.onec[:, 0:1], start=False, stop=sp, skip_group_check=True)),
                         reads=[C.pm_b[k2], C.onec_b], writes=[C.ps_b[OB]])
            P.op("dve", (lambda e: e.reciprocal(out=C.rc[:], in_=C.ps[OB][:, 128:129])), reads=[C.ps_b[OB]], writes=[C.rc_b])
            P.op("dve", (lambda e, o=C.yb[:, h * 128:(h + 1) * 128]: e.tensor_scalar(out=o, in0=C.ps[OB][:, 0:128], scalar1=C.rc[:], scalar2=None, op0=ALU.mult)),
                 reads=[C.ps_b[OB], C.rc_b], writes=[C.yb_b])
        for half in range(2):
            for j in range(4):
                c = half * 4 + j
                P.op("pe", (lambda e, o=psT[:, j * 128:(j + 1) * 128], i_=C.yb[:, c * 128:(c + 1) * 128]: e.transpose(o, i_, C.ident[:])),
                     reads=[C.yb_b, C.ident_b], writes=[C.ps_b[TB]])
            P.op("act", (lambda e, o=ybT[:, half * 4:half * 4 + 4, qi_ * 128:(qi_ + 1) * 128], i_=psT[:, 0:512].rearrange("p (c t) -> p c t", t=128):
                         e.activation(out=o, in_=i_, func=AF.Copy)),
                 reads=[C.ps_b[TB]], writes=[ybTb])
    mT = C.R2[:, :].bitcast(BF16).rearrange("p (c t) -> p c t", t=NT)
    mTb = Buf("mT")
    C.claim("R2", [mTb])
    wi = wbase
    lc = [0]
    for c2 in range(8):
        w, wb = C.wget(wi); wi += 1
        for cc in range(2):
            ch = 2 * c2 + cc
            bk = nb()
            for k in range(8):
                mm(P, C.ps[bk][:], w[:, k, cc * 128:(cc + 1) * 128], ybT[:, k, :], k == 0, k == 7, [wb, ybTb], [C.ps_b[bk]])
            k2 = lc[0] % 2
            lc[0] += 1
            P.dma("sp", (lambda e, o=C.gt1[:] if k2 == 0 else C.gt2[:], s_=dr["gb"][0][ch * 128:(ch + 1) * 128, t0:t0 + NT]: e.dma_start(out=o, in_=s_)),
                  reads=[dr["gb"][1]], writes=[C.gt1_b if k2 == 0 else C.gt2_b], lane=f"lg{k2}")
            P.dma("sp", (lambda e, o=C.tmpa[k2][:], s_=dr["apart"][0][ch * 128:(ch + 1) * 128, t0:t0 + NT]: e.dma_start(out=o, in_=s_)),
                  reads=[dr["apart"][1]], writes=[C.tmpa_b[k2]], lane=f"la{k2}")
            gbt, gbb = (C.gt1, C.gt1_b) if k2 == 0 else (C.gt2, C.gt2_b)
            P.op("dve", (lambda e, g_=gbt[:], i_=C.ps[bk][:]: e.tensor_tensor(out=g_, in0=g_, in1=i_, op=ALU.mult)),
                 reads=[gbb, C.ps_b[bk]], writes=[gbb])
            P.op("dve", (lambda e, o=mT[:, ch, :], g_=gbt[:], a=C.tmpa[k2][:]: e.tensor_tensor(out=o, in0=g_, in1=a, op=ALU.add)),
                 reads=[gbb, C.tmpa_b[k2]], writes=[mTb])
    fT = C.R1[:, :].rearrange("p (c t) -> p c t", t=NT)
    fTb = Buf("fT")
    C.claim("R1", [fTb])
    r2b = Buf("r2x")
    wi = emit_outproj_claim(C, wi, mT, mTb, x1, x2, t0, cs[:, 48:64], 1.0, cb, fT, fTb, r2b)
    return wi


def emit_outproj_claim(C, wi, mT, mTb, x1, x2, t0, gpost, alpha, cb, fT, fTb, r2b):
    class _Lazy:
        pass
    P = C.P
    orig_dma = P.dma
    state = {"claimed": False}

    def dma_hook(eng, fn, reads, writes, lane):
        if (not state["claimed"]) and r2b in writes:
            C.claim("R2", [r2b])
            state["claimed"] = True
        return orig_dma(eng, fn, reads, writes, lane)
    P.dma = dma_hook
    try:
        wi = emit_outproj(C, wi, mT, lambda f: mTb, DC, x1, x2, t0, gpost, alpha, cb, fT, fTb, r2b)
    finally:
        P.dma = orig_dma
    return wi


def _interleave(ga, gb_):
    la, lb = list(ga), None
    return la


def run_interleaved(gens_a, gens_b):
    na, nb_ = len(gens_a), len(gens_b)
    ia = ib = 0
    while ia < na or ib < nb_:
        fa = ia / na if na else 1.0
        fb = ib / nb_ if nb_ else 1.0
        if ia < na and (fa <= fb or ib >= nb_):
            gens_a[ia](); ia += 1
        else:
            gens_b[ib](); ib += 1


def emit_mixb_layer(C, wbase, NTL, S, x1, x2, cs, cb, dr):
    P = C.P
    NBLK = NTL * 4
    score = [C.R3[:, 0:4096], C.R4[:, 0:4096]]
    scb = [Buf("score0"), Buf("score1")]
    work = C.R3[:, 4096:8192]
    wkb = Buf("work")
    mask = C.R3[:, 8192:10240].bitcast(BF16)
    mkb = Buf("mask")
    C.claim("R3", [scb[0], wkb, mkb])
    C.claim("R4", [scb[1]])
    maskT = [C.mT0[:], C.mT1[:]]
    maskTb = [C.mT0_b, C.mT1_b]
    qib = [C.qib, C.qib2]; qibb = [C.qib_b, C.qib2_b]
    qbk = [C.qbk, C.qbk2]; qbkb = [C.qbk_b, C.qbk2_b]
    wabs = [C.wabs, C.wabs2]; wabsb = [C.wabs_b, C.wabs2_b]
    wsg = [C.wsg, C.wsg2]; wsgb = [C.wsg_b, C.wsg2_b]
    wx = [C.wx, C.wx2]; wxb = [C.wx_b, C.wx2_b]
    rr = [0]

    def nb():
        b = rr[0] % 4
        rr[0] += 1
        return b
    OBS, TB, SCB = (4, 6), 7, 5
    rcs = [C.rc, C.rc2]; rcsb = [C.rc_b, C.rc2_b]
    pmc = [0]
    hdc = [0]
    dg = [C.dg0, C.dg1]; dgb = [C.dg0_b, C.dg1_b]
    rlc = [0]
    psT = C.ps[TB][:].bitcast(BF16)
    kvc = [0]
    st = {}

    def phase_ab(qb):
        th = []
        p = qb % 2
        q0 = qb * 128
        nkb = qb + 1
        Sc = nkb * 128
        ngrp = (nkb + 3) // 4
        sc, scbp = score[p], scb[p]

        def loads():
            P.dma("sp", (lambda e, s_=dr["qiT"][0][:, q0:q0 + 128].rearrange("(c p) t -> p c t", p=128), o=qib[p][:]: e.dma_start(out=o, in_=s_)),
                  reads=[dr["qiT"][1]], writes=[qibb[p]], lane=f"lq0{p}")
            P.dma("sp", (lambda e, s_=dr["qT"][0][:, q0:q0 + 128].rearrange("(c p) t -> p c t", p=128), o=qbk[p][:]: e.dma_start(out=o, in_=s_)),
                  reads=[dr["qT"][1]], writes=[qbkb[p]], lane=f"lq1{p}")
            P.dma("sp", (lambda e, s_=dr["widx"][0][q0:q0 + 128, :], o=wx[p][:]: e.dma_start(out=o, in_=s_)),
                  reads=[dr["widx"][1]], writes=[wxb[p]], lane=f"lq2{p}")
            P.op("act", (lambda e, o=wabs[p][:], i_=wx[p][:]: e.activation(out=o, in_=i_, func=AF.Abs)), reads=[wxb[p]], writes=[wabsb[p]])
            P.op("act", (lambda e, o=wsg[p][:], i_=wx[p][:]: e.activation(out=o, in_=i_, func=AF.Sign)), reads=[wxb[p]], writes=[wsgb[p]])
            for h in range(16):
                P.op("pool", (lambda e, o=dg[p][:, h, :], s_=wsg[p][:, h:h + 1]: e.tensor_scalar(out=o, in0=C.ident[:], scalar1=s_, scalar2=1.0, op0=ALU.mult, op1=ALU.mult)),
                     reads=[C.ident_b, wsgb[p]], writes=[dgb[p]])
        th.append(loads)
        for grp in range(ngrp):
            c0 = grp * 512
            n = min(Sc, c0 + 512) - c0
            gs = {}

            def mk_dots(h, grp=grp, c0=c0, n=n, gs=gs):
                def dots():
                    c, half = h // 2, h % 2
                    rows = slice(half * 64, half * 64 + 64)
                    bk = nb()
                    mm(P, C.ps[bk][:, 0:n], qib[p][rows, c, :], C.kis[rows, c0:c0 + n], True, True, [qibb[p], C.kis_b], [C.ps_b[bk]])
                    k2 = rlc[0] % 4
                    rlc[0] += 1
                    gs[h] = k2
                    P.op("act", (lambda e, o=C.rl16[k2][:, 0:n], i_=C.ps[bk][:, 0:n], s_=wabs[p][:, h:h + 1]: e.activation(out=o, in_=i_, func=AF.Relu, scale=s_)),
                         reads=[C.ps_b[bk], wabsb[p]], writes=[C.rl16_b[k2]])
                return dots

            def mk_hsum(h, grp=grp, c0=c0, n=n, gs=gs):
                def hsum():
                    k2 = gs[h]
                    mm(P, C.ps[SCB][:, 0:n], dg[p][:, h, :], C.rl16[k2][:, 0:n], h == 0, h == 15, [dgb[p], C.rl16_b[k2]], [C.ps_b[SCB]])
                    if h == 15:
                        P.op("act", (lambda e, o=sc[:, c0:c0 + n], i_=C.ps[SCB][:, 0:n]: e.activation(out=o, in_=i_, func=AF.Copy)),
                             reads=[C.ps_b[SCB]], writes=[scbp])
                return hsum
            LAI = 2
            for h0 in range(LAI):
                th.append(mk_dots(h0))
            for h in range(16):
                if h + LAI < 16:
                    th.append(mk_dots(h + LAI))
                th.append(mk_hsum(h))

        if Sc >= BIS_MIN:
            def binit():
                P.op("dve", (lambda e, s_=sc[:, 0:Sc]: e.max(out=C.m8[:], in_=s_)), reads=[scbp], writes=[C.m8_b])
                P.op("dve", (lambda e, s_=sc[:, 0:Sc]: e.tensor_reduce(out=C.blo[:], in_=s_, axis=mybir.AxisListType.X, op=ALU.min)), reads=[scbp], writes=[C.blo_b])
                P.op("dve", (lambda e: e.tensor_tensor(out=C.brng[:], in0=C.m8[:, 0:1], in1=C.blo[:], op=ALU.subtract)), reads=[C.m8_b, C.blo_b], writes=[C.brng_b])
                P.op("dve", (lambda e: e.tensor_scalar(out=C.bstep[:], in0=C.pw2[:], scalar1=C.brng[:], scalar2=None, op0=ALU.mult)), reads=[C.pw2_b, C.brng_b], writes=[C.bstep_b])
            th.append(binit)

        def causal():
            dsl = sc[:, (nkb - 1) * 128:nkb * 128]
            P.op("pool", (lambda e, d_=dsl: e.affine_select(out=d_, in_=d_, pattern=[[-1, 128]], compare_op=ALU.is_ge, fill=fillreg(e, NEG), base=0, channel_multiplier=1)),
                 reads=[scbp], writes=[scbp])
        th.append(causal)
        use_bis = Sc >= BIS_MIN
        if use_bis:
            def bis_init():
                pass
            for k in range(BIS_IT):
                def it(k=k):
                    P.op("dve", (lambda e: e.tensor_tensor(out=C.bmid[:], in0=C.blo[:], in1=C.bstep[:, k:k + 1], op=ALU.add)),
                         reads=[C.blo_b, C.bstep_b], writes=[C.bmid_b])
                    P.op("dve", (lambda e, w_=work[:, 0:Sc], s_=sc[:, 0:Sc]: e.tensor_scalar(out=w_, in0=s_, scalar1=C.bmid[:], scalar2=None, op0=ALU.is_ge, op1=ALU.add, accum_out=C.bcnt[:])),
                         reads=[scbp, C.bmid_b], writes=[wkb, C.bcnt_b])
                    P.op("dve", (lambda e: e.tensor_scalar(out=C.bfs[:], in0=C.bcnt[:], scalar1=float(TOPK) - 0.5, scalar2=C.bstep[:, k:k + 1], op0=ALU.is_ge, op1=ALU.mult)),
                         reads=[C.bcnt_b, C.bstep_b], writes=[C.bfs_b])
                    P.op("dve", (lambda e: e.tensor_tensor(out=C.blo[:], in0=C.blo[:], in1=C.bfs[:], op=ALU.add)),
                         reads=[C.blo_b, C.bfs_b], writes=[C.blo_b])
                th.append(it)
        elif Sc > TOPK:
            for r in range(TOPK // 8):
                def rnd(r=r):
                    src = sc[:, 0:Sc] if r == 0 else work[:, 0:Sc]
                    srcb = scbp if r == 0 else wkb
                    P.op("dve", (lambda e, s_=src: e.max(out=C.m8[:], in_=s_)), reads=[srcb], writes=[C.m8_b])
                    if r < TOPK // 8 - 1:
                        P.op("dve", (lambda e, s_=src, w_=work[:, 0:Sc]: e.match_replace(out=w_, in_to_replace=C.m8[:], in_values=s_, imm_value=NEG)),
                             reads=[srcb, C.m8_b], writes=[wkb])
                th.append(rnd)

        def mk():
            if Sc >= BIS_MIN:
                thr, thrb = C.blo[:], C.blo_b
            elif Sc > TOPK:
                thr, thrb = C.m8[:, 7:8], C.m8_b
            else:
                thr, thrb = C.thrneg[:], C.thrneg_b
            P.op("dve", (lambda e, t_=thr, m_=mask[:, 0:Sc], s_=sc[:, 0:Sc]: e.tensor_scalar(out=m_, in0=s_, scalar1=t_, scalar2=None, op0=(ALU.is_lt if ATTN_PE else ALU.is_ge))),
                 reads=[scbp, thrb], writes=[mkb])
        th.append(mk)
        for grp in range(ngrp):
            def tr(grp=grp):
                kbs = list(range(grp * 4, min(nkb, grp * 4 + 4)))
                for j, kb in enumerate(kbs):
                    P.op("pe", (lambda e, o=psT[:, j * 128:(j + 1) * 128], i_=mask[:, kb * 128:(kb + 1) * 128]: e.transpose(o, i_, C.ident[:])),
                         reads=[mkb, C.ident_b], writes=[C.ps_b[TB]])
                n = len(kbs) * 128
                P.op("act", (lambda e, o=maskT[p][:, grp * 512:grp * 512 + n], i_=psT[:, 0:n]: e.activation(out=o, in_=i_, func=AF.Copy)),
                     reads=[C.ps_b[TB]], writes=[maskTb[p]])
            th.append(tr)
        return th

    def phase_c(qb):
        th = []
        p = qb % 2
        qi_ = qb % 4
        nkb = qb + 1
        Sc = nkb * 128
        ngrp = (nkb + 3) // 4
        kh, khb, vh, vhb, ybT, ybTb = st["kh"], st["khb"], st["vh"], st["vhb"], st["ybT"], st["ybTb"]
        pms, pmsb = st["pms"], st["pmsb"]
        for h in range(8):
            hs = {}

            def ld(h=h, hs=hs):
                s2 = kvc[0] % 2
                kvc[0] += 1
                hs["s2"] = s2
                hs["ob"] = OBS[hdc[0] % 2]
                hs["rc"] = hdc[0] % 2
                hdc[0] += 1
                P.dma("sp", (lambda e, o=kh[s2][:, 0:Sc], s_=dr["kT"][0][h * 128:(h + 1) * 128, 0:Sc]: e.dma_start(out=o, in_=s_)),
                      reads=[dr["kT"][1]], writes=[khb[s2]], lane=f"lk{s2}")
                P.dma("sp", (lambda e, o=vh[s2][:, 0:nkb, :], s_=dr["v"][0][0:Sc, h * 128:(h + 1) * 128].rearrange("(b p) c -> p b c", p=128): e.dma_start(out=o, in_=s_)),
                      reads=[dr["v"][1]], writes=[vhb[s2]], lane=f"lv{s2}")
            th.append(ld)
            def mk_qk(grp, h=h, hs=hs):
                def qk():
                    s2 = hs["s2"]
                    kbs = list(range(grp * 4, min(nkb, grp * 4 + 4)))
                    n = len(kbs) * 128
                    if ATTN_PE:
                        bk = nb()
                        for j, kb in enumerate(kbs):
                            w_ = nkb - 1 - kb
                            near = w_ <= 1
                            o_ = C.ps[bk][:, j * 128:(j + 1) * 128]
                            P.op("pe", (lambda e, o=o_, l=kh[s2][:, kb * 128:(kb + 1) * 128], r=qbk[p][:, h, :]: e.matmul(o, l, r, start=True, stop=False, skip_group_check=True)),
                                 reads=[khb[s2], qbkb[p]], writes=[C.ps_b[bk]])
                            P.op("pe", (lambda e, o=o_, r=maskT[p][:, kb * 128:(kb + 1) * 128], sp=(not near): e.matmul(o, C.negI[:], r, start=False, stop=sp, skip_group_check=True)),
                                 reads=[C.negI_b, maskTb[p]], writes=[C.ps_b[bk]])
                            if near:
                                P.op("pe", (lambda e, o=o_, r=C.Fb[:, (h * 2 + w_) * 128:(h * 2 + w_ + 1) * 128]: e.matmul(o, C.ident[:], r, start=False, stop=True, skip_group_check=True)),
                                     reads=[C.ident_b, C.Fb_b], writes=[C.ps_b[bk]])
                        k2 = pmc[0] % 4
                        pmc[0] += 1
                        P.op("act", (lambda e, o=pms[k2][:, 0:n], i_=C.ps[bk][:, 0:n]: e.activation(out=o, in_=i_, func=AF.Exp, bias=C.cb8[:, h:h + 1], scale=ATT_SCALE)),
                             reads=[C.ps_b[bk], C.cb8_b], writes=[pmsb[k2]])
                    else:
                        bk = nb()
                        for j, kb in enumerate(kbs):
                            mm(P, C.ps[bk][:, j * 128:(j + 1) * 128], kh[s2][:, kb * 128:(kb + 1) * 128], qbk[p][:, h, :], True, True, [khb[s2], qbkb[p]], [C.ps_b[bk]])
                        k3 = C.cnt % 2
                        C.cnt += 1
                        P.op("act", (lambda e, o=C.tmpa[k3][:, 0:n], i_=C.ps[bk][:, 0:n]: e.activation(out=o, in_=i_, func=AF.Exp, bias=C.cb8[:, h:h + 1], scale=ATT_SCALE)),
                             reads=[C.ps_b[bk], C.cb8_b], writes=[C.tmpa_b[k3]])
                        k2 = pmc[0] % 4
                        pmc[0] += 1
                        P.op("dve", (lambda e, o=pms[k2][:, 0:n], a=C.tmpa[k3][:, 0:n], b=maskT[p][:, grp * 512:grp * 512 + n]: e.tensor_tensor(out=o, in0=a, in1=b, op=ALU.mult)),
                             reads=[C.tmpa_b[k3], maskTb[p]], writes=[pmsb[k2]])
                        for j, kb in enumerate(kbs):
                            w_ = nkb - 1 - kb
                            if w_ <= 1:
                                P.op("dve", (lambda e, o=pms[k2][:, j * 128:(j + 1) * 128], f_=C.Fe[:, (h * 2 + w_) * 128:(h * 2 + w_ + 1) * 128]: e.tensor_tensor(out=o, in0=o, in1=f_, op=ALU.mult)),
                                     reads=[pmsb[k2], C.Fe_b], writes=[pmsb[k2]])
                    hs[("k2", grp)] = k2
                return qk

            def mk_pv(grp, h=h, hs=hs):
                def pv():
                    s2, OB = hs["s2"], hs["ob"]
                    k2 = hs[("k2", grp)]
                    kbs = list(range(grp * 4, min(nkb, grp * 4 + 4)))
                    for j, kb in enumerate(kbs):
                        P.op("pe", (lambda e, o=C.ps[OB][:, 0:128], l=pms[k2][:, j * 128:(j + 1) * 128], r=vh[s2][:, kb, :], st_=(kb == 0), sp=(kb == nkb - 1):
                                    e.matmul(o, l, r, start=st_, stop=sp, skip_group_check=True)),
                             reads=[pmsb[k2], vhb[s2]], writes=[C.ps_b[OB]])
                        P.op("pe", (lambda e, o=C.ps[OB][:, 128:129], l=pms[k2][:, j * 128:(j + 1) * 128], sp=(kb == nkb - 1):
                                    e.matmul(o, l, C.onec[:, 0:1], start=False, stop=sp, skip_group_check=True)),
                             reads=[pmsb[k2], C.onec_b], writes=[C.ps_b[OB]])

                return pv
            LA = 2
            for g0 in range(min(LA, ngrp)):
                th.append(mk_qk(g0))
            for grp in range(ngrp):
                if grp + LA < ngrp:
                    th.append(mk_qk(grp + LA))
                th.append(mk_pv(grp))

            def fin(h=h, hs=hs):
                OB, ri = hs["ob"], hs["rc"]
                P.op("dve", (lambda e, o=rcs[ri][:], i_=C.ps[OB][:, 128:129]: e.reciprocal(out=o, in_=i_)), reads=[C.ps_b[OB]], writes=[rcsb[ri]])
                P.op("dve", (lambda e, o=C.yb[:, h * 128:(h + 1) * 128], i_=C.ps[OB][:, 0:128], s_=rcs[ri][:]: e.tensor_scalar(out=o, in0=i_, scalar1=s_, scalar2=None, op0=ALU.mult)),
                     reads=[C.ps_b[OB], rcsb[ri]], writes=[C.yb_b])
            th.append(fin)
        for half in range(2):
            def ytr(half=half):
                for j in range(4):
                    c = half * 4 + j
                    P.op("pe", (lambda e, o=psT[:, j * 128:(j + 1) * 128], i_=C.yb[:, c * 128:(c + 1) * 128]: e.transpose(o, i_, C.ident[:])),
                         reads=[C.yb_b, C.ident_b], writes=[C.ps_b[TB]])
                P.op("act", (lambda e, o=ybT[:, half * 4:half * 4 + 4, qi_ * 128:(qi_ + 1) * 128], i_=psT[:, 0:512].rearrange("p (c t) -> p c t", t=128):
                             e.activation(out=o, in_=i_, func=AF.Copy)),
                     reads=[C.ps_b[TB]], writes=[ybTb])
            th.append(ytr)
        return th

    def open_tile():
        kh = [C.R1[:, i * 2048:(i + 1) * 2048].bitcast(BF16) for i in range(2)]
        khb = [Buf("kh0"), Buf("kh1")]
        ybT = C.R1[:, 4096:6144].bitcast(BF16).rearrange("p (c t) -> p c t", t=NT)
        ybTb = Buf("ybT")
        vh = [C.R2[:, i * 2048:(i + 1) * 2048].bitcast(BF16).rearrange("p (b c) -> p b c", c=128) for i in range(2)]
        vhb = [Buf("vh0"), Buf("vh1")]
        pms = [C.R1[:, 6144 + i * 256:6144 + (i + 1) * 256].bitcast(BF16) for i in range(4)]
        pmsb = [Buf(f"pm{i}") for i in range(4)]
        C.claim("R1", khb + [ybTb] + pmsb); C.claim("R2", vhb)
        st.update(kh=kh, khb=khb, ybT=ybT, ybTb=ybTb, vh=vh, vhb=vhb, pms=pms, pmsb=pmsb)

    def tail(tt, wi):
        t0 = tt * NT
        ybT, ybTb = st["ybT"], st["ybTb"]
        mT = C.R2[:, :].bitcast(BF16).rearrange("p (c t) -> p c t", t=NT)
        mTb = Buf("mT")
        C.claim("R2", [mTb])
        lc = [0]
        for c2 in range(8):
            w, wb = C.wget(wi); wi += 1
            for cc in range(2):
                ch = 2 * c2 + cc
                bk = nb()
                for k in range(8):
                    mm(P, C.ps[bk][:], w[:, k, cc * 128:(cc + 1) * 128], ybT[:, k, :], k == 0, k == 7, [wb, ybTb], [C.ps_b[bk]])
                k2 = lc[0] % 2
                lc[0] += 1
                gbt, gbb = (C.gt1, C.gt1_b) if k2 == 0 else (C.gt2, C.gt2_b)
                P.dma("sp", (lambda e, o=gbt[:], s_=dr["gb"][0][ch * 128:(ch + 1) * 128, t0:t0 + NT]: e.dma_start(out=o, in_=s_)),
                      reads=[dr["gb"][1]], writes=[gbb], lane=f"lg{k2}")
                P.dma("sp", (lambda e, o=C.ta2[k2][:], s_=dr["apart"][0][ch * 128:(ch + 1) * 128, t0:t0 + NT]: e.dma_start(out=o, in_=s_)),
                      reads=[dr["apart"][1]], writes=[C.ta2_b[k2]], lane=f"la{k2}")
                P.op("dve", (lambda e, g_=gbt[:], i_=C.ps[bk][:]: e.tensor_tensor(out=g_, in0=g_, in1=i_, op=ALU.mult)),
                     reads=[gbb, C.ps_b[bk]], writes=[gbb])
                P.op("dve", (lambda e, o=mT[:, ch, :], g_=gbt[:], a=C.ta2[k2][:]: e.tensor_tensor(out=o, in0=g_, in1=a, op=ALU.add)),
                     reads=[gbb, C.ta2_b[k2]], writes=[mTb])
        fT = C.R1[:, :].rearrange("p (c t) -> p c t", t=NT)
        fTb = Buf("fT")
        C.claim("R1", [fTb])
        r2b = Buf("r2x")
        wi = emit_outproj_claim(C, wi, mT, mTb, x1, x2, t0, cs[:, 48:64], 1.0, cb, fT, fTb, r2b)
        return wi

    wi = wbase
    for f in phase_ab(0):
        f()
    for qb in range(NBLK):
        if qb % 4 == 0:
            open_tile()
        ca = phase_ab(qb + 1) if qb + 1 < NBLK else []
        cc_ = phase_c(qb)
        run_interleaved(ca, cc_)
        if qb % 4 == 3:
            wi = tail(qb // 4, wi)
    return wi


L_ = 2
NUM_BUCKETS = 32
MAX_DISTANCE = 128
IN_COLS = 6224


def build_program(S, depth):
    nc = bass.Bass("TRN2", target_bir_lowering=False)
    NTL = S // NT

    def din(name, shape, dt=F32):
        return nc.dram_tensor(name, list(shape), dt, kind="ExternalInput").ap()

    def dscr(name, shape, dt=F32):
        return nc.dram_tensor(name, list(shape), dt, kind="Internal").ap()
    x = din("x", [D, S])
    y = nc.dram_tensor("y", [D, S], F32, kind="ExternalOutput").ap()
    W = {}
    for l in range(depth):
        W[l] = dict(
            f1i=din(f"f1i{l}", [D, 2 * DFF]), f1o=din(f"f1o{l}", [DFF, D]),
            f2i=din(f"f2i{l}", [D, 2 * DFF]), f2o=din(f"f2o{l}", [DFF, D]),
            win=din(f"win{l}", [D, IN_COLS]), wg=din(f"wg{l}", [D, 2 * D]),
            wba=din(f"wba{l}", [AW, D]), wbb=din(f"wbb{l}", [AW, D]), wo=din(f"wo{l}", [D, D]),
            cpk=din(f"cpk{l}", [128, 96]), bro=din(f"bro{l}", [128, 3072]), wsT=din(f"wsT{l}", [128, 8, 128]),
        )
    toep = din("toep", [128, 2048])
    cb8d = din("cb8", [128, 8])
    xa = (dscr("xa", [D, S]), Buf("xa"))
    xb = (dscr("xb", [D, S]), Buf("xb"))
    xc = (dscr("xc", [D, S]), Buf("xc"))
    dr = {
        "qT": (dscr("qT", [AW, S], BF16), Buf("qT")), "qiT": (dscr("qiT", [AW, S], BF16), Buf("qiT")),
        "kT": (dscr("kT", [AW, S], BF16), Buf("kT")), "v": (dscr("v", [S, AW], BF16), Buf("v")),
        "kiT": (dscr("kiT", [128, S], BF16), Buf("kiT")), "widx": (dscr("widx", [S, 16]), Buf("widx")),
        "apart": (dscr("apart", [D, S]), Buf("apart")), "gb": (dscr("gb", [D, S]), Buf("gb")),
    }
    P = Prog(nc)
    C = Ctx(P)
    cs = [P.sbuf(f"consts{l}", [128, 96], F32) for l in range(depth)]
    cb = [Buf(f"consts{l}") for l in range(depth)]
    for l in range(depth):
        P.dma("sp", (lambda e, l=l: e.dma_start(out=cs[l][:], in_=W[l]["cpk"])), reads=[], writes=[cb[l]], lane="ldc")
    emit_attn_consts(C, toep, cb8d)
    for l in range(depth):
        w = W[l]
        for t in range(NTL):
            C.plan_extend(ffn_plan(w["f1i"], w["f1o"]))
        for t in range(NTL):
            C.plan_extend(mixa_plan(w["win"], w["wg"], w["wba"]))
        for t in range(NTL):
            C.plan_extend(mixb_tail_plan(w["wbb"], w["wo"]))
        for t in range(NTL):
            C.plan_extend(ffn_plan(w["f2i"], w["f2o"]))
    wi = 0
    xin = (x, Buf("x"))
    for l in range(depth):
        w = W[l]
        xout = (y, Buf("y")) if l == depth - 1 else xc
        for t in range(NTL):
            wi = emit_ffn(C, wi, xin, xa, t * NT, cs[l][:, 0:16], cs[l][:, 16:32], cb[l])
        emit_mix_consts(C, w["bro"], w["wsT"], None)
        for t in range(NTL):
            wi = emit_mixa(C, wi, xa, t * NT, S, cs[l], cb[l], dr)
        P.dma("sp", (lambda e: e.dma_start(out=C.kis[:, 0:S], in_=dr["kiT"][0])), reads=[dr["kiT"][1]], writes=[C.kis_b], lane="ldk")
        wi = emit_mixb_layer(C, wi, NTL, S, xa, xb, cs[l], cb[l], dr)
        for t in range(NTL):
            wi = emit_ffn(C, wi, xb, xout, t * NT, cs[l][:, 64:80], cs[l][:, 80:96], cb[l])
        xin = xc
    assert wi == len(C.plan), (wi, len(C.plan))
    P.finalize()
    return nc


def t5_bucket_np(n):
    max_exact = NUM_BUCKETS // 2
    nf = np.maximum(n, 1).astype(np.float32)
    large = max_exact + (np.log(nf / max_exact) / np.log(MAX_DISTANCE / max_exact) * (NUM_BUCKETS - max_exact)).astype(np.int32)
    large = np.minimum(large, NUM_BUCKETS - 1)
    return np.where(n < max_exact, n, large)


def host_prep(inp, depth):
    f = lambda a: np.ascontiguousarray(np.asarray(a, dtype=np.float32))
    m = {}

    def pc(v):
        return np.asarray(v, dtype=np.float32).reshape(16, 128).T
    for l in range(depth):
        m[f"f1i{l}"] = f(inp["ffn1_w_in"][l]); m[f"f1o{l}"] = f(inp["ffn1_w_out"][l])
        m[f"f2i{l}"] = f(inp["ffn2_w_in"][l]); m[f"f2o{l}"] = f(inp["ffn2_w_out"][l])
        m[f"win{l}"] = f(inp["w_in"][l]); m[f"wg{l}"] = f(inp["w_gate"][l])
        m[f"wba{l}"] = f(inp["w_branch_a"][l]); m[f"wbb{l}"] = f(inp["w_branch_b"][l]); m[f"wo{l}"] = f(inp["w_out"][l])
        m[f"cpk{l}"] = f(np.concatenate([pc(inp[k][l]) for k in ("ffn1_norm_pre", "ffn1_norm_post", "mix_norm_pre", "mix_norm_post", "ffn2_norm_pre", "ffn2_norm_post")], axis=1))
        row = np.concatenate([np.asarray(inp["sgu_ln_g"][l], np.float32), np.asarray(inp["sgu_ln_b"][l], np.float32), np.asarray(inp["sgu_b"][l], np.float32).reshape(-1)])
        m[f"bro{l}"] = f(np.broadcast_to(row[None, :], (128, 3072)))
        m[f"wsT{l}"] = f(np.transpose(np.asarray(inp["sgu_w_s"][l], np.float32), (2, 0, 1)))
    rb = np.asarray(inp["rel_bias"], np.float32)
    s_ = np.arange(128)[:, None]
    t_ = np.arange(128)[None, :]
    toep = np.zeros((128, 8, 2, 128), np.float32)
    for w_ in range(2):
        dist = np.maximum(t_ - s_ + 128 * w_, 0)
        bk = t5_bucket_np(dist)
        toep[:, :, w_, :] = np.transpose(rb[bk], (0, 2, 1))
    m["toep"] = f(toep.reshape(128, 2048))
    m["cb8"] = f(np.broadcast_to(rb[NUM_BUCKETS - 1][None, :], (128, 8)))
    return m


BATCH = 4
DEPTH = 2
_NC_CACHE = {}


def kernel(**inputs):
    inp = {k: np.asarray(v) for k, v in inputs.items()}
    S = inp["x"].shape[1]
    if "nc" not in _NC_CACHE:
        _NC_CACHE["nc"] = build_program(S, DEPTH)
    nc = _NC_CACHE["nc"]
    shared = host_prep(inp, DEPTH)
    in_maps = []
    for b in range(BATCH):
        m = dict(shared)
        m["x"] = np.ascontiguousarray(inp["x"][b].T.astype(np.float32))
        in_maps.append(m)
    res = run_bass_kernel_spmd(nc, in_maps, core_ids=list(range(BATCH)))
    out = np.stack([np.asarray(res.results[b]["y"]).T for b in range(BATCH)], axis=0)
    return np.ascontiguousarray(out.astype(np.float32))
```
